# Optimizing a Trainium2 kernel written in Bass

```python
import math
import jax
import jax.numpy as jnp
from jax import lax
import numpy as np

D_MODEL = 1024
BATCH = 8
SEQ = 8192
DEPTH = 4

GRID_W = 64
CTX_LEN = 256
N_EVEN = (DEPTH + 1) // 2
N_ODD = DEPTH // 2
N_VRES = N_ODD - 1
EPS = 1e-6

MLA_HEADS = 8
MLA_Q_LORA = 256
MLA_KV_LORA = 128
MLA_NOPE = 64
MLA_ROPE = 32
MLA_V = 64
MLA_QK = MLA_NOPE + MLA_ROPE
MLA_SCALE = MLA_QK ** -0.5
ROPE_BASE = 10000.0
Q_BLOCK = 128

GDN_HEADS = 4
GDN_DK = 128
GDN_DV = 128
GDN_CONV = 5
GDN_CHUNK = 64

RWKV_HEAD = 64
RWKV_HEADS = D_MODEL // RWKV_HEAD
RWKV_DECAY_LORA = max(32, int(round(1.8 * D_MODEL ** 0.5 / 32)) * 32)
RWKV_AAA_LORA = max(32, int(round(1.8 * D_MODEL ** 0.5 / 32)) * 32)
RWKV_MV_LORA = max(32, int(round(1.3 * D_MODEL ** 0.5 / 32)) * 32)
RWKV_GATE_LORA = max(32, int(round(0.6 * D_MODEL ** 0.8 / 32)) * 32)
GN_EPS = 64e-5

MOE_GROUPS = 4
MOE_PER_GROUP = 8
MOE_EXPERTS = MOE_GROUPS * MOE_PER_GROUP
MOE_TOPK = 2
MOE_HIDDEN = 512
MOE_BLOCK = 256

MLA_COLS = MLA_Q_LORA + MLA_KV_LORA + MLA_ROPE
GDN_QKV = GDN_HEADS * (2 * GDN_DK + GDN_DV)
GDN_Z = GDN_HEADS * GDN_DV
GDN_AB = 2 * 2 * GDN_HEADS
IN_COLS = MLA_COLS + GDN_QKV + GDN_Z + GDN_AB
MIX_OUT = MLA_HEADS * MLA_V + GDN_HEADS * GDN_DV

kernel_name = 'hybrid_mla_gdn_rwkv7_hmoe_dit'


def rms_norm(x, gain):
    xf = x.astype(jnp.float32)
    y = xf * lax.rsqrt(jnp.mean(xf * xf, -1, keepdims=True) + EPS)
    return (y * gain.astype(jnp.float32)).astype(x.dtype)


def l2_norm(x):
    xf = x.astype(jnp.float32)
    return (xf * lax.rsqrt(jnp.sum(xf * xf, -1, keepdims=True) + EPS)).astype(x.dtype)


def modulate(x, shift, scale):
    return x * (1 + scale) + shift


def _dir(t, d):
    return jnp.flip(t, axis=1) if d == 1 else t


def axial_rope_angles(n_tok):
    rows = n_tok // GRID_W
    row = jnp.repeat(jnp.arange(rows, dtype=jnp.float32), GRID_W)
    col = jnp.tile(jnp.arange(GRID_W, dtype=jnp.float32), rows)
    n_freq = MLA_ROPE // 4
    inv = ROPE_BASE ** (-jnp.arange(n_freq, dtype=jnp.float32) / n_freq)
    ang = jnp.stack([row[:, None] * inv, col[:, None] * inv], axis=1)
    return jnp.cos(ang), jnp.sin(ang)


def apply_axial_rope(x, cos, sin):
    B, S, H, _ = x.shape
    xr = x.reshape(B, S, H, 2, 2, MLA_ROPE // 4)
    x1, x2 = xr[..., 0, :], xr[..., 1, :]
    c = cos[None, :, None].astype(x.dtype)
    s = sin[None, :, None].astype(x.dtype)
    out = jnp.stack([x1 * c - x2 * s, x2 * c + x1 * s], axis=-2)
    return out.reshape(B, S, H, MLA_ROPE)


def mla_qkv(u, qa_g, w_qb, kva_g, w_kvb, qn_g, kn_g, rope):
    B, T, _ = u.shape
    H = MLA_HEADS
    c_q = rms_norm(u[..., :MLA_Q_LORA], qa_g)
    c_kv = rms_norm(u[..., MLA_Q_LORA:MLA_Q_LORA + MLA_KV_LORA], kva_g)
    k_pe = u[..., MLA_Q_LORA + MLA_KV_LORA:]
    q = (c_q @ w_qb).reshape(B, T, H, MLA_QK)
    kv = (c_kv @ w_kvb).reshape(B, T, H, MLA_NOPE + MLA_V)
    k = jnp.concatenate([kv[..., :MLA_NOPE], jnp.broadcast_to(k_pe[:, :, None, :], (B, T, H, MLA_ROPE))], -1)
    q = rms_norm(q, qn_g)
    k = rms_norm(k, kn_g)
    if rope is not None:
        cos, sin = rope
        q = jnp.concatenate([q[..., :MLA_NOPE], apply_axial_rope(q[..., MLA_NOPE:], cos, sin)], -1)
        k = jnp.concatenate([k[..., :MLA_NOPE], apply_axial_rope(k[..., MLA_NOPE:], cos, sin)], -1)
    return q, k, kv[..., MLA_NOPE:]


def latent_attention(q, k_lat, v_lat, k_ctx, v_ctx):
    B, S, H, Dh = q.shape
    k = jnp.concatenate([k_lat, k_ctx], 1)
    v = jnp.concatenate([v_lat, v_ctx], 1)
    nb = S // Q_BLOCK
    qb = jnp.swapaxes(q.reshape(B, nb, Q_BLOCK, H, Dh), 0, 1)

    def block(qi):
        s = jnp.einsum('bqhd,bkhd->bhqk', qi, k).astype(jnp.float32) * MLA_SCALE
        p = jax.nn.softmax(s, axis=-1).astype(v.dtype)
        return jnp.einsum('bhqk,bkhd->bqhd', p, v)

    o = lax.map(block, qb)
    return jnp.swapaxes(o, 0, 1).reshape(B, S, H * MLA_V)


def context_attention(q, k, v):
    B, L, H, _ = q.shape
    s = jnp.einsum('bqhd,bkhd->bhqk', q, k).astype(jnp.float32) * MLA_SCALE
    p = jax.nn.softmax(s, axis=-1).astype(v.dtype)
    return jnp.einsum('bhqk,bkhd->bqhd', p, v).reshape(B, L, H * MLA_V)


def short_conv(x, w):
    C = x.shape[-1]
    return lax.conv_general_dilated(x, w[:, None, :].astype(x.dtype), window_strides=(1,),
                                    padding=[(GDN_CONV // 2, GDN_CONV // 2)],
                                    dimension_numbers=('NWC', 'WIO', 'NWC'), feature_group_count=C)


def gdn_prep(u, conv_w, a_log, dt_bias):
    B, T, _ = u.shape
    nk = GDN_HEADS * GDN_DK
    qkv = jax.nn.silu(short_conv(u[..., :GDN_QKV], conv_w))
    q = l2_norm(qkv[..., :nk].reshape(B, T, GDN_HEADS, GDN_DK)) * GDN_DK ** -0.5
    k = l2_norm(qkv[..., nk:2 * nk].reshape(B, T, GDN_HEADS, GDN_DK))
    v = qkv[..., 2 * nk:].reshape(B, T, GDN_HEADS, GDN_DV)
    z = u[..., GDN_QKV:GDN_QKV + GDN_Z].reshape(B, T, GDN_HEADS, GDN_DV)
    ab = u[..., GDN_QKV + GDN_Z:].astype(jnp.float32).reshape(B, T, 2, 2, GDN_HEADS)
    g = -jnp.exp(a_log.astype(jnp.float32)) * jax.nn.softplus(ab[:, :, :, 0] + dt_bias.astype(jnp.float32))
    beta = jax.nn.sigmoid(ab[:, :, :, 1])
    return q, k, v, z, g, beta


def gated_delta_chunked(q, k, v, g, beta, s0):
    f32 = jnp.float32
    B, T, H, DK = q.shape
    DV = v.shape[-1]
    C = GDN_CHUNK
    n = T // C

    def blocks(t):
        t = t.astype(f32).reshape((B, n, C) + t.shape[2:])
        return jnp.moveaxis(jnp.moveaxis(t, 1, 0), 2, 3)

    q, k, v, g, beta = (blocks(t) for t in (q, k, v, g, beta))
    gc = jnp.cumsum(g, axis=-1)
    causal = jnp.tril(jnp.ones((C, C), bool))
    strict = jnp.tril(jnp.ones((C, C), bool), -1)
    decay = jnp.exp(jnp.where(causal, gc[..., :, None] - gc[..., None, :], -jnp.inf))
    kb = k * beta[..., None]
    lower = jnp.where(strict, jnp.einsum('nbhid,nbhjd->nbhij', kb, k) * decay, 0.0)
    rhs = jnp.concatenate([v * beta[..., None], kb * jnp.exp(gc)[..., None]], -1)
    sol = lax.linalg.triangular_solve(lower + jnp.eye(C, dtype=f32), rhs, left_side=True, lower=True,
                                      unit_diagonal=True)
    u_, w_ = sol[..., :DV], sol[..., DV:]
    a_qk = jnp.where(causal, jnp.einsum('nbhid,nbhjd->nbhij', q, k) * decay, 0.0)

    def step(S, xs):
        qi, ki, ui, wi, gi, ai = xs
        v_new = ui - jnp.einsum('bhck,bhkv->bhcv', wi, S)
        o = jnp.einsum('bhck,bhkv->bhcv', qi * jnp.exp(gi)[..., None], S) + jnp.einsum('bhij,bhjv->bhiv', ai, v_new)
        g_last = gi[..., -1:]
        S = S * jnp.exp(g_last)[..., None] + jnp.einsum('bhck,bhcv->bhkv', ki * jnp.exp(g_last - gi)[..., None], v_new)
        return S, o

    S, o = lax.scan(step, s0.astype(f32), (q, k, u_, w_, gc, a_qk))
    o = jnp.moveaxis(jnp.moveaxis(o, 3, 2), 0, 1).reshape(B, T, H, DV)
    return o, S


def gdn_bidirectional(uc, ul, conv_w, a_log, dt_bias, out_g):
    qc, kc, vc, zc, gc, bc = gdn_prep(uc, conv_w, a_log, dt_bias)
    ql, kl, vl, zl, gl, bl = gdn_prep(ul, conv_w, a_log, dt_bias)
    B = ul.shape[0]
    oc = 0.0
    ol = 0.0
    for d in range(2):
        s0 = jnp.zeros((B, GDN_HEADS, GDN_DK, GDN_DV), jnp.float32)
        o_c, s_c = gated_delta_chunked(_dir(qc, d), _dir(kc, d), _dir(vc, d), _dir(gc[:, :, d], d),
                                       _dir(bc[:, :, d], d), s0)
        o_l, _ = gated_delta_chunked(_dir(ql, d), _dir(kl, d), _dir(vl, d), _dir(gl[:, :, d], d),
                                     _dir(bl[:, :, d], d), s_c)
        oc = oc + _dir(o_c, d)
        ol = ol + _dir(o_l, d)

    def finish(o, z):
        Bz, T = z.shape[:2]
        return (rms_norm(o, out_g).astype(z.dtype) * jax.nn.silu(z)).reshape(Bz, T, GDN_HEADS * GDN_DV)

    return finish(oc, zc), finish(ol, zl)


def attn_delta_mixer(xc, xl, cos, sin, w_in, w_out, qa_g, w_qb, kva_g, w_kvb, qn_g, kn_g,
                     conv_w, a_log, dt_bias, out_g, need_ctx):
    uc = xc @ w_in
    ul = xl @ w_in
    qc, kc, vc = mla_qkv(uc[..., :MLA_COLS], qa_g, w_qb, kva_g, w_kvb, qn_g, kn_g, None)
    ql, kl, vl = mla_qkv(ul[..., :MLA_COLS], qa_g, w_qb, kva_g, w_kvb, qn_g, kn_g, (cos, sin))
    a_lat = latent_attention(ql, kl, vl, kc, vc)
    d_ctx, d_lat = gdn_bidirectional(uc[..., MLA_COLS:], ul[..., MLA_COLS:], conv_w, a_log, dt_bias, out_g)
    y_lat = jnp.concatenate([a_lat, d_lat], -1) @ w_out
    y_ctx = None
    if need_ctx:
        y_ctx = jnp.concatenate([context_attention(qc, kc, vc), d_ctx], -1) @ w_out
    return y_ctx, y_lat


def centred_shift(x):
    xp = jnp.pad(x, ((0, 0), (1, 1), (0, 0)))
    return 0.5 * (xp[:, :-2] + xp[:, 2:]) - x


def rwkv7_project(x, v_first, mu, wr, wk, wv, w0, w1, w2, a0, a1, a2, g1, g2, k_k, k_a, vres):
    B, T, D = x.shape
    hd = lambda t: t.reshape(B, T, RWKV_HEADS, RWKV_HEAD)
    xx = centred_shift(x)
    xr, xw, xk, xv, xa, xg = [x + xx * mu[i] for i in range(6)]
    r = xr @ wr
    k = xk @ wk
    v = xv @ wv
    if vres is None:
        v_first = v
    else:
        v0, v1, v2 = vres
        v = v + (v_first - v) * jax.nn.sigmoid(v0 + (xv @ v1) @ v2)
    gate = jax.nn.sigmoid(xg @ g1) @ g2
    kk = l2_norm(hd(k * k_k))
    decays, keys, rates = [], [], []
    for d in range(2):
        w_log = -jax.nn.softplus(-(w0[d] + jnp.tanh(xw @ w1[d]) @ w2[d])) - 0.5
        a = jax.nn.sigmoid(a0[d] + (xa @ a1[d]) @ a2[d])
        decays.append(hd(jnp.exp(-jnp.exp(w_log.astype(jnp.float32)))))
        keys.append(hd(k * (1 + (a - 1) * k_a)))
        rates.append(hd(a))
    return hd(r), hd(v), gate, kk, decays, keys, rates, v_first


def rwkv7_scan(r, w, k, v, a, b, s0):
    def step(S, xs):
        rt, wt, kt, vt, at, bt = xs
        sa = jnp.einsum('bhvk,bhk->bhv', S, at)
        S = S * wt[:, :, None, :] + sa[..., None] * bt[:, :, None, :] + vt[..., None] * kt[:, :, None, :]
        return S, jnp.einsum('bhvk,bhk->bhv', S, rt)

    xs = tuple(jnp.moveaxis(t.astype(jnp.float32), 1, 0) for t in (r, w, k, v, a, b))
    S, y = lax.scan(step, s0, xs)
    return jnp.moveaxis(y, 0, 1), S


def rwkv7_scan_inputs(p, d):
    r, v, gate, kk, decays, keys, rates, _ = p
    return (_dir(r, d), _dir(decays[d], d), _dir(keys[d], d), _dir(v, d), _dir(-kk, d), _dir(kk * rates[d], d))


def rwkv7_output(y, p, r_k, ln_w, ln_b, wo):
    r, v, gate, kk, decays, keys, rates, _ = p
    B, T, H, N = y.shape
    mean = jnp.mean(y, -1, keepdims=True)
    var = jnp.mean(jnp.square(y - mean), -1, keepdims=True)
    yn = ((y - mean) * lax.rsqrt(var + GN_EPS)).astype(gate.dtype).reshape(B, T, H * N) * ln_w + ln_b
    k_bonus = 0.5 * (keys[0] + keys[1])
    bonus = (jnp.sum(r * k_bonus * r_k, -1, keepdims=True) * v).reshape(B, T, H * N)
    return ((yn + bonus) * gate) @ wo


def rwkv7_mixer(xc, xl, vf_c, vf_l, mu, wr, wk, wv, wo, w0, w1, w2, a0, a1, a2, g1, g2, k_k, k_a, r_k,
                ln_w, ln_b, vres, need_ctx):
    pc = rwkv7_project(xc, vf_c, mu, wr, wk, wv, w0, w1, w2, a0, a1, a2, g1, g2, k_k, k_a, vres)
    pl = rwkv7_project(xl, vf_l, mu, wr, wk, wv, w0, w1, w2, a0, a1, a2, g1, g2, k_k, k_a, vres)
    B = xl.shape[0]
    yc = 0.0
    yl = 0.0
    for d in range(2):
        s0 = jnp.zeros((B, RWKV_HEADS, RWKV_HEAD, RWKV_HEAD), jnp.float32)
        o_c, s_c = rwkv7_scan(*rwkv7_scan_inputs(pc, d), s0)
        o_l, _ = rwkv7_scan(*rwkv7_scan_inputs(pl, d), s_c)
        yc = yc + _dir(o_c, d)
        yl = yl + _dir(o_l, d)
    y_lat = rwkv7_output(yl, pl, r_k, ln_w, ln_b, wo)
    y_ctx = rwkv7_output(yc, pc, r_k, ln_w, ln_b, wo) if need_ctx else None
    return y_ctx, y_lat, pc[-1], pl[-1]


def hier_moe(h, w_group, b_group, w_expert, b_expert, w1, w3, w2):
    N, D = h.shape
    pg = jax.nn.softmax((h @ w_group).astype(jnp.float32) + b_group.astype(jnp.float32), axis=-1)
    pg_top, g_idx = lax.top_k(pg, 1)
    le = (h @ w_expert).astype(jnp.float32) + b_expert.astype(jnp.float32)
    sel = g_idx * MOE_PER_GROUP + jnp.arange(MOE_PER_GROUP)[None, :]
    pe = jax.nn.softmax(jnp.take_along_axis(le, sel, axis=1), axis=-1)
    pe_top, e_loc = lax.top_k(pe, MOE_TOPK)
    wts = pg_top * pe_top / jnp.sum(pe_top, -1, keepdims=True)
    eid = (g_idx * MOE_PER_GROUP + e_loc).reshape(-1)
    tok = jnp.repeat(jnp.arange(N, dtype=jnp.int32), MOE_TOPK)
    w_f = wts.reshape(-1).astype(h.dtype)
    A = N * MOE_TOPK
    onehot = (eid[:, None] == jnp.arange(MOE_EXPERTS)[None, :]).astype(jnp.int32)
    rank = jnp.take_along_axis(jnp.cumsum(onehot, 0), eid[:, None], 1)[:, 0] - 1
    counts = jnp.sum(onehot, 0)
    padded = (counts + MOE_BLOCK - 1) // MOE_BLOCK * MOE_BLOCK
    pend = jnp.cumsum(padded)
    dest = (pend - padded)[eid] + rank
    n_blocks = -(-A // MOE_BLOCK) + MOE_EXPERTS
    slot_tok = jnp.zeros((n_blocks * MOE_BLOCK,), jnp.int32).at[dest].set(tok)
    slot_w = jnp.zeros((n_blocks * MOE_BLOCK,), h.dtype).at[dest].set(w_f)
    blk_e = jnp.minimum(jnp.searchsorted(pend, jnp.arange(n_blocks, dtype=jnp.int32) * MOE_BLOCK, side='right'),
                        MOE_EXPERTS - 1)

    def body(out, xs):
        toks, ws, e = xs
        xb = h[toks]
        hid = jax.nn.silu(xb @ w1[e]) * (xb @ w3[e])
        return out.at[toks].add((hid @ w2[e]) * ws[:, None]), None

    out, _ = lax.scan(body, jnp.zeros_like(h),
                      (slot_tok.reshape(n_blocks, MOE_BLOCK), slot_w.reshape(n_blocks, MOE_BLOCK), blk_e))
    return out


def setup_inputs(seed: int = 0) -> dict:
    key = jax.random.key(seed)
    keys = iter(jax.random.split(key, 64))
    f32 = jnp.float32

    def normal(shape, std):
        return jax.random.normal(next(keys), shape, f32) * std

    def uniform(shape, lo, hi):
        return jax.random.uniform(next(keys), shape, f32, lo, hi)

    def gain(shape):
        return 1.0 + normal(shape, 0.02)

    D = D_MODEL
    dt = jnp.exp(uniform((N_EVEN, 2, GDN_HEADS), math.log(1e-3), math.log(1e-1)))
    return {
        'x': normal((BATCH, SEQ, D), 1.0),
        'c': normal((BATCH, D), 1.0),
        'ctx': normal((BATCH, CTX_LEN, D), 1.0),
        'c_ctx': normal((D,), 1.0),
        'ada_w': normal((DEPTH, D, 6 * D), 0.5 * D ** -0.5),
        'ada_b': normal((DEPTH, 6 * D), 0.02),
        'norm_mix': gain((DEPTH, D)),
        'norm_ffn': gain((DEPTH, D)),
        'hy_w_in': normal((N_EVEN, D, IN_COLS), D ** -0.5),
        'hy_w_out': normal((N_EVEN, MIX_OUT, D), MIX_OUT ** -0.5),
        'mla_qa_norm': gain((N_EVEN, MLA_Q_LORA)),
        'mla_w_qb': normal((N_EVEN, MLA_Q_LORA, MLA_HEADS * MLA_QK), MLA_Q_LORA ** -0.5),
        'mla_kva_norm': gain((N_EVEN, MLA_KV_LORA)),
        'mla_w_kvb': normal((N_EVEN, MLA_KV_LORA, MLA_HEADS * (MLA_NOPE + MLA_V)), MLA_KV_LORA ** -0.5),
        'mla_q_norm': gain((N_EVEN, MLA_QK)),
        'mla_k_norm': gain((N_EVEN, MLA_QK)),
        'gdn_conv': normal((N_EVEN, GDN_CONV, GDN_QKV), GDN_CONV ** -0.5),
        'gdn_a_log': jnp.log(uniform((N_EVEN, 2, GDN_HEADS), 1.0, 16.0)),
        'gdn_dt_bias': dt + jnp.log(-jnp.expm1(-dt)),
        'gdn_out_norm': gain((N_EVEN, GDN_DV)),
        'rk_mu': uniform((N_ODD, 6, D), 0.0, 1.0),
        'rk_wr': normal((N_ODD, D, D), D ** -0.5),
        'rk_wk': normal((N_ODD, D, D), D ** -0.5),
        'rk_wv': normal((N_ODD, D, D), D ** -0.5),
        'rk_wo': normal((N_ODD, D, D), D ** -0.5),
        'rk_w0': uniform((N_ODD, 2, D), -2.0, 1.0),
        'rk_w1': normal((N_ODD, 2, D, RWKV_DECAY_LORA), D ** -0.5),
        'rk_w2': normal((N_ODD, 2, RWKV_DECAY_LORA, D), 0.5 * RWKV_DECAY_LORA ** -0.5),
        'rk_a0': normal((N_ODD, 2, D), 0.5),
        'rk_a1': normal((N_ODD, 2, D, RWKV_AAA_LORA), D ** -0.5),
        'rk_a2': normal((N_ODD, 2, RWKV_AAA_LORA, D), 0.5 * RWKV_AAA_LORA ** -0.5),
        'rk_g1': normal((N_ODD, D, RWKV_GATE_LORA), D ** -0.5),
        'rk_g2': normal((N_ODD, RWKV_GATE_LORA, D), RWKV_GATE_LORA ** -0.5),
        'rk_kk': 0.85 + normal((N_ODD, D), 0.05),
        'rk_ka': 1.0 + normal((N_ODD, D), 0.05),
        'rk_rk': normal((N_ODD, RWKV_HEADS, RWKV_HEAD), 0.1),
        'rk_ln_w': gain((N_ODD, D)),
        'rk_ln_b': normal((N_ODD, D), 0.02),
        'rk_v0': normal((N_VRES, D), 0.5),
        'rk_v1': normal((N_VRES, D, RWKV_MV_LORA), D ** -0.5),
        'rk_v2': normal((N_VRES, RWKV_MV_LORA, D), 0.5 * RWKV_MV_LORA ** -0.5),
        'moe_w_group': normal((DEPTH, D, MOE_GROUPS), D ** -0.5),
        'moe_b_group': normal((DEPTH, MOE_GROUPS), 0.01),
        'moe_w_expert': normal((DEPTH, D, MOE_EXPERTS), D ** -0.5),
        'moe_b_expert': normal((DEPTH, MOE_EXPERTS), 0.01),
        'moe_w1': normal((DEPTH, MOE_EXPERTS, D, MOE_HIDDEN), D ** -0.5),
        'moe_w3': normal((DEPTH, MOE_EXPERTS, D, MOE_HIDDEN), D ** -0.5),
        'moe_w2': normal((DEPTH, MOE_EXPERTS, MOE_HIDDEN, D), MOE_HIDDEN ** -0.5),
    }


def reference(x, c, ctx, c_ctx, ada_w, ada_b, norm_mix, norm_ffn,
              hy_w_in, hy_w_out, mla_qa_norm, mla_w_qb, mla_kva_norm, mla_w_kvb, mla_q_norm, mla_k_norm,
              gdn_conv, gdn_a_log, gdn_dt_bias, gdn_out_norm,
              rk_mu, rk_wr, rk_wk, rk_wv, rk_wo, rk_w0, rk_w1, rk_w2, rk_a0, rk_a1, rk_a2,
              rk_g1, rk_g2, rk_kk, rk_ka, rk_rk, rk_ln_w, rk_ln_b, rk_v0, rk_v1, rk_v2,
              moe_w_group, moe_b_group, moe_w_expert, moe_b_expert, moe_w1, moe_w3, moe_w2):
    B, S, D = x.shape
    L = ctx.shape[1]
    cos, sin = axial_rope_angles(S)
    sc_lat = jax.nn.silu(c)
    sc_ctx = jax.nn.silu(c_ctx)[None]
    h_lat, h_ctx = x, ctx
    vf_lat = None
    vf_ctx = None
    for l in range(DEPTH):
        need_ctx = l < DEPTH - 1
        m_lat = (sc_lat @ ada_w[l] + ada_b[l]).reshape(B, 1, 6, D)
        m_ctx = (sc_ctx @ ada_w[l] + ada_b[l]).reshape(1, 1, 6, D)
        u_lat = modulate(rms_norm(h_lat, norm_mix[l]), m_lat[:, :, 0], m_lat[:, :, 1])
        u_ctx = modulate(rms_norm(h_ctx, norm_mix[l]), m_ctx[:, :, 0], m_ctx[:, :, 1])
        j = l // 2
        if l % 2 == 0:
            y_ctx, y_lat = attn_delta_mixer(u_ctx, u_lat, cos, sin, hy_w_in[j], hy_w_out[j], mla_qa_norm[j],
                                            mla_w_qb[j], mla_kva_norm[j], mla_w_kvb[j], mla_q_norm[j],
                                            mla_k_norm[j], gdn_conv[j], gdn_a_log[j], gdn_dt_bias[j],
                                            gdn_out_norm[j], need_ctx)
        else:
            vres = None if j == 0 else (rk_v0[j - 1], rk_v1[j - 1], rk_v2[j - 1])
            y_ctx, y_lat, v_c, v_l = rwkv7_mixer(u_ctx, u_lat, vf_ctx, vf_lat, rk_mu[j], rk_wr[j], rk_wk[j],
                                                 rk_wv[j], rk_wo[j], rk_w0[j], rk_w1[j], rk_w2[j], rk_a0[j],
                                                 rk_a1[j], rk_a2[j], rk_g1[j], rk_g2[j], rk_kk[j], rk_ka[j],
                                                 rk_rk[j], rk_ln_w[j], rk_ln_b[j], vres, need_ctx)
            if j == 0:
                vf_ctx, vf_lat = v_c, v_l
        h_lat = h_lat + m_lat[:, :, 2] * y_lat
        f_lat = modulate(rms_norm(h_lat, norm_ffn[l]), m_lat[:, :, 3], m_lat[:, :, 4]).reshape(B * S, D)
        n_ctx_tok = 0
        if need_ctx:
            h_ctx = h_ctx + m_ctx[:, :, 2] * y_ctx
            f_ctx = modulate(rms_norm(h_ctx, norm_ffn[l]), m_ctx[:, :, 3], m_ctx[:, :, 4]).reshape(B * L, D)
            tokens = jnp.concatenate([f_ctx, f_lat], 0)
            n_ctx_tok = B * L
        else:
            tokens = f_lat
        moe_out = hier_moe(tokens, moe_w_group[l], moe_b_group[l], moe_w_expert[l], moe_b_expert[l],
                           moe_w1[l], moe_w3[l], moe_w2[l])
        h_lat = h_lat + m_lat[:, :, 5] * moe_out[n_ctx_tok:].reshape(B, S, D)
        if need_ctx:
            h_ctx = h_ctx + m_ctx[:, :, 5] * moe_out[:n_ctx_tok].reshape(B, L, D)
    return h_lat
```

```python
import contextlib
import numpy as np
import concourse.bass as bass
import concourse.mybir as mybir
from concourse.bass_utils import run_bass_kernel_spmd

F32 = mybir.dt.float32
BF16 = mybir.dt.bfloat16
I32 = mybir.dt.int32
AF = mybir.ActivationFunctionType
ALU = mybir.AluOpType
AX = mybir.AxisListType

ENGS = ("pe", "act", "dve", "pool", "sp")
SAME_ENGINE_SYNC = True
DMA_WINDOW = 8
SEM_MAXV = 30000


class Op:
    __slots__ = ("eng", "fn", "reads", "writes", "is_dma", "deps", "signals", "event", "pre_wait", "barrier")

    def __init__(self, eng, fn, reads, writes, is_dma, barrier=False):
        self.eng = eng
        self.fn = fn
        self.reads = reads
        self.writes = writes
        self.is_dma = is_dma
        self.deps = set()
        self.signals = False
        self.event = None
        self.pre_wait = None
        self.barrier = barrier


class V:
    __slots__ = ("ap", "key")

    def __init__(self, ap, key):
        self.ap = ap
        self.key = key


def _a(x):
    return x.ap if isinstance(x, V) else x


def _key(x):
    if isinstance(x, V):
        return x.key
    if isinstance(x, (str, tuple)):
        return x
    if hasattr(x, "tensor"):
        return x.tensor.name
    return x.name


class Phase:
    def __init__(self, kb):
        self.kb = kb
        self.es = contextlib.ExitStack()

    def sb(self, shape, dtype=F32, name="t"):
        self.kb._n += 1
        return self.es.enter_context(self.kb.nc.sbuf_tensor(f"{name}_{self.kb._n}", list(shape), dtype))

    def ps(self, shape=(128, 512), dtype=F32, name="p"):
        self.kb._n += 1
        nm = f"{name}_{self.kb._n}"
        self.kb.psum_names.add(nm)
        return self.es.enter_context(self.kb.nc.psum_tensor(nm, list(shape), dtype))

    def __enter__(self):
        return self

    def __exit__(self, *a):
        self.kb.barrier()
        self.es.close()
        return False


class KB:
    def __init__(self, nc):
        self.nc = nc
        self.ops = []
        self._n = 0
        self.psum_names = set()

    def phase(self):
        return Phase(self)

    def sb(self, shape, dtype=F32, name="g"):
        self._n += 1
        return self.nc.alloc_sbuf_tensor(f"{name}_{self._n}", list(shape), dtype)

    def dram(self, shape, dtype=F32, name="dr"):
        self._n += 1
        return self.nc.dram_tensor(f"{name}_{self._n}", list(shape), dtype, kind="Internal")

    def barrier(self):
        self.ops.append(Op(None, None, [], [], False, barrier=True))

    def op(self, eng, fn, reads, writes, is_dma=False):
        o = Op(eng, fn, [_key(r) for r in reads], [_key(w) for w in writes], is_dma)
        self.ops.append(o)
        return o

    def mm(self, out, lhsT, rhs, start=True, stop=True, **kw):
        return self.op("pe", lambda e: e.matmul(_a(out), _a(lhsT), _a(rhs), start=start, stop=stop, **kw), [lhsT, rhs], [out])

    def tr(self, out, in_, ident):
        return self.op("pe", lambda e: e.transpose(_a(out), _a(in_), _a(ident)), [in_, ident], [out])

    def act(self, out, in_, func, bias=None, scale=None, accum_out=None):
        kw = {}
        reads = [in_]
        if bias is not None:
            kw["bias"] = bias
            if not isinstance(bias, (int, float)):
                reads.append(bias)
        if scale is not None:
            kw["scale"] = scale
            if not isinstance(scale, (int, float)):
                reads.append(scale)
        writes = [out]
        if accum_out is not None:
            kw["accum_out"] = accum_out
            writes.append(accum_out)
        kw = {k: _a(x) for k, x in kw.items()}
        return self.op("act", lambda e: e.activation(_a(out), _a(in_), func, **kw), reads, writes)

    def v(self, eng, method, out, ins, *args, **kw):
        isap = lambda a: hasattr(a, "tensor") or isinstance(a, V)
        reads = [a for a in ins if isap(a)]
        reads += [a for a in args if isap(a)]
        reads += [a for a in kw.values() if isap(a)]
        ins2 = [_a(a) for a in ins]
        args2 = [_a(a) for a in args]
        kw2 = {k: _a(x) for k, x in kw.items()}
        return self.op(eng, lambda e: getattr(e, method)(_a(out), *ins2, *args2, **kw2), reads, [out])

    def dma(self, out, in_, eng="sp", rkey=None, wkey=None, **kw):
        return self.op(eng, lambda e: e.dma_start(out=_a(out), in_=_a(in_), **kw), [rkey or in_], [wkey or out], is_dma=True)

    def gather(self, out, src, idx, rkey=None):
        return self.op("pool", lambda e: e.indirect_dma_start(out=out, out_offset=None, in_=src,
                       in_offset=bass.IndirectOffsetOnAxis(ap=idx, axis=0)), [rkey or src, idx], [out], is_dma=True)

    def scatter(self, dst, src, idx, wkey=None):
        return self.op("pool", lambda e: e.indirect_dma_start(out=dst, out_offset=bass.IndirectOffsetOnAxis(ap=idx, axis=0),
                       in_=src, in_offset=None), [src, idx], [wkey or dst], is_dma=True)

    def finalize(self):
        nc = self.nc
        allops = self.ops
        ops = [o for o in allops if not o.barrier]
        idx_of = {id(o): i for i, o in enumerate(ops)}
        last_w = {}
        readers = {}
        last_on = {e: None for e in ENGS}
        recent_dma = {e: [] for e in ENGS}
        pending = {e: set() for e in ENGS}
        for o in allops:
            if o.barrier:
                src = set()
                for e in ENGS:
                    if last_on[e] is not None:
                        src.add(last_on[e])
                    src.update(recent_dma[e])
                for e in ENGS:
                    pending[e] |= src
                continue
            i = idx_of[id(o)]
            deps = set()
            for k in o.reads:
                if k in last_w:
                    deps.add(last_w[k])
                if k in self.psum_names:
                    for r in readers.get(k, ()):
                        if ops[r].eng != o.eng:
                            deps.add(r)
            for k in o.writes:
                if k in last_w:
                    deps.add(last_w[k])
                deps.update(readers.get(k, ()))
            deps |= pending[o.eng]
            pending[o.eng] = set()
            deps.discard(i)
            for k in o.reads:
                readers.setdefault(k, []).append(i)
            for k in o.writes:
                last_w[k] = i
                readers[k] = []
            fd = set()
            for d in deps:
                p = ops[d]
                if p.eng == o.eng and not p.is_dma and (o.eng == "pe" or not SAME_ENGINE_SYNC):
                    continue
                fd.add(d)
            o.deps = fd
            for d in fd:
                ops[d].signals = True
            last_on[o.eng] = i
            if o.is_dma:
                recent_dma[o.eng] = (recent_dma[o.eng] + [i])[-DMA_WINDOW:]
        n_sig = {e: 0 for e in ENGS}
        n_dma = {e: 0 for e in ENGS}
        for o in ops:
            if o.is_dma:
                o.signals = True
                n_dma[o.eng] += 1
            elif o.signals:
                n_sig[o.eng] += 1
        sems = {e: [nc.alloc_semaphore(f"s_{e}_{j}") for j in range(max(1, -(-n_sig[e] // SEM_MAXV)))] for e in ENGS}
        dsems = {e: [nc.alloc_semaphore(f"d_{e}_{j}") for j in range(DMA_WINDOW)] for e in ENGS if n_dma[e]}
        cnt = {e: 0 for e in ENGS}
        dcnt = {e: 0 for e in ENGS}
        for o in ops:
            if o.is_dma:
                n = dcnt[o.eng]
                dcnt[o.eng] += 1
                sem = dsems[o.eng][n % DMA_WINDOW]
                o.event = (sem, 16 * (n // DMA_WINDOW + 1))
                if n >= DMA_WINDOW:
                    o.pre_wait = (sem, 16 * (n // DMA_WINDOW))
            elif o.signals:
                n = cnt[o.eng]
                cnt[o.eng] += 1
                o.event = (sems[o.eng][n // SEM_MAXV], n % SEM_MAXV + 1)
        self.stats = dict(n_ops=len(ops), per_eng={e: sum(1 for o in ops if o.eng == e) for e in ENGS}, n_sig=n_sig, n_dma=n_dma)
        per_eng = {e: [o for o in ops if o.eng == e] for e in ENGS}
        final_waits = []
        for e in ENGS:
            for j in range(min(DMA_WINDOW, dcnt[e])):
                final_waits.append((dsems[e][j], 16 * ((dcnt[e] - 1 - j) // DMA_WINDOW + 1)))

        def emit(engname, eh):
            waited = {}

            def wait(sem, v):
                if waited.get(sem.name, 0) >= v:
                    return
                waited[sem.name] = v
                eh.wait_ge(sem, v)

            for o in per_eng[engname]:
                if o.pre_wait is not None:
                    wait(*o.pre_wait)
                for d in sorted(o.deps):
                    wait(*ops[d].event)
                ins = o.fn(eh)
                if o.event is not None:
                    ins.then_inc(o.event[0], 16 if o.is_dma else 1)
            if engname == "sp":
                for s, v in final_waits:
                    wait(s, v)

        with nc.Block() as block:
            @block.tensor
            def _(e):
                emit("pe", e)

            @block.scalar
            def _(e):
                emit("act", e)

            @block.vector
            def _(e):
                emit("dve", e)

            @block.gpsimd
            def _(e):
                emit("pool", e)

            @block.sync
            def _(e):
                emit("sp", e)
D = 1024
CTX = 256
GRID_W = 64
EPS = 1e-6
NH = 8
QK = 96
GH = 4
UF_ROWS = 1984
UT_COLS = 528
MOE_BS = 512


class Cfg:
    def __init__(self, nlt=16, layers=(0, 1, 2, 3), debug=False):
        self.nlt = nlt
        self.T = CTX + 512 * nlt
        self.tiles = [(0, CTX)] + [(CTX + 512 * i, 512) for i in range(nlt)]
        self.nsub = self.T // 128
        self.layers = tuple(layers)
        self.debug = debug
        self.nblk = -(-(2 * self.T) // MOE_BS) + 32
        self.nslot = self.nblk * MOE_BS


def rope_perm():
    p = np.zeros(32, np.int64)
    for ax in range(2):
        for half in range(2):
            for f in range(8):
                p[ax * 16 + half * 8 + f] = ax * 16 + (1 - half) * 8 + f
    return p


def rope_tables(cfg):
    S = cfg.T - CTX
    pos = np.arange(S)
    row = (pos // GRID_W).astype(np.float32)
    col = (pos % GRID_W).astype(np.float32)
    inv = (10000.0 ** (-np.arange(8, dtype=np.float32) / 8)).astype(np.float32)
    cosT = np.zeros((96, cfg.T), np.float32)
    sinT = np.zeros((96, cfg.T), np.float32)
    cosT[64:96, :CTX] = 1.0
    for ax in range(2):
        p = row if ax == 0 else col
        ang = p[None, :] * inv[:, None]
        for half in range(2):
            r0 = 64 + ax * 16 + half * 8
            cosT[r0:r0 + 8, CTX:] = np.cos(ang)
            sinT[r0:r0 + 8, CTX:] = np.sin(ang) * (-1.0 if half == 0 else 1.0)
    return cosT, sinT


def host_consts(cfg):
    c = {}
    c["ident_f"] = np.eye(128, dtype=np.float32)
    i = np.arange(128)
    same = (i[:, None] // 64) == (i[None, :] // 64)
    c["m_le"] = (same & (i[:, None] <= i[None, :])).astype(np.float32)
    c["m_ge"] = (same & (i[:, None] >= i[None, :])).astype(np.float32)
    c["m_lt"] = (same & (i[:, None] < i[None, :])).astype(np.float32)
    c["m_gt"] = (same & (i[:, None] > i[None, :])).astype(np.float32)
    c["m_same"] = same.astype(np.float32)
    c["ones_f"] = np.ones((128, 128), np.float32)
    c["m_h0"] = np.repeat((i[:, None] < 64), 128, 1).astype(np.float32)
    c["m_h1"] = np.repeat((i[:, None] >= 64), 128, 1).astype(np.float32)
    c["tri_lt_full"] = (i[:, None] < i[None, :]).astype(np.float32)
    cosT, sinT = rope_tables(cfg)
    c["cosT"] = cosT
    c["sinT"] = sinT
    c["thr"] = np.tile((np.arange(34, dtype=np.float32) * MOE_BS)[None, :], (128, 1))
    rm = np.ones((128, 512), np.float32)
    rm[:, ::64] = 0.0
    c["rmask"] = rm
    hs = np.zeros((128, 2), np.float32)
    hs[:64, 0] = 1.0
    hs[64:, 1] = 1.0
    c["hsel"] = hs
    c["iota_p"] = i.astype(np.float32).reshape(128, 1)
    c["blk_start"] = np.tile((np.arange(cfg.nblk, dtype=np.float32) * MOE_BS)[None, :], (128, 1))
    return c


CONST_SHAPES = lambda cfg: {
    "ident_f": (128, 128), "m_le": (128, 128), "m_ge": (128, 128), "m_lt": (128, 128), "m_gt": (128, 128),
    "m_same": (128, 128), "ones_f": (128, 128), "m_h0": (128, 128), "m_h1": (128, 128), "tri_lt_full": (128, 128),
    "cosT": (96, cfg.T), "sinT": (96, cfg.T), "iota_p": (128, 1), "rmask": (128, 512), "hsel": (128, 2), "thr": (128, 34), "blk_start": (128, cfg.nblk),
}


def host_even_smalls(inp, j):
    perm = rope_perm()
    s = {}
    s["qa_g"] = np.ascontiguousarray(inp["mla_qa_norm"][j].reshape(2, 128).T)
    s["kva_g"] = np.ascontiguousarray(inp["mla_kva_norm"][j].reshape(128, 1))
    for nm, key in (("qn_col", "mla_q_norm"), ("kn_col", "mla_k_norm")):
        g = inp[key][j]
        colv = np.zeros((96, 2), np.float32)
        colv[:, 0] = g
        colv[64:96, 1] = g[64 + perm]
        s[nm] = colv
    s["convw"] = np.ascontiguousarray(inp["gdn_conv"][j].reshape(5, 12, 128).transpose(2, 1, 0))
    s["gdn_vec"] = np.concatenate([inp["gdn_a_log"][j].reshape(-1), inp["gdn_dt_bias"][j].reshape(-1)]).reshape(1, 16).astype(np.float32)
    s["outg"] = np.ascontiguousarray(inp["gdn_out_norm"][j].reshape(1, 128))
    return s


EVEN_SMALL_SHAPES = {"qa_g": (128, 2), "kva_g": (128, 1), "qn_col": (96, 2), "kn_col": (96, 2),
                     "convw": (128, 12, 5), "gdn_vec": (1, 16), "outg": (1, 128)}


def host_odd_smalls(inp, j):
    s = {}
    col = lambda v: np.ascontiguousarray(v.reshape(8, 128).T)
    s["mu"] = np.ascontiguousarray(inp["rk_mu"][j].reshape(6, 8, 128).transpose(2, 0, 1))
    s["w0"] = np.ascontiguousarray(inp["rk_w0"][j].reshape(2, 8, 128).transpose(2, 0, 1))
    s["a0"] = np.ascontiguousarray(inp["rk_a0"][j].reshape(2, 8, 128).transpose(2, 0, 1))
    s["kkcol"] = col(inp["rk_kk"][j])
    s["kacol"] = col(inp["rk_ka"][j])
    s["rkcol"] = col(inp["rk_rk"][j].reshape(-1))
    s["lnw"] = np.ascontiguousarray(inp["rk_ln_w"][j].reshape(1, -1))
    s["lnb"] = np.ascontiguousarray(inp["rk_ln_b"][j].reshape(1, -1))
    s["v0"] = np.ascontiguousarray(inp["rk_v0"][j - 1].reshape(1, -1)) if j > 0 else np.zeros((1, D), np.float32)
    return s


ODD_SMALL_SHAPES = {"mu": (128, 6, 8), "w0": (128, 2, 8), "a0": (128, 2, 8), "kkcol": (128, 8), "kacol": (128, 8),
                    "rkcol": (128, 8), "lnw": (1, D), "lnb": (1, D), "v0": (1, D)}
MLA_SCALE = 96 ** -0.5


def rstd_inplace(kb, t, inv_n, eps, src=None):
    kb.v("dve", "tensor_scalar", t, [src if src is not None else t], inv_n, eps, ALU.mult, ALU.add)
    kb.act(t, t, AF.Sqrt)
    kb.v("dve", "reciprocal", t, [t])


class Prog:
    def __init__(self, cfg):
        self.cfg = cfg
        self.nc = bass.Bass("TRN2", target_bir_lowering=False)
        self.kb = KB(self.nc)
        self.in_shapes = {}
        self.dbg = {}
        self._rr = 0

    def inp(self, name, shape, dtype=F32):
        self.in_shapes[name] = (tuple(shape), dtype)
        return self.nc.dram_tensor(name, list(shape), dtype, kind="ExternalInput").ap()

    def scratch(self, name, shape, dtype=F32):
        if self.cfg.debug:
            t = self.nc.dram_tensor("dbg_" + name, list(shape), dtype, kind="ExternalOutput")
            self.dbg[name] = "dbg_" + name
        else:
            t = self.nc.dram_tensor("scr_" + name, list(shape), dtype, kind="Internal")
        return t.ap()

    def evac(self, out, in_):
        self._rr += 1
        if self._rr % 2:
            self.kb.act(out, in_, AF.Copy)
        else:
            self.kb.v("dve", "tensor_copy", out, [in_])

    def setup(self):
        kb, cfg = self.kb, self.cfg
        self.h0 = self.inp("h0", [cfg.T, D])
        self.cvec = self.inp("cvec", [128, 16])
        self.out = self.nc.dram_tensor("out", [cfg.T - CTX, D], F32, kind="ExternalOutput").ap()
        self.H = self.scratch("H", [cfg.T, D])
        self.cin = {k: self.inp("c_" + k, s) for k, s in CONST_SHAPES(cfg).items()}
        self.ident_f = kb.sb([128, 128], F32, "identf")
        self.ident = kb.sb([128, 128], BF16, "ident")
        self.ones = kb.sb([128, 128], BF16, "ones")
        kb.dma(self.ident_f[:], self.cin["ident_f"])
        kb.v("dve", "tensor_copy", self.ident[:], [self.ident_f[:]])
        kb.v("pool", "memset", self.ones[:], [], 1.0)
        self.modL = [kb.sb([128, D], F32, f"modL{i}") for i in range(6)]
        self.modC = [kb.sb([128, D], F32, f"modC{i}") for i in range(6)]
        with kb.phase() as ph:
            bufs = [ph.sb([128, D], F32, "cp") for _ in range(4)]
            for s in range(cfg.nsub):
                b = bufs[s % 4]
                kb.dma(b[:], self.h0[s * 128:(s + 1) * 128, :])
                kb.dma(self.H[s * 128:(s + 1) * 128, :], b[:])

    def finish(self):
        kb, cfg = self.kb, self.cfg
        with kb.phase() as ph:
            bufs = [ph.sb([128, D], F32, "cp") for _ in range(4)]
            for s in range(2, cfg.nsub):
                b = bufs[s % 4]
                kb.dma(b[:], self.H[s * 128:(s + 1) * 128, :])
                kb.dma(self.out[(s - 2) * 128:(s - 1) * 128, :], b[:])
        kb.finalize()

    def phase_mod(self, l):
        kb = self.kb
        ada_w = self.inp(f"ada_w__{l}", [D, 6 * D])
        ada_b = self.inp(f"ada_b__{l}", [1, 6 * D])
        nmix = self.inp(f"norm_mix__{l}", [1, D])
        nffn = self.inp(f"norm_ffn__{l}", [1, D])
        with kb.phase() as ph:
            sc = ph.sb([128, 16], F32, "sc")
            kb.dma(sc[:], self.cvec)
            kb.act(sc[:], sc[:], AF.Silu)
            lhs = ph.sb([128, 16, 128], F32, "lhs")
            for c in range(16):
                kb.v("dve", "tensor_copy", lhs[:, c, :], [sc[:, c:c + 1].to_broadcast([128, 128])])
            bb = ph.sb([128, 6 * D], F32, "bb")
            kb.dma(bb[:], ada_b.broadcast_to([128, 6 * D]))
            wst = [ph.sb([128, 8, 512], F32, "wst") for _ in range(2)]
            pl = [ph.ps() for _ in range(2)]
            pc = [ph.ps() for _ in range(2)]
            awv = ada_w.rearrange("(c p) n -> p c n", p=128)
            for n in range(12):
                w = wst[n % 2]
                kb.dma(w[:], awv[:, :, n * 512:(n + 1) * 512])
                for c in range(8):
                    kb.mm(pl[n % 2][:], lhs[:, c, :], w[:, c, :], start=(c == 0), stop=(c == 7))
                for c in range(8):
                    kb.mm(pc[n % 2][:], lhs[:, 8 + c, :], w[:, c, :], start=(c == 0), stop=(c == 7))
                jj, hf = n // 2, n % 2
                kb.v("dve", "tensor_tensor", self.modL[jj][:, hf * 512:(hf + 1) * 512], [pl[n % 2][:], bb[:, n * 512:(n + 1) * 512]], ALU.add)
                kb.v("dve", "tensor_tensor", self.modC[jj][:, hf * 512:(hf + 1) * 512], [pc[n % 2][:], bb[:, n * 512:(n + 1) * 512]], ALU.add)
            nm = ph.sb([128, D], F32, "nm")
            nf = ph.sb([128, D], F32, "nf")
            kb.dma(nm[:], nmix.broadcast_to([128, D]))
            kb.dma(nf[:], nffn.broadcast_to([128, D]))
            for mod in (self.modL, self.modC):
                kb.v("dve", "scalar_tensor_tensor", mod[1][:], [mod[1][:], 1.0, nm[:]], ALU.add, ALU.mult)
                kb.v("dve", "scalar_tensor_tensor", mod[4][:], [mod[4][:], 1.0, nf[:]], ALU.add, ALU.mult)

    def norm_sub(self, ht, junk, ss, tmp, xn, G, S):
        kb = self.kb
        kb.act(junk, ht, AF.Square, accum_out=ss)
        rstd_inplace(kb, ss, 1.0 / D, EPS)
        kb.v("dve", "scalar_tensor_tensor", tmp, [ht, ss, G], ALU.mult, ALU.mult)
        kb.v("pool", "tensor_tensor", xn, [tmp, S], ALU.add)

    def norm_tiles_to_xT(self, ph, src, gi, si, consume):
        kb, cfg = self.kb, self.cfg
        xTs = [ph.sb([128, 8, 512], BF16, "xT") for _ in range(2)]
        hts = [ph.sb([128, D], F32, "ht") for _ in range(2)]
        junk = ph.sb([128, D], F32, "junk")
        sss = [ph.sb([128, 1], F32, "ss") for _ in range(2)]
        tmps = [ph.sb([128, D], F32, "tmp") for _ in range(2)]
        xns = [ph.sb([128, D], BF16, "xn") for _ in range(2)]
        ptr = [ph.ps([128, 1024], BF16, "ptr") for _ in range(2)]
        k = 0
        for ti, (t0, TT) in enumerate(cfg.tiles):
            xT = xTs[ti % 2]
            mod = self.modC if ti == 0 else self.modL
            for s in range(TT // 128):
                ht, ss, tmp, xn, pt = hts[k % 2], sss[k % 2], tmps[k % 2], xns[k % 2], ptr[k % 2]
                k += 1
                r0 = t0 + s * 128
                kb.dma(ht[:], src[r0:r0 + 128, :])
                self.norm_sub(ht[:], junk[:], ss[:], tmp[:], xn[:], mod[gi][:], mod[si][:])
                for c in range(8):
                    kb.tr(pt[:, c * 128:(c + 1) * 128], xn[:, c * 128:(c + 1) * 128], self.ident[:])
                self.evac(xT[:, :, s * 128:(s + 1) * 128], pt[:].rearrange("p (c t) -> p c t", c=8))
            consume(ti, t0, TT, xT)

    def even_layer(self, l):
        j = l // 2
        self.phase_mod(l)
        sm = {k: self.inp(f"e{j}_{k}", s) for k, s in EVEN_SMALL_SHAPES.items()}
        self.phase_e1(j)
        self.phase_e2(j, sm)
        self.phase_e3(j)
        self.phase_e4(j, sm)
        self.phase_e5(j, sm)
        self.phase_e6(l, j, sm)
        self.moe(l)

    def phase_e1(self, j):
        kb, cfg = self.kb, self.cfg
        W = self.inp(f"hy_w_in__{j}", [D, 2480])
        self.UF = self.scratch(f"UF{j}", [UF_ROWS, cfg.T])
        self.UT = self.scratch(f"UT{j}", [cfg.T, UT_COLS])
        with kb.phase() as ph:
            Wfm = ph.sb([128, 8, UF_ROWS], BF16, "Wfm")
            Wtm = ph.sb([128, 8, UT_COLS], BF16, "Wtm")
            Wv = W.rearrange("(c p) n -> p c n", p=128)
            kb.dma(Wfm[:, :, 0:416], Wv[:, :, 0:416], eng="pool")
            for (d0, s0) in ((0, 8), (8, 0), (16, 24), (24, 16)):
                kb.dma(Wfm[:, :, 416 + d0:416 + d0 + 8], Wv[:, :, 384 + s0:384 + s0 + 8], eng="pool")
            kb.dma(Wfm[:, :, 448:1984], Wv[:, :, 416:1952], eng="pool")
            kb.dma(Wtm[:, :, :], Wv[:, :, 1952:2480], eng="pool")
            pmm = [ph.ps() for _ in range(4)]
            ost = [ph.sb([128, 512], F32, "ost") for _ in range(4)]
            cnt = [0]

            def consume(ti, t0, TT, xT):
                for m in range(16):
                    rows = min(128, UF_ROWS - m * 128)
                    pm, ob = pmm[cnt[0] % 4], ost[cnt[0] % 4]
                    cnt[0] += 1
                    for c in range(8):
                        kb.mm(pm[0:rows, 0:TT], Wfm[:, c, m * 128:m * 128 + rows], xT[:, c, 0:TT], start=(c == 0), stop=(c == 7))
                    self.evac(ob[0:rows, 0:TT], pm[0:rows, 0:TT])
                    kb.dma(self.UF[m * 128:m * 128 + rows, t0:t0 + TT], ob[0:rows, 0:TT])
                for s in range(TT // 128):
                    for (n0, n1) in ((0, 512), (512, 528)):
                        pm, ob = pmm[cnt[0] % 4], ost[cnt[0] % 4]
                        cnt[0] += 1
                        for c in range(8):
                            kb.mm(pm[:, 0:n1 - n0], xT[:, c, s * 128:(s + 1) * 128], Wtm[:, c, n0:n1], start=(c == 0), stop=(c == 7))
                        self.evac(ob[:, 0:n1 - n0], pm[:, 0:n1 - n0])
                        kb.dma(self.UT[t0 + s * 128:t0 + (s + 1) * 128, n0:n1], ob[:, 0:n1 - n0])

            self.norm_tiles_to_xT(ph, self.H, 1, 0, consume)

    def phase_e2(self, j, sm):
        kb, cfg = self.kb, self.cfg
        Wqb = self.inp(f"mla_w_qb__{j}", [256, 768])
        Wkvb = self.inp(f"mla_w_kvb__{j}", [128, 1024])
        self.QF = self.scratch(f"QF{j}", [NH, 96, cfg.T], BF16)
        self.KF = self.scratch(f"KF{j}", [NH, 96, cfg.T], BF16)
        self.VT = self.scratch(f"VT{j}", [cfg.T, 512], BF16)
        with kb.phase() as ph:
            Wq = ph.sb([128, 2, 768], BF16, "Wq")
            Wqs = ph.sb([128, 2, NH, 32], BF16, "Wqs")
            Wk = ph.sb([128, NH, 64], BF16, "Wk")
            Wvv = ph.sb([128, NH, 64], BF16, "Wv")
            kb.dma(Wq[:], Wqb.rearrange("(c p) n -> p c n", p=128), eng="pool")
            Wq4 = Wqb.rearrange("(c p) (h d) -> p c h d", p=128, d=96)
            for (d0, s0) in ((0, 8), (8, 0), (16, 24), (24, 16)):
                for c in range(2):
                    kb.dma(Wqs[:, c, :, d0:d0 + 8], Wq4[:, c, :, 64 + s0:64 + s0 + 8], eng="pool")
            Wkv3 = Wkvb.rearrange("p (h d) -> p h d", d=128)
            kb.dma(Wk[:], Wkv3[:, :, 0:64], eng="pool")
            kb.dma(Wvv[:], Wkv3[:, :, 64:128], eng="pool")
            qa_g = ph.sb([128, 2], F32, "qa_g")
            kva_g = ph.sb([128, 1], F32, "kva_g")
            qn = ph.sb([96, 2], F32, "qn")
            kn = ph.sb([96, 2], F32, "kn")
            kb.dma(qa_g[:], sm["qa_g"])
            kb.dma(kva_g[:], sm["kva_g"])
            kb.dma(qn[:], sm["qn_col"])
            kb.dma(kn[:], sm["kn_col"])
            cq = ph.sb([128, 2, 512], F32, "cq")
            ckv = ph.sb([128, 512], F32, "ckv")
            kpe = ph.sb([96, 512], F32, "kpe")
            kpes = ph.sb([96, 512], F32, "kpes")
            cs = ph.sb([96, 512], F32, "cs")
            sn = ph.sb([96, 512], F32, "sn")
            sq = ph.sb([128, 2, 512], BF16, "sq")
            rs = ph.sb([128, 512], F32, "rs")
            cqn = ph.sb([128, 2, 512], BF16, "cqn")
            ckvn = ph.sb([128, 512], BF16, "ckvn")
            rk = ph.sb([96, 512], F32, "rk")
            t1s = [ph.sb([96, 512], F32, "t1") for _ in range(2)]
            t2s = [ph.sb([96, 512], F32, "t2") for _ in range(2)]
            sqhs = [ph.sb([96, 512], BF16, "sqh") for _ in range(2)]
            rshs = [ph.sb([96, 512], F32, "rsh") for _ in range(2)]
            ohs = [ph.sb([96, 512], BF16, "oh") for _ in range(2)]
            vsb = [ph.sb([128, 512], BF16, "vsb") for _ in range(2)]
            pA = [ph.ps() for _ in range(2)]
            pB = [ph.ps() for _ in range(2)]
            pS = [ph.ps() for _ in range(2)]
            pR = ph.ps()
            pV = ph.ps()
            u = 0
            for ti, (t0, TT) in enumerate(cfg.tiles):
                kb.dma(cq[:, :, 0:TT], self.UF[0:256, t0:t0 + TT].rearrange("(c p) t -> p c t", p=128))
                kb.dma(ckv[:, 0:TT], self.UF[256:384, t0:t0 + TT])
                kb.dma(kpe[64:96, 0:TT], self.UF[384:416, t0:t0 + TT])
                kb.dma(kpes[64:96, 0:TT], self.UF[416:448, t0:t0 + TT])
                kb.dma(cs[64:96, 0:TT], self.cin["cosT"][64:96, t0:t0 + TT])
                kb.dma(sn[64:96, 0:TT], self.cin["sinT"][64:96, t0:t0 + TT])
                kb.act(sq[:, :, 0:TT], cq[:, :, 0:TT], AF.Square)
                for c in range(2):
                    kb.mm(pR[:, 0:TT], self.ones[:], sq[:, c, 0:TT], start=(c == 0), stop=(c == 1))
                rstd_inplace(kb, rs[:, 0:TT], 1.0 / 256, EPS, src=pR[:, 0:TT])
                for c in range(2):
                    kb.v("dve", "scalar_tensor_tensor", cqn[:, c, 0:TT], [cq[:, c, 0:TT], qa_g[:, c:c + 1], rs[:, 0:TT]], ALU.mult, ALU.mult)
                kb.act(sq[:, 0, 0:TT], ckv[:, 0:TT], AF.Square)
                kb.mm(pR[:, 0:TT], self.ones[:], sq[:, 0, 0:TT])
                rstd_inplace(kb, rs[:, 0:TT], 1.0 / 128, EPS, src=pR[:, 0:TT])
                kb.v("dve", "scalar_tensor_tensor", ckvn[:, 0:TT], [ckv[:, 0:TT], kva_g[:, 0:1], rs[:, 0:TT]], ALU.mult, ALU.mult)
                kb.v("dve", "scalar_tensor_tensor", rk[64:96, 0:TT], [kpe[64:96, 0:TT], kn[64:96, 0:1], cs[64:96, 0:TT]], ALU.mult, ALU.mult)
                kb.v("dve", "scalar_tensor_tensor", t2s[0][64:96, 0:TT], [kpes[64:96, 0:TT], kn[64:96, 1:2], sn[64:96, 0:TT]], ALU.mult, ALU.mult)
                kb.v("pool", "tensor_tensor", rk[64:96, 0:TT], [rk[64:96, 0:TT], t2s[0][64:96, 0:TT]], ALU.add)
                for h in range(NH):
                    a, b2, s_, t1, t2, sqh, rsh, oh = pA[u % 2], pB[u % 2], pS[u % 2], t1s[u % 2], t2s[u % 2], sqhs[u % 2], rshs[u % 2], ohs[u % 2]
                    u += 1
                    for c in range(2):
                        kb.mm(a[0:96, 0:TT], Wq[:, c, h * 96:(h + 1) * 96], cqn[:, c, 0:TT], start=(c == 0), stop=(c == 1))
                    for c in range(2):
                        kb.mm(b2[64:96, 0:TT], Wqs[:, c, h, :], cqn[:, c, 0:TT], start=(c == 0), stop=(c == 1))
                    kb.act(sqh[0:96, 0:TT], a[0:96, 0:TT], AF.Square)
                    kb.mm(s_[0:96, 0:TT], self.ones[0:96, 0:96], sqh[0:96, 0:TT])
                    rstd_inplace(kb, rsh[0:96, 0:TT], 1.0 / 96, EPS, src=s_[0:96, 0:TT])
                    kb.v("dve", "scalar_tensor_tensor", oh[0:64, 0:TT], [a[0:64, 0:TT], qn[0:64, 0:1], rsh[0:64, 0:TT]], ALU.mult, ALU.mult)
                    kb.v("dve", "scalar_tensor_tensor", t1[64:96, 0:TT], [a[64:96, 0:TT], qn[64:96, 0:1], cs[64:96, 0:TT]], ALU.mult, ALU.mult)
                    kb.v("dve", "scalar_tensor_tensor", t2[64:96, 0:TT], [b2[64:96, 0:TT], qn[64:96, 1:2], sn[64:96, 0:TT]], ALU.mult, ALU.mult)
                    kb.v("pool", "tensor_tensor", t1[64:96, 0:TT], [t1[64:96, 0:TT], t2[64:96, 0:TT]], ALU.add)
                    kb.v("dve", "tensor_tensor", oh[64:96, 0:TT], [t1[64:96, 0:TT], rsh[64:96, 0:TT]], ALU.mult)
                    kb.dma(self.QF[h, :, t0:t0 + TT], oh[0:96, 0:TT])
                    a, s_, sqh, rsh, oh = pA[u % 2], pS[u % 2], sqhs[u % 2], rshs[u % 2], ohs[u % 2]
                    u += 1
                    kb.mm(a[0:64, 0:TT], Wk[:, h, :], ckvn[:, 0:TT])
                    kb.act(sqh[0:64, 0:TT], a[0:64, 0:TT], AF.Square)
                    kb.act(sqh[64:96, 0:TT], kpe[64:96, 0:TT], AF.Square)
                    kb.mm(s_[0:96, 0:TT], self.ones[0:96, 0:96], sqh[0:96, 0:TT])
                    rstd_inplace(kb, rsh[0:96, 0:TT], 1.0 / 96, EPS, src=s_[0:96, 0:TT])
                    kb.v("dve", "scalar_tensor_tensor", oh[0:64, 0:TT], [a[0:64, 0:TT], kn[0:64, 0:1], rsh[0:64, 0:TT]], ALU.mult, ALU.mult)
                    kb.v("dve", "tensor_tensor", oh[64:96, 0:TT], [rk[64:96, 0:TT], rsh[64:96, 0:TT]], ALU.mult)
                    kb.dma(self.KF[h, :, t0:t0 + TT], oh[0:96, 0:TT])
                for s in range(TT // 128):
                    vb = vsb[s % 2]
                    kb.mm(pV[:], ckvn[:, s * 128:(s + 1) * 128], Wvv[:].rearrange("p h d -> p (h d)"))
                    self.evac(vb[:], pV[:])
                    kb.dma(self.VT[t0 + s * 128:t0 + (s + 1) * 128, :], vb[:])

    def phase_e3(self, j):
        kb, cfg = self.kb, self.cfg
        self.AF_ = self.scratch(f"AF{j}", [512, cfg.T], BF16)
        nsub = cfg.nsub
        with kb.phase() as ph:
            Vall = ph.sb([128, nsub, 512], BF16, "Vall")
            kb.dma(Vall[:], self.VT.rearrange("(b p) n -> p b n", p=128))
            Khs = [ph.sb([96, cfg.T], BF16, "Kh") for _ in range(2)]
            Qts = [ph.sb([96, 512], BF16, "Qt") for _ in range(2)]
            PTs = [ph.sb([128, 512], BF16, "PT") for _ in range(4)]
            rden = ph.sb([64, 512], F32, "rden")
            osb = [ph.sb([64, 512], BF16, "osb") for _ in range(2)]
            pS = [ph.ps() for _ in range(4)]
            pO = [ph.ps() for _ in range(2)]
            pD = [ph.ps() for _ in range(2)]
            u = 0
            q = 0
            for h in range(NH):
                Kh = Khs[h % 2]
                kb.dma(Kh[:], self.KF[h])
                for ti, (t0, TT) in enumerate(cfg.tiles):
                    Qt, po, pd, ob = Qts[q % 2], pO[q % 2], pD[q % 2], osb[q % 2]
                    q += 1
                    kb.dma(Qt[:, 0:TT], self.QF[h, :, t0:t0 + TT])
                    nkb = 2 if ti == 0 else nsub
                    for kbk in range(nkb):
                        ps, pt = pS[u % 4], PTs[u % 4]
                        u += 1
                        kb.mm(ps[:, 0:TT], Kh[:, kbk * 128:(kbk + 1) * 128], Qt[:, 0:TT])
                        kb.act(pt[:, 0:TT], ps[:, 0:TT], AF.Exp, scale=MLA_SCALE)
                        kb.mm(po[0:64, 0:TT], Vall[:, kbk, h * 64:(h + 1) * 64], pt[:, 0:TT], start=(kbk == 0), stop=(kbk == nkb - 1))
                        kb.mm(pd[0:64, 0:TT], self.ones[:, 0:64], pt[:, 0:TT], start=(kbk == 0), stop=(kbk == nkb - 1))
                    kb.v("dve", "reciprocal", rden[:, 0:TT], [pd[0:64, 0:TT]])
                    kb.v("dve", "tensor_tensor", ob[:, 0:TT], [po[0:64, 0:TT], rden[:, 0:TT]], ALU.mult)
                    kb.dma(self.AF_[h * 64:(h + 1) * 64, t0:t0 + TT], ob[:, 0:TT])
    def load_const_tile(self, ph, name, dtype=F32):
        shp = CONST_SHAPES(self.cfg)[name]
        t = ph.sb(list(shp), F32, name)
        self.kb.dma(t[:], self.cin[name])
        if dtype == F32:
            return t
        t2 = ph.sb(list(shp), dtype, name + "b")
        self.kb.v("dve", "tensor_copy", t2[:], [t[:]])
        return t2

    def phase_e4(self, j, sm):
        kb, cfg = self.kb, self.cfg
        T = cfg.T
        self.GQ = self.scratch(f"GQ{j}", [GH, 128, T], BF16)
        self.GK = self.scratch(f"GK{j}", [GH, 128, T], BF16)
        self.GKT = self.scratch(f"GKT{j}", [T, 512], BF16)
        self.GVT = self.scratch(f"GVT{j}", [T, 512], BF16)
        last = len(cfg.tiles) - 1
        with kb.phase() as ph:
            cw = ph.sb([128, 12, 5], F32, "cw")
            kb.dma(cw[:], sm["convw"])
            xs = [ph.sb([128, 516], F32, "x") for _ in range(2)]
            accs = [ph.sb([128, 512], F32, "acc") for _ in range(2)]
            ys = [ph.sb([128, 512], F32, "y") for _ in range(2)]
            sqs = [ph.sb([128, 512], BF16, "sq") for _ in range(2)]
            rs = ph.sb([128, 512], F32, "rs")
            yns = [ph.sb([128, 512], BF16, "yn") for _ in range(2)]
            tsb = [ph.sb([128, 4, 128], BF16, "tsb") for _ in range(2)]
            pR = [ph.ps() for _ in range(2)]
            ptr = [ph.ps([128, 1024], BF16, "ptr") for _ in range(2)]
            u = 0
            for ti, (t0, TT) in enumerate(cfg.tiles):
                for cc in range(12):
                    x, acc, y, sq, yn, pr, pt, tb = xs[u % 2], accs[u % 2], ys[u % 2], sqs[u % 2], yns[u % 2], pR[u % 2], ptr[u % 2], tsb[u % 2]
                    u += 1
                    r0 = 448 + cc * 128
                    kb.dma(x[:, 2:TT + 2], self.UF[r0:r0 + 128, t0:t0 + TT])
                    if ti in (0, 1):
                        kb.v("pool", "memset", x[:, 0:2], [], 0.0)
                    else:
                        kb.dma(x[:, 0:2], self.UF[r0:r0 + 128, t0 - 2:t0])
                    if ti in (0, last):
                        kb.v("pool", "memset", x[:, TT + 2:TT + 4], [], 0.0)
                    else:
                        kb.dma(x[:, TT + 2:TT + 4], self.UF[r0:r0 + 128, t0 + TT:t0 + TT + 2])
                    kb.v("dve", "tensor_scalar", acc[:, 0:TT], [x[:, 0:TT]], cw[:, cc, 0:1], None, ALU.mult)
                    for jj in range(1, 5):
                        kb.v("dve", "scalar_tensor_tensor", acc[:, 0:TT], [x[:, jj:jj + TT], cw[:, cc, jj:jj + 1], acc[:, 0:TT]], ALU.mult, ALU.add)
                    kb.act(y[:, 0:TT], acc[:, 0:TT], AF.Silu)
                    if cc < 8:
                        kb.act(sq[:, 0:TT], y[:, 0:TT], AF.Square)
                        kb.mm(pr[:, 0:TT], self.ones[:], sq[:, 0:TT])
                        rstd_inplace(kb, rs[:, 0:TT], 1.0, EPS, src=pr[:, 0:TT])
                        kb.v("dve", "scalar_tensor_tensor", yn[:, 0:TT], [y[:, 0:TT], (128 ** -0.5) if cc < 4 else 1.0, rs[:, 0:TT]], ALU.mult, ALU.mult)
                        dst = self.GQ[cc] if cc < 4 else self.GK[cc - 4]
                        kb.dma(dst[:, t0:t0 + TT], yn[:, 0:TT])
                    else:
                        kb.v("pool", "tensor_copy", yn[:, 0:TT], [y[:, 0:TT]])
                    if cc >= 4:
                        ns = TT // 128
                        for s in range(ns):
                            kb.tr(pt[:, s * 128:(s + 1) * 128], yn[:, s * 128:(s + 1) * 128], self.ident[:])
                        self.evac(tb[:, 0:ns, :], pt[:, 0:TT].rearrange("p (s c) -> p s c", c=128))
                        dstT = self.GKT if cc < 8 else self.GVT
                        hh = (cc - 4) % 4
                        kb.dma(dstT[t0:t0 + TT, hh * 128:(hh + 1) * 128].rearrange("(s p) c -> p s c", p=128), tb[:, 0:ns, :])

    def phase_e5(self, j, sm):
        kb, cfg = self.kb, self.cfg
        T, nsub = cfg.T, cfg.nsub
        self.OD = [self.scratch(f"OD{j}_{d}", [T, 512]) for d in range(2)]
        fwd = list(range(nsub))
        bwd = [1, 0] + list(range(nsub - 1, 1, -1))
        with kb.phase() as ph:
            mle = self.load_const_tile(ph, "m_le")
            mge = self.load_const_tile(ph, "m_ge")
            mlt = self.load_const_tile(ph, "m_lt")
            mgt = self.load_const_tile(ph, "m_gt")
            msame = self.load_const_tile(ph, "m_same")
            mh0 = self.load_const_tile(ph, "m_h0")
            mh1 = self.load_const_tile(ph, "m_h1")
            onesf = self.load_const_tile(ph, "ones_f")
            gv = ph.sb([128, 16], F32, "gv")
            kb.dma(gv[:], sm["gdn_vec"].broadcast_to([128, 16]))
            negA = ph.sb([128, 8], F32, "negA")
            kb.act(negA[:], gv[:, 0:8], AF.Exp)
            kb.v("dve", "tensor_scalar", negA[:], [negA[:]], -1.0, None, ALU.mult)
            S = [[ph.sb([128, 128], F32, f"S{d}{h}") for h in range(GH)] for d in range(2)]
            Sb = [[ph.sb([128, 128], BF16, f"Sb{d}{h}") for h in range(GH)] for d in range(2)]
            for d in range(2):
                for h in range(GH):
                    kb.v("pool", "memset", S[d][h][:], [], 0.0)
                    kb.v("pool", "memset", Sb[d][h][:], [], 0.0)
            banks = [ph.ps() for _ in range(7)]
            tbank = ph.ps([128, 1024], BF16, "tbank")
            slot = [0]
            tslot = [0]

            def ps128t():
                i = tslot[0] % 8
                tslot[0] += 1
                return V(tbank[:, i * 128:(i + 1) * 128], tbank.name)

            def ps128(bf=False):
                i = slot[0] % 7
                slot[0] += 1
                b = banks[i]
                return V(b[:, 0:128], b.name)

            def sub(v, r, cs=None):
                a = v.ap[r, :] if cs is None else v.ap[r, cs]
                return V(a, v.key)

            UB = []
            for d in range(2):
                b = {}
                b["ab"] = ph.sb([128, 16], F32, "ab")
                b["KT"] = ph.sb([128, GH, 128], BF16, "KT")
                b["QT"] = ph.sb([128, GH, 128], BF16, "QT")
                b["Ktm"] = ph.sb([128, 512], BF16, "Ktm")
                b["Vtm"] = ph.sb([128, 512], BF16, "Vtm")
                for nm in ("xa", "g", "beta", "nbeta", "gc", "gt", "eg", "ek", "beg", "egl0", "egl1"):
                    b[nm] = ph.sb([128, 4], F32, nm)
                b["osb"] = ph.sb([128, 512], F32, "osb")
                UB.append(b)
            HB = []
            for i in range(2):
                b = {}
                for nm in ("diag", "e1", "e2", "Dst", "DT", "EG", "usb"):
                    b[nm] = ph.sb([128, 128], F32, nm)
                for nm in ("X0", "X1", "XT0", "XT1", "AT0", "AT1", "Vb", "Kbg", "Khat", "QgT", "AqkT", "wT", "vnew"):
                    b[nm] = ph.sb([128, 128], BF16, nm)
                HB.append(b)
            hcount = 0
            for step in range(nsub):
                for d in range(2):
                    sb_ = fwd[step] if d == 0 else bwd[step]
                    r0 = sb_ * 128
                    B = UB[d]
                    kb.dma(B["ab"][:], self.UT[r0:r0 + 128, 512:528])
                    kb.dma(B["KT"][:], self.GK[:, :, r0:r0 + 128].rearrange("h c t -> c h t"))
                    kb.dma(B["QT"][:], self.GQ[:, :, r0:r0 + 128].rearrange("h c t -> c h t"))
                    kb.dma(B["Ktm"][:], self.GKT[r0:r0 + 128, :])
                    kb.dma(B["Vtm"][:], self.GVT[r0:r0 + 128, :])
                    ab = B["ab"]
                    kb.v("dve", "tensor_tensor", B["xa"][:], [ab[:, d * 8:d * 8 + 4], gv[:, 8 + d * 4:12 + d * 4]], ALU.add)
                    kb.act(B["xa"][:], B["xa"][:], AF.Exp)
                    kb.act(B["xa"][:], B["xa"][:], AF.Ln, bias=1.0)
                    kb.v("dve", "tensor_tensor", B["g"][:], [B["xa"][:], negA[:, d * 4:d * 4 + 4]], ALU.mult)
                    kb.act(B["beta"][:], ab[:, d * 8 + 4:d * 8 + 8], AF.Sigmoid)
                    kb.v("dve", "tensor_scalar", B["nbeta"][:], [B["beta"][:]], -1.0, None, ALU.mult)
                    Mc = mle if d == 0 else mge
                    p_gc, p_gt, p_0, p_1 = ps128(), ps128(), ps128(), ps128()
                    kb.mm(sub(p_gc, slice(0, 128), slice(0, 4)), Mc[:], B["g"][:])
                    kb.mm(sub(p_gt, slice(0, 128), slice(0, 4)), msame[:], B["g"][:])
                    kb.mm(sub(p_0, slice(0, 128), slice(0, 4)), mh0[:], B["g"][:])
                    kb.mm(sub(p_1, slice(0, 128), slice(0, 4)), mh1[:], B["g"][:])
                    kb.v("dve", "tensor_copy", B["gc"][:], [sub(p_gc, slice(0, 128), slice(0, 4))])
                    kb.v("dve", "tensor_tensor", B["gt"][:], [sub(p_gt, slice(0, 128), slice(0, 4)), B["gc"][:]], ALU.subtract)
                    kb.act(B["ek"][:], B["gt"][:], AF.Exp)
                    kb.act(B["eg"][:], B["gc"][:], AF.Exp)
                    kb.act(B["egl0"][:], sub(p_0, slice(0, 128), slice(0, 4)), AF.Exp)
                    kb.act(B["egl1"][:], sub(p_1, slice(0, 128), slice(0, 4)), AF.Exp)
                    kb.v("dve", "tensor_tensor", B["beg"][:], [B["beta"][:], B["eg"][:]], ALU.mult)
                    for h in range(GH):
                        Hb = HB[hcount % 2]
                        hcount += 1
                        hs = slice(h * 128, (h + 1) * 128)
                        gcol = B["gc"][:, h:h + 1]
                        kb.v("dve", "tensor_scalar", Hb["diag"][:], [self.ident_f[:]], gcol, None, ALU.mult)
                        pg = ps128()
                        kb.mm(pg, onesf[:], Hb["diag"][:])
                        kb.v("dve", "tensor_scalar", Hb["e1"][:], [pg], gcol, 0.0, ALU.subtract, ALU.max)
                        kb.act(Hb["e1"][:], Hb["e1"][:], AF.Exp, scale=-1.0)
                        kb.v("pool", "tensor_tensor", Hb["Dst"][:], [Hb["e1"][:], (mgt if d == 0 else mlt)[:]], ALU.mult)
                        kb.v("dve", "tensor_scalar", Hb["e2"][:], [pg], gcol, 0.0, ALU.subtract, ALU.min)
                        kb.act(Hb["e2"][:], Hb["e2"][:], AF.Exp)
                        kb.v("pool", "tensor_tensor", Hb["DT"][:], [Hb["e2"][:], (mle if d == 0 else mge)[:]], ALU.mult)
                        kb.act(Hb["EG"][:], pg, AF.Exp)
                        pkk = ps128()
                        kb.mm(pkk, B["KT"][:, h, :], B["KT"][:, h, :])
                        kb.v("dve", "scalar_tensor_tensor", Hb["X0"][:], [pkk, B["nbeta"][:, h:h + 1], Hb["Dst"][:]], ALU.mult, ALU.mult)
                        pkq = ps128()
                        kb.mm(pkq, B["KT"][:, h, :], B["QT"][:, h, :])
                        kb.v("dve", "tensor_tensor", Hb["AqkT"][:], [pkq, Hb["DT"][:]], ALU.mult)
                        pxt_b = ps128t()
                        kb.tr(pxt_b, Hb["X0"][:], self.ident[:])
                        kb.v("dve", "tensor_copy", Hb["XT0"][:], [pxt_b])
                        kb.v("pool", "tensor_tensor", Hb["AT0"][:], [Hb["XT0"][:], self.ident[:]], ALU.add)
                        X, XT, AT = Hb["X0"], Hb["XT0"], Hb["AT0"]
                        for js in range(1, 6):
                            Xn = Hb["X1"] if X is Hb["X0"] else Hb["X0"]
                            XTn = Hb["XT1"] if XT is Hb["XT0"] else Hb["XT0"]
                            ATn = Hb["AT1"] if AT is Hb["AT0"] else Hb["AT0"]
                            px = ps128()
                            kb.mm(px, XT[:], X[:])
                            if js < 5:
                                pxT = ps128()
                                kb.mm(pxT, X[:], XT[:])
                            kb.act(Xn[:], px, AF.Copy)
                            if js < 5:
                                kb.v("dve", "tensor_copy", XTn[:], [pxT])
                            pa = ps128()
                            kb.mm(pa, Xn[:], AT[:])
                            kb.v("dve", "tensor_tensor", ATn[:], [pa, AT[:]], ALU.add)
                            X, XT, AT = Xn, XTn, ATn
                        kb.v("pool", "tensor_scalar", Hb["Vb"][:], [B["Vtm"][:, hs]], B["beta"][:, h:h + 1], 1.0, ALU.mult, ALU.mult)
                        kb.v("pool", "tensor_scalar", Hb["Kbg"][:], [B["Ktm"][:, hs]], B["beg"][:, h:h + 1], 1.0, ALU.mult, ALU.mult)
                        kb.act(Hb["Khat"][:], B["Ktm"][:, hs], AF.Copy, scale=B["ek"][:, h:h + 1])
                        kb.v("pool", "tensor_tensor", Hb["QgT"][:], [B["QT"][:, h, :], Hb["EG"][:]], ALU.mult)
                        pu = ps128()
                        kb.mm(pu, AT[:], Hb["Vb"][:])
                        kb.act(Hb["usb"][:], pu, AF.Copy)
                        pw = ps128()
                        kb.mm(pw, Hb["Kbg"][:], AT[:])
                        kb.act(Hb["wT"][:], pw, AF.Copy)
                        for half in ((0, 1) if d == 0 else (1, 0)):
                            r = slice(half * 64, half * 64 + 64)
                            egl = B["egl0"] if half == 0 else B["egl1"]
                            p1 = ps128()
                            kb.mm(sub(p1, r), Hb["wT"][:, r], Sb[d][h][:])
                            kb.v("dve", "tensor_tensor", Hb["vnew"][r, :], [Hb["usb"][r, :], sub(p1, r)], ALU.subtract)
                            p2 = ps128()
                            kb.mm(sub(p2, r), Hb["QgT"][:, r], Sb[d][h][:], start=True, stop=False)
                            kb.mm(sub(p2, r), Hb["AqkT"][r, r], Hb["vnew"][r, :], start=False, stop=True)
                            kb.act(B["osb"][r, hs], sub(p2, r), AF.Copy)
                            p3 = ps128()
                            kb.mm(p3, Hb["Khat"][r, :], Hb["vnew"][r, :])
                            kb.v("dve", "scalar_tensor_tensor", S[d][h][:], [S[d][h][:], egl[:, h:h + 1], p3], ALU.mult, ALU.add)
                            kb.act(Sb[d][h][:], S[d][h][:], AF.Copy)
                    kb.dma(self.OD[d][r0:r0 + 128, :], B["osb"][:])

    def phase_e6(self, l, j, sm):
        kb, cfg = self.kb, self.cfg
        Wout = self.inp(f"hy_w_out__{j}", [D, D])
        self.Fm = self.scratch(f"F{l}", [cfg.T, D], BF16)
        with kb.phase() as ph:
            Wo = ph.sb([128, 8, D], BF16, "Wo")
            kb.dma(Wo[:], Wout.rearrange("(c p) n -> p c n", p=128), eng="pool")
            og = ph.sb([128, 128], F32, "og")
            kb.dma(og[:], sm["outg"].broadcast_to([128, 128]))
            o0s = [ph.sb([128, 512], F32, "o0") for _ in range(2)]
            o1s = [ph.sb([128, 512], F32, "o1") for _ in range(2)]
            zs = [ph.sb([128, 512], F32, "z") for _ in range(2)]
            sqo = ph.sb([128, 512], F32, "sqo")
            ssq = [ph.sb([128, 4], F32, "ssq") for _ in range(2)]
            on = ph.sb([128, 512], F32, "on")
            dtm = [ph.sb([128, 512], BF16, "dtm") for _ in range(2)]
            XT = [ph.sb([128, 8, 128], BF16, "XT") for _ in range(2)]
            ptr = [ph.ps([128, 1024], BF16, "ptr") for _ in range(2)]
            py = [ph.ps() for _ in range(4)]
            hts = [ph.sb([128, D], F32, "ht") for _ in range(2)]
            tmp = [ph.sb([128, D], F32, "tmp") for _ in range(2)]
            junk = ph.sb([128, D], F32, "junk")
            sss = [ph.sb([128, 1], F32, "ss") for _ in range(2)]
            tmp2 = [ph.sb([128, D], F32, "tmp2") for _ in range(2)]
            fb = [ph.sb([128, D], BF16, "fb") for _ in range(2)]
            for s in range(cfg.nsub):
                mod = self.modC if s < 2 else self.modL
                r0 = s * 128
                i = s % 2
                kb.dma(o0s[i][:], self.OD[0][r0:r0 + 128, :])
                kb.dma(o1s[i][:], self.OD[1][r0:r0 + 128, :])
                kb.dma(zs[i][:], self.UT[r0:r0 + 128, 0:512])
                kb.dma(XT[i][:, 0:4, :], self.AF_[:, r0:r0 + 128].rearrange("(c p) t -> p c t", p=128))
                kb.dma(hts[i][:], self.H[r0:r0 + 128, :])
                kb.v("pool", "tensor_tensor", o0s[i][:], [o0s[i][:], o1s[i][:]], ALU.add)
                kb.v("dve", "tensor_tensor", sqo[:], [o0s[i][:], o0s[i][:]], ALU.mult)
                kb.v("dve", "tensor_reduce", ssq[i][:], [sqo[:].rearrange("p (h v) -> p h v", h=4)], AX.X, ALU.add)
                rstd_inplace(kb, ssq[i][:], 1.0 / 128, EPS)
                for h in range(4):
                    hs = slice(h * 128, (h + 1) * 128)
                    kb.v("dve", "scalar_tensor_tensor", on[:, hs], [o0s[i][:, hs], ssq[i][:, h:h + 1], og[:]], ALU.mult, ALU.mult)
                kb.act(zs[i][:], zs[i][:], AF.Silu)
                kb.v("pool", "tensor_tensor", dtm[i][:], [on[:], zs[i][:]], ALU.mult)
                for c in range(4):
                    kb.tr(ptr[i][:, c * 128:(c + 1) * 128], dtm[i][:, c * 128:(c + 1) * 128], self.ident[:])
                self.evac(XT[i][:, 4:8, :], ptr[i][:, 0:512].rearrange("p (c t) -> p c t", c=4))
                for n in range(2):
                    pp = py[(2 * s + n) % 4]
                    for c in range(8):
                        kb.mm(pp[:], XT[i][:, c, :], Wo[:, c, n * 512:(n + 1) * 512], start=(c == 0), stop=(c == 7))
                    kb.v("dve", "tensor_tensor", tmp[i][:, n * 512:(n + 1) * 512], [pp[:], mod[2][:, n * 512:(n + 1) * 512]], ALU.mult)
                kb.v("pool", "tensor_tensor", hts[i][:], [tmp[i][:], hts[i][:]], ALU.add)
                kb.dma(self.H[r0:r0 + 128, :], hts[i][:])
                self.norm_sub(hts[i][:], junk[:], sss[i][:], tmp2[i][:], fb[i][:], mod[4][:], mod[3][:])
                kb.dma(self.Fm[r0:r0 + 128, :], fb[i][:])
    def moe_setup(self):
        kb, cfg = self.kb, self.cfg
        if hasattr(self, "OH"):
            return
        ns, nb = cfg.nsub, cfg.nblk
        self.OH = kb.sb([128, ns, 64], BF16, "OH")
        self.WTS = kb.sb([128, ns, 2], F32, "WTS")
        self.DEST = kb.sb([128, ns, 2], I32, "DEST")
        self.I1 = kb.sb([128, nb, 8], I32, "I1")
        self.I2 = kb.sb([128, nb, 4], I32, "I2")
        self.offs = kb.sb([128, 32], F32, "offs")
        self.XS = self.scratch("XS", [cfg.nslot, D], BF16)
        self.YS = self.scratch("YS", [cfg.nslot, D], F32)

    def moe(self, l):
        self.moe_setup()
        kb, cfg = self.kb, self.cfg
        ns, nb = cfg.nsub, cfg.nblk
        wg = self.inp(f"moe_w_group__{l}", [D, 4])
        bg = self.inp(f"moe_b_group__{l}", [1, 4])
        we = self.inp(f"moe_w_expert__{l}", [D, 32])
        be = self.inp(f"moe_b_expert__{l}", [1, 32])
        w1 = self.inp(f"moe_w1__{l}", [32 * D, 512])
        w3 = self.inp(f"moe_w3__{l}", [32 * D, 512])
        w2 = self.inp(f"moe_w2__{l}", [32 * 512, D])
        OH, WTS, DEST, I1, I2, offs = self.OH, self.WTS, self.DEST, self.I1, self.I2, self.offs
        with kb.phase() as ph:
            Wr = ph.sb([128, 8, 36], BF16, "Wr")
            kb.dma(Wr[:, :, 0:4], wg.rearrange("(c p) n -> p c n", p=128), eng="pool")
            kb.dma(Wr[:, :, 4:36], we.rearrange("(c p) n -> p c n", p=128), eng="pool")
            br = ph.sb([128, 36], F32, "br")
            kb.dma(br[:, 0:4], bg.broadcast_to([128, 4]))
            kb.dma(br[:, 4:36], be.broadcast_to([128, 32]))
            zt = ph.sb([128, D], BF16, "zt")
            kb.v("pool", "memset", zt[:], [], 0.0)
            for i in range(cfg.nslot // 128):
                kb.dma(self.XS[i * 128:(i + 1) * 128, :], zt[:], wkey=("XS", "z", i))
            fts = [ph.sb([128, D], BF16, "ft") for _ in range(2)]
            fT = [ph.sb([128, 8, 128], BF16, "fT") for _ in range(2)]
            ptr = [ph.ps([128, 1024], BF16, "ptr") for _ in range(2)]
            plg = [ph.ps() for _ in range(2)]
            pcnt = ph.ps()
            sm_ = [{nm: ph.sb([128, w], F32, nm) for nm, w in (("lg", 36), ("mx", 1), ("nmx", 1), ("eg", 4), ("sg", 1), ("pg", 1), ("G1", 4),
                                                               ("pen", 4), ("lem", 32), ("top", 8), ("dif", 1), ("r", 1), ("den", 1))} for _ in range(2)]
            osum = [ph.sb([128, 32], BF16, "osum") for _ in range(2)]
            for s in range(ns):
                i = s % 2
                ft, t = fts[i], sm_[i]
                kb.dma(ft[:], self.Fm[s * 128:(s + 1) * 128, :])
                for c in range(8):
                    kb.tr(ptr[i][:, c * 128:(c + 1) * 128], ft[:, c * 128:(c + 1) * 128], self.ident[:])
                self.evac(fT[i][:], ptr[i][:].rearrange("p (c t) -> p c t", c=8))
                for c in range(8):
                    kb.mm(plg[i][:, 0:36], fT[i][:, c, :], Wr[:, c, :], start=(c == 0), stop=(c == 7))
                kb.v("dve", "tensor_tensor", t["lg"][:], [plg[i][:, 0:36], br[:]], ALU.add)
                kb.v("dve", "reduce_max", t["mx"][:], [t["lg"][:, 0:4]], AX.X)
                kb.v("dve", "tensor_scalar", t["nmx"][:], [t["mx"][:]], -1.0, None, ALU.mult)
                kb.act(t["eg"][:], t["lg"][:, 0:4], AF.Exp, bias=t["nmx"][:], accum_out=t["sg"][:])
                kb.v("dve", "reciprocal", t["pg"][:], [t["sg"][:]])
                kb.v("dve", "tensor_scalar", t["G1"][:], [t["lg"][:, 0:4]], t["mx"][:], None, ALU.is_equal)
                kb.v("dve", "tensor_scalar", t["pen"][:], [t["G1"][:]], 1e30, -1e30, ALU.mult, ALU.add)
                for g in range(4):
                    kb.v("dve", "tensor_scalar", t["lem"][:, g * 8:(g + 1) * 8], [t["lg"][:, 4 + g * 8:12 + g * 8]], t["pen"][:, g:g + 1], None, ALU.add)
                kb.v("dve", "max", t["top"][:], [t["lem"][:]])
                kb.v("dve", "tensor_scalar", OH[:, s, 0:32], [t["lem"][:]], t["top"][:, 0:1], None, ALU.is_equal)
                kb.v("dve", "tensor_scalar", OH[:, s, 32:64], [t["lem"][:]], t["top"][:, 1:2], None, ALU.is_equal)
                kb.v("dve", "tensor_tensor", t["dif"][:], [t["top"][:, 1:2], t["top"][:, 0:1]], ALU.subtract)
                kb.act(t["r"][:], t["dif"][:], AF.Exp)
                kb.v("dve", "tensor_scalar", t["den"][:], [t["r"][:]], 1.0, None, ALU.add)
                kb.v("dve", "reciprocal", t["den"][:], [t["den"][:]])
                kb.v("dve", "tensor_tensor", WTS[:, s, 0:1], [t["pg"][:], t["den"][:]], ALU.mult)
                kb.v("dve", "tensor_tensor", WTS[:, s, 1:2], [WTS[:, s, 0:1], t["r"][:]], ALU.mult)
                kb.v("pool", "tensor_tensor", osum[i][:], [OH[:, s, 0:32], OH[:, s, 32:64]], ALU.add)
                kb.mm(pcnt[:, 0:32], self.ones[:], osum[i][:], start=(s == 0), stop=(s == ns - 1))
            cnt = ph.sb([128, 32], F32, "cnt")
            kb.v("dve", "tensor_copy", cnt[:], [pcnt[:, 0:32]])
            thr = ph.sb([128, 34], F32, "thr")
            kb.dma(thr[:], self.cin["thr"])
            cmp = ph.sb([128, 32, 34], F32, "cmp")
            kb.v("dve", "tensor_tensor", cmp[:], [cnt[:].unsqueeze(2).to_broadcast([128, 32, 34]), thr[:].unsqueeze(1).to_broadcast([128, 32, 34])], ALU.is_gt)
            padded = ph.sb([128, 32], F32, "padded")
            kb.v("dve", "tensor_reduce", padded[:], [cmp[:]], AX.X, ALU.add)
            kb.v("dve", "tensor_scalar", padded[:], [padded[:]], float(MOE_BS), None, ALU.mult)
            onesr = ph.sb([128, 32], F32, "onesr")
            kb.v("pool", "memset", onesr[:], [], 1.0)
            pend = ph.sb([128, 32], F32, "pend")
            kb.v("dve", "tensor_tensor_scan", pend[:], [onesr[:], padded[:]], 0.0, ALU.mult, ALU.add)
            kb.v("dve", "tensor_tensor", offs[:], [pend[:], padded[:]], ALU.subtract)
            bst = ph.sb([128, nb], F32, "bst")
            kb.dma(bst[:], self.cin["blk_start"])
            cmp2 = ph.sb([128, nb, 32], F32, "cmp2")
            kb.v("dve", "tensor_tensor", cmp2[:], [pend[:].unsqueeze(1).to_broadcast([128, nb, 32]), bst[:].unsqueeze(2).to_broadcast([128, nb, 32])], ALU.is_le)
            blke = ph.sb([128, nb], F32, "blke")
            kb.v("dve", "tensor_reduce", blke[:], [cmp2[:]], AX.X, ALU.add)
            kb.v("dve", "tensor_scalar", blke[:], [blke[:]], 31.0, None, ALU.min)
            iop = ph.sb([128, 1], F32, "iop")
            kb.dma(iop[:], self.cin["iota_p"])
            b1 = ph.sb([128, nb], F32, "b1")
            b2 = ph.sb([128, nb], F32, "b2")
            kb.v("dve", "tensor_scalar", b1[:], [blke[:]], 1024.0, iop[:, 0:1], ALU.mult, ALU.add)
            kb.v("dve", "tensor_scalar", b2[:], [blke[:]], 512.0, iop[:, 0:1], ALU.mult, ALU.add)
            for c in range(8):
                kb.v("dve", "tensor_scalar", I1[:, :, c], [b1[:]], float(c * 128), None, ALU.add)
            for c in range(4):
                kb.v("dve", "tensor_scalar", I2[:, :, c], [b2[:]], float(c * 128), None, ALU.add)
            tri = ph.sb([128, 128], F32, "trif")
            kb.dma(tri[:], self.cin["tri_lt_full"])
            trib = ph.sb([128, 128], BF16, "trib")
            kb.v("dve", "tensor_copy", trib[:], [tri[:]])
            kb.barrier()
            basep = ph.sb([128, 32], F32, "basep")
            kb.v("dve", "tensor_copy", basep[:], [offs[:]])
            pcx = [ph.ps() for _ in range(2)]
            pos = [ph.sb([128, 32], F32, "pos") for _ in range(2)]
            tm = [ph.sb([128, 32], F32, "tm") for _ in range(2)]
            dd = [ph.sb([128, 2], F32, "dd") for _ in range(2)]
            for s in range(ns):
                i = s % 2
                kb.v("pool", "tensor_tensor", osum[i][:], [OH[:, s, 0:32], OH[:, s, 32:64]], ALU.add)
                kb.mm(pcx[i][:, 0:32], trib[:], osum[i][:])
                kb.mm(pcx[i][:, 32:64], self.ones[:], osum[i][:])
                kb.v("dve", "tensor_tensor", pos[i][:], [pcx[i][:, 0:32], basep[:]], ALU.add)
                kb.v("dve", "tensor_tensor", basep[:], [pcx[i][:, 32:64], basep[:]], ALU.add)
                for k in range(2):
                    kb.v("dve", "tensor_tensor", tm[i][:], [OH[:, s, k * 32:(k + 1) * 32], pos[i][:]], ALU.mult)
                    kb.v("dve", "tensor_reduce", dd[i][:, k:k + 1], [tm[i][:]], AX.X, ALU.add)
                kb.v("dve", "tensor_copy", DEST[:, s, :], [dd[i][:]])
                ft = fts[i]
                kb.dma(ft[:], self.Fm[s * 128:(s + 1) * 128, :])
                for k in range(2):
                    kb.scatter(self.XS, ft[:], DEST[:, s, k:k + 1], wkey=("XS", "sc"))
        with kb.phase() as ph:
            W1 = [ph.sb([128, 8, 512], BF16, "W1") for _ in range(2)]
            W3 = [ph.sb([128, 8, 512], BF16, "W3") for _ in range(2)]
            W2 = [ph.sb([128, 4, D], BF16, "W2") for _ in range(2)]
            xs = [ph.sb([128, D], BF16, "xs") for _ in range(2)]
            xT = [ph.sb([128, 8, 512], BF16, "xT") for _ in range(2)]
            sil = [ph.sb([128, 512], F32, "sil") for _ in range(2)]
            hid = [ph.sb([128, 4, 512], BF16, "hid") for _ in range(2)]
            ysb = [ph.sb([128, D], F32, "ysb") for _ in range(2)]
            ptr = [ph.ps([128, 1024], BF16, "ptr") for _ in range(2)]
            p1 = [ph.ps() for _ in range(2)]
            p3 = [ph.ps() for _ in range(2)]
            py = [ph.ps() for _ in range(2)]
            u = 0
            for b in range(nb):
                i = b % 2
                for c in range(8):
                    kb.gather(W1[i][:, c, :], w1, I1[:, b, c:c + 1])
                    kb.gather(W3[i][:, c, :], w3, I1[:, b, c:c + 1])
                for c in range(4):
                    kb.gather(W2[i][:, c, :], w2, I2[:, b, c:c + 1])
                for s in range(4):
                    x = xs[u % 2]
                    pt = ptr[u % 2]
                    u += 1
                    r0 = b * MOE_BS + s * 128
                    kb.dma(x[:], self.XS[r0:r0 + 128, :], rkey=("XS", "sc"))
                    for c in range(8):
                        kb.tr(pt[:, c * 128:(c + 1) * 128], x[:, c * 128:(c + 1) * 128], self.ident[:])
                    self.evac(xT[i][:, :, s * 128:(s + 1) * 128], pt[:].rearrange("p (c t) -> p c t", c=8))
                for hc in range(4):
                    a, b3 = p1[hc % 2], p3[hc % 2]
                    for c in range(8):
                        kb.mm(a[:], W1[i][:, c, hc * 128:(hc + 1) * 128], xT[i][:, c, :], start=(c == 0), stop=(c == 7))
                    for c in range(8):
                        kb.mm(b3[:], W3[i][:, c, hc * 128:(hc + 1) * 128], xT[i][:, c, :], start=(c == 0), stop=(c == 7))
                    kb.act(sil[hc % 2][:], a[:], AF.Silu)
                    kb.v("dve", "tensor_tensor", hid[i][:, hc, :], [b3[:], sil[hc % 2][:]], ALU.mult)
                for s in range(4):
                    yb = ysb[s % 2]
                    for n in range(2):
                        pp = py[n]
                        for hc in range(4):
                            kb.mm(pp[:], hid[i][:, hc, s * 128:(s + 1) * 128], W2[i][:, hc, n * 512:(n + 1) * 512], start=(hc == 0), stop=(hc == 3))
                        self.evac(yb[:, n * 512:(n + 1) * 512], pp[:])
                    r0 = b * MOE_BS + s * 128
                    kb.dma(self.YS[r0:r0 + 128, :], yb[:], wkey=("YS", "w"))
        with kb.phase() as ph:
            y1 = [ph.sb([128, D], F32, "y1") for _ in range(2)]
            y2 = [ph.sb([128, D], F32, "y2") for _ in range(2)]
            hts = [ph.sb([128, D], F32, "ht") for _ in range(2)]
            for s in range(ns):
                i = s % 2
                mod = self.modC if s < 2 else self.modL
                kb.gather(y1[i][:], self.YS, DEST[:, s, 0:1], rkey=("YS", "w"))
                kb.gather(y2[i][:], self.YS, DEST[:, s, 1:2], rkey=("YS", "w"))
                kb.dma(hts[i][:], self.H[s * 128:(s + 1) * 128, :])
                kb.v("dve", "tensor_scalar", y1[i][:], [y1[i][:]], WTS[:, s, 0:1], None, ALU.mult)
                kb.v("dve", "scalar_tensor_tensor", y1[i][:], [y2[i][:], WTS[:, s, 1:2], y1[i][:]], ALU.mult, ALU.add)
                kb.v("pool", "tensor_tensor", y1[i][:], [y1[i][:], mod[5][:]], ALU.mult)
                kb.v("dve", "tensor_tensor", hts[i][:], [hts[i][:], y1[i][:]], ALU.add)
                kb.dma(self.H[s * 128:(s + 1) * 128, :], hts[i][:])
    def odd_layer(self, l):
        j = l // 2
        self.phase_mod(l)
        sm = {k: self.inp(f"o{j}_{k}", s) for k, s in ODD_SMALL_SHAPES.items()}
        self.phase_r1()
        self.phase_r2a(l, j, sm)
        self.phase_r2b(j, sm)
        self.phase_r3(j)
        self.phase_r4(l, j, sm)
        self.moe(l)

    def phase_r1(self):
        kb, cfg = self.kb, self.cfg
        if not hasattr(self, "XN"):
            self.XN = self.scratch("XN", [D, cfg.T], BF16)
        with kb.phase() as ph:
            def consume(ti, t0, TT, xT):
                kb.dma(self.XN[:, t0:t0 + TT].rearrange("(c p) t -> p c t", p=128), xT[:, :, 0:TT])
            self.norm_tiles_to_xT(ph, self.H, 1, 0, consume)

    def wload(self, ph, src, shape, name, view=None):
        t = ph.sb(shape, BF16, name)
        self.kb.dma(t[:], view if view is not None else src, eng="pool")
        return t

    def phase_r2a(self, l, j, sm):
        kb, cfg = self.kb, self.cfg
        T = cfg.T
        has_vres = j > 0
        wr = self.inp(f"rk_wr__{j}", [D, D])
        wk = self.inp(f"rk_wk__{j}", [D, D])
        wv = self.inp(f"rk_wv__{j}", [D, D])
        w1 = self.inp(f"rk_w1__{j}", [2, D, 64])
        a1 = self.inp(f"rk_a1__{j}", [2, D, 64])
        g1 = self.inp(f"rk_g1__{j}", [D, 160])
        g2 = self.inp(f"rk_g2__{j}", [160, D])
        if has_vres:
            v1 = self.inp(f"rk_v1__{j - 1}", [D, 32])
            v2 = self.inp(f"rk_v2__{j - 1}", [32, D])
        if not hasattr(self, "RF"):
            self.RF = self.scratch("RF", [D, T], BF16)
            self.KFm = self.scratch("KFm", [D, T], BF16)
            self.KKF = self.scratch("KKF", [D, T], BF16)
            self.TW = self.scratch("TW", [2, 64, T], BF16)
            self.TA = self.scratch("TA", [2, 64, T], BF16)
            self.VTM = self.scratch("VTM", [T, D], BF16)
            self.VF = self.scratch("VF", [T, D], F32)
            self.GATE = self.scratch("GATE", [T, D], BF16)
        last = len(cfg.tiles) - 1
        with kb.phase() as ph:
            r3 = lambda w: w.rearrange("(c p) n -> p c n", p=128)
            Wr = self.wload(ph, None, [128, 8, D], "Wr", r3(wr))
            Wk = self.wload(ph, None, [128, 8, D], "Wk", r3(wk))
            Wv = self.wload(ph, None, [128, 8, D], "Wv", r3(wv))
            W1 = [self.wload(ph, None, [128, 8, 64], "W1", r3(w1[d])) for d in range(2)]
            A1 = [self.wload(ph, None, [128, 8, 64], "A1", r3(a1[d])) for d in range(2)]
            G1 = self.wload(ph, None, [128, 8, 160], "G1", r3(g1))
            G2a = self.wload(ph, None, [128, D], "G2a", g2[0:128, :])
            G2b = self.wload(ph, None, [32, D], "G2b", g2[128:160, :])
            if has_vres:
                V1 = self.wload(ph, None, [128, 8, 32], "V1", r3(v1))
                V2 = self.wload(ph, None, [32, D], "V2", v2)
                v0b = ph.sb([128, D], F32, "v0b")
                kb.dma(v0b[:], sm["v0"].broadcast_to([128, D]))
            mu = ph.sb([128, 6, 8], F32, "mu")
            kb.dma(mu[:], sm["mu"])
            kkc = ph.sb([128, 8], F32, "kkc")
            kb.dma(kkc[:], sm["kkcol"])
            bones = ph.sb([128, 128], F32, "bonesf")
            kb.dma(bones[:], self.cin["m_same"])
            bonesb = ph.sb([128, 128], BF16, "bonesb")
            kb.v("dve", "tensor_copy", bonesb[:], [bones[:]])
            x = ph.sb([128, 8, 514], BF16, "x")
            tmpf = ph.sb([128, 8, 512], F32, "tmpf")
            xx = ph.sb([128, 8, 512], BF16, "xx")
            xms = [ph.sb([128, 8, 512], BF16, "xm") for _ in range(2)]
            pm = [ph.ps() for _ in range(4)]
            pR = ph.ps()
            ob = [ph.sb([128, 512], BF16, "ob") for _ in range(3)]
            of = [ph.sb([128, 512], F32, "of") for _ in range(2)]
            sq = ph.sb([128, 512], BF16, "sq")
            rs = ph.sb([128, 512], F32, "rs")
            sg = ph.sb([128, 2, 512], BF16, "sg")
            lvT = ph.sb([32, 512], BF16, "lvT")
            vf = [ph.sb([128, 512], F32, "vf") for _ in range(2)]
            cnt = [0]

            def mix(i, TT):
                xm = xms[cnt[0] % 2]
                cnt[0] += 1
                for c in range(8):
                    kb.v("dve", "scalar_tensor_tensor", xm[:, c, 0:TT], [xx[:, c, 0:TT], mu[:, i, c:c + 1], x[:, c, 1:TT + 1]], ALU.mult, ALU.add)
                return xm

            def proj_fm(xm, Wt, ncol0, rows, TT, pmi):
                p = pm[pmi % 4]
                for c in range(8):
                    kb.mm(p[0:rows, 0:TT], Wt[:, c, ncol0:ncol0 + rows], xm[:, c, 0:TT], start=(c == 0), stop=(c == 7))
                return p

            u = 0
            for ti, (t0, TT) in enumerate(cfg.tiles):
                lo = 0 if ti in (0, 1) else 1
                hi = 0 if ti in (0, last) else 1
                kb.dma(x[:, :, 1 - lo:TT + 1 + hi], self.XN[:, t0 - lo:t0 + TT + hi].rearrange("(c p) t -> p c t", p=128))
                if not lo:
                    kb.v("pool", "memset", x[:, :, 0:1], [], 0.0)
                if not hi:
                    kb.v("pool", "memset", x[:, :, TT + 1:TT + 2], [], 0.0)
                kb.v("dve", "tensor_tensor", tmpf[:, :, 0:TT], [x[:, :, 0:TT], x[:, :, 2:TT + 2]], ALU.add)
                kb.v("dve", "scalar_tensor_tensor", xx[:, :, 0:TT], [tmpf[:, :, 0:TT], 0.5, x[:, :, 1:TT + 1]], ALU.mult, ALU.subtract)
                xm = mix(0, TT)
                for m in range(8):
                    p = proj_fm(xm, Wr, m * 128, 128, TT, u)
                    o = ob[u % 3]
                    u += 1
                    self.evac(o[:, 0:TT], p[:, 0:TT])
                    kb.dma(self.RF[m * 128:(m + 1) * 128, t0:t0 + TT], o[:, 0:TT])
                xm = mix(2, TT)
                for m in range(8):
                    p = proj_fm(xm, Wk, m * 128, 128, TT, u)
                    o = ob[u % 3]
                    f = of[u % 2]
                    u += 1
                    kb.act(o[:, 0:TT], p[:, 0:TT], AF.Copy)
                    kb.dma(self.KFm[m * 128:(m + 1) * 128, t0:t0 + TT], o[:, 0:TT])
                    kb.v("dve", "tensor_scalar", f[:, 0:TT], [p[:, 0:TT]], kkc[:, m:m + 1], None, ALU.mult)
                    kb.act(sq[:, 0:TT], f[:, 0:TT], AF.Square)
                    kb.mm(pR[:, 0:TT], bonesb[:], sq[:, 0:TT])
                    rstd_inplace(kb, rs[:, 0:TT], 1.0, EPS, src=pR[:, 0:TT])
                    o2 = ob[u % 3]
                    u += 1
                    kb.v("dve", "tensor_tensor", o2[:, 0:TT], [f[:, 0:TT], rs[:, 0:TT]], ALU.mult)
                    kb.dma(self.KKF[m * 128:(m + 1) * 128, t0:t0 + TT], o2[:, 0:TT])
                xm = mix(3, TT)
                if has_vres:
                    p = proj_fm(xm, V1, 0, 32, TT, u)
                    u += 1
                    kb.act(lvT[:, 0:TT], p[0:32, 0:TT], AF.Copy)
                for s in range(TT // 128):
                    r0 = t0 + s * 128
                    for n in range(2):
                        p = pm[u % 4]
                        o = ob[u % 3]
                        f = of[u % 2]
                        u += 1
                        for c in range(8):
                            kb.mm(p[:], xm[:, c, s * 128:(s + 1) * 128], Wv[:, c, n * 512:(n + 1) * 512], start=(c == 0), stop=(c == 7))
                        if not has_vres:
                            kb.act(f[:], p[:], AF.Copy)
                            kb.dma(self.VF[r0:r0 + 128, n * 512:(n + 1) * 512], f[:])
                            kb.v("dve", "tensor_copy", o[:], [p[:]])
                        else:
                            p2 = pm[u % 4]
                            u += 1
                            vft = vf[n]
                            kb.dma(vft[:], self.VF[r0:r0 + 128, n * 512:(n + 1) * 512])
                            kb.mm(p2[:], lvT[:, s * 128:(s + 1) * 128], V2[:, n * 512:(n + 1) * 512])
                            kb.v("dve", "tensor_tensor", f[:], [p2[:], v0b[:, n * 512:(n + 1) * 512]], ALU.add)
                            kb.act(f[:], f[:], AF.Sigmoid)
                            kb.v("dve", "tensor_tensor", vft[:], [vft[:], p[:]], ALU.subtract)
                            kb.v("pool", "tensor_tensor", vft[:], [vft[:], f[:]], ALU.mult)
                            kb.v("dve", "tensor_tensor", o[:], [vft[:], p[:]], ALU.add)
                        kb.dma(self.VTM[r0:r0 + 128, n * 512:(n + 1) * 512], o[:])
                xm = mix(5, TT)
                p = proj_fm(xm, G1, 0, 128, TT, u)
                u += 1
                kb.act(sg[:, 0, 0:TT], p[:, 0:TT], AF.Sigmoid)
                p = proj_fm(xm, G1, 128, 32, TT, u)
                u += 1
                kb.act(sg[0:32, 1, 0:TT], p[0:32, 0:TT], AF.Sigmoid)
                for s in range(TT // 128):
                    r0 = t0 + s * 128
                    for n in range(2):
                        p = pm[u % 4]
                        o = ob[u % 3]
                        u += 1
                        kb.mm(p[:], sg[:, 0, s * 128:(s + 1) * 128], G2a[:, n * 512:(n + 1) * 512], start=True, stop=False)
                        kb.mm(p[:], sg[0:32, 1, s * 128:(s + 1) * 128], G2b[:, n * 512:(n + 1) * 512], start=False, stop=True)
                        self.evac(o[:], p[:])
                        kb.dma(self.GATE[r0:r0 + 128, n * 512:(n + 1) * 512], o[:])
                xm = mix(1, TT)
                for d in range(2):
                    p = proj_fm(xm, W1[d], 0, 64, TT, u)
                    o = ob[u % 3]
                    u += 1
                    kb.act(o[0:64, 0:TT], p[0:64, 0:TT], AF.Tanh)
                    kb.dma(self.TW[d, :, t0:t0 + TT], o[0:64, 0:TT])
                xm = mix(4, TT)
                for d in range(2):
                    p = proj_fm(xm, A1[d], 0, 64, TT, u)
                    o = ob[u % 3]
                    u += 1
                    kb.act(o[0:64, 0:TT], p[0:64, 0:TT], AF.Copy)
                    kb.dma(self.TA[d, :, t0:t0 + TT], o[0:64, 0:TT])

    def phase_r2b(self, j, sm):
        kb, cfg = self.kb, self.cfg
        T = cfg.T
        nch = T // 64
        w2 = self.inp(f"rk_w2__{j}", [2, 64, D])
        a2 = self.inp(f"rk_a2__{j}", [2, 64, D])
        if not hasattr(self, "RT"):
            for nm in ("RT", "AT", "BT", "KT"):
                setattr(self, nm, [self.scratch(f"{nm}{d}", [D, T], BF16) for d in range(2)])
            for nm in ("ATM", "KHAT", "BHAT"):
                setattr(self, nm, [self.scratch(f"{nm}{d}", [T, D], BF16) for d in range(2)])
            self.GCd = [self.scratch(f"GC{d}", [D, nch], F32) for d in range(2)]
            self.SBN = self.scratch("SBN", [T, 16], F32)
        with kb.phase() as ph:
            W2 = [self.wload(ph, None, [64, D], "W2", w2[d]) for d in range(2)]
            A2 = [self.wload(ph, None, [64, D], "A2", a2[d]) for d in range(2)]
            w0 = ph.sb([128, 2, 8], F32, "w0")
            a0 = ph.sb([128, 2, 8], F32, "a0")
            kac = ph.sb([128, 8], F32, "kac")
            omka = ph.sb([128, 8], F32, "omka")
            rkc = ph.sb([128, 8], F32, "rkc")
            kb.dma(w0[:], sm["w0"])
            kb.dma(a0[:], sm["a0"])
            kb.dma(kac[:], sm["kacol"])
            kb.dma(rkc[:], sm["rkcol"])
            kb.v("dve", "tensor_scalar", omka[:], [kac[:]], -1.0, 1.0, ALU.mult, ALU.add)
            rmask = ph.sb([128, 512], F32, "rmask")
            kb.dma(rmask[:], self.cin["rmask"])
            hsel = ph.sb([128, 2], F32, "hself")
            kb.dma(hsel[:], self.cin["hsel"])
            hselb = ph.sb([128, 2], BF16, "hselb")
            kb.v("dve", "tensor_copy", hselb[:], [hsel[:]])
            tw = [ph.sb([64, 512], BF16, "tw") for _ in range(2)]
            ta = [ph.sb([64, 512], BF16, "ta") for _ in range(2)]
            rt = [ph.sb([128, 512], BF16, "rt") for _ in range(2)]
            kt = [ph.sb([128, 512], BF16, "kt") for _ in range(2)]
            kkt = [ph.sb([128, 512], BF16, "kkt") for _ in range(2)]
            F = lambda nm: ph.sb([128, 512], F32, nm)
            lw, av, keys, bv, cs, tot, e1, e2, ex, tmp, prod = F("lw"), F("av"), F("keys"), F("bv"), F("cs"), F("tot"), F("e1"), F("e2"), F("ex"), F("tmp"), F("prod")
            obs = [ph.sb([128, 512], BF16, "ob") for _ in range(4)]
            tms = [ph.sb([128, 3, 512], BF16, "tms") for _ in range(2)]
            tsb = [ph.sb([128, 4, 128], BF16, "tsb") for _ in range(3)]
            gcs = ph.sb([128, 8], F32, "gcs")
            prodb = ph.sb([128, 512], BF16, "prodb")
            sbn = ph.sb([128, 4, 16], F32, "sbn")
            pz = [ph.ps() for _ in range(2)]
            pa = [ph.ps() for _ in range(2)]
            ptr = [ph.ps([128, 1024], BF16, "ptr") for _ in range(3)]
            psb = ph.ps()
            u = 0
            tq = 0
            for ti, (t0, TT) in enumerate(cfg.tiles):
                ns = TT // 128
                nc_ = TT // 64
                for d in range(2):
                    kb.dma(tw[d][:, 0:TT], self.TW[d, :, t0:t0 + TT])
                    kb.dma(ta[d][:, 0:TT], self.TA[d, :, t0:t0 + TT])
                for c in range(8):
                    i = c % 2
                    kb.dma(rt[i][:, 0:TT], self.RF[c * 128:(c + 1) * 128, t0:t0 + TT])
                    kb.dma(kt[i][:, 0:TT], self.KFm[c * 128:(c + 1) * 128, t0:t0 + TT])
                    kb.dma(kkt[i][:, 0:TT], self.KKF[c * 128:(c + 1) * 128, t0:t0 + TT])
                    for d in range(2):
                        z, a_ = pz[u % 2], pa[u % 2]
                        tm3 = tms[u % 2]
                        u += 1
                        kb.mm(z[:, 0:TT], W2[d][:, c * 128:(c + 1) * 128], tw[d][:, 0:TT])
                        kb.mm(a_[:, 0:TT], A2[d][:, c * 128:(c + 1) * 128], ta[d][:, 0:TT])
                        kb.act(lw[:, 0:TT], z[:, 0:TT], AF.Sigmoid, bias=w0[:, d, c:c + 1])
                        kb.v("dve", "tensor_scalar", lw[:, 0:TT], [lw[:, 0:TT]], -0.6065306597126334, None, ALU.mult)
                        kb.act(av[:, 0:TT], a_[:, 0:TT], AF.Sigmoid, bias=a0[:, d, c:c + 1])
                        kb.v("dve", "tensor_scalar", tmp[:, 0:TT], [av[:, 0:TT]], kac[:, c:c + 1], omka[:, c:c + 1], ALU.mult, ALU.add)
                        kb.v("dve", "tensor_tensor", keys[:, 0:TT], [tmp[:, 0:TT], kt[i][:, 0:TT]], ALU.mult)
                        kb.v("pool", "tensor_tensor", bv[:, 0:TT], [av[:, 0:TT], kkt[i][:, 0:TT]], ALU.mult)
                        kb.v("dve", "tensor_tensor_scan", cs[:, 0:TT], [rmask[:, 0:TT], lw[:, 0:TT]], 0.0, ALU.mult, ALU.add)
                        cs3 = cs[:, 0:TT].rearrange("p (n k) -> p n k", k=64)
                        kb.v("pool", "tensor_copy", tot[:, 0:TT].rearrange("p (n k) -> p n k", k=64), [cs3[:, :, 63:64].to_broadcast([128, nc_, 64])])
                        if d == 0:
                            E1 = cs
                        else:
                            kb.v("dve", "tensor_tensor", e1[:, 0:TT], [tot[:, 0:TT], cs[:, 0:TT]], ALU.subtract)
                            kb.v("dve", "tensor_tensor", e1[:, 0:TT], [e1[:, 0:TT], lw[:, 0:TT]], ALU.add)
                            E1 = e1
                        kb.v("pool", "tensor_tensor", e2[:, 0:TT], [E1[:, 0:TT], lw[:, 0:TT]], ALU.subtract)
                        o = obs[tq % 4]; tq += 1
                        kb.act(ex[:, 0:TT], E1[:, 0:TT], AF.Exp)
                        kb.v("dve", "tensor_tensor", o[:, 0:TT], [ex[:, 0:TT], rt[i][:, 0:TT]], ALU.mult)
                        kb.dma(self.RT[d][c * 128:(c + 1) * 128, t0:t0 + TT], o[:, 0:TT])
                        o = obs[tq % 4]; tq += 1
                        kb.act(ex[:, 0:TT], e2[:, 0:TT], AF.Exp)
                        kb.v("dve", "scalar_tensor_tensor", o[:, 0:TT], [ex[:, 0:TT], -1.0, kkt[i][:, 0:TT]], ALU.mult, ALU.mult)
                        kb.dma(self.AT[d][c * 128:(c + 1) * 128, t0:t0 + TT], o[:, 0:TT])
                        kb.v("pool", "tensor_copy", tm3[:, 0, 0:TT], [o[:, 0:TT]])
                        kb.act(ex[:, 0:TT], E1[:, 0:TT], AF.Exp, scale=-1.0)
                        o = obs[tq % 4]; tq += 1
                        kb.v("dve", "tensor_tensor", o[:, 0:TT], [ex[:, 0:TT], bv[:, 0:TT]], ALU.mult)
                        kb.dma(self.BT[d][c * 128:(c + 1) * 128, t0:t0 + TT], o[:, 0:TT])
                        o = obs[tq % 4]; tq += 1
                        kb.v("dve", "tensor_tensor", o[:, 0:TT], [ex[:, 0:TT], keys[:, 0:TT]], ALU.mult)
                        kb.dma(self.KT[d][c * 128:(c + 1) * 128, t0:t0 + TT], o[:, 0:TT])
                        kb.v("pool", "tensor_tensor", tmp[:, 0:TT], [tot[:, 0:TT], E1[:, 0:TT]], ALU.subtract)
                        kb.act(ex[:, 0:TT], tmp[:, 0:TT], AF.Exp)
                        kb.v("dve", "tensor_tensor", tm3[:, 1, 0:TT], [ex[:, 0:TT], keys[:, 0:TT]], ALU.mult)
                        kb.v("dve", "tensor_tensor", tm3[:, 2, 0:TT], [ex[:, 0:TT], bv[:, 0:TT]], ALU.mult)
                        kb.act(gcs[:, 0:nc_], cs3[:, :, 63], AF.Exp)
                        kb.dma(self.GCd[d][c * 128:(c + 1) * 128, t0 // 64:t0 // 64 + nc_], gcs[:, 0:nc_])
                        for k3, dst in enumerate((self.ATM[d], self.KHAT[d], self.BHAT[d])):
                            pt, tb = ptr[k3], tsb[k3]
                            for s in range(ns):
                                kb.tr(pt[:, s * 128:(s + 1) * 128], tm3[:, k3, s * 128:(s + 1) * 128], self.ident[:])
                            self.evac(tb[:, 0:ns, :], pt[:, 0:TT].rearrange("p (s c) -> p s c", c=128))
                            kb.dma(dst[t0:t0 + TT, c * 128:(c + 1) * 128].rearrange("(s p) c -> p s c", p=128), tb[:, 0:ns, :])
                        if d == 0:
                            kb.v("dve", "scalar_tensor_tensor", prod[:, 0:TT], [keys[:, 0:TT], rkc[:, c:c + 1], rt[i][:, 0:TT]], ALU.mult, ALU.mult)
                        else:
                            kb.v("dve", "scalar_tensor_tensor", tmp[:, 0:TT], [keys[:, 0:TT], rkc[:, c:c + 1], rt[i][:, 0:TT]], ALU.mult, ALU.mult)
                            kb.v("dve", "scalar_tensor_tensor", prodb[:, 0:TT], [tmp[:, 0:TT], 1.0, prod[:, 0:TT]], ALU.mult, ALU.add)
                    for s in range(ns):
                        kb.mm(psb[:, (s * 8 + c) * 2:(s * 8 + c) * 2 + 2], prodb[:, s * 128:(s + 1) * 128], hselb[:])
                kb.v("dve", "tensor_scalar", sbn[:, 0:ns, :], [psb[:, 0:ns * 16].rearrange("p (s h) -> p s h", h=16)], 0.5, None, ALU.mult)
                kb.dma(self.SBN[t0:t0 + TT, :].rearrange("(s p) h -> p s h", p=128), sbn[:, 0:ns, :])
    def phase_r3(self, j):
        kb, cfg = self.kb, self.cfg
        T, nsub = cfg.T, cfg.nsub
        if not hasattr(self, "YD"):
            self.YD = [self.scratch(f"YD{d}", [T, D]) for d in range(2)]
        fwd = list(range(nsub))
        bwd = [1, 0] + list(range(nsub - 1, 1, -1))
        with kb.phase() as ph:
            mle = self.load_const_tile(ph, "m_le")
            mge = self.load_const_tile(ph, "m_ge")
            mlt = self.load_const_tile(ph, "m_lt")
            mgt = self.load_const_tile(ph, "m_gt")
            Hs = [[ph.sb([128, 64], F32, f"Hs{d}{c}") for c in range(8)] for d in range(2)]
            Hb = [[ph.sb([128, 64], BF16, f"Hb{d}{c}") for c in range(8)] for d in range(2)]
            for d in range(2):
                for c in range(8):
                    kb.v("pool", "memset", Hs[d][c][:], [], 0.0)
                    kb.v("pool", "memset", Hb[d][c][:], [], 0.0)
            banks = [ph.ps() for _ in range(8)]
            slot = [0]

            def ps128():
                i = slot[0] % 8
                slot[0] += 1
                return banks[i]

            UB = []
            for d in range(2):
                b = {}
                for nm in ("RT", "AT", "BT", "KT"):
                    b[nm] = ph.sb([128, 8, 128], BF16, nm)
                for nm in ("ATM", "KHAT", "BHAT", "V"):
                    b[nm] = ph.sb([128, D], BF16, nm)
                b["GC"] = ph.sb([128, 8, 2], F32, "GC")
                b["ysb"] = ph.sb([128, D], F32, "ysb")
                UB.append(b)
            HB = []
            for i in range(2):
                b = {}
                for nm in ("X0", "X1", "XT0", "XT1", "AT0", "AT1", "AakT", "ArkT", "ArbT", "PT"):
                    b[nm] = ph.sb([128, 128], BF16, nm)
                b["Z"] = ph.sb([128, 64], BF16, "Z")
                b["U"] = ph.sb([128, 64], BF16, "U")
                b["U0"] = ph.sb([128, 64], F32, "U0")
                HB.append(b)
            hcount = 0
            for step in range(nsub):
                for d in range(2):
                    sb_ = fwd[step] if d == 0 else bwd[step]
                    r0 = sb_ * 128
                    B = UB[d]
                    for nm, src in (("RT", self.RT), ("AT", self.AT), ("BT", self.BT), ("KT", self.KT)):
                        kb.dma(B[nm][:], src[d][:, r0:r0 + 128].rearrange("(c p) t -> p c t", p=128))
                    for nm, src in (("ATM", self.ATM[d]), ("KHAT", self.KHAT[d]), ("BHAT", self.BHAT[d]), ("V", self.VTM)):
                        kb.dma(B[nm][:], src[r0:r0 + 128, :])
                    kb.dma(B["GC"][:], self.GCd[d][:, sb_ * 2:sb_ * 2 + 2].rearrange("(c p) n -> p c n", p=128))
                    MsT = mlt if d == 0 else mgt
                    Ms = mgt if d == 0 else mlt
                    MiT = mle if d == 0 else mge
                    for h in range(16):
                        c = h // 2
                        rs = slice(64 * (h % 2), 64 * (h % 2) + 64)
                        hs = slice(h * 64, (h + 1) * 64)
                        Hh = HB[hcount % 2]
                        hcount += 1
                        RT, AT_, BT, KT = B["RT"][rs, c, :], B["AT"][rs, c, :], B["BT"][rs, c, :], B["KT"][rs, c, :]
                        p = ps128()
                        kb.mm(p[:, 0:128], BT, AT_)
                        kb.v("dve", "tensor_tensor", Hh["XT0"][:], [p[:, 0:128], MsT[:]], ALU.mult)
                        p = ps128()
                        kb.mm(p[:, 0:128], AT_, BT)
                        kb.v("dve", "tensor_tensor", Hh["X0"][:], [p[:, 0:128], Ms[:]], ALU.mult)
                        p = ps128()
                        kb.mm(p[:, 0:128], KT, AT_)
                        kb.v("dve", "tensor_tensor", Hh["AakT"][:], [p[:, 0:128], MsT[:]], ALU.mult)
                        p = ps128()
                        kb.mm(p[:, 0:128], KT, RT)
                        kb.v("dve", "tensor_tensor", Hh["ArkT"][:], [p[:, 0:128], MiT[:]], ALU.mult)
                        p = ps128()
                        kb.mm(p[:, 0:128], BT, RT)
                        kb.v("dve", "tensor_tensor", Hh["ArbT"][:], [p[:, 0:128], MiT[:]], ALU.mult)
                        kb.v("pool", "tensor_tensor", Hh["AT0"][:], [Hh["XT0"][:], self.ident[:]], ALU.add)
                        X, XT, AT = Hh["X0"], Hh["XT0"], Hh["AT0"]
                        for js in range(1, 6):
                            Xn = Hh["X1"] if X is Hh["X0"] else Hh["X0"]
                            XTn = Hh["XT1"] if XT is Hh["XT0"] else Hh["XT0"]
                            ATn = Hh["AT1"] if AT is Hh["AT0"] else Hh["AT0"]
                            px = ps128()
                            kb.mm(px[:, 0:128], XT[:], X[:])
                            if js < 5:
                                pxT = ps128()
                                kb.mm(pxT[:, 0:128], X[:], XT[:])
                            kb.act(Xn[:], px[:, 0:128], AF.Copy)
                            if js < 5:
                                kb.v("dve", "tensor_copy", XTn[:], [pxT[:, 0:128]])
                            pa = ps128()
                            kb.mm(pa[:, 0:128], Xn[:], AT[:])
                            kb.v("dve", "tensor_tensor", ATn[:], [pa[:, 0:128], AT[:]], ALU.add)
                            X, XT, AT = Xn, XTn, ATn
                        p = ps128()
                        kb.mm(p[:, 0:64], Hh["AakT"][:], B["V"][:, hs])
                        kb.act(Hh["Z"][:], p[:, 0:64], AF.Copy)
                        p = ps128()
                        kb.mm(p[:, 0:64], AT[:], Hh["Z"][:])
                        kb.act(Hh["U0"][:], p[:, 0:64], AF.Copy)
                        p = ps128()
                        kb.mm(p[rs, 0:128], B["ATM"][:, hs], AT[:])
                        kb.act(Hh["PT"][rs, :], p[rs, 0:128], AF.Copy)
                        for half in ((0, 1) if d == 0 else (1, 0)):
                            q = slice(half * 64, half * 64 + 64)
                            p1 = ps128()
                            kb.mm(p1[q, 0:64], Hh["PT"][rs, q], Hb[d][c][rs, :])
                            kb.v("dve", "tensor_tensor", Hh["U"][q, :], [p1[q, 0:64], Hh["U0"][q, :]], ALU.add)
                            p2a = ps128()
                            kb.mm(p2a[q, 0:64], B["RT"][rs, c, q], Hb[d][c][rs, :])
                            kb.act(B["ysb"][q, hs], p2a[q, 0:64], AF.Copy)
                            p2 = ps128()
                            kb.mm(p2[q, 0:64], Hh["ArkT"][q, q], B["V"][q, hs], start=True, stop=False)
                            kb.mm(p2[q, 0:64], Hh["ArbT"][q, q], Hh["U"][q, :], start=False, stop=True)
                            kb.v("dve", "tensor_tensor", B["ysb"][q, hs], [p2[q, 0:64], B["ysb"][q, hs]], ALU.add)
                            p3 = ps128()
                            kb.mm(p3[rs, 0:64], B["KHAT"][q, hs], B["V"][q, hs], start=True, stop=False)
                            kb.mm(p3[rs, 0:64], B["BHAT"][q, hs], Hh["U"][q, :], start=False, stop=True)
                            kb.v("dve", "scalar_tensor_tensor", Hs[d][c][rs, :], [Hs[d][c][rs, :], B["GC"][rs, c, half:half + 1], p3[rs, 0:64]], ALU.mult, ALU.add)
                            kb.act(Hb[d][c][rs, :], Hs[d][c][rs, :], AF.Copy)
                    kb.dma(self.YD[d][r0:r0 + 128, :], B["ysb"][:])

    def phase_r4(self, l, j, sm):
        kb, cfg = self.kb, self.cfg
        Wout = self.inp(f"rk_wo__{j}", [D, D])
        self.Fm = self.scratch(f"F{l}", [cfg.T, D], BF16)
        with kb.phase() as ph:
            Wo = ph.sb([128, 8, D], BF16, "Wo")
            kb.dma(Wo[:], Wout.rearrange("(c p) n -> p c n", p=128), eng="pool")
            lnw = ph.sb([128, D], F32, "lnw")
            lnb = ph.sb([128, D], F32, "lnb")
            kb.dma(lnw[:], sm["lnw"].broadcast_to([128, D]))
            kb.dma(lnb[:], sm["lnb"].broadcast_to([128, D]))
            y0 = [ph.sb([128, D], F32, "y0") for _ in range(2)]
            y1 = [ph.sb([128, D], F32, "y1") for _ in range(2)]
            vt = [ph.sb([128, D], BF16, "vt") for _ in range(2)]
            gt = [ph.sb([128, D], BF16, "gt") for _ in range(2)]
            sbn = [ph.sb([128, 16], F32, "sbn") for _ in range(2)]
            st = [ph.sb([128, 16], F32, "st") for _ in range(2)]
            sqt = ph.sb([128, D], F32, "sqt")
            bon = ph.sb([128, D], F32, "bon")
            zt = [ph.sb([128, D], BF16, "zt") for _ in range(2)]
            XT = [ph.sb([128, 8, 128], BF16, "XT") for _ in range(2)]
            ptr = [ph.ps([128, 1024], BF16, "ptr") for _ in range(2)]
            py = [ph.ps() for _ in range(4)]
            hts = [ph.sb([128, D], F32, "ht") for _ in range(2)]
            tmp = [ph.sb([128, D], F32, "tmp") for _ in range(2)]
            junk = ph.sb([128, D], F32, "junk")
            sss = [ph.sb([128, 1], F32, "ss") for _ in range(2)]
            tmp2 = [ph.sb([128, D], F32, "tmp2") for _ in range(2)]
            fb = [ph.sb([128, D], BF16, "fb") for _ in range(2)]
            v3 = lambda t: t[:].rearrange("p (h n) -> p h n", n=64)
            b3 = lambda t: t[:].unsqueeze(2).to_broadcast([128, 16, 64])
            for s in range(cfg.nsub):
                mod = self.modC if s < 2 else self.modL
                r0 = s * 128
                i = s % 2
                kb.dma(y0[i][:], self.YD[0][r0:r0 + 128, :])
                kb.dma(y1[i][:], self.YD[1][r0:r0 + 128, :])
                kb.dma(vt[i][:], self.VTM[r0:r0 + 128, :])
                kb.dma(gt[i][:], self.GATE[r0:r0 + 128, :])
                kb.dma(sbn[i][:], self.SBN[r0:r0 + 128, :])
                kb.dma(hts[i][:], self.H[r0:r0 + 128, :])
                kb.v("pool", "tensor_tensor", y0[i][:], [y0[i][:], y1[i][:]], ALU.add)
                kb.v("dve", "tensor_reduce", st[i][:], [v3(y0[i])], AX.X, ALU.add)
                kb.v("dve", "tensor_scalar", st[i][:], [st[i][:]], 1.0 / 64, None, ALU.mult)
                kb.v("dve", "tensor_tensor", v3(y0[i]), [v3(y0[i]), b3(st[i])], ALU.subtract)
                kb.v("pool", "tensor_tensor", sqt[:], [y0[i][:], y0[i][:]], ALU.mult)
                kb.v("dve", "tensor_reduce", st[i][:], [v3(sqt)], AX.X, ALU.add)
                rstd_inplace(kb, st[i][:], 1.0 / 64, 64e-5)
                kb.v("dve", "tensor_tensor", v3(y0[i]), [v3(y0[i]), b3(st[i])], ALU.mult)
                kb.v("pool", "tensor_tensor", y0[i][:], [y0[i][:], lnw[:]], ALU.mult)
                kb.v("pool", "tensor_tensor", y0[i][:], [y0[i][:], lnb[:]], ALU.add)
                kb.v("dve", "tensor_tensor", v3(bon), [v3(vt[i]), b3(sbn[i])], ALU.mult)
                kb.v("dve", "tensor_tensor", y0[i][:], [y0[i][:], bon[:]], ALU.add)
                kb.v("pool", "tensor_tensor", zt[i][:], [y0[i][:], gt[i][:]], ALU.mult)
                for c in range(8):
                    kb.tr(ptr[i][:, c * 128:(c + 1) * 128], zt[i][:, c * 128:(c + 1) * 128], self.ident[:])
                self.evac(XT[i][:], ptr[i][:].rearrange("p (c t) -> p c t", c=8))
                for n in range(2):
                    pp = py[(2 * s + n) % 4]
                    for c in range(8):
                        kb.mm(pp[:], XT[i][:, c, :], Wo[:, c, n * 512:(n + 1) * 512], start=(c == 0), stop=(c == 7))
                    kb.v("dve", "tensor_tensor", tmp[i][:, n * 512:(n + 1) * 512], [pp[:], mod[2][:, n * 512:(n + 1) * 512]], ALU.mult)
                kb.v("pool", "tensor_tensor", hts[i][:], [tmp[i][:], hts[i][:]], ALU.add)
                kb.dma(self.H[r0:r0 + 128, :], hts[i][:])
                self.norm_sub(hts[i][:], junk[:], sss[i][:], tmp2[i][:], fb[i][:], mod[4][:], mod[3][:])
                kb.dma(self.Fm[r0:r0 + 128, :], fb[i][:])
import re


def resolve_input(name, cfg, inp, b, consts, cache):
    if name == "h0":
        return np.ascontiguousarray(np.concatenate([inp["ctx"][b], inp["x"][b][:cfg.T - CTX]], 0))
    if name == "cvec":
        return np.ascontiguousarray(np.concatenate([inp["c"][b].reshape(8, 128).T, inp["c_ctx"].reshape(8, 128).T], 1))
    if name.startswith("c_"):
        return consts[name[2:]]
    key = ("shared", name)
    if key in cache:
        return cache[key]
    m = re.match(r"^e(\d+)_(\w+)$", name)
    if m:
        j = int(m.group(1))
        if ("es", j) not in cache:
            cache[("es", j)] = host_even_smalls(inp, j)
        arr = cache[("es", j)][m.group(2)]
    else:
        m = re.match(r"^o(\d+)_(\w+)$", name)
        if m:
            j = int(m.group(1))
            if ("os", j) not in cache:
                cache[("os", j)] = host_odd_smalls(inp, j)
            arr = cache[("os", j)][m.group(2)]
        else:
            m = re.match(r"^(.+)__(\d+)$", name)
            base, idx = m.group(1), int(m.group(2))
            a = inp[base][idx]
            if a.ndim == 1:
                a = a.reshape(1, -1)
            elif base in ("moe_w1", "moe_w3", "moe_w2"):
                a = a.reshape(-1, a.shape[-1])
            arr = np.ascontiguousarray(a)
    cache[key] = arr
    return arr


_PROG_CACHE = {}


def get_prog(cfg_key):
    if cfg_key not in _PROG_CACHE:
        nlt, layers, debug = cfg_key
        cfg = Cfg(nlt=nlt, layers=layers, debug=debug)
        p = Prog(cfg)
        p.setup()
        for l in cfg.layers:
            if l % 2 == 0:
                p.even_layer(l)
            else:
                p.odd_layer(l)
        p.finish()
        _PROG_CACHE[cfg_key] = p
    return _PROG_CACHE[cfg_key]


def run_prog(p, inp, batches):
    cfg = p.cfg
    consts = host_consts(cfg)
    cache = {}
    in_maps = []
    for b in batches:
        m = {}
        for name, (shape, dt) in p.in_shapes.items():
            a = resolve_input(name, cfg, inp, b, consts, cache)
            assert tuple(a.shape) == tuple(shape), (name, a.shape, shape)
            m[name] = a
        in_maps.append(m)
    res = run_bass_kernel_spmd(p.nc, in_maps, core_ids=list(range(len(batches))))
    return res


def kernel(**inputs):
    inp = {k: np.asarray(v) for k, v in inputs.items()}
    p = get_prog((16, (0, 1, 2, 3), False))
    res = run_prog(p, inp, list(range(8)))
    return np.stack([np.asarray(r["out"], dtype=np.float32) for r in res.results], 0)
```

```python
import contextlib
import numpy as np
import concourse.bass as bass
import concourse.mybir as mybir
from concourse.bass_utils import run_bass_kernel_spmd

F32 = mybir.dt.float32
BF16 = mybir.dt.bfloat16
I32 = mybir.dt.int32
AF = mybir.ActivationFunctionType
ALU = mybir.AluOpType
AX = mybir.AxisListType

ENGS = ("pe", "act", "dve", "pool", "sp")
SAME_ENGINE_SYNC = True
DMA_WINDOW = 8
SEM_MAXV = 30000


class Op:
    __slots__ = ("eng", "fn", "reads", "writes", "is_dma", "deps", "signals", "event", "pre_wait", "barrier")

    def __init__(self, eng, fn, reads, writes, is_dma, barrier=False):
        self.eng = eng
        self.fn = fn
        self.reads = reads
        self.writes = writes
        self.is_dma = is_dma
        self.deps = set()
        self.signals = False
        self.event = None
        self.pre_wait = None
        self.barrier = barrier


class V:
    __slots__ = ("ap", "key")

    def __init__(self, ap, key):
        self.ap = ap
        self.key = key


def _a(x):
    return x.ap if isinstance(x, V) else x


def _key(x):
    if isinstance(x, V):
        return x.key
    if isinstance(x, (str, tuple)):
        return x
    if hasattr(x, "tensor"):
        return x.tensor.name
    return x.name


class Phase:
    def __init__(self, kb):
        self.kb = kb
        self.es = contextlib.ExitStack()

    def sb(self, shape, dtype=F32, name="t"):
        self.kb._n += 1
        return self.es.enter_context(self.kb.nc.sbuf_tensor(f"{name}_{self.kb._n}", list(shape), dtype))

    def ps(self, shape=(128, 512), dtype=F32, name="p"):
        self.kb._n += 1
        nm = f"{name}_{self.kb._n}"
        self.kb.psum_names.add(nm)
        return self.es.enter_context(self.kb.nc.psum_tensor(nm, list(shape), dtype))

    def __enter__(self):
        return self

    def __exit__(self, *a):
        self.kb.barrier()
        self.es.close()
        return False


def interleave(makers, width):
    it = iter(makers)
    active = []
    for slot in range(width):
        m = next(it, None)
        if m is not None:
            active.append((slot, m(slot)))
    while active:
        for entry in list(active):
            slot, g = entry
            try:
                next(g)
            except StopIteration:
                i = active.index(entry)
                m = next(it, None)
                if m is not None:
                    active[i] = (slot, m(slot))
                else:
                    active.pop(i)


class KB:
    def __init__(self, nc):
        self.nc = nc
        self.ops = []
        self._n = 0
        self.psum_names = set()

    def phase(self):
        return Phase(self)

    def sb(self, shape, dtype=F32, name="g"):
        self._n += 1
        return self.nc.alloc_sbuf_tensor(f"{name}_{self._n}", list(shape), dtype)

    def dram(self, shape, dtype=F32, name="dr"):
        self._n += 1
        return self.nc.dram_tensor(f"{name}_{self._n}", list(shape), dtype, kind="Internal")

    def barrier(self):
        self.ops.append(Op(None, None, [], [], False, barrier=True))

    def op(self, eng, fn, reads, writes, is_dma=False):
        o = Op(eng, fn, [_key(r) for r in reads], [_key(w) for w in writes], is_dma)
        self.ops.append(o)
        return o

    def mm(self, out, lhsT, rhs, start=True, stop=True, **kw):
        return self.op("pe", lambda e: e.matmul(_a(out), _a(lhsT), _a(rhs), start=start, stop=stop, **kw), [lhsT, rhs], [out])

    def tr(self, out, in_, ident):
        return self.op("pe", lambda e: e.transpose(_a(out), _a(in_), _a(ident)), [in_, ident], [out])

    def act(self, out, in_, func, bias=None, scale=None, accum_out=None):
        kw = {}
        reads = [in_]
        if bias is not None:
            kw["bias"] = bias
            if not isinstance(bias, (int, float)):
                reads.append(bias)
        if scale is not None:
            kw["scale"] = scale
            if not isinstance(scale, (int, float)):
                reads.append(scale)
        writes = [out]
        if accum_out is not None:
            kw["accum_out"] = accum_out
            writes.append(accum_out)
        kw = {k: _a(x) for k, x in kw.items()}
        return self.op("act", lambda e: e.activation(_a(out), _a(in_), func, **kw), reads, writes)

    def v(self, eng, method, out, ins, *args, **kw):
        isap = lambda a: hasattr(a, "tensor") or isinstance(a, V)
        reads = [a for a in ins if isap(a)]
        reads += [a for a in args if isap(a)]
        reads += [a for a in kw.values() if isap(a)]
        ins2 = [_a(a) for a in ins]
        args2 = [_a(a) for a in args]
        kw2 = {k: _a(x) for k, x in kw.items()}
        return self.op(eng, lambda e: getattr(e, method)(_a(out), *ins2, *args2, **kw2), reads, [out])

    def dma(self, out, in_, eng="sp", rkey=None, wkey=None, extra_reads=(), **kw):
        return self.op(eng, lambda e: e.dma_start(out=_a(out), in_=_a(in_), **kw), [rkey or in_] + list(extra_reads), [wkey or out], is_dma=True)

    def gather(self, out, src, idx, rkey=None):
        return self.op("pool", lambda e: e.indirect_dma_start(out=out, out_offset=None, in_=src,
                       in_offset=bass.IndirectOffsetOnAxis(ap=idx, axis=0)), [rkey or src, idx], [out], is_dma=True)

    def scatter(self, dst, src, idx, wkey=None):
        return self.op("pool", lambda e: e.indirect_dma_start(out=dst, out_offset=bass.IndirectOffsetOnAxis(ap=idx, axis=0),
                       in_=src, in_offset=None), [src, idx], [wkey or dst], is_dma=True)

    def finalize(self):
        nc = self.nc
        allops = self.ops
        ops = [o for o in allops if not o.barrier]
        idx_of = {id(o): i for i, o in enumerate(ops)}
        last_w = {}
        readers = {}
        last_on = {e: None for e in ENGS}
        recent_dma = {e: [] for e in ENGS}
        pending = {e: set() for e in ENGS}
        for o in allops:
            if o.barrier:
                src = set()
                for e in ENGS:
                    if last_on[e] is not None:
                        src.add(last_on[e])
                    src.update(recent_dma[e])
                for e in ENGS:
                    pending[e] |= src
                continue
            i = idx_of[id(o)]
            deps = set()
            for k in o.reads:
                if k in last_w:
                    deps.add(last_w[k])
                if k in self.psum_names:
                    for r in readers.get(k, ()):
                        if ops[r].eng != o.eng:
                            deps.add(r)
            for k in o.writes:
                if k in last_w:
                    deps.add(last_w[k])
                deps.update(readers.get(k, ()))
            deps |= pending[o.eng]
            pending[o.eng] = set()
            deps.discard(i)
            for k in o.reads:
                readers.setdefault(k, []).append(i)
            for k in o.writes:
                last_w[k] = i
                readers[k] = []
            fd = set()
            for d in deps:
                p = ops[d]
                if p.eng == o.eng and not p.is_dma and (o.eng == "pe" or not SAME_ENGINE_SYNC):
                    continue
                fd.add(d)
            o.deps = fd
            for d in fd:
                ops[d].signals = True
            last_on[o.eng] = i
            if o.is_dma:
                recent_dma[o.eng] = (recent_dma[o.eng] + [i])[-DMA_WINDOW:]
        n_sig = {e: 0 for e in ENGS}
        n_dma = {e: 0 for e in ENGS}
        for o in ops:
            if o.is_dma:
                o.signals = True
                n_dma[o.eng] += 1
            elif o.signals:
                n_sig[o.eng] += 1
        sems = {e: [nc.alloc_semaphore(f"s_{e}_{j}") for j in range(max(1, -(-n_sig[e] // SEM_MAXV)))] for e in ENGS}
        dsems = {e: [nc.alloc_semaphore(f"d_{e}_{j}") for j in range(DMA_WINDOW)] for e in ENGS if n_dma[e]}
        cnt = {e: 0 for e in ENGS}
        dcnt = {e: 0 for e in ENGS}
        for o in ops:
            if o.is_dma:
                n = dcnt[o.eng]
                dcnt[o.eng] += 1
                sem = dsems[o.eng][n % DMA_WINDOW]
                o.event = (sem, 16 * (n // DMA_WINDOW + 1))
                if n >= DMA_WINDOW:
                    o.pre_wait = (sem, 16 * (n // DMA_WINDOW))
            elif o.signals:
                n = cnt[o.eng]
                cnt[o.eng] += 1
                o.event = (sems[o.eng][n // SEM_MAXV], n % SEM_MAXV + 1)
        self.stats = dict(n_ops=len(ops), per_eng={e: sum(1 for o in ops if o.eng == e) for e in ENGS}, n_sig=n_sig, n_dma=n_dma)
        per_eng = {e: [o for o in ops if o.eng == e] for e in ENGS}
        final_waits = []
        for e in ENGS:
            for j in range(min(DMA_WINDOW, dcnt[e])):
                final_waits.append((dsems[e][j], 16 * ((dcnt[e] - 1 - j) // DMA_WINDOW + 1)))

        def emit(engname, eh):
            waited = {}

            def wait(sem, v):
                if waited.get(sem.name, 0) >= v:
                    return
                waited[sem.name] = v
                eh.wait_ge(sem, v)

            for o in per_eng[engname]:
                if o.pre_wait is not None:
                    wait(*o.pre_wait)
                for d in sorted(o.deps):
                    wait(*ops[d].event)
                ins = o.fn(eh)
                if o.event is not None:
                    ins.then_inc(o.event[0], 16 if o.is_dma else 1)
            if engname == "sp":
                for s, v in final_waits:
                    wait(s, v)

        with nc.Block() as block:
            @block.tensor
            def _(e):
                emit("pe", e)

            @block.scalar
            def _(e):
                emit("act", e)

            @block.vector
            def _(e):
                emit("dve", e)

            @block.gpsimd
            def _(e):
                emit("pool", e)

            @block.sync
            def _(e):
                emit("sp", e)
D = 1024
CTX = 256
GRID_W = 64
EPS = 1e-6
NH = 8
QK = 96
GH = 4
UF_ROWS = 1984
UT_COLS = 528
MOE_BS = 512


class Cfg:
    def __init__(self, nlt=16, layers=(0, 1, 2, 3), debug=False):
        self.nlt = nlt
        self.T = CTX + 512 * nlt
        self.tiles = [(0, CTX)] + [(CTX + 512 * i, 512) for i in range(nlt)]
        self.nsub = self.T // 128
        self.layers = tuple(layers)
        self.debug = debug
        self.nblk = -(-(2 * self.T) // MOE_BS) + 32
        self.nslot = self.nblk * MOE_BS


def rope_perm():
    p = np.zeros(32, np.int64)
    for ax in range(2):
        for half in range(2):
            for f in range(8):
                p[ax * 16 + half * 8 + f] = ax * 16 + (1 - half) * 8 + f
    return p


def rope_tables(cfg):
    S = cfg.T - CTX
    pos = np.arange(S)
    row = (pos // GRID_W).astype(np.float32)
    col = (pos % GRID_W).astype(np.float32)
    inv = (10000.0 ** (-np.arange(8, dtype=np.float32) / 8)).astype(np.float32)
    cosT = np.zeros((96, cfg.T), np.float32)
    sinT = np.zeros((96, cfg.T), np.float32)
    cosT[64:96, :CTX] = 1.0
    for ax in range(2):
        p = row if ax == 0 else col
        ang = p[None, :] * inv[:, None]
        for half in range(2):
            r0 = 64 + ax * 16 + half * 8
            cosT[r0:r0 + 8, CTX:] = np.cos(ang)
            sinT[r0:r0 + 8, CTX:] = np.sin(ang) * (-1.0 if half == 0 else 1.0)
    return cosT, sinT


def host_consts(cfg):
    c = {}
    c["ident_f"] = np.eye(128, dtype=np.float32)
    i = np.arange(128)
    same = (i[:, None] // 64) == (i[None, :] // 64)
    c["m_le"] = (same & (i[:, None] <= i[None, :])).astype(np.float32)
    c["m_ge"] = (same & (i[:, None] >= i[None, :])).astype(np.float32)
    c["m_lt"] = (same & (i[:, None] < i[None, :])).astype(np.float32)
    c["m_gt"] = (same & (i[:, None] > i[None, :])).astype(np.float32)
    c["m_same"] = same.astype(np.float32)
    c["ones_f"] = np.ones((128, 128), np.float32)
    c["m_h0"] = np.repeat((i[:, None] < 64), 128, 1).astype(np.float32)
    c["m_h1"] = np.repeat((i[:, None] >= 64), 128, 1).astype(np.float32)
    c["tri_lt_full"] = (i[:, None] < i[None, :]).astype(np.float32)
    cosT, sinT = rope_tables(cfg)
    c["cosT"] = cosT
    c["sinT"] = sinT
    c["thr"] = np.tile((np.arange(34, dtype=np.float32) * MOE_BS)[None, :], (128, 1))
    rm = np.ones((128, 512), np.float32)
    rm[:, ::64] = 0.0
    c["rmask"] = rm
    hs = np.zeros((128, 2), np.float32)
    hs[:64, 0] = 1.0
    hs[64:, 1] = 1.0
    c["hsel"] = hs
    c["iota_p"] = i.astype(np.float32).reshape(128, 1)
    c["blk_start"] = np.tile((np.arange(cfg.nblk, dtype=np.float32) * MOE_BS)[None, :], (128, 1))
    return c


CONST_SHAPES = lambda cfg: {
    "ident_f": (128, 128), "m_le": (128, 128), "m_ge": (128, 128), "m_lt": (128, 128), "m_gt": (128, 128),
    "m_same": (128, 128), "ones_f": (128, 128), "m_h0": (128, 128), "m_h1": (128, 128), "tri_lt_full": (128, 128),
    "cosT": (96, cfg.T), "sinT": (96, cfg.T), "iota_p": (128, 1), "rmask": (128, 512), "hsel": (128, 2), "thr": (128, 34), "blk_start": (128, cfg.nblk),
}


def host_even_smalls(inp, j):
    perm = rope_perm()
    s = {}
    s["qa_g"] = np.ascontiguousarray(inp["mla_qa_norm"][j].reshape(2, 128).T)
    s["kva_g"] = np.ascontiguousarray(inp["mla_kva_norm"][j].reshape(128, 1))
    for nm, key in (("qn_col", "mla_q_norm"), ("kn_col", "mla_k_norm")):
        g = inp[key][j]
        colv = np.zeros((96, 2), np.float32)
        colv[:, 0] = g
        colv[64:96, 1] = g[64 + perm]
        s[nm] = colv
    s["convw"] = np.ascontiguousarray(inp["gdn_conv"][j].reshape(5, 12, 128).transpose(2, 1, 0))
    s["gdn_vec"] = np.concatenate([inp["gdn_a_log"][j].reshape(-1), inp["gdn_dt_bias"][j].reshape(-1)]).reshape(1, 16).astype(np.float32)
    s["outg"] = np.ascontiguousarray(inp["gdn_out_norm"][j].reshape(1, 128))
    return s


EVEN_SMALL_SHAPES = {"qa_g": (128, 2), "kva_g": (128, 1), "qn_col": (96, 2), "kn_col": (96, 2),
                     "convw": (128, 12, 5), "gdn_vec": (1, 16), "outg": (1, 128)}


def host_odd_smalls(inp, j):
    s = {}
    col = lambda v: np.ascontiguousarray(v.reshape(8, 128).T)
    s["mu"] = np.ascontiguousarray(inp["rk_mu"][j].reshape(6, 8, 128).transpose(2, 0, 1))
    s["w0"] = np.ascontiguousarray(inp["rk_w0"][j].reshape(2, 8, 128).transpose(2, 0, 1))
    s["a0"] = np.ascontiguousarray(inp["rk_a0"][j].reshape(2, 8, 128).transpose(2, 0, 1))
    s["kkcol"] = col(inp["rk_kk"][j])
    s["kacol"] = col(inp["rk_ka"][j])
    s["rkcol"] = col(inp["rk_rk"][j].reshape(-1))
    s["lnw"] = np.ascontiguousarray(inp["rk_ln_w"][j].reshape(1, -1))
    s["lnb"] = np.ascontiguousarray(inp["rk_ln_b"][j].reshape(1, -1))
    s["v0"] = np.ascontiguousarray(inp["rk_v0"][j - 1].reshape(1, -1)) if j > 0 else np.zeros((1, D), np.float32)
    return s


ODD_SMALL_SHAPES = {"mu": (128, 6, 8), "w0": (128, 2, 8), "a0": (128, 2, 8), "kkcol": (128, 8), "kacol": (128, 8),
                    "rkcol": (128, 8), "lnw": (1, D), "lnb": (1, D), "v0": (1, D)}
MLA_SCALE = 96 ** -0.5


def rstd_inplace(kb, t, inv_n, eps, src=None):
    kb.v("dve", "tensor_scalar", t, [src if src is not None else t], inv_n, eps, ALU.mult, ALU.add)
    kb.act(t, t, AF.Sqrt)
    kb.v("dve", "reciprocal", t, [t])


class Prog:
    def __init__(self, cfg):
        self.cfg = cfg
        self.nc = bass.Bass("TRN2", target_bir_lowering=False)
        self.kb = KB(self.nc)
        self.in_shapes = {}
        self.dbg = {}
        self._rr = 0

    def inp(self, name, shape, dtype=F32):
        self.in_shapes[name] = (tuple(shape), dtype)
        return self.nc.dram_tensor(name, list(shape), dtype, kind="ExternalInput").ap()

    def scratch(self, name, shape, dtype=F32):
        if self.cfg.debug:
            t = self.nc.dram_tensor("dbg_" + name, list(shape), dtype, kind="ExternalOutput")
            self.dbg[name] = "dbg_" + name
        else:
            t = self.nc.dram_tensor("scr_" + name, list(shape), dtype, kind="Internal")
        return t.ap()

    def evac(self, out, in_):
        self._rr += 1
        if self._rr % 2:
            self.kb.act(out, in_, AF.Copy)
        else:
            self.kb.v("dve", "tensor_copy", out, [in_])

    def setup(self):
        kb, cfg = self.kb, self.cfg
        self.h0 = self.inp("h0", [cfg.T, D])
        self.cvec = self.inp("cvec", [128, 16])
        self.out = self.nc.dram_tensor("out", [cfg.T - CTX, D], F32, kind="ExternalOutput").ap()
        self.H = self.scratch("H", [cfg.T, D])
        self.cin = {k: self.inp("c_" + k, s) for k, s in CONST_SHAPES(cfg).items()}
        self.ident_f = kb.sb([128, 128], F32, "identf")
        self.ident = kb.sb([128, 128], BF16, "ident")
        self.ones = kb.sb([128, 128], BF16, "ones")
        kb.dma(self.ident_f[:], self.cin["ident_f"])
        kb.v("dve", "tensor_copy", self.ident[:], [self.ident_f[:]])
        kb.v("pool", "memset", self.ones[:], [], 1.0)
        self.modL = [kb.sb([128, D], F32, f"modL{i}") for i in range(6)]
        self.modC = [kb.sb([128, D], F32, f"modC{i}") for i in range(6)]
        with kb.phase() as ph:
            bufs = [ph.sb([128, D], F32, "cp") for _ in range(4)]
            for s in range(cfg.nsub):
                b = bufs[s % 4]
                kb.dma(b[:], self.h0[s * 128:(s + 1) * 128, :])
                kb.dma(self.H[s * 128:(s + 1) * 128, :], b[:])

    def finish(self):
        kb, cfg = self.kb, self.cfg
        with kb.phase() as ph:
            bufs = [ph.sb([128, D], F32, "cp") for _ in range(4)]
            for s in range(2, cfg.nsub):
                b = bufs[s % 4]
                kb.dma(b[:], self.H[s * 128:(s + 1) * 128, :])
                kb.dma(self.out[(s - 2) * 128:(s - 1) * 128, :], b[:])
        kb.finalize()

    def phase_mod(self, l):
        kb = self.kb
        ada_w = self.inp(f"ada_w__{l}", [D, 6 * D])
        ada_b = self.inp(f"ada_b__{l}", [1, 6 * D])
        nmix = self.inp(f"norm_mix__{l}", [1, D])
        nffn = self.inp(f"norm_ffn__{l}", [1, D])
        with kb.phase() as ph:
            sc = ph.sb([128, 16], F32, "sc")
            kb.dma(sc[:], self.cvec)
            kb.act(sc[:], sc[:], AF.Silu)
            lhs = ph.sb([128, 16, 128], F32, "lhs")
            for c in range(16):
                kb.v("dve", "tensor_copy", lhs[:, c, :], [sc[:, c:c + 1].to_broadcast([128, 128])])
            bb = ph.sb([128, 6 * D], F32, "bb")
            kb.dma(bb[:], ada_b.broadcast_to([128, 6 * D]))
            wst = [ph.sb([128, 8, 512], F32, "wst") for _ in range(2)]
            pl = [ph.ps() for _ in range(2)]
            pc = [ph.ps() for _ in range(2)]
            awv = ada_w.rearrange("(c p) n -> p c n", p=128)
            for n in range(12):
                w = wst[n % 2]
                kb.dma(w[:], awv[:, :, n * 512:(n + 1) * 512])
                for c in range(8):
                    kb.mm(pl[n % 2][:], lhs[:, c, :], w[:, c, :], start=(c == 0), stop=(c == 7))
                for c in range(8):
                    kb.mm(pc[n % 2][:], lhs[:, 8 + c, :], w[:, c, :], start=(c == 0), stop=(c == 7))
                jj, hf = n // 2, n % 2
                kb.v("dve", "tensor_tensor", self.modL[jj][:, hf * 512:(hf + 1) * 512], [pl[n % 2][:], bb[:, n * 512:(n + 1) * 512]], ALU.add)
                kb.v("dve", "tensor_tensor", self.modC[jj][:, hf * 512:(hf + 1) * 512], [pc[n % 2][:], bb[:, n * 512:(n + 1) * 512]], ALU.add)
            nm = ph.sb([128, D], F32, "nm")
            nf = ph.sb([128, D], F32, "nf")
            kb.dma(nm[:], nmix.broadcast_to([128, D]))
            kb.dma(nf[:], nffn.broadcast_to([128, D]))
            for mod in (self.modL, self.modC):
                kb.v("dve", "scalar_tensor_tensor", mod[1][:], [mod[1][:], 1.0, nm[:]], ALU.add, ALU.mult)
                kb.v("dve", "scalar_tensor_tensor", mod[4][:], [mod[4][:], 1.0, nf[:]], ALU.add, ALU.mult)

    def norm_sub(self, ht, junk, ss, tmp, xn, G, S):
        kb = self.kb
        kb.act(junk, ht, AF.Square, accum_out=ss)
        rstd_inplace(kb, ss, 1.0 / D, EPS)
        kb.v("dve", "scalar_tensor_tensor", tmp, [ht, ss, G], ALU.mult, ALU.mult)
        kb.v("pool", "tensor_tensor", xn, [tmp, S], ALU.add)

    def norm_tiles_to_xT(self, ph, src, gi, si, consume):
        kb, cfg = self.kb, self.cfg
        xTs = [ph.sb([128, 8, 512], BF16, "xT") for _ in range(2)]
        hts = [ph.sb([128, D], F32, "ht") for _ in range(2)]
        junk = ph.sb([128, D], F32, "junk")
        sss = [ph.sb([128, 1], F32, "ss") for _ in range(2)]
        tmps = [ph.sb([128, D], F32, "tmp") for _ in range(2)]
        xns = [ph.sb([128, D], BF16, "xn") for _ in range(2)]
        ptr = [ph.ps([128, 1024], BF16, "ptr") for _ in range(2)]
        k = 0
        for ti, (t0, TT) in enumerate(cfg.tiles):
            xT = xTs[ti % 2]
            mod = self.modC if ti == 0 else self.modL
            for s in range(TT // 128):
                ht, ss, tmp, xn, pt = hts[k % 2], sss[k % 2], tmps[k % 2], xns[k % 2], ptr[k % 2]
                k += 1
                r0 = t0 + s * 128
                kb.dma(ht[:], src[r0:r0 + 128, :])
                self.norm_sub(ht[:], junk[:], ss[:], tmp[:], xn[:], mod[gi][:], mod[si][:])
                for c in range(8):
                    kb.tr(pt[:, c * 128:(c + 1) * 128], xn[:, c * 128:(c + 1) * 128], self.ident[:])
                self.evac(xT[:, :, s * 128:(s + 1) * 128], pt[:].rearrange("p (c t) -> p c t", c=8))
            consume(ti, t0, TT, xT)

    def even_layer(self, l):
        j = l // 2
        self.phase_mod(l)
        sm = {k: self.inp(f"e{j}_{k}", s) for k, s in EVEN_SMALL_SHAPES.items()}
        self.phase_e1(j)
        self.phase_e2(j, sm)
        self.phase_e3(j)
        self.phase_e4(j, sm)
        self.phase_e5(j, sm)
        self.phase_e6(l, j, sm)
        self.moe(l)

    def phase_e1(self, j):
        kb, cfg = self.kb, self.cfg
        W = self.inp(f"hy_w_in__{j}", [D, 2480])
        self.UF = self.scratch(f"UF{j}", [UF_ROWS, cfg.T])
        self.UT = self.scratch(f"UT{j}", [cfg.T, UT_COLS])
        with kb.phase() as ph:
            Wfm = ph.sb([128, 8, UF_ROWS], BF16, "Wfm")
            Wtm = ph.sb([128, 8, UT_COLS], BF16, "Wtm")
            Wv = W.rearrange("(c p) n -> p c n", p=128)
            kb.dma(Wfm[:, :, 0:416], Wv[:, :, 0:416], eng="pool")
            for (d0, s0) in ((0, 8), (8, 0), (16, 24), (24, 16)):
                kb.dma(Wfm[:, :, 416 + d0:416 + d0 + 8], Wv[:, :, 384 + s0:384 + s0 + 8], eng="pool")
            kb.dma(Wfm[:, :, 448:1984], Wv[:, :, 416:1952], eng="pool")
            kb.dma(Wtm[:, :, :], Wv[:, :, 1952:2480], eng="pool")
            pmm = [ph.ps() for _ in range(4)]
            ost = [ph.sb([128, 512], F32, "ost") for _ in range(4)]
            cnt = [0]

            def consume(ti, t0, TT, xT):
                for m in range(16):
                    rows = min(128, UF_ROWS - m * 128)
                    pm, ob = pmm[cnt[0] % 4], ost[cnt[0] % 4]
                    cnt[0] += 1
                    for c in range(8):
                        kb.mm(pm[0:rows, 0:TT], Wfm[:, c, m * 128:m * 128 + rows], xT[:, c, 0:TT], start=(c == 0), stop=(c == 7))
                    self.evac(ob[0:rows, 0:TT], pm[0:rows, 0:TT])
                    kb.dma(self.UF[m * 128:m * 128 + rows, t0:t0 + TT], ob[0:rows, 0:TT])
                for s in range(TT // 128):
                    for (n0, n1) in ((0, 512), (512, 528)):
                        pm, ob = pmm[cnt[0] % 4], ost[cnt[0] % 4]
                        cnt[0] += 1
                        for c in range(8):
                            kb.mm(pm[:, 0:n1 - n0], xT[:, c, s * 128:(s + 1) * 128], Wtm[:, c, n0:n1], start=(c == 0), stop=(c == 7))
                        self.evac(ob[:, 0:n1 - n0], pm[:, 0:n1 - n0])
                        kb.dma(self.UT[t0 + s * 128:t0 + (s + 1) * 128, n0:n1], ob[:, 0:n1 - n0])

            self.norm_tiles_to_xT(ph, self.H, 1, 0, consume)

    def phase_e2(self, j, sm):
        kb, cfg = self.kb, self.cfg
        Wqb = self.inp(f"mla_w_qb__{j}", [256, 768])
        Wkvb = self.inp(f"mla_w_kvb__{j}", [128, 1024])
        self.QF = self.scratch(f"QF{j}", [NH, 96, cfg.T], BF16)
        self.KF = self.scratch(f"KF{j}", [NH, 96, cfg.T], BF16)
        self.VT = self.scratch(f"VT{j}", [cfg.T, 512], BF16)
        with kb.phase() as ph:
            Wq = ph.sb([128, 2, 768], BF16, "Wq")
            Wqs = ph.sb([128, 2, NH, 32], BF16, "Wqs")
            Wk = ph.sb([128, NH, 64], BF16, "Wk")
            Wvv = ph.sb([128, NH, 64], BF16, "Wv")
            kb.dma(Wq[:], Wqb.rearrange("(c p) n -> p c n", p=128), eng="pool")
            Wq4 = Wqb.rearrange("(c p) (h d) -> p c h d", p=128, d=96)
            for (d0, s0) in ((0, 8), (8, 0), (16, 24), (24, 16)):
                for c in range(2):
                    kb.dma(Wqs[:, c, :, d0:d0 + 8], Wq4[:, c, :, 64 + s0:64 + s0 + 8], eng="pool")
            Wkv3 = Wkvb.rearrange("p (h d) -> p h d", d=128)
            kb.dma(Wk[:], Wkv3[:, :, 0:64], eng="pool")
            kb.dma(Wvv[:], Wkv3[:, :, 64:128], eng="pool")
            qa_g = ph.sb([128, 2], F32, "qa_g")
            kva_g = ph.sb([128, 1], F32, "kva_g")
            qn = ph.sb([96, 2], F32, "qn")
            kn = ph.sb([96, 2], F32, "kn")
            kb.dma(qa_g[:], sm["qa_g"])
            kb.dma(kva_g[:], sm["kva_g"])
            kb.dma(qn[:], sm["qn_col"])
            kb.dma(kn[:], sm["kn_col"])
            cq = ph.sb([128, 2, 512], F32, "cq")
            ckv = ph.sb([128, 512], F32, "ckv")
            kpe = ph.sb([96, 512], F32, "kpe")
            kpes = ph.sb([96, 512], F32, "kpes")
            cs = ph.sb([96, 512], F32, "cs")
            sn = ph.sb([96, 512], F32, "sn")
            sq = ph.sb([128, 2, 512], BF16, "sq")
            rs = ph.sb([128, 512], F32, "rs")
            cqn = ph.sb([128, 2, 512], BF16, "cqn")
            ckvn = ph.sb([128, 512], BF16, "ckvn")
            rk = ph.sb([96, 512], F32, "rk")
            t1s = [ph.sb([96, 512], F32, "t1") for _ in range(2)]
            t2s = [ph.sb([96, 512], F32, "t2") for _ in range(2)]
            sqhs = [ph.sb([96, 512], BF16, "sqh") for _ in range(2)]
            rshs = [ph.sb([96, 512], F32, "rsh") for _ in range(2)]
            ohs = [ph.sb([96, 512], BF16, "oh") for _ in range(2)]
            vsb = [ph.sb([128, 512], BF16, "vsb") for _ in range(2)]
            pA = [ph.ps() for _ in range(2)]
            pB = [ph.ps() for _ in range(2)]
            pS = [ph.ps() for _ in range(2)]
            pR = ph.ps()
            pV = ph.ps()
            u = 0
            for ti, (t0, TT) in enumerate(cfg.tiles):
                kb.dma(cq[:, :, 0:TT], self.UF[0:256, t0:t0 + TT].rearrange("(c p) t -> p c t", p=128))
                kb.dma(ckv[:, 0:TT], self.UF[256:384, t0:t0 + TT])
                kb.dma(kpe[64:96, 0:TT], self.UF[384:416, t0:t0 + TT])
                kb.dma(kpes[64:96, 0:TT], self.UF[416:448, t0:t0 + TT])
                kb.dma(cs[64:96, 0:TT], self.cin["cosT"][64:96, t0:t0 + TT])
                kb.dma(sn[64:96, 0:TT], self.cin["sinT"][64:96, t0:t0 + TT])
                kb.act(sq[:, :, 0:TT], cq[:, :, 0:TT], AF.Square)
                for c in range(2):
                    kb.mm(pR[:, 0:TT], self.ones[:], sq[:, c, 0:TT], start=(c == 0), stop=(c == 1))
                rstd_inplace(kb, rs[:, 0:TT], 1.0 / 256, EPS, src=pR[:, 0:TT])
                for c in range(2):
                    kb.v("dve", "scalar_tensor_tensor", cqn[:, c, 0:TT], [cq[:, c, 0:TT], qa_g[:, c:c + 1], rs[:, 0:TT]], ALU.mult, ALU.mult)
                kb.act(sq[:, 0, 0:TT], ckv[:, 0:TT], AF.Square)
                kb.mm(pR[:, 0:TT], self.ones[:], sq[:, 0, 0:TT])
                rstd_inplace(kb, rs[:, 0:TT], 1.0 / 128, EPS, src=pR[:, 0:TT])
                kb.v("dve", "scalar_tensor_tensor", ckvn[:, 0:TT], [ckv[:, 0:TT], kva_g[:, 0:1], rs[:, 0:TT]], ALU.mult, ALU.mult)
                kb.v("dve", "scalar_tensor_tensor", rk[64:96, 0:TT], [kpe[64:96, 0:TT], kn[64:96, 0:1], cs[64:96, 0:TT]], ALU.mult, ALU.mult)
                kb.v("dve", "scalar_tensor_tensor", t2s[0][64:96, 0:TT], [kpes[64:96, 0:TT], kn[64:96, 1:2], sn[64:96, 0:TT]], ALU.mult, ALU.mult)
                kb.v("pool", "tensor_tensor", rk[64:96, 0:TT], [rk[64:96, 0:TT], t2s[0][64:96, 0:TT]], ALU.add)
                for h in range(NH):
                    a, b2, s_, t1, t2, sqh, rsh, oh = pA[u % 2], pB[u % 2], pS[u % 2], t1s[u % 2], t2s[u % 2], sqhs[u % 2], rshs[u % 2], ohs[u % 2]
                    u += 1
                    for c in range(2):
                        kb.mm(a[0:96, 0:TT], Wq[:, c, h * 96:(h + 1) * 96], cqn[:, c, 0:TT], start=(c == 0), stop=(c == 1))
                    for c in range(2):
                        kb.mm(b2[64:96, 0:TT], Wqs[:, c, h, :], cqn[:, c, 0:TT], start=(c == 0), stop=(c == 1))
                    kb.act(sqh[0:96, 0:TT], a[0:96, 0:TT], AF.Square)
                    kb.mm(s_[0:96, 0:TT], self.ones[0:96, 0:96], sqh[0:96, 0:TT])
                    rstd_inplace(kb, rsh[0:96, 0:TT], 1.0 / 96, EPS, src=s_[0:96, 0:TT])
                    kb.v("dve", "scalar_tensor_tensor", oh[0:64, 0:TT], [a[0:64, 0:TT], qn[0:64, 0:1], rsh[0:64, 0:TT]], ALU.mult, ALU.mult)
                    kb.v("dve", "scalar_tensor_tensor", t1[64:96, 0:TT], [a[64:96, 0:TT], qn[64:96, 0:1], cs[64:96, 0:TT]], ALU.mult, ALU.mult)
                    kb.v("dve", "scalar_tensor_tensor", t2[64:96, 0:TT], [b2[64:96, 0:TT], qn[64:96, 1:2], sn[64:96, 0:TT]], ALU.mult, ALU.mult)
                    kb.v("pool", "tensor_tensor", t1[64:96, 0:TT], [t1[64:96, 0:TT], t2[64:96, 0:TT]], ALU.add)
                    kb.v("dve", "tensor_tensor", oh[64:96, 0:TT], [t1[64:96, 0:TT], rsh[64:96, 0:TT]], ALU.mult)
                    kb.dma(self.QF[h, :, t0:t0 + TT], oh[0:96, 0:TT])
                    a, s_, sqh, rsh, oh = pA[u % 2], pS[u % 2], sqhs[u % 2], rshs[u % 2], ohs[u % 2]
                    u += 1
                    kb.mm(a[0:64, 0:TT], Wk[:, h, :], ckvn[:, 0:TT])
                    kb.act(sqh[0:64, 0:TT], a[0:64, 0:TT], AF.Square)
                    kb.act(sqh[64:96, 0:TT], kpe[64:96, 0:TT], AF.Square)
                    kb.mm(s_[0:96, 0:TT], self.ones[0:96, 0:96], sqh[0:96, 0:TT])
                    rstd_inplace(kb, rsh[0:96, 0:TT], 1.0 / 96, EPS, src=s_[0:96, 0:TT])
                    kb.v("dve", "scalar_tensor_tensor", oh[0:64, 0:TT], [a[0:64, 0:TT], kn[0:64, 0:1], rsh[0:64, 0:TT]], ALU.mult, ALU.mult)
                    kb.v("dve", "tensor_tensor", oh[64:96, 0:TT], [rk[64:96, 0:TT], rsh[64:96, 0:TT]], ALU.mult)
                    kb.dma(self.KF[h, :, t0:t0 + TT], oh[0:96, 0:TT])
                for s in range(TT // 128):
                    vb = vsb[s % 2]
                    kb.mm(pV[:], ckvn[:, s * 128:(s + 1) * 128], Wvv[:].rearrange("p h d -> p (h d)"))
                    self.evac(vb[:], pV[:])
                    kb.dma(self.VT[t0 + s * 128:t0 + (s + 1) * 128, :], vb[:])

    def phase_e3(self, j):
        kb, cfg = self.kb, self.cfg
        self.AF_ = self.scratch(f"AF{j}", [512, cfg.T], BF16)
        nsub = cfg.nsub
        with kb.phase() as ph:
            Vall = ph.sb([128, nsub, 512], BF16, "Vall")
            kb.dma(Vall[:], self.VT.rearrange("(b p) n -> p b n", p=128))
            Khs = [ph.sb([96, cfg.T], BF16, "Kh") for _ in range(2)]
            Qts = [ph.sb([96, 512], BF16, "Qt") for _ in range(2)]
            PTs = [ph.sb([128, 512], BF16, "PT") for _ in range(4)]
            rden = ph.sb([64, 512], F32, "rden")
            osb = [ph.sb([64, 512], BF16, "osb") for _ in range(2)]
            pS = [ph.ps() for _ in range(4)]
            pO = [ph.ps() for _ in range(2)]
            pD = [ph.ps() for _ in range(2)]
            DEPTH = 3
            units = []
            gi = 0
            for h in range(NH):
                for ti, (t0, TT) in enumerate(cfg.tiles):
                    nkb = 2 if ti == 0 else nsub
                    for kbk in range(nkb):
                        units.append((h, ti, t0, TT, kbk, nkb, gi))
                    gi += 1
            cur_h = [-1]
            cur_g = [-1]
            for idx in range(len(units) + DEPTH):
                if idx < len(units):
                    h, ti, t0, TT, kbk, nkb, g = units[idx]
                    if h != cur_h[0]:
                        cur_h[0] = h
                        kb.dma(Khs[h % 2][:], self.KF[h])
                    if g != cur_g[0]:
                        cur_g[0] = g
                        kb.dma(Qts[g % 2][:, 0:TT], self.QF[h, :, t0:t0 + TT])
                    ps, pt = pS[idx % 4], PTs[idx % 4]
                    kb.mm(ps[:, 0:TT], Khs[h % 2][:, kbk * 128:(kbk + 1) * 128], Qts[g % 2][:, 0:TT])
                    kb.act(pt[:, 0:TT], ps[:, 0:TT], AF.Exp, scale=MLA_SCALE)
                j2 = idx - DEPTH
                if j2 >= 0:
                    h, ti, t0, TT, kbk, nkb, g = units[j2]
                    pt = PTs[j2 % 4]
                    po, pd, ob = pO[g % 2], pD[g % 2], osb[g % 2]
                    kb.mm(po[0:64, 0:TT], Vall[:, kbk, h * 64:(h + 1) * 64], pt[:, 0:TT], start=(kbk == 0), stop=(kbk == nkb - 1))
                    kb.mm(pd[0:64, 0:TT], self.ones[:, 0:64], pt[:, 0:TT], start=(kbk == 0), stop=(kbk == nkb - 1))
                    if kbk == nkb - 1:
                        kb.v("dve", "reciprocal", rden[:, 0:TT], [pd[0:64, 0:TT]])
                        kb.v("dve", "tensor_tensor", ob[:, 0:TT], [po[0:64, 0:TT], rden[:, 0:TT]], ALU.mult)
                        kb.dma(self.AF_[h * 64:(h + 1) * 64, t0:t0 + TT], ob[:, 0:TT])
    def load_const_tile(self, ph, name, dtype=F32):
        shp = CONST_SHAPES(self.cfg)[name]
        t = ph.sb(list(shp), F32, name)
        self.kb.dma(t[:], self.cin[name])
        if dtype == F32:
            return t
        t2 = ph.sb(list(shp), dtype, name + "b")
        self.kb.v("dve", "tensor_copy", t2[:], [t[:]])
        return t2

    def phase_e4(self, j, sm):
        kb, cfg = self.kb, self.cfg
        T = cfg.T
        self.GQ = self.scratch(f"GQ{j}", [GH, 128, T], BF16)
        self.GK = self.scratch(f"GK{j}", [GH, 128, T], BF16)
        self.GKT = self.scratch(f"GKT{j}", [T, 512], BF16)
        self.GVT = self.scratch(f"GVT{j}", [T, 512], BF16)
        last = len(cfg.tiles) - 1
        with kb.phase() as ph:
            cw = ph.sb([128, 12, 5], F32, "cw")
            kb.dma(cw[:], sm["convw"])
            xs = [ph.sb([128, 516], F32, "x") for _ in range(2)]
            accs = [ph.sb([128, 512], F32, "acc") for _ in range(2)]
            ys = [ph.sb([128, 512], F32, "y") for _ in range(2)]
            sqs = [ph.sb([128, 512], BF16, "sq") for _ in range(2)]
            rs = ph.sb([128, 512], F32, "rs")
            yns = [ph.sb([128, 512], BF16, "yn") for _ in range(2)]
            tsb = [ph.sb([128, 4, 128], BF16, "tsb") for _ in range(2)]
            pR = [ph.ps() for _ in range(2)]
            ptr = [ph.ps([128, 1024], BF16, "ptr") for _ in range(2)]
            u = 0
            for ti, (t0, TT) in enumerate(cfg.tiles):
                for cc in range(12):
                    x, acc, y, sq, yn, pr, pt, tb = xs[u % 2], accs[u % 2], ys[u % 2], sqs[u % 2], yns[u % 2], pR[u % 2], ptr[u % 2], tsb[u % 2]
                    u += 1
                    r0 = 448 + cc * 128
                    kb.dma(x[:, 2:TT + 2], self.UF[r0:r0 + 128, t0:t0 + TT])
                    if ti in (0, 1):
                        kb.v("pool", "memset", x[:, 0:2], [], 0.0)
                    else:
                        kb.dma(x[:, 0:2], self.UF[r0:r0 + 128, t0 - 2:t0])
                    if ti in (0, last):
                        kb.v("pool", "memset", x[:, TT + 2:TT + 4], [], 0.0)
                    else:
                        kb.dma(x[:, TT + 2:TT + 4], self.UF[r0:r0 + 128, t0 + TT:t0 + TT + 2])
                    kb.v("dve", "tensor_scalar", acc[:, 0:TT], [x[:, 0:TT]], cw[:, cc, 0:1], None, ALU.mult)
                    for jj in range(1, 5):
                        kb.v("dve", "scalar_tensor_tensor", acc[:, 0:TT], [x[:, jj:jj + TT], cw[:, cc, jj:jj + 1], acc[:, 0:TT]], ALU.mult, ALU.add)
                    kb.act(y[:, 0:TT], acc[:, 0:TT], AF.Silu)
                    if cc < 8:
                        kb.act(sq[:, 0:TT], y[:, 0:TT], AF.Square)
                        kb.mm(pr[:, 0:TT], self.ones[:], sq[:, 0:TT])
                        rstd_inplace(kb, rs[:, 0:TT], 1.0, EPS, src=pr[:, 0:TT])
                        kb.v("dve", "scalar_tensor_tensor", yn[:, 0:TT], [y[:, 0:TT], (128 ** -0.5) if cc < 4 else 1.0, rs[:, 0:TT]], ALU.mult, ALU.mult)
                        dst = self.GQ[cc] if cc < 4 else self.GK[cc - 4]
                        kb.dma(dst[:, t0:t0 + TT], yn[:, 0:TT])
                    else:
                        kb.v("pool", "tensor_copy", yn[:, 0:TT], [y[:, 0:TT]])
                    if cc >= 4:
                        ns = TT // 128
                        for s in range(ns):
                            kb.tr(pt[:, s * 128:(s + 1) * 128], yn[:, s * 128:(s + 1) * 128], self.ident[:])
                        self.evac(tb[:, 0:ns, :], pt[:, 0:TT].rearrange("p (s c) -> p s c", c=128))
                        dstT = self.GKT if cc < 8 else self.GVT
                        hh = (cc - 4) % 4
                        kb.dma(dstT[t0:t0 + TT, hh * 128:(hh + 1) * 128].rearrange("(s p) c -> p s c", p=128), tb[:, 0:ns, :])

    def phase_e5(self, j, sm):
        kb, cfg = self.kb, self.cfg
        T, nsub = cfg.T, cfg.nsub
        self.OD = [self.scratch(f"OD{j}_{d}", [T, 512]) for d in range(2)]
        fwd = list(range(nsub))
        bwd = [1, 0] + list(range(nsub - 1, 1, -1))
        with kb.phase() as ph:
            mle = self.load_const_tile(ph, "m_le")
            mge = self.load_const_tile(ph, "m_ge")
            mlt = self.load_const_tile(ph, "m_lt")
            mgt = self.load_const_tile(ph, "m_gt")
            msame = self.load_const_tile(ph, "m_same")
            mh0 = self.load_const_tile(ph, "m_h0")
            mh1 = self.load_const_tile(ph, "m_h1")
            onesf = self.load_const_tile(ph, "ones_f")
            gv = ph.sb([128, 16], F32, "gv")
            kb.dma(gv[:], sm["gdn_vec"].broadcast_to([128, 16]))
            negA = ph.sb([128, 8], F32, "negA")
            kb.act(negA[:], gv[:, 0:8], AF.Exp)
            kb.v("dve", "tensor_scalar", negA[:], [negA[:]], -1.0, None, ALU.mult)
            S = [[ph.sb([128, 128], F32, f"S{d}{h}") for h in range(GH)] for d in range(2)]
            Sb = [[ph.sb([128, 128], BF16, f"Sb{d}{h}") for h in range(GH)] for d in range(2)]
            for d in range(2):
                for h in range(GH):
                    kb.v("pool", "memset", S[d][h][:], [], 0.0)
                    kb.v("pool", "memset", Sb[d][h][:], [], 0.0)
            banks = [ph.ps() for _ in range(7)]
            tbank = ph.ps([128, 1024], BF16, "tbank")
            slot = [0]
            tslot = [0]

            def ps128t():
                i = tslot[0] % 8
                tslot[0] += 1
                return V(tbank[:, i * 128:(i + 1) * 128], tbank.name)

            def ps128(bf=False):
                i = slot[0] % 7
                slot[0] += 1
                b = banks[i]
                return V(b[:, 0:128], b.name)

            def sub(v, r, cs=None):
                a = v.ap[r, :] if cs is None else v.ap[r, cs]
                return V(a, v.key)

            UB = []
            for d in range(4):
                b = {}
                b["ab"] = ph.sb([128, 16], F32, "ab")
                b["KT"] = ph.sb([128, GH, 128], BF16, "KT")
                b["QT"] = ph.sb([128, GH, 128], BF16, "QT")
                b["Ktm"] = ph.sb([128, 512], BF16, "Ktm")
                b["Vtm"] = ph.sb([128, 512], BF16, "Vtm")
                for nm in ("xa", "g", "beta", "nbeta", "gc", "gt", "eg", "ek", "beg", "egl0", "egl1"):
                    b[nm] = ph.sb([128, 4], F32, nm)
                b["osb"] = ph.sb([128, 512], F32, "osb")
                UB.append(b)
            HB = []
            for i in range(8):
                b = {}
                for nm in ("diag", "e1", "e2", "Dst", "DT", "EG", "usb"):
                    b[nm] = ph.sb([128, 128], F32, nm)
                for nm in ("X0", "X1", "XT0", "XT1", "AT0", "AT1", "Vb", "Kbg", "Khat", "QgT", "AqkT", "wT", "vnew"):
                    b[nm] = ph.sb([128, 128], BF16, nm)
                HB.append(b)
            def unit(d, h, B, Hb):
                hs = slice(h * 128, (h + 1) * 128)
                gcol = B["gc"][:, h:h + 1]
                kb.v("dve", "tensor_scalar", Hb["diag"][:], [self.ident_f[:]], gcol, None, ALU.mult)
                pg = ps128()
                kb.mm(pg, onesf[:], Hb["diag"][:])
                kb.v("dve", "tensor_scalar", Hb["e1"][:], [pg], gcol, 0.0, ALU.subtract, ALU.max)
                kb.v("dve", "tensor_scalar", Hb["e2"][:], [pg], gcol, 0.0, ALU.subtract, ALU.min)
                kb.act(Hb["EG"][:], pg, AF.Exp)
                kb.act(Hb["e1"][:], Hb["e1"][:], AF.Exp, scale=-1.0)
                kb.act(Hb["e2"][:], Hb["e2"][:], AF.Exp)
                kb.v("pool", "tensor_tensor", Hb["Dst"][:], [Hb["e1"][:], (mgt if d == 0 else mlt)[:]], ALU.mult)
                kb.v("pool", "tensor_tensor", Hb["DT"][:], [Hb["e2"][:], (mle if d == 0 else mge)[:]], ALU.mult)
                yield
                pkk = ps128()
                kb.mm(pkk, B["KT"][:, h, :], B["KT"][:, h, :])
                kb.v("dve", "scalar_tensor_tensor", Hb["X0"][:], [pkk, B["nbeta"][:, h:h + 1], Hb["Dst"][:]], ALU.mult, ALU.mult)
                pkq = ps128()
                kb.mm(pkq, B["KT"][:, h, :], B["QT"][:, h, :])
                kb.v("dve", "tensor_tensor", Hb["AqkT"][:], [pkq, Hb["DT"][:]], ALU.mult)
                yield
                pxt_b = ps128t()
                kb.tr(pxt_b, Hb["X0"][:], self.ident[:])
                kb.v("dve", "tensor_copy", Hb["XT0"][:], [pxt_b])
                kb.v("pool", "tensor_tensor", Hb["AT0"][:], [Hb["XT0"][:], self.ident[:]], ALU.add)
                yield
                X, XT, AT = Hb["X0"], Hb["XT0"], Hb["AT0"]
                for js in range(1, 6):
                    Xn = Hb["X1"] if X is Hb["X0"] else Hb["X0"]
                    XTn = Hb["XT1"] if XT is Hb["XT0"] else Hb["XT0"]
                    ATn = Hb["AT1"] if AT is Hb["AT0"] else Hb["AT0"]
                    px = ps128()
                    kb.mm(px, XT[:], X[:])
                    if js < 5:
                        pxT = ps128()
                        kb.mm(pxT, X[:], XT[:])
                    kb.act(Xn[:], px, AF.Copy)
                    if js < 5:
                        kb.v("dve", "tensor_copy", XTn[:], [pxT])
                    yield
                    pa = ps128()
                    kb.mm(pa, Xn[:], AT[:])
                    kb.v("dve", "tensor_tensor", ATn[:], [pa, AT[:]], ALU.add)
                    X, XT, AT = Xn, XTn, ATn
                    yield
                kb.v("pool", "tensor_scalar", Hb["Vb"][:], [B["Vtm"][:, hs]], B["beta"][:, h:h + 1], 1.0, ALU.mult, ALU.mult)
                kb.v("pool", "tensor_scalar", Hb["Kbg"][:], [B["Ktm"][:, hs]], B["beg"][:, h:h + 1], 1.0, ALU.mult, ALU.mult)
                kb.act(Hb["Khat"][:], B["Ktm"][:, hs], AF.Copy, scale=B["ek"][:, h:h + 1])
                kb.v("pool", "tensor_tensor", Hb["QgT"][:], [B["QT"][:, h, :], Hb["EG"][:]], ALU.mult)
                pu = ps128()
                kb.mm(pu, AT[:], Hb["Vb"][:])
                kb.act(Hb["usb"][:], pu, AF.Copy)
                pw = ps128()
                kb.mm(pw, Hb["Kbg"][:], AT[:])
                kb.act(Hb["wT"][:], pw, AF.Copy)
                yield
                for half in ((0, 1) if d == 0 else (1, 0)):
                    r = slice(half * 64, half * 64 + 64)
                    egl = B["egl0"] if half == 0 else B["egl1"]
                    oq = V(B["osb"][r, hs], (B["osb"].name, h))
                    p1 = ps128()
                    kb.mm(sub(p1, r), Hb["wT"][:, r], Sb[d][h][:])
                    kb.v("dve", "tensor_tensor", Hb["vnew"][r, :], [Hb["usb"][r, :], sub(p1, r)], ALU.subtract)
                    yield
                    p2 = ps128()
                    kb.mm(sub(p2, r), Hb["QgT"][:, r], Sb[d][h][:], start=True, stop=False)
                    kb.mm(sub(p2, r), Hb["AqkT"][r, r], Hb["vnew"][r, :], start=False, stop=True)
                    kb.act(oq, sub(p2, r), AF.Copy)
                    p3 = ps128()
                    kb.mm(p3, Hb["Khat"][r, :], Hb["vnew"][r, :])
                    kb.v("dve", "scalar_tensor_tensor", S[d][h][:], [S[d][h][:], egl[:, h:h + 1], p3], ALU.mult, ALU.add)
                    kb.act(Sb[d][h][:], S[d][h][:], AF.Copy)
                    yield

            for step in range(nsub):
                makers = []
                Bs = []
                for d in range(2):
                    sb_ = fwd[step] if d == 0 else bwd[step]
                    r0 = sb_ * 128
                    B = UB[(step % 2) * 2 + d]
                    Bs.append((B, r0, d))
                    kb.dma(B["ab"][:], self.UT[r0:r0 + 128, 512:528])
                    kb.dma(B["KT"][:], self.GK[:, :, r0:r0 + 128].rearrange("h c t -> c h t"))
                    kb.dma(B["QT"][:], self.GQ[:, :, r0:r0 + 128].rearrange("h c t -> c h t"))
                    kb.dma(B["Ktm"][:], self.GKT[r0:r0 + 128, :])
                    kb.dma(B["Vtm"][:], self.GVT[r0:r0 + 128, :])
                    ab = B["ab"]
                    kb.v("dve", "tensor_tensor", B["xa"][:], [ab[:, d * 8:d * 8 + 4], gv[:, 8 + d * 4:12 + d * 4]], ALU.add)
                    kb.act(B["xa"][:], B["xa"][:], AF.Exp)
                    kb.act(B["xa"][:], B["xa"][:], AF.Ln, bias=1.0)
                    kb.v("dve", "tensor_tensor", B["g"][:], [B["xa"][:], negA[:, d * 4:d * 4 + 4]], ALU.mult)
                    kb.act(B["beta"][:], ab[:, d * 8 + 4:d * 8 + 8], AF.Sigmoid)
                    kb.v("dve", "tensor_scalar", B["nbeta"][:], [B["beta"][:]], -1.0, None, ALU.mult)
                    Mc = mle if d == 0 else mge
                    p_gc, p_gt, p_0, p_1 = ps128(), ps128(), ps128(), ps128()
                    kb.mm(sub(p_gc, slice(0, 128), slice(0, 4)), Mc[:], B["g"][:])
                    kb.mm(sub(p_gt, slice(0, 128), slice(0, 4)), msame[:], B["g"][:])
                    kb.mm(sub(p_0, slice(0, 128), slice(0, 4)), mh0[:], B["g"][:])
                    kb.mm(sub(p_1, slice(0, 128), slice(0, 4)), mh1[:], B["g"][:])
                    kb.v("dve", "tensor_copy", B["gc"][:], [sub(p_gc, slice(0, 128), slice(0, 4))])
                    kb.v("dve", "tensor_tensor", B["gt"][:], [sub(p_gt, slice(0, 128), slice(0, 4)), B["gc"][:]], ALU.subtract)
                    kb.act(B["ek"][:], B["gt"][:], AF.Exp)
                    kb.act(B["eg"][:], B["gc"][:], AF.Exp)
                    kb.act(B["egl0"][:], sub(p_0, slice(0, 128), slice(0, 4)), AF.Exp)
                    kb.act(B["egl1"][:], sub(p_1, slice(0, 128), slice(0, 4)), AF.Exp)
                    kb.v("dve", "tensor_tensor", B["beg"][:], [B["beta"][:], B["eg"][:]], ALU.mult)
                for h in range(GH):
                    for (B, r0, d) in Bs:
                        makers.append(lambda slot_, d=d, h=h, B=B: unit(d, h, B, HB[slot_]))
                interleave(makers, 8)
                for (B, r0, d) in Bs:
                    kb.dma(self.OD[d][r0:r0 + 128, :], B["osb"][:], extra_reads=[(B["osb"].name, h) for h in range(GH)])

    def phase_e6(self, l, j, sm):
        kb, cfg = self.kb, self.cfg
        Wout = self.inp(f"hy_w_out__{j}", [D, D])
        self.Fm = self.scratch(f"F{l}", [cfg.T, D], BF16)
        with kb.phase() as ph:
            Wo = ph.sb([128, 8, D], BF16, "Wo")
            kb.dma(Wo[:], Wout.rearrange("(c p) n -> p c n", p=128), eng="pool")
            og = ph.sb([128, 128], F32, "og")
            kb.dma(og[:], sm["outg"].broadcast_to([128, 128]))
            o0s = [ph.sb([128, 512], F32, "o0") for _ in range(2)]
            o1s = [ph.sb([128, 512], F32, "o1") for _ in range(2)]
            zs = [ph.sb([128, 512], F32, "z") for _ in range(2)]
            sqo = ph.sb([128, 512], F32, "sqo")
            ssq = [ph.sb([128, 4], F32, "ssq") for _ in range(2)]
            on = ph.sb([128, 512], F32, "on")
            dtm = [ph.sb([128, 512], BF16, "dtm") for _ in range(2)]
            XT = [ph.sb([128, 8, 128], BF16, "XT") for _ in range(2)]
            ptr = [ph.ps([128, 1024], BF16, "ptr") for _ in range(2)]
            py = [ph.ps() for _ in range(4)]
            hts = [ph.sb([128, D], F32, "ht") for _ in range(2)]
            tmp = [ph.sb([128, D], F32, "tmp") for _ in range(2)]
            junk = ph.sb([128, D], F32, "junk")
            sss = [ph.sb([128, 1], F32, "ss") for _ in range(2)]
            tmp2 = [ph.sb([128, D], F32, "tmp2") for _ in range(2)]
            fb = [ph.sb([128, D], BF16, "fb") for _ in range(2)]
            for s in range(cfg.nsub):
                mod = self.modC if s < 2 else self.modL
                r0 = s * 128
                i = s % 2
                kb.dma(o0s[i][:], self.OD[0][r0:r0 + 128, :])
                kb.dma(o1s[i][:], self.OD[1][r0:r0 + 128, :])
                kb.dma(zs[i][:], self.UT[r0:r0 + 128, 0:512])
                kb.dma(XT[i][:, 0:4, :], self.AF_[:, r0:r0 + 128].rearrange("(c p) t -> p c t", p=128))
                kb.dma(hts[i][:], self.H[r0:r0 + 128, :])
                kb.v("pool", "tensor_tensor", o0s[i][:], [o0s[i][:], o1s[i][:]], ALU.add)
                kb.v("dve", "tensor_tensor", sqo[:], [o0s[i][:], o0s[i][:]], ALU.mult)
                kb.v("dve", "tensor_reduce", ssq[i][:], [sqo[:].rearrange("p (h v) -> p h v", h=4)], AX.X, ALU.add)
                rstd_inplace(kb, ssq[i][:], 1.0 / 128, EPS)
                for h in range(4):
                    hs = slice(h * 128, (h + 1) * 128)
                    kb.v("dve", "scalar_tensor_tensor", on[:, hs], [o0s[i][:, hs], ssq[i][:, h:h + 1], og[:]], ALU.mult, ALU.mult)
                kb.act(zs[i][:], zs[i][:], AF.Silu)
                kb.v("pool", "tensor_tensor", dtm[i][:], [on[:], zs[i][:]], ALU.mult)
                for c in range(4):
                    kb.tr(ptr[i][:, c * 128:(c + 1) * 128], dtm[i][:, c * 128:(c + 1) * 128], self.ident[:])
                self.evac(XT[i][:, 4:8, :], ptr[i][:, 0:512].rearrange("p (c t) -> p c t", c=4))
                for n in range(2):
                    pp = py[(2 * s + n) % 4]
                    for c in range(8):
                        kb.mm(pp[:], XT[i][:, c, :], Wo[:, c, n * 512:(n + 1) * 512], start=(c == 0), stop=(c == 7))
                    kb.v("dve", "tensor_tensor", tmp[i][:, n * 512:(n + 1) * 512], [pp[:], mod[2][:, n * 512:(n + 1) * 512]], ALU.mult)
                kb.v("pool", "tensor_tensor", hts[i][:], [tmp[i][:], hts[i][:]], ALU.add)
                kb.dma(self.H[r0:r0 + 128, :], hts[i][:])
                self.norm_sub(hts[i][:], junk[:], sss[i][:], tmp2[i][:], fb[i][:], mod[4][:], mod[3][:])
                kb.dma(self.Fm[r0:r0 + 128, :], fb[i][:])
    def moe_setup(self):
        kb, cfg = self.kb, self.cfg
        if hasattr(self, "OH"):
            return
        ns, nb = cfg.nsub, cfg.nblk
        self.OH = kb.sb([128, ns, 64], BF16, "OH")
        self.WTS = kb.sb([128, ns, 2], F32, "WTS")
        self.DEST = kb.sb([128, ns, 2], I32, "DEST")
        self.I1 = kb.sb([128, nb, 8], I32, "I1")
        self.I2 = kb.sb([128, nb, 4], I32, "I2")
        self.offs = kb.sb([128, 32], F32, "offs")
        self.XS = self.scratch("XS", [cfg.nslot, D], BF16)
        self.YS = self.scratch("YS", [cfg.nslot, D], F32)

    def moe(self, l):
        self.moe_setup()
        kb, cfg = self.kb, self.cfg
        ns, nb = cfg.nsub, cfg.nblk
        wg = self.inp(f"moe_w_group__{l}", [D, 4])
        bg = self.inp(f"moe_b_group__{l}", [1, 4])
        we = self.inp(f"moe_w_expert__{l}", [D, 32])
        be = self.inp(f"moe_b_expert__{l}", [1, 32])
        w1 = self.inp(f"moe_w1__{l}", [32 * D, 512])
        w3 = self.inp(f"moe_w3__{l}", [32 * D, 512])
        w2 = self.inp(f"moe_w2__{l}", [32 * 512, D])
        OH, WTS, DEST, I1, I2, offs = self.OH, self.WTS, self.DEST, self.I1, self.I2, self.offs
        with kb.phase() as ph:
            Wr = ph.sb([128, 8, 36], BF16, "Wr")
            kb.dma(Wr[:, :, 0:4], wg.rearrange("(c p) n -> p c n", p=128), eng="pool")
            kb.dma(Wr[:, :, 4:36], we.rearrange("(c p) n -> p c n", p=128), eng="pool")
            br = ph.sb([128, 36], F32, "br")
            kb.dma(br[:, 0:4], bg.broadcast_to([128, 4]))
            kb.dma(br[:, 4:36], be.broadcast_to([128, 32]))
            zt = ph.sb([128, D], BF16, "zt")
            kb.v("pool", "memset", zt[:], [], 0.0)
            for i in range(cfg.nslot // 128):
                kb.dma(self.XS[i * 128:(i + 1) * 128, :], zt[:], wkey=("XS", "z", i))
            fts = [ph.sb([128, D], BF16, "ft") for _ in range(2)]
            fT = [ph.sb([128, 8, 128], BF16, "fT") for _ in range(2)]
            ptr = [ph.ps([128, 1024], BF16, "ptr") for _ in range(2)]
            plg = [ph.ps() for _ in range(2)]
            pcnt = ph.ps()
            sm_ = [{nm: ph.sb([128, w], F32, nm) for nm, w in (("lg", 36), ("mx", 1), ("nmx", 1), ("eg", 4), ("sg", 1), ("pg", 1), ("G1", 4),
                                                               ("pen", 4), ("lem", 32), ("top", 8), ("dif", 1), ("r", 1), ("den", 1))} for _ in range(2)]
            osum = [ph.sb([128, 32], BF16, "osum") for _ in range(2)]
            for s in range(ns):
                i = s % 2
                ft, t = fts[i], sm_[i]
                kb.dma(ft[:], self.Fm[s * 128:(s + 1) * 128, :])
                for c in range(8):
                    kb.tr(ptr[i][:, c * 128:(c + 1) * 128], ft[:, c * 128:(c + 1) * 128], self.ident[:])
                self.evac(fT[i][:], ptr[i][:].rearrange("p (c t) -> p c t", c=8))
                for c in range(8):
                    kb.mm(plg[i][:, 0:36], fT[i][:, c, :], Wr[:, c, :], start=(c == 0), stop=(c == 7))
                kb.v("dve", "tensor_tensor", t["lg"][:], [plg[i][:, 0:36], br[:]], ALU.add)
                kb.v("dve", "reduce_max", t["mx"][:], [t["lg"][:, 0:4]], AX.X)
                kb.v("dve", "tensor_scalar", t["nmx"][:], [t["mx"][:]], -1.0, None, ALU.mult)
                kb.act(t["eg"][:], t["lg"][:, 0:4], AF.Exp, bias=t["nmx"][:], accum_out=t["sg"][:])
                kb.v("dve", "reciprocal", t["pg"][:], [t["sg"][:]])
                kb.v("dve", "tensor_scalar", t["G1"][:], [t["lg"][:, 0:4]], t["mx"][:], None, ALU.is_equal)
                kb.v("dve", "tensor_scalar", t["pen"][:], [t["G1"][:]], 1e30, -1e30, ALU.mult, ALU.add)
                for g in range(4):
                    kb.v("dve", "tensor_scalar", t["lem"][:, g * 8:(g + 1) * 8], [t["lg"][:, 4 + g * 8:12 + g * 8]], t["pen"][:, g:g + 1], None, ALU.add)
                kb.v("dve", "max", t["top"][:], [t["lem"][:]])
                kb.v("dve", "tensor_scalar", OH[:, s, 0:32], [t["lem"][:]], t["top"][:, 0:1], None, ALU.is_equal)
                kb.v("dve", "tensor_scalar", OH[:, s, 32:64], [t["lem"][:]], t["top"][:, 1:2], None, ALU.is_equal)
                kb.v("dve", "tensor_tensor", t["dif"][:], [t["top"][:, 1:2], t["top"][:, 0:1]], ALU.subtract)
                kb.act(t["r"][:], t["dif"][:], AF.Exp)
                kb.v("dve", "tensor_scalar", t["den"][:], [t["r"][:]], 1.0, None, ALU.add)
                kb.v("dve", "reciprocal", t["den"][:], [t["den"][:]])
                kb.v("dve", "tensor_tensor", WTS[:, s, 0:1], [t["pg"][:], t["den"][:]], ALU.mult)
                kb.v("dve", "tensor_tensor", WTS[:, s, 1:2], [WTS[:, s, 0:1], t["r"][:]], ALU.mult)
                kb.v("pool", "tensor_tensor", osum[i][:], [OH[:, s, 0:32], OH[:, s, 32:64]], ALU.add)
                kb.mm(pcnt[:, 0:32], self.ones[:], osum[i][:], start=(s == 0), stop=(s == ns - 1))
            cnt = ph.sb([128, 32], F32, "cnt")
            kb.v("dve", "tensor_copy", cnt[:], [pcnt[:, 0:32]])
            thr = ph.sb([128, 34], F32, "thr")
            kb.dma(thr[:], self.cin["thr"])
            cmp = ph.sb([128, 32, 34], F32, "cmp")
            kb.v("dve", "tensor_tensor", cmp[:], [cnt[:].unsqueeze(2).to_broadcast([128, 32, 34]), thr[:].unsqueeze(1).to_broadcast([128, 32, 34])], ALU.is_gt)
            padded = ph.sb([128, 32], F32, "padded")
            kb.v("dve", "tensor_reduce", padded[:], [cmp[:]], AX.X, ALU.add)
            kb.v("dve", "tensor_scalar", padded[:], [padded[:]], float(MOE_BS), None, ALU.mult)
            onesr = ph.sb([128, 32], F32, "onesr")
            kb.v("pool", "memset", onesr[:], [], 1.0)
            pend = ph.sb([128, 32], F32, "pend")
            kb.v("dve", "tensor_tensor_scan", pend[:], [onesr[:], padded[:]], 0.0, ALU.mult, ALU.add)
            kb.v("dve", "tensor_tensor", offs[:], [pend[:], padded[:]], ALU.subtract)
            bst = ph.sb([128, nb], F32, "bst")
            kb.dma(bst[:], self.cin["blk_start"])
            cmp2 = ph.sb([128, nb, 32], F32, "cmp2")
            kb.v("dve", "tensor_tensor", cmp2[:], [pend[:].unsqueeze(1).to_broadcast([128, nb, 32]), bst[:].unsqueeze(2).to_broadcast([128, nb, 32])], ALU.is_le)
            blke = ph.sb([128, nb], F32, "blke")
            kb.v("dve", "tensor_reduce", blke[:], [cmp2[:]], AX.X, ALU.add)
            kb.v("dve", "tensor_scalar", blke[:], [blke[:]], 31.0, None, ALU.min)
            iop = ph.sb([128, 1], F32, "iop")
            kb.dma(iop[:], self.cin["iota_p"])
            b1 = ph.sb([128, nb], F32, "b1")
            b2 = ph.sb([128, nb], F32, "b2")
            kb.v("dve", "tensor_scalar", b1[:], [blke[:]], 1024.0, iop[:, 0:1], ALU.mult, ALU.add)
            kb.v("dve", "tensor_scalar", b2[:], [blke[:]], 512.0, iop[:, 0:1], ALU.mult, ALU.add)
            for c in range(8):
                kb.v("dve", "tensor_scalar", I1[:, :, c], [b1[:]], float(c * 128), None, ALU.add)
            for c in range(4):
                kb.v("dve", "tensor_scalar", I2[:, :, c], [b2[:]], float(c * 128), None, ALU.add)
            tri = ph.sb([128, 128], F32, "trif")
            kb.dma(tri[:], self.cin["tri_lt_full"])
            trib = ph.sb([128, 128], BF16, "trib")
            kb.v("dve", "tensor_copy", trib[:], [tri[:]])
            kb.barrier()
            basep = ph.sb([128, 32], F32, "basep")
            kb.v("dve", "tensor_copy", basep[:], [offs[:]])
            pcx = [ph.ps() for _ in range(2)]
            pos = [ph.sb([128, 32], F32, "pos") for _ in range(2)]
            tm = [ph.sb([128, 32], F32, "tm") for _ in range(2)]
            dd = [ph.sb([128, 2], F32, "dd") for _ in range(2)]
            for s in range(ns):
                i = s % 2
                kb.v("pool", "tensor_tensor", osum[i][:], [OH[:, s, 0:32], OH[:, s, 32:64]], ALU.add)
                kb.mm(pcx[i][:, 0:32], trib[:], osum[i][:])
                kb.mm(pcx[i][:, 32:64], self.ones[:], osum[i][:])
                kb.v("dve", "tensor_tensor", pos[i][:], [pcx[i][:, 0:32], basep[:]], ALU.add)
                kb.v("dve", "tensor_tensor", basep[:], [pcx[i][:, 32:64], basep[:]], ALU.add)
                for k in range(2):
                    kb.v("dve", "tensor_tensor", tm[i][:], [OH[:, s, k * 32:(k + 1) * 32], pos[i][:]], ALU.mult)
                    kb.v("dve", "tensor_reduce", dd[i][:, k:k + 1], [tm[i][:]], AX.X, ALU.add)
                kb.v("dve", "tensor_copy", DEST[:, s, :], [dd[i][:]])
                ft = fts[i]
                kb.dma(ft[:], self.Fm[s * 128:(s + 1) * 128, :])
                for k in range(2):
                    kb.scatter(self.XS, ft[:], DEST[:, s, k:k + 1], wkey=("XS", "sc"))
        with kb.phase() as ph:
            W1 = [ph.sb([128, 8, 512], BF16, "W1") for _ in range(2)]
            W3 = [ph.sb([128, 8, 512], BF16, "W3") for _ in range(2)]
            W2 = [ph.sb([128, 4, D], BF16, "W2") for _ in range(2)]
            xs = [ph.sb([128, D], BF16, "xs") for _ in range(2)]
            xT = [ph.sb([128, 8, 512], BF16, "xT") for _ in range(2)]
            sil = [ph.sb([128, 512], F32, "sil") for _ in range(2)]
            hid = [ph.sb([128, 4, 512], BF16, "hid") for _ in range(2)]
            ysb = [ph.sb([128, D], F32, "ysb") for _ in range(2)]
            ptr = [ph.ps([128, 1024], BF16, "ptr") for _ in range(2)]
            p1 = [ph.ps() for _ in range(2)]
            p3 = [ph.ps() for _ in range(2)]
            py = [ph.ps() for _ in range(2)]
            u = 0
            for b in range(nb):
                i = b % 2
                for c in range(8):
                    kb.gather(W1[i][:, c, :], w1, I1[:, b, c:c + 1])
                    kb.gather(W3[i][:, c, :], w3, I1[:, b, c:c + 1])
                for c in range(4):
                    kb.gather(W2[i][:, c, :], w2, I2[:, b, c:c + 1])
                for s in range(4):
                    x = xs[u % 2]
                    pt = ptr[u % 2]
                    u += 1
                    r0 = b * MOE_BS + s * 128
                    kb.dma(x[:], self.XS[r0:r0 + 128, :], rkey=("XS", "sc"))
                    for c in range(8):
                        kb.tr(pt[:, c * 128:(c + 1) * 128], x[:, c * 128:(c + 1) * 128], self.ident[:])
                    self.evac(xT[i][:, :, s * 128:(s + 1) * 128], pt[:].rearrange("p (c t) -> p c t", c=8))
                for hc in range(4):
                    a, b3 = p1[hc % 2], p3[hc % 2]
                    for c in range(8):
                        kb.mm(a[:], W1[i][:, c, hc * 128:(hc + 1) * 128], xT[i][:, c, :], start=(c == 0), stop=(c == 7))
                    for c in range(8):
                        kb.mm(b3[:], W3[i][:, c, hc * 128:(hc + 1) * 128], xT[i][:, c, :], start=(c == 0), stop=(c == 7))
                    kb.act(sil[hc % 2][:], a[:], AF.Silu)
                    kb.v("dve", "tensor_tensor", hid[i][:, hc, :], [b3[:], sil[hc % 2][:]], ALU.mult)
                for s in range(4):
                    yb = ysb[s % 2]
                    for n in range(2):
                        pp = py[n]
                        for hc in range(4):
                            kb.mm(pp[:], hid[i][:, hc, s * 128:(s + 1) * 128], W2[i][:, hc, n * 512:(n + 1) * 512], start=(hc == 0), stop=(hc == 3))
                        self.evac(yb[:, n * 512:(n + 1) * 512], pp[:])
                    r0 = b * MOE_BS + s * 128
                    kb.dma(self.YS[r0:r0 + 128, :], yb[:], wkey=("YS", "w"))
        with kb.phase() as ph:
            y1 = [ph.sb([128, D], F32, "y1") for _ in range(2)]
            y2 = [ph.sb([128, D], F32, "y2") for _ in range(2)]
            hts = [ph.sb([128, D], F32, "ht") for _ in range(2)]
            for s in range(ns):
                i = s % 2
                mod = self.modC if s < 2 else self.modL
                kb.gather(y1[i][:], self.YS, DEST[:, s, 0:1], rkey=("YS", "w"))
                kb.gather(y2[i][:], self.YS, DEST[:, s, 1:2], rkey=("YS", "w"))
                kb.dma(hts[i][:], self.H[s * 128:(s + 1) * 128, :])
                kb.v("dve", "tensor_scalar", y1[i][:], [y1[i][:]], WTS[:, s, 0:1], None, ALU.mult)
                kb.v("dve", "scalar_tensor_tensor", y1[i][:], [y2[i][:], WTS[:, s, 1:2], y1[i][:]], ALU.mult, ALU.add)
                kb.v("pool", "tensor_tensor", y1[i][:], [y1[i][:], mod[5][:]], ALU.mult)
                kb.v("dve", "tensor_tensor", hts[i][:], [hts[i][:], y1[i][:]], ALU.add)
                kb.dma(self.H[s * 128:(s + 1) * 128, :], hts[i][:])
    def odd_layer(self, l):
        j = l // 2
        self.phase_mod(l)
        sm = {k: self.inp(f"o{j}_{k}", s) for k, s in ODD_SMALL_SHAPES.items()}
        self.phase_r1()
        self.phase_r2a(l, j, sm)
        self.phase_r2b(j, sm)
        self.phase_r3(j)
        self.phase_r4(l, j, sm)
        self.moe(l)

    def phase_r1(self):
        kb, cfg = self.kb, self.cfg
        if not hasattr(self, "XN"):
            self.XN = self.scratch("XN", [D, cfg.T], BF16)
        with kb.phase() as ph:
            def consume(ti, t0, TT, xT):
                kb.dma(self.XN[:, t0:t0 + TT].rearrange("(c p) t -> p c t", p=128), xT[:, :, 0:TT])
            self.norm_tiles_to_xT(ph, self.H, 1, 0, consume)

    def wload(self, ph, src, shape, name, view=None):
        t = ph.sb(shape, BF16, name)
        self.kb.dma(t[:], view if view is not None else src, eng="pool")
        return t

    def phase_r2a(self, l, j, sm):
        kb, cfg = self.kb, self.cfg
        T = cfg.T
        has_vres = j > 0
        wr = self.inp(f"rk_wr__{j}", [D, D])
        wk = self.inp(f"rk_wk__{j}", [D, D])
        wv = self.inp(f"rk_wv__{j}", [D, D])
        w1 = self.inp(f"rk_w1__{j}", [2, D, 64])
        a1 = self.inp(f"rk_a1__{j}", [2, D, 64])
        g1 = self.inp(f"rk_g1__{j}", [D, 160])
        g2 = self.inp(f"rk_g2__{j}", [160, D])
        if has_vres:
            v1 = self.inp(f"rk_v1__{j - 1}", [D, 32])
            v2 = self.inp(f"rk_v2__{j - 1}", [32, D])
        if not hasattr(self, "RF"):
            self.RF = self.scratch("RF", [D, T], BF16)
            self.KFm = self.scratch("KFm", [D, T], BF16)
            self.KKF = self.scratch("KKF", [D, T], BF16)
            self.TW = self.scratch("TW", [2, 64, T], BF16)
            self.TA = self.scratch("TA", [2, 64, T], BF16)
            self.VTM = self.scratch("VTM", [T, D], BF16)
            self.VF = self.scratch("VF", [T, D], F32)
            self.GATE = self.scratch("GATE", [T, D], BF16)
        last = len(cfg.tiles) - 1
        with kb.phase() as ph:
            r3 = lambda w: w.rearrange("(c p) n -> p c n", p=128)
            Wr = self.wload(ph, None, [128, 8, D], "Wr", r3(wr))
            Wk = self.wload(ph, None, [128, 8, D], "Wk", r3(wk))
            Wv = self.wload(ph, None, [128, 8, D], "Wv", r3(wv))
            W1 = [self.wload(ph, None, [128, 8, 64], "W1", r3(w1[d])) for d in range(2)]
            A1 = [self.wload(ph, None, [128, 8, 64], "A1", r3(a1[d])) for d in range(2)]
            G1 = self.wload(ph, None, [128, 8, 160], "G1", r3(g1))
            G2a = self.wload(ph, None, [128, D], "G2a", g2[0:128, :])
            G2b = self.wload(ph, None, [32, D], "G2b", g2[128:160, :])
            if has_vres:
                V1 = self.wload(ph, None, [128, 8, 32], "V1", r3(v1))
                V2 = self.wload(ph, None, [32, D], "V2", v2)
                v0b = ph.sb([128, D], F32, "v0b")
                kb.dma(v0b[:], sm["v0"].broadcast_to([128, D]))
            mu = ph.sb([128, 6, 8], F32, "mu")
            kb.dma(mu[:], sm["mu"])
            kkc = ph.sb([128, 8], F32, "kkc")
            kb.dma(kkc[:], sm["kkcol"])
            bones = ph.sb([128, 128], F32, "bonesf")
            kb.dma(bones[:], self.cin["m_same"])
            bonesb = ph.sb([128, 128], BF16, "bonesb")
            kb.v("dve", "tensor_copy", bonesb[:], [bones[:]])
            x = ph.sb([128, 8, 514], BF16, "x")
            tmpf = ph.sb([128, 8, 512], F32, "tmpf")
            xx = ph.sb([128, 8, 512], BF16, "xx")
            xms = [ph.sb([128, 8, 512], BF16, "xm") for _ in range(2)]
            pm = [ph.ps() for _ in range(4)]
            pR = ph.ps()
            ob = [ph.sb([128, 512], BF16, "ob") for _ in range(3)]
            of = [ph.sb([128, 512], F32, "of") for _ in range(2)]
            sq = ph.sb([128, 512], BF16, "sq")
            rs = ph.sb([128, 512], F32, "rs")
            sg = ph.sb([128, 2, 512], BF16, "sg")
            lvT = ph.sb([32, 512], BF16, "lvT")
            vf = [ph.sb([128, 512], F32, "vf") for _ in range(2)]
            cnt = [0]

            def mix(i, TT):
                xm = xms[cnt[0] % 2]
                cnt[0] += 1
                for c in range(8):
                    kb.v("dve", "scalar_tensor_tensor", xm[:, c, 0:TT], [xx[:, c, 0:TT], mu[:, i, c:c + 1], x[:, c, 1:TT + 1]], ALU.mult, ALU.add)
                return xm

            def proj_fm(xm, Wt, ncol0, rows, TT, pmi):
                p = pm[pmi % 4]
                for c in range(8):
                    kb.mm(p[0:rows, 0:TT], Wt[:, c, ncol0:ncol0 + rows], xm[:, c, 0:TT], start=(c == 0), stop=(c == 7))
                return p

            u = 0
            for ti, (t0, TT) in enumerate(cfg.tiles):
                lo = 0 if ti in (0, 1) else 1
                hi = 0 if ti in (0, last) else 1
                kb.dma(x[:, :, 1 - lo:TT + 1 + hi], self.XN[:, t0 - lo:t0 + TT + hi].rearrange("(c p) t -> p c t", p=128))
                if not lo:
                    kb.v("pool", "memset", x[:, :, 0:1], [], 0.0)
                if not hi:
                    kb.v("pool", "memset", x[:, :, TT + 1:TT + 2], [], 0.0)
                kb.v("dve", "tensor_tensor", tmpf[:, :, 0:TT], [x[:, :, 0:TT], x[:, :, 2:TT + 2]], ALU.add)
                kb.v("dve", "scalar_tensor_tensor", xx[:, :, 0:TT], [tmpf[:, :, 0:TT], 0.5, x[:, :, 1:TT + 1]], ALU.mult, ALU.subtract)
                xm = mix(0, TT)
                for m in range(8):
                    p = proj_fm(xm, Wr, m * 128, 128, TT, u)
                    o = ob[u % 3]
                    u += 1
                    self.evac(o[:, 0:TT], p[:, 0:TT])
                    kb.dma(self.RF[m * 128:(m + 1) * 128, t0:t0 + TT], o[:, 0:TT])
                xm = mix(2, TT)
                for m in range(8):
                    p = proj_fm(xm, Wk, m * 128, 128, TT, u)
                    o = ob[u % 3]
                    f = of[u % 2]
                    u += 1
                    kb.act(o[:, 0:TT], p[:, 0:TT], AF.Copy)
                    kb.dma(self.KFm[m * 128:(m + 1) * 128, t0:t0 + TT], o[:, 0:TT])
                    kb.v("dve", "tensor_scalar", f[:, 0:TT], [p[:, 0:TT]], kkc[:, m:m + 1], None, ALU.mult)
                    kb.act(sq[:, 0:TT], f[:, 0:TT], AF.Square)
                    kb.mm(pR[:, 0:TT], bonesb[:], sq[:, 0:TT])
                    rstd_inplace(kb, rs[:, 0:TT], 1.0, EPS, src=pR[:, 0:TT])
                    o2 = ob[u % 3]
                    u += 1
                    kb.v("dve", "tensor_tensor", o2[:, 0:TT], [f[:, 0:TT], rs[:, 0:TT]], ALU.mult)
                    kb.dma(self.KKF[m * 128:(m + 1) * 128, t0:t0 + TT], o2[:, 0:TT])
                xm = mix(3, TT)
                if has_vres:
                    p = proj_fm(xm, V1, 0, 32, TT, u)
                    u += 1
                    kb.act(lvT[:, 0:TT], p[0:32, 0:TT], AF.Copy)
                for s in range(TT // 128):
                    r0 = t0 + s * 128
                    for n in range(2):
                        p = pm[u % 4]
                        o = ob[u % 3]
                        f = of[u % 2]
                        u += 1
                        for c in range(8):
                            kb.mm(p[:], xm[:, c, s * 128:(s + 1) * 128], Wv[:, c, n * 512:(n + 1) * 512], start=(c == 0), stop=(c == 7))
                        if not has_vres:
                            kb.act(f[:], p[:], AF.Copy)
                            kb.dma(self.VF[r0:r0 + 128, n * 512:(n + 1) * 512], f[:])
                            kb.v("dve", "tensor_copy", o[:], [p[:]])
                        else:
                            p2 = pm[u % 4]
                            u += 1
                            vft = vf[n]
                            kb.dma(vft[:], self.VF[r0:r0 + 128, n * 512:(n + 1) * 512])
                            kb.mm(p2[:], lvT[:, s * 128:(s + 1) * 128], V2[:, n * 512:(n + 1) * 512])
                            kb.v("dve", "tensor_tensor", f[:], [p2[:], v0b[:, n * 512:(n + 1) * 512]], ALU.add)
                            kb.act(f[:], f[:], AF.Sigmoid)
                            kb.v("dve", "tensor_tensor", vft[:], [vft[:], p[:]], ALU.subtract)
                            kb.v("pool", "tensor_tensor", vft[:], [vft[:], f[:]], ALU.mult)
                            kb.v("dve", "tensor_tensor", o[:], [vft[:], p[:]], ALU.add)
                        kb.dma(self.VTM[r0:r0 + 128, n * 512:(n + 1) * 512], o[:])
                xm = mix(5, TT)
                p = proj_fm(xm, G1, 0, 128, TT, u)
                u += 1
                kb.act(sg[:, 0, 0:TT], p[:, 0:TT], AF.Sigmoid)
                p = proj_fm(xm, G1, 128, 32, TT, u)
                u += 1
                kb.act(sg[0:32, 1, 0:TT], p[0:32, 0:TT], AF.Sigmoid)
                for s in range(TT // 128):
                    r0 = t0 + s * 128
                    for n in range(2):
                        p = pm[u % 4]
                        o = ob[u % 3]
                        u += 1
                        kb.mm(p[:], sg[:, 0, s * 128:(s + 1) * 128], G2a[:, n * 512:(n + 1) * 512], start=True, stop=False)
                        kb.mm(p[:], sg[0:32, 1, s * 128:(s + 1) * 128], G2b[:, n * 512:(n + 1) * 512], start=False, stop=True)
                        self.evac(o[:], p[:])
                        kb.dma(self.GATE[r0:r0 + 128, n * 512:(n + 1) * 512], o[:])
                xm = mix(1, TT)
                for d in range(2):
                    p = proj_fm(xm, W1[d], 0, 64, TT, u)
                    o = ob[u % 3]
                    u += 1
                    kb.act(o[0:64, 0:TT], p[0:64, 0:TT], AF.Tanh)
                    kb.dma(self.TW[d, :, t0:t0 + TT], o[0:64, 0:TT])
                xm = mix(4, TT)
                for d in range(2):
                    p = proj_fm(xm, A1[d], 0, 64, TT, u)
                    o = ob[u % 3]
                    u += 1
                    kb.act(o[0:64, 0:TT], p[0:64, 0:TT], AF.Copy)
                    kb.dma(self.TA[d, :, t0:t0 + TT], o[0:64, 0:TT])

    def phase_r2b(self, j, sm):
        kb, cfg = self.kb, self.cfg
        T = cfg.T
        nch = T // 64
        w2 = self.inp(f"rk_w2__{j}", [2, 64, D])
        a2 = self.inp(f"rk_a2__{j}", [2, 64, D])
        if not hasattr(self, "RT"):
            for nm in ("RT", "AT", "BT", "KT"):
                setattr(self, nm, [self.scratch(f"{nm}{d}", [D, T], BF16) for d in range(2)])
            for nm in ("ATM", "KHAT", "BHAT"):
                setattr(self, nm, [self.scratch(f"{nm}{d}", [T, D], BF16) for d in range(2)])
            self.GCd = [self.scratch(f"GC{d}", [D, nch], F32) for d in range(2)]
            self.SBN = self.scratch("SBN", [T, 16], F32)
        with kb.phase() as ph:
            W2 = [self.wload(ph, None, [64, D], "W2", w2[d]) for d in range(2)]
            A2 = [self.wload(ph, None, [64, D], "A2", a2[d]) for d in range(2)]
            w0 = ph.sb([128, 2, 8], F32, "w0")
            a0 = ph.sb([128, 2, 8], F32, "a0")
            kac = ph.sb([128, 8], F32, "kac")
            omka = ph.sb([128, 8], F32, "omka")
            rkc = ph.sb([128, 8], F32, "rkc")
            kb.dma(w0[:], sm["w0"])
            kb.dma(a0[:], sm["a0"])
            kb.dma(kac[:], sm["kacol"])
            kb.dma(rkc[:], sm["rkcol"])
            kb.v("dve", "tensor_scalar", omka[:], [kac[:]], -1.0, 1.0, ALU.mult, ALU.add)
            rmask = ph.sb([128, 512], F32, "rmask")
            kb.dma(rmask[:], self.cin["rmask"])
            hsel = ph.sb([128, 2], F32, "hself")
            kb.dma(hsel[:], self.cin["hsel"])
            hselb = ph.sb([128, 2], BF16, "hselb")
            kb.v("dve", "tensor_copy", hselb[:], [hsel[:]])
            tw = [ph.sb([64, 512], BF16, "tw") for _ in range(2)]
            ta = [ph.sb([64, 512], BF16, "ta") for _ in range(2)]
            rt = [ph.sb([128, 512], BF16, "rt") for _ in range(2)]
            kt = [ph.sb([128, 512], BF16, "kt") for _ in range(2)]
            kkt = [ph.sb([128, 512], BF16, "kkt") for _ in range(2)]
            F = lambda nm: ph.sb([128, 512], F32, nm)
            lw, av, keys, bv, cs, tot, e1, e2, ex, tmp, prod = F("lw"), F("av"), F("keys"), F("bv"), F("cs"), F("tot"), F("e1"), F("e2"), F("ex"), F("tmp"), F("prod")
            obs = [ph.sb([128, 512], BF16, "ob") for _ in range(4)]
            tms = [ph.sb([128, 3, 512], BF16, "tms") for _ in range(2)]
            tsb = [ph.sb([128, 4, 128], BF16, "tsb") for _ in range(3)]
            gcs = ph.sb([128, 8], F32, "gcs")
            prodb = ph.sb([128, 512], BF16, "prodb")
            sbn = ph.sb([128, 4, 16], F32, "sbn")
            pz = [ph.ps() for _ in range(2)]
            pa = [ph.ps() for _ in range(2)]
            ptr = [ph.ps([128, 1024], BF16, "ptr") for _ in range(3)]
            psb = ph.ps()
            u = 0
            tq = 0
            for ti, (t0, TT) in enumerate(cfg.tiles):
                ns = TT // 128
                nc_ = TT // 64
                for d in range(2):
                    kb.dma(tw[d][:, 0:TT], self.TW[d, :, t0:t0 + TT])
                    kb.dma(ta[d][:, 0:TT], self.TA[d, :, t0:t0 + TT])
                for c in range(8):
                    i = c % 2
                    kb.dma(rt[i][:, 0:TT], self.RF[c * 128:(c + 1) * 128, t0:t0 + TT])
                    kb.dma(kt[i][:, 0:TT], self.KFm[c * 128:(c + 1) * 128, t0:t0 + TT])
                    kb.dma(kkt[i][:, 0:TT], self.KKF[c * 128:(c + 1) * 128, t0:t0 + TT])
                    for d in range(2):
                        z, a_ = pz[u % 2], pa[u % 2]
                        tm3 = tms[u % 2]
                        u += 1
                        kb.mm(z[:, 0:TT], W2[d][:, c * 128:(c + 1) * 128], tw[d][:, 0:TT])
                        kb.mm(a_[:, 0:TT], A2[d][:, c * 128:(c + 1) * 128], ta[d][:, 0:TT])
                        kb.act(lw[:, 0:TT], z[:, 0:TT], AF.Sigmoid, bias=w0[:, d, c:c + 1])
                        kb.v("dve", "tensor_scalar", lw[:, 0:TT], [lw[:, 0:TT]], -0.6065306597126334, None, ALU.mult)
                        kb.act(av[:, 0:TT], a_[:, 0:TT], AF.Sigmoid, bias=a0[:, d, c:c + 1])
                        kb.v("dve", "tensor_scalar", tmp[:, 0:TT], [av[:, 0:TT]], kac[:, c:c + 1], omka[:, c:c + 1], ALU.mult, ALU.add)
                        kb.v("dve", "tensor_tensor", keys[:, 0:TT], [tmp[:, 0:TT], kt[i][:, 0:TT]], ALU.mult)
                        kb.v("pool", "tensor_tensor", bv[:, 0:TT], [av[:, 0:TT], kkt[i][:, 0:TT]], ALU.mult)
                        kb.v("dve", "tensor_tensor_scan", cs[:, 0:TT], [rmask[:, 0:TT], lw[:, 0:TT]], 0.0, ALU.mult, ALU.add)
                        cs3 = cs[:, 0:TT].rearrange("p (n k) -> p n k", k=64)
                        kb.v("pool", "tensor_copy", tot[:, 0:TT].rearrange("p (n k) -> p n k", k=64), [cs3[:, :, 63:64].to_broadcast([128, nc_, 64])])
                        if d == 0:
                            E1 = cs
                        else:
                            kb.v("dve", "tensor_tensor", e1[:, 0:TT], [tot[:, 0:TT], cs[:, 0:TT]], ALU.subtract)
                            kb.v("dve", "tensor_tensor", e1[:, 0:TT], [e1[:, 0:TT], lw[:, 0:TT]], ALU.add)
                            E1 = e1
                        kb.v("pool", "tensor_tensor", e2[:, 0:TT], [E1[:, 0:TT], lw[:, 0:TT]], ALU.subtract)
                        o = obs[tq % 4]; tq += 1
                        kb.act(ex[:, 0:TT], E1[:, 0:TT], AF.Exp)
                        kb.v("dve", "tensor_tensor", o[:, 0:TT], [ex[:, 0:TT], rt[i][:, 0:TT]], ALU.mult)
                        kb.dma(self.RT[d][c * 128:(c + 1) * 128, t0:t0 + TT], o[:, 0:TT])
                        o = obs[tq % 4]; tq += 1
                        kb.act(ex[:, 0:TT], e2[:, 0:TT], AF.Exp)
                        kb.v("dve", "scalar_tensor_tensor", o[:, 0:TT], [ex[:, 0:TT], -1.0, kkt[i][:, 0:TT]], ALU.mult, ALU.mult)
                        kb.dma(self.AT[d][c * 128:(c + 1) * 128, t0:t0 + TT], o[:, 0:TT])
                        kb.v("pool", "tensor_copy", tm3[:, 0, 0:TT], [o[:, 0:TT]])
                        kb.act(ex[:, 0:TT], E1[:, 0:TT], AF.Exp, scale=-1.0)
                        o = obs[tq % 4]; tq += 1
                        kb.v("dve", "tensor_tensor", o[:, 0:TT], [ex[:, 0:TT], bv[:, 0:TT]], ALU.mult)
                        kb.dma(self.BT[d][c * 128:(c + 1) * 128, t0:t0 + TT], o[:, 0:TT])
                        o = obs[tq % 4]; tq += 1
                        kb.v("dve", "tensor_tensor", o[:, 0:TT], [ex[:, 0:TT], keys[:, 0:TT]], ALU.mult)
                        kb.dma(self.KT[d][c * 128:(c + 1) * 128, t0:t0 + TT], o[:, 0:TT])
                        kb.v("pool", "tensor_tensor", tmp[:, 0:TT], [tot[:, 0:TT], E1[:, 0:TT]], ALU.subtract)
                        kb.act(ex[:, 0:TT], tmp[:, 0:TT], AF.Exp)
                        kb.v("dve", "tensor_tensor", tm3[:, 1, 0:TT], [ex[:, 0:TT], keys[:, 0:TT]], ALU.mult)
                        kb.v("dve", "tensor_tensor", tm3[:, 2, 0:TT], [ex[:, 0:TT], bv[:, 0:TT]], ALU.mult)
                        kb.act(gcs[:, 0:nc_], cs3[:, :, 63], AF.Exp)
                        kb.dma(self.GCd[d][c * 128:(c + 1) * 128, t0 // 64:t0 // 64 + nc_], gcs[:, 0:nc_])
                        for k3, dst in enumerate((self.ATM[d], self.KHAT[d], self.BHAT[d])):
                            pt, tb = ptr[k3], tsb[k3]
                            for s in range(ns):
                                kb.tr(pt[:, s * 128:(s + 1) * 128], tm3[:, k3, s * 128:(s + 1) * 128], self.ident[:])
                            self.evac(tb[:, 0:ns, :], pt[:, 0:TT].rearrange("p (s c) -> p s c", c=128))
                            kb.dma(dst[t0:t0 + TT, c * 128:(c + 1) * 128].rearrange("(s p) c -> p s c", p=128), tb[:, 0:ns, :])
                        if d == 0:
                            kb.v("dve", "scalar_tensor_tensor", prod[:, 0:TT], [keys[:, 0:TT], rkc[:, c:c + 1], rt[i][:, 0:TT]], ALU.mult, ALU.mult)
                        else:
                            kb.v("dve", "scalar_tensor_tensor", tmp[:, 0:TT], [keys[:, 0:TT], rkc[:, c:c + 1], rt[i][:, 0:TT]], ALU.mult, ALU.mult)
                            kb.v("dve", "scalar_tensor_tensor", prodb[:, 0:TT], [tmp[:, 0:TT], 1.0, prod[:, 0:TT]], ALU.mult, ALU.add)
                    for s in range(ns):
                        kb.mm(psb[:, (s * 8 + c) * 2:(s * 8 + c) * 2 + 2], prodb[:, s * 128:(s + 1) * 128], hselb[:])
                kb.v("dve", "tensor_scalar", sbn[:, 0:ns, :], [psb[:, 0:ns * 16].rearrange("p (s h) -> p s h", h=16)], 0.5, None, ALU.mult)
                kb.dma(self.SBN[t0:t0 + TT, :].rearrange("(s p) h -> p s h", p=128), sbn[:, 0:ns, :])
    def phase_r3(self, j):
        kb, cfg = self.kb, self.cfg
        T, nsub = cfg.T, cfg.nsub
        W = 8
        if not hasattr(self, "YD"):
            self.YD = [self.scratch(f"YD{d}", [T, D]) for d in range(2)]
        fwd = list(range(nsub))
        bwd = [1, 0] + list(range(nsub - 1, 1, -1))
        with kb.phase() as ph:
            mle = self.load_const_tile(ph, "m_le")
            mge = self.load_const_tile(ph, "m_ge")
            mlt = self.load_const_tile(ph, "m_lt")
            mgt = self.load_const_tile(ph, "m_gt")
            Hs = [[ph.sb([128, 64], F32, f"Hs{d}_{h}") for h in range(16)] for d in range(2)]
            Hb = [[ph.sb([128, 64], BF16, f"Hb{d}_{h}") for h in range(16)] for d in range(2)]
            for d in range(2):
                for h in range(16):
                    kb.v("pool", "memset", Hs[d][h][:], [], 0.0)
                    kb.v("pool", "memset", Hb[d][h][:], [], 0.0)
            banks = [ph.ps() for _ in range(8)]
            slot = [0]

            def ps128():
                i = slot[0] % 8
                slot[0] += 1
                return banks[i]

            UB = []
            for i in range(4):
                b = {}
                for nm in ("RT", "AT", "BT", "KT"):
                    b[nm] = ph.sb([128, 8, 128], BF16, nm)
                for nm in ("ATM", "KHAT", "BHAT", "V"):
                    b[nm] = ph.sb([128, D], BF16, nm)
                b["GC"] = ph.sb([128, 8, 2], F32, "GC")
                b["ysb"] = ph.sb([128, D], F32, "ysb")
                UB.append(b)
            HB = []
            for i in range(W):
                b = {}
                for nm in ("X0", "X1", "XT0", "XT1", "AT0", "AT1", "AakT", "ArkT", "ArbT", "PT"):
                    b[nm] = ph.sb([128, 128], BF16, nm)
                b["Z"] = ph.sb([128, 64], BF16, "Z")
                b["U"] = ph.sb([128, 64], BF16, "U")
                b["U0"] = ph.sb([128, 64], F32, "U0")
                HB.append(b)

            def unit(d, h, B, Hh):
                MsT = mlt if d == 0 else mgt
                Ms = mgt if d == 0 else mlt
                MiT = mle if d == 0 else mge
                c = h // 2
                rs = slice(64 * (h % 2), 64 * (h % 2) + 64)
                hs = slice(h * 64, (h + 1) * 64)
                RT, AT_, BT, KT = B["RT"][rs, c, :], B["AT"][rs, c, :], B["BT"][rs, c, :], B["KT"][rs, c, :]
                for (l_, r_, dst, msk) in ((BT, AT_, "XT0", MsT), (AT_, BT, "X0", Ms), (KT, AT_, "AakT", MsT), (KT, RT, "ArkT", MiT), (BT, RT, "ArbT", MiT)):
                    p = ps128()
                    kb.mm(p[:, 0:128], l_, r_)
                    kb.v("dve", "tensor_tensor", Hh[dst][:], [p[:, 0:128], msk[:]], ALU.mult)
                    yield
                kb.v("pool", "tensor_tensor", Hh["AT0"][:], [Hh["XT0"][:], self.ident[:]], ALU.add)
                X, XT, AT = Hh["X0"], Hh["XT0"], Hh["AT0"]
                for js in range(1, 6):
                    Xn = Hh["X1"] if X is Hh["X0"] else Hh["X0"]
                    XTn = Hh["XT1"] if XT is Hh["XT0"] else Hh["XT0"]
                    ATn = Hh["AT1"] if AT is Hh["AT0"] else Hh["AT0"]
                    px = ps128()
                    kb.mm(px[:, 0:128], XT[:], X[:])
                    if js < 5:
                        pxT = ps128()
                        kb.mm(pxT[:, 0:128], X[:], XT[:])
                    kb.act(Xn[:], px[:, 0:128], AF.Copy)
                    if js < 5:
                        kb.v("dve", "tensor_copy", XTn[:], [pxT[:, 0:128]])
                    yield
                    pa = ps128()
                    kb.mm(pa[:, 0:128], Xn[:], AT[:])
                    kb.v("dve", "tensor_tensor", ATn[:], [pa[:, 0:128], AT[:]], ALU.add)
                    X, XT, AT = Xn, XTn, ATn
                    yield
                p = ps128()
                kb.mm(p[:, 0:64], Hh["AakT"][:], B["V"][:, hs])
                kb.act(Hh["Z"][:], p[:, 0:64], AF.Copy)
                p = ps128()
                kb.mm(p[rs, 0:128], B["ATM"][:, hs], AT[:])
                kb.act(Hh["PT"][rs, :], p[rs, 0:128], AF.Copy)
                yield
                p = ps128()
                kb.mm(p[:, 0:64], AT[:], Hh["Z"][:])
                kb.act(Hh["U0"][:], p[:, 0:64], AF.Copy)
                yield
                for half in ((0, 1) if d == 0 else (1, 0)):
                    q = slice(half * 64, half * 64 + 64)
                    yq = V(B["ysb"][q, hs], (B["ysb"].name, h))
                    p1 = ps128()
                    kb.mm(p1[q, 0:64], Hh["PT"][rs, q], Hb[d][h][rs, :])
                    kb.v("dve", "tensor_tensor", Hh["U"][q, :], [p1[q, 0:64], Hh["U0"][q, :]], ALU.add)
                    p2a = ps128()
                    kb.mm(p2a[q, 0:64], B["RT"][rs, c, q], Hb[d][h][rs, :])
                    kb.act(yq, p2a[q, 0:64], AF.Copy)
                    yield
                    p2 = ps128()
                    kb.mm(p2[q, 0:64], Hh["ArkT"][q, q], B["V"][q, hs], start=True, stop=False)
                    kb.mm(p2[q, 0:64], Hh["ArbT"][q, q], Hh["U"][q, :], start=False, stop=True)
                    kb.v("dve", "tensor_tensor", yq, [p2[q, 0:64], yq], ALU.add)
                    p3 = ps128()
                    kb.mm(p3[rs, 0:64], B["KHAT"][q, hs], B["V"][q, hs], start=True, stop=False)
                    kb.mm(p3[rs, 0:64], B["BHAT"][q, hs], Hh["U"][q, :], start=False, stop=True)
                    kb.v("dve", "scalar_tensor_tensor", Hs[d][h][rs, :], [Hs[d][h][rs, :], B["GC"][rs, c, half:half + 1], p3[rs, 0:64]], ALU.mult, ALU.add)
                    kb.act(Hb[d][h][rs, :], Hs[d][h][rs, :], AF.Copy)
                    yield

            for step in range(nsub):
                makers = []
                Bs = []
                for d in range(2):
                    sb_ = fwd[step] if d == 0 else bwd[step]
                    r0 = sb_ * 128
                    B = UB[(step % 2) * 2 + d]
                    Bs.append((B, r0, d))
                    for nm, src in (("RT", self.RT), ("AT", self.AT), ("BT", self.BT), ("KT", self.KT)):
                        kb.dma(B[nm][:], src[d][:, r0:r0 + 128].rearrange("(c p) t -> p c t", p=128))
                    for nm, src in (("ATM", self.ATM[d]), ("KHAT", self.KHAT[d]), ("BHAT", self.BHAT[d]), ("V", self.VTM)):
                        kb.dma(B[nm][:], src[r0:r0 + 128, :])
                    kb.dma(B["GC"][:], self.GCd[d][:, sb_ * 2:sb_ * 2 + 2].rearrange("(c p) n -> p c n", p=128))
                for h in range(16):
                    for (B, r0, d) in Bs:
                        makers.append(lambda slot_, d=d, h=h, B=B: unit(d, h, B, HB[slot_]))
                interleave(makers, W)
                for (B, r0, d) in Bs:
                    kb.dma(self.YD[d][r0:r0 + 128, :], B["ysb"][:], extra_reads=[(B["ysb"].name, h) for h in range(16)])

    def phase_r4(self, l, j, sm):
        kb, cfg = self.kb, self.cfg
        Wout = self.inp(f"rk_wo__{j}", [D, D])
        self.Fm = self.scratch(f"F{l}", [cfg.T, D], BF16)
        with kb.phase() as ph:
            Wo = ph.sb([128, 8, D], BF16, "Wo")
            kb.dma(Wo[:], Wout.rearrange("(c p) n -> p c n", p=128), eng="pool")
            lnw = ph.sb([128, D], F32, "lnw")
            lnb = ph.sb([128, D], F32, "lnb")
            kb.dma(lnw[:], sm["lnw"].broadcast_to([128, D]))
            kb.dma(lnb[:], sm["lnb"].broadcast_to([128, D]))
            y0 = [ph.sb([128, D], F32, "y0") for _ in range(2)]
            y1 = [ph.sb([128, D], F32, "y1") for _ in range(2)]
            vt = [ph.sb([128, D], BF16, "vt") for _ in range(2)]
            gt = [ph.sb([128, D], BF16, "gt") for _ in range(2)]
            sbn = [ph.sb([128, 16], F32, "sbn") for _ in range(2)]
            st = [ph.sb([128, 16], F32, "st") for _ in range(2)]
            sqt = ph.sb([128, D], F32, "sqt")
            bon = ph.sb([128, D], F32, "bon")
            zt = [ph.sb([128, D], BF16, "zt") for _ in range(2)]
            XT = [ph.sb([128, 8, 128], BF16, "XT") for _ in range(2)]
            ptr = [ph.ps([128, 1024], BF16, "ptr") for _ in range(2)]
            py = [ph.ps() for _ in range(4)]
            hts = [ph.sb([128, D], F32, "ht") for _ in range(2)]
            tmp = [ph.sb([128, D], F32, "tmp") for _ in range(2)]
            junk = ph.sb([128, D], F32, "junk")
            sss = [ph.sb([128, 1], F32, "ss") for _ in range(2)]
            tmp2 = [ph.sb([128, D], F32, "tmp2") for _ in range(2)]
            fb = [ph.sb([128, D], BF16, "fb") for _ in range(2)]
            v3 = lambda t: t[:].rearrange("p (h n) -> p h n", n=64)
            b3 = lambda t: t[:].unsqueeze(2).to_broadcast([128, 16, 64])
            for s in range(cfg.nsub):
                mod = self.modC if s < 2 else self.modL
                r0 = s * 128
                i = s % 2
                kb.dma(y0[i][:], self.YD[0][r0:r0 + 128, :])
                kb.dma(y1[i][:], self.YD[1][r0:r0 + 128, :])
                kb.dma(vt[i][:], self.VTM[r0:r0 + 128, :])
                kb.dma(gt[i][:], self.GATE[r0:r0 + 128, :])
                kb.dma(sbn[i][:], self.SBN[r0:r0 + 128, :])
                kb.dma(hts[i][:], self.H[r0:r0 + 128, :])
                kb.v("pool", "tensor_tensor", y0[i][:], [y0[i][:], y1[i][:]], ALU.add)
                kb.v("dve", "tensor_reduce", st[i][:], [v3(y0[i])], AX.X, ALU.add)
                kb.v("dve", "tensor_scalar", st[i][:], [st[i][:]], 1.0 / 64, None, ALU.mult)
                kb.v("dve", "tensor_tensor", v3(y0[i]), [v3(y0[i]), b3(st[i])], ALU.subtract)
                kb.v("pool", "tensor_tensor", sqt[:], [y0[i][:], y0[i][:]], ALU.mult)
                kb.v("dve", "tensor_reduce", st[i][:], [v3(sqt)], AX.X, ALU.add)
                rstd_inplace(kb, st[i][:], 1.0 / 64, 64e-5)
                kb.v("dve", "tensor_tensor", v3(y0[i]), [v3(y0[i]), b3(st[i])], ALU.mult)
                kb.v("pool", "tensor_tensor", y0[i][:], [y0[i][:], lnw[:]], ALU.mult)
                kb.v("pool", "tensor_tensor", y0[i][:], [y0[i][:], lnb[:]], ALU.add)
                kb.v("dve", "tensor_tensor", v3(bon), [v3(vt[i]), b3(sbn[i])], ALU.mult)
                kb.v("dve", "tensor_tensor", y0[i][:], [y0[i][:], bon[:]], ALU.add)
                kb.v("pool", "tensor_tensor", zt[i][:], [y0[i][:], gt[i][:]], ALU.mult)
                for c in range(8):
                    kb.tr(ptr[i][:, c * 128:(c + 1) * 128], zt[i][:, c * 128:(c + 1) * 128], self.ident[:])
                self.evac(XT[i][:], ptr[i][:].rearrange("p (c t) -> p c t", c=8))
                for n in range(2):
                    pp = py[(2 * s + n) % 4]
                    for c in range(8):
                        kb.mm(pp[:], XT[i][:, c, :], Wo[:, c, n * 512:(n + 1) * 512], start=(c == 0), stop=(c == 7))
                    kb.v("dve", "tensor_tensor", tmp[i][:, n * 512:(n + 1) * 512], [pp[:], mod[2][:, n * 512:(n + 1) * 512]], ALU.mult)
                kb.v("pool", "tensor_tensor", hts[i][:], [tmp[i][:], hts[i][:]], ALU.add)
                kb.dma(self.H[r0:r0 + 128, :], hts[i][:])
                self.norm_sub(hts[i][:], junk[:], sss[i][:], tmp2[i][:], fb[i][:], mod[4][:], mod[3][:])
                kb.dma(self.Fm[r0:r0 + 128, :], fb[i][:])
import re


def resolve_input(name, cfg, inp, b, consts, cache):
    if name == "h0":
        return np.ascontiguousarray(np.concatenate([inp["ctx"][b], inp["x"][b][:cfg.T - CTX]], 0))
    if name == "cvec":
        return np.ascontiguousarray(np.concatenate([inp["c"][b].reshape(8, 128).T, inp["c_ctx"].reshape(8, 128).T], 1))
    if name.startswith("c_"):
        return consts[name[2:]]
    key = ("shared", name)
    if key in cache:
        return cache[key]
    m = re.match(r"^e(\d+)_(\w+)$", name)
    if m:
        j = int(m.group(1))
        if ("es", j) not in cache:
            cache[("es", j)] = host_even_smalls(inp, j)
        arr = cache[("es", j)][m.group(2)]
    else:
        m = re.match(r"^o(\d+)_(\w+)$", name)
        if m:
            j = int(m.group(1))
            if ("os", j) not in cache:
                cache[("os", j)] = host_odd_smalls(inp, j)
            arr = cache[("os", j)][m.group(2)]
        else:
            m = re.match(r"^(.+)__(\d+)$", name)
            base, idx = m.group(1), int(m.group(2))
            a = inp[base][idx]
            if a.ndim == 1:
                a = a.reshape(1, -1)
            elif base in ("moe_w1", "moe_w3", "moe_w2"):
                a = a.reshape(-1, a.shape[-1])
            arr = np.ascontiguousarray(a)
    cache[key] = arr
    return arr


_PROG_CACHE = {}


def get_prog(cfg_key):
    if cfg_key not in _PROG_CACHE:
        nlt, layers, debug = cfg_key
        cfg = Cfg(nlt=nlt, layers=layers, debug=debug)
        p = Prog(cfg)
        p.setup()
        for l in cfg.layers:
            if l % 2 == 0:
                p.even_layer(l)
            else:
                p.odd_layer(l)
        p.finish()
        _PROG_CACHE[cfg_key] = p
    return _PROG_CACHE[cfg_key]


def run_prog(p, inp, batches):
    cfg = p.cfg
    consts = host_consts(cfg)
    cache = {}
    in_maps = []
    for b in batches:
        m = {}
        for name, (shape, dt) in p.in_shapes.items():
            a = resolve_input(name, cfg, inp, b, consts, cache)
            assert tuple(a.shape) == tuple(shape), (name, a.shape, shape)
            m[name] = a
        in_maps.append(m)
    res = run_bass_kernel_spmd(p.nc, in_maps, core_ids=list(range(len(batches))))
    return res


def kernel(**inputs):
    inp = {k: np.asarray(v) for k, v in inputs.items()}
    p = get_prog((16, (0, 1, 2, 3), False))
    res = run_prog(p, inp, list(range(8)))
    return np.stack([np.asarray(r["out"], dtype=np.float32) for r in res.results], 0)
```

```python
import contextlib
import numpy as np
import concourse.bass as bass
import concourse.mybir as mybir
from concourse.bass_utils import run_bass_kernel_spmd

F32 = mybir.dt.float32
BF16 = mybir.dt.bfloat16
I32 = mybir.dt.int32
AF = mybir.ActivationFunctionType
ALU = mybir.AluOpType
AX = mybir.AxisListType

ENGS = ("pe", "act", "dve", "pool", "sp")
SAME_ENGINE_SYNC = True
DMA_WINDOW = 8
SEM_MAXV = 30000


class Op:
    __slots__ = ("eng", "fn", "reads", "writes", "is_dma", "deps", "signals", "event", "pre_wait", "barrier")

    def __init__(self, eng, fn, reads, writes, is_dma, barrier=False):
        self.eng = eng
        self.fn = fn
        self.reads = reads
        self.writes = writes
        self.is_dma = is_dma
        self.deps = set()
        self.signals = False
        self.event = None
        self.pre_wait = None
        self.barrier = barrier


class V:
    __slots__ = ("ap", "key")

    def __init__(self, ap, key):
        self.ap = ap
        self.key = key


def _a(x):
    return x.ap if isinstance(x, V) else x


def _key(x):
    if isinstance(x, V):
        return x.key
    if isinstance(x, (str, tuple)):
        return x
    if hasattr(x, "tensor"):
        return x.tensor.name
    return x.name


class Phase:
    def __init__(self, kb):
        self.kb = kb
        self.es = contextlib.ExitStack()

    def sb(self, shape, dtype=F32, name="t"):
        self.kb._n += 1
        return self.es.enter_context(self.kb.nc.sbuf_tensor(f"{name}_{self.kb._n}", list(shape), dtype))

    def ps(self, shape=(128, 512), dtype=F32, name="p"):
        self.kb._n += 1
        nm = f"{name}_{self.kb._n}"
        self.kb.psum_names.add(nm)
        return self.es.enter_context(self.kb.nc.psum_tensor(nm, list(shape), dtype))

    def __enter__(self):
        return self

    def __exit__(self, *a):
        self.kb.barrier()
        self.es.close()
        return False


def interleave(makers, width):
    it = iter(makers)
    active = []
    for slot in range(width):
        m = next(it, None)
        if m is not None:
            active.append((slot, m(slot)))
    while active:
        for entry in list(active):
            slot, g = entry
            try:
                next(g)
            except StopIteration:
                i = active.index(entry)
                m = next(it, None)
                if m is not None:
                    active[i] = (slot, m(slot))
                else:
                    active.pop(i)


class KB:
    def __init__(self, nc):
        self.nc = nc
        self.ops = []
        self._n = 0
        self.psum_names = set()

    def phase(self):
        return Phase(self)

    def sb(self, shape, dtype=F32, name="g"):
        self._n += 1
        return self.nc.alloc_sbuf_tensor(f"{name}_{self._n}", list(shape), dtype)

    def dram(self, shape, dtype=F32, name="dr"):
        self._n += 1
        return self.nc.dram_tensor(f"{name}_{self._n}", list(shape), dtype, kind="Internal")

    def barrier(self):
        self.ops.append(Op(None, None, [], [], False, barrier=True))

    def op(self, eng, fn, reads, writes, is_dma=False):
        o = Op(eng, fn, [_key(r) for r in reads], [_key(w) for w in writes], is_dma)
        self.ops.append(o)
        return o

    def mm(self, out, lhsT, rhs, start=True, stop=True, **kw):
        return self.op("pe", lambda e: e.matmul(_a(out), _a(lhsT), _a(rhs), start=start, stop=stop, **kw), [lhsT, rhs], [out])

    def tr(self, out, in_, ident):
        return self.op("pe", lambda e: e.transpose(_a(out), _a(in_), _a(ident)), [in_, ident], [out])

    def act(self, out, in_, func, bias=None, scale=None, accum_out=None):
        kw = {}
        reads = [in_]
        if bias is not None:
            kw["bias"] = bias
            if not isinstance(bias, (int, float)):
                reads.append(bias)
        if scale is not None:
            kw["scale"] = scale
            if not isinstance(scale, (int, float)):
                reads.append(scale)
        writes = [out]
        if accum_out is not None:
            kw["accum_out"] = accum_out
            writes.append(accum_out)
        kw = {k: _a(x) for k, x in kw.items()}
        return self.op("act", lambda e: e.activation(_a(out), _a(in_), func, **kw), reads, writes)

    def v(self, eng, method, out, ins, *args, **kw):
        isap = lambda a: hasattr(a, "tensor") or isinstance(a, V)
        reads = [a for a in ins if isap(a)]
        reads += [a for a in args if isap(a)]
        reads += [a for a in kw.values() if isap(a)]
        ins2 = [_a(a) for a in ins]
        args2 = [_a(a) for a in args]
        kw2 = {k: _a(x) for k, x in kw.items()}
        return self.op(eng, lambda e: getattr(e, method)(_a(out), *ins2, *args2, **kw2), reads, [out])

    def dma(self, out, in_, eng="sp", rkey=None, wkey=None, extra_reads=(), **kw):
        return self.op(eng, lambda e: e.dma_start(out=_a(out), in_=_a(in_), **kw), [rkey or in_] + list(extra_reads), [wkey or out], is_dma=True)

    def gather(self, out, src, idx, rkey=None):
        return self.op("pool", lambda e: e.indirect_dma_start(out=out, out_offset=None, in_=src,
                       in_offset=bass.IndirectOffsetOnAxis(ap=idx, axis=0)), [rkey or src, idx], [out], is_dma=True)

    def scatter(self, dst, src, idx, wkey=None):
        return self.op("pool", lambda e: e.indirect_dma_start(out=dst, out_offset=bass.IndirectOffsetOnAxis(ap=idx, axis=0),
                       in_=src, in_offset=None), [src, idx], [wkey or dst], is_dma=True)

    def finalize(self):
        nc = self.nc
        allops = self.ops
        ops = [o for o in allops if not o.barrier]
        idx_of = {id(o): i for i, o in enumerate(ops)}
        last_w = {}
        readers = {}
        last_on = {e: None for e in ENGS}
        recent_dma = {e: [] for e in ENGS}
        pending = {e: set() for e in ENGS}
        for o in allops:
            if o.barrier:
                src = set()
                for e in ENGS:
                    if last_on[e] is not None:
                        src.add(last_on[e])
                    src.update(recent_dma[e])
                for e in ENGS:
                    pending[e] |= src
                continue
            i = idx_of[id(o)]
            deps = set()
            for k in o.reads:
                if k in last_w:
                    deps.add(last_w[k])
                if k in self.psum_names:
                    for r in readers.get(k, ()):
                        if ops[r].eng != o.eng:
                            deps.add(r)
            for k in o.writes:
                if k in last_w:
                    deps.add(last_w[k])
                deps.update(readers.get(k, ()))
            deps |= pending[o.eng]
            pending[o.eng] = set()
            deps.discard(i)
            for k in o.reads:
                readers.setdefault(k, []).append(i)
            for k in o.writes:
                last_w[k] = i
                readers[k] = []
            fd = set()
            for d in deps:
                p = ops[d]
                if p.eng == o.eng and not p.is_dma and (o.eng == "pe" or not SAME_ENGINE_SYNC):
                    continue
                fd.add(d)
            best = {}
            keep = set()
            for d in fd:
                pe_ = ops[d]
                if pe_.is_dma or pe_.eng not in ("pe", "act", "dve"):
                    keep.add(d)
                elif pe_.eng not in best or d > best[pe_.eng]:
                    best[pe_.eng] = d
            fd = keep | set(best.values())
            o.deps = fd
            for d in fd:
                ops[d].signals = True
            last_on[o.eng] = i
            if o.is_dma:
                recent_dma[o.eng] = (recent_dma[o.eng] + [i])[-DMA_WINDOW:]
        n_sig = {e: 0 for e in ENGS}
        n_dma = {e: 0 for e in ENGS}
        for o in ops:
            if o.is_dma:
                o.signals = True
                n_dma[o.eng] += 1
            elif o.signals:
                n_sig[o.eng] += 1
        sems = {e: [nc.alloc_semaphore(f"s_{e}_{j}") for j in range(max(1, -(-n_sig[e] // SEM_MAXV)))] for e in ENGS}
        dsems = {e: [nc.alloc_semaphore(f"d_{e}_{j}") for j in range(DMA_WINDOW)] for e in ENGS if n_dma[e]}
        cnt = {e: 0 for e in ENGS}
        dcnt = {e: 0 for e in ENGS}
        for o in ops:
            if o.is_dma:
                n = dcnt[o.eng]
                dcnt[o.eng] += 1
                sem = dsems[o.eng][n % DMA_WINDOW]
                o.event = (sem, 16 * (n // DMA_WINDOW + 1))
                if n >= DMA_WINDOW:
                    o.pre_wait = (sem, 16 * (n // DMA_WINDOW))
            elif o.signals:
                n = cnt[o.eng]
                cnt[o.eng] += 1
                o.event = (sems[o.eng][n // SEM_MAXV], n % SEM_MAXV + 1)
        self.stats = dict(n_ops=len(ops), per_eng={e: sum(1 for o in ops if o.eng == e) for e in ENGS}, n_sig=n_sig, n_dma=n_dma)
        per_eng = {e: [o for o in ops if o.eng == e] for e in ENGS}
        final_waits = []
        for e in ENGS:
            for j in range(min(DMA_WINDOW, dcnt[e])):
                final_waits.append((dsems[e][j], 16 * ((dcnt[e] - 1 - j) // DMA_WINDOW + 1)))

        def emit(engname, eh):
            waited = {}

            def wait(sem, v):
                if waited.get(sem.name, 0) >= v:
                    return
                waited[sem.name] = v
                eh.wait_ge(sem, v)

            for o in per_eng[engname]:
                if o.pre_wait is not None:
                    wait(*o.pre_wait)
                for d in sorted(o.deps):
                    wait(*ops[d].event)
                ins = o.fn(eh)
                if o.event is not None:
                    ins.then_inc(o.event[0], 16 if o.is_dma else 1)
            if engname == "sp":
                for s, v in final_waits:
                    wait(s, v)

        with nc.Block() as block:
            @block.tensor
            def _(e):
                emit("pe", e)

            @block.scalar
            def _(e):
                emit("act", e)

            @block.vector
            def _(e):
                emit("dve", e)

            @block.gpsimd
            def _(e):
                emit("pool", e)

            @block.sync
            def _(e):
                emit("sp", e)
D = 1024
CTX = 256
GRID_W = 64
EPS = 1e-6
NH = 8
QK = 96
GH = 4
UF_ROWS = 1984
UT_COLS = 528
MOE_BS = 512


class Cfg:
    def __init__(self, nlt=16, layers=(0, 1, 2, 3), debug=False):
        self.nlt = nlt
        self.T = CTX + 512 * nlt
        self.tiles = [(0, CTX)] + [(CTX + 512 * i, 512) for i in range(nlt)]
        self.nsub = self.T // 128
        self.layers = tuple(layers)
        self.debug = debug
        self.nblk = -(-(2 * self.T) // MOE_BS) + 32
        self.nslot = self.nblk * MOE_BS


def rope_perm():
    p = np.zeros(32, np.int64)
    for ax in range(2):
        for half in range(2):
            for f in range(8):
                p[ax * 16 + half * 8 + f] = ax * 16 + (1 - half) * 8 + f
    return p


def rope_tables(cfg):
    S = cfg.T - CTX
    pos = np.arange(S)
    row = (pos // GRID_W).astype(np.float32)
    col = (pos % GRID_W).astype(np.float32)
    inv = (10000.0 ** (-np.arange(8, dtype=np.float32) / 8)).astype(np.float32)
    cosT = np.zeros((96, cfg.T), np.float32)
    sinT = np.zeros((96, cfg.T), np.float32)
    cosT[64:96, :CTX] = 1.0
    for ax in range(2):
        p = row if ax == 0 else col
        ang = p[None, :] * inv[:, None]
        for half in range(2):
            r0 = 64 + ax * 16 + half * 8
            cosT[r0:r0 + 8, CTX:] = np.cos(ang)
            sinT[r0:r0 + 8, CTX:] = np.sin(ang) * (-1.0 if half == 0 else 1.0)
    return cosT, sinT


def host_consts(cfg):
    c = {}
    c["ident_f"] = np.eye(128, dtype=np.float32)
    i = np.arange(128)
    same = (i[:, None] // 64) == (i[None, :] // 64)
    c["m_le"] = (same & (i[:, None] <= i[None, :])).astype(np.float32)
    c["m_ge"] = (same & (i[:, None] >= i[None, :])).astype(np.float32)
    c["m_lt"] = (same & (i[:, None] < i[None, :])).astype(np.float32)
    c["m_gt"] = (same & (i[:, None] > i[None, :])).astype(np.float32)
    c["m_same"] = same.astype(np.float32)
    c["ones_f"] = np.ones((128, 128), np.float32)
    c["m_h0"] = np.repeat((i[:, None] < 64), 128, 1).astype(np.float32)
    c["m_h1"] = np.repeat((i[:, None] >= 64), 128, 1).astype(np.float32)
    c["tri_lt_full"] = (i[:, None] < i[None, :]).astype(np.float32)
    cosT, sinT = rope_tables(cfg)
    c["cosT"] = cosT
    c["sinT"] = sinT
    c["thr"] = np.tile((np.arange(34, dtype=np.float32) * MOE_BS)[None, :], (128, 1))
    rm = np.ones((128, 512), np.float32)
    rm[:, ::64] = 0.0
    c["rmask"] = rm
    hs = np.zeros((128, 2), np.float32)
    hs[:64, 0] = 1.0
    hs[64:, 1] = 1.0
    c["hsel"] = hs
    c["iota_p"] = i.astype(np.float32).reshape(128, 1)
    c["blk_start"] = np.tile((np.arange(cfg.nblk, dtype=np.float32) * MOE_BS)[None, :], (128, 1))
    return c


CONST_SHAPES = lambda cfg: {
    "ident_f": (128, 128), "m_le": (128, 128), "m_ge": (128, 128), "m_lt": (128, 128), "m_gt": (128, 128),
    "m_same": (128, 128), "ones_f": (128, 128), "m_h0": (128, 128), "m_h1": (128, 128), "tri_lt_full": (128, 128),
    "cosT": (96, cfg.T), "sinT": (96, cfg.T), "iota_p": (128, 1), "rmask": (128, 512), "hsel": (128, 2), "thr": (128, 34), "blk_start": (128, cfg.nblk),
}


def host_even_smalls(inp, j):
    perm = rope_perm()
    s = {}
    s["qa_g"] = np.ascontiguousarray(inp["mla_qa_norm"][j].reshape(2, 128).T)
    s["kva_g"] = np.ascontiguousarray(inp["mla_kva_norm"][j].reshape(128, 1))
    for nm, key in (("qn_col", "mla_q_norm"), ("kn_col", "mla_k_norm")):
        g = inp[key][j]
        colv = np.zeros((96, 2), np.float32)
        colv[:, 0] = g
        colv[64:96, 1] = g[64 + perm]
        s[nm] = colv
    s["convw"] = np.ascontiguousarray(inp["gdn_conv"][j].reshape(5, 12, 128).transpose(2, 1, 0))
    s["gdn_vec"] = np.concatenate([inp["gdn_a_log"][j].reshape(-1), inp["gdn_dt_bias"][j].reshape(-1)]).reshape(1, 16).astype(np.float32)
    s["outg"] = np.ascontiguousarray(inp["gdn_out_norm"][j].reshape(1, 128))
    return s


EVEN_SMALL_SHAPES = {"qa_g": (128, 2), "kva_g": (128, 1), "qn_col": (96, 2), "kn_col": (96, 2),
                     "convw": (128, 12, 5), "gdn_vec": (1, 16), "outg": (1, 128)}


def host_odd_smalls(inp, j):
    s = {}
    col = lambda v: np.ascontiguousarray(v.reshape(8, 128).T)
    s["mu"] = np.ascontiguousarray(inp["rk_mu"][j].reshape(6, 8, 128).transpose(2, 0, 1))
    s["w0"] = np.ascontiguousarray(inp["rk_w0"][j].reshape(2, 8, 128).transpose(2, 0, 1))
    s["a0"] = np.ascontiguousarray(inp["rk_a0"][j].reshape(2, 8, 128).transpose(2, 0, 1))
    s["kkcol"] = col(inp["rk_kk"][j])
    s["kacol"] = col(inp["rk_ka"][j])
    s["rkcol"] = col(inp["rk_rk"][j].reshape(-1))
    s["lnw"] = np.ascontiguousarray(inp["rk_ln_w"][j].reshape(1, -1))
    s["lnb"] = np.ascontiguousarray(inp["rk_ln_b"][j].reshape(1, -1))
    s["v0"] = np.ascontiguousarray(inp["rk_v0"][j - 1].reshape(1, -1)) if j > 0 else np.zeros((1, D), np.float32)
    return s


ODD_SMALL_SHAPES = {"mu": (128, 6, 8), "w0": (128, 2, 8), "a0": (128, 2, 8), "kkcol": (128, 8), "kacol": (128, 8),
                    "rkcol": (128, 8), "lnw": (1, D), "lnb": (1, D), "v0": (1, D)}
MLA_SCALE = 96 ** -0.5


def rstd_inplace(kb, t, inv_n, eps, src=None):
    kb.v("dve", "tensor_scalar", t, [src if src is not None else t], inv_n, eps, ALU.mult, ALU.add)
    kb.act(t, t, AF.Sqrt)
    kb.v("dve", "reciprocal", t, [t])


class Prog:
    def __init__(self, cfg):
        self.cfg = cfg
        self.nc = bass.Bass("TRN2", target_bir_lowering=False)
        self.kb = KB(self.nc)
        self.in_shapes = {}
        self.dbg = {}
        self._rr = 0

    def inp(self, name, shape, dtype=F32):
        self.in_shapes[name] = (tuple(shape), dtype)
        return self.nc.dram_tensor(name, list(shape), dtype, kind="ExternalInput").ap()

    def scratch(self, name, shape, dtype=F32):
        if self.cfg.debug:
            t = self.nc.dram_tensor("dbg_" + name, list(shape), dtype, kind="ExternalOutput")
            self.dbg[name] = "dbg_" + name
        else:
            t = self.nc.dram_tensor("scr_" + name, list(shape), dtype, kind="Internal")
        return t.ap()

    def evac(self, out, in_):
        self._rr += 1
        if self._rr % 2:
            self.kb.act(out, in_, AF.Copy)
        else:
            self.kb.v("dve", "tensor_copy", out, [in_])

    def setup(self):
        kb, cfg = self.kb, self.cfg
        self.h0 = self.inp("h0", [cfg.T, D])
        self.cvec = self.inp("cvec", [128, 16])
        self.out = self.nc.dram_tensor("out", [cfg.T - CTX, D], F32, kind="ExternalOutput").ap()
        self.H = self.scratch("H", [cfg.T, D])
        self.cin = {k: self.inp("c_" + k, s) for k, s in CONST_SHAPES(cfg).items()}
        self.ident_f = kb.sb([128, 128], F32, "identf")
        self.ident = kb.sb([128, 128], BF16, "ident")
        self.ones = kb.sb([128, 128], BF16, "ones")
        kb.dma(self.ident_f[:], self.cin["ident_f"])
        kb.v("dve", "tensor_copy", self.ident[:], [self.ident_f[:]])
        kb.v("pool", "memset", self.ones[:], [], 1.0)
        self.modL = [kb.sb([128, D], F32, f"modL{i}") for i in range(6)]
        self.modC = [kb.sb([128, D], F32, f"modC{i}") for i in range(6)]
        with kb.phase() as ph:
            bufs = [ph.sb([128, D], F32, "cp") for _ in range(4)]
            for s in range(cfg.nsub):
                b = bufs[s % 4]
                kb.dma(b[:], self.h0[s * 128:(s + 1) * 128, :])
                kb.dma(self.H[s * 128:(s + 1) * 128, :], b[:])

    def finish(self):
        kb, cfg = self.kb, self.cfg
        with kb.phase() as ph:
            bufs = [ph.sb([128, D], F32, "cp") for _ in range(4)]
            for s in range(2, cfg.nsub):
                b = bufs[s % 4]
                kb.dma(b[:], self.H[s * 128:(s + 1) * 128, :])
                kb.dma(self.out[(s - 2) * 128:(s - 1) * 128, :], b[:])
        kb.finalize()

    def phase_mod(self, l):
        kb = self.kb
        ada_w = self.inp(f"ada_w__{l}", [D, 6 * D])
        ada_b = self.inp(f"ada_b__{l}", [1, 6 * D])
        nmix = self.inp(f"norm_mix__{l}", [1, D])
        nffn = self.inp(f"norm_ffn__{l}", [1, D])
        with kb.phase() as ph:
            sc = ph.sb([128, 16], F32, "sc")
            kb.dma(sc[:], self.cvec)
            kb.act(sc[:], sc[:], AF.Silu)
            lhs = ph.sb([128, 16, 128], F32, "lhs")
            for c in range(16):
                kb.v("dve", "tensor_copy", lhs[:, c, :], [sc[:, c:c + 1].to_broadcast([128, 128])])
            bb = ph.sb([128, 6 * D], F32, "bb")
            kb.dma(bb[:], ada_b.broadcast_to([128, 6 * D]))
            wst = [ph.sb([128, 8, 512], F32, "wst") for _ in range(2)]
            pl = [ph.ps() for _ in range(2)]
            pc = [ph.ps() for _ in range(2)]
            awv = ada_w.rearrange("(c p) n -> p c n", p=128)
            for n in range(12):
                w = wst[n % 2]
                kb.dma(w[:], awv[:, :, n * 512:(n + 1) * 512])
                for c in range(8):
                    kb.mm(pl[n % 2][:], lhs[:, c, :], w[:, c, :], start=(c == 0), stop=(c == 7))
                for c in range(8):
                    kb.mm(pc[n % 2][:], lhs[:, 8 + c, :], w[:, c, :], start=(c == 0), stop=(c == 7))
                jj, hf = n // 2, n % 2
                kb.v("dve", "tensor_tensor", self.modL[jj][:, hf * 512:(hf + 1) * 512], [pl[n % 2][:], bb[:, n * 512:(n + 1) * 512]], ALU.add)
                kb.v("dve", "tensor_tensor", self.modC[jj][:, hf * 512:(hf + 1) * 512], [pc[n % 2][:], bb[:, n * 512:(n + 1) * 512]], ALU.add)
            nm = ph.sb([128, D], F32, "nm")
            nf = ph.sb([128, D], F32, "nf")
            kb.dma(nm[:], nmix.broadcast_to([128, D]))
            kb.dma(nf[:], nffn.broadcast_to([128, D]))
            for mod in (self.modL, self.modC):
                kb.v("dve", "scalar_tensor_tensor", mod[1][:], [mod[1][:], 1.0, nm[:]], ALU.add, ALU.mult)
                kb.v("dve", "scalar_tensor_tensor", mod[4][:], [mod[4][:], 1.0, nf[:]], ALU.add, ALU.mult)

    def norm_sub(self, ht, junk, ss, tmp, xn, G, S):
        kb = self.kb
        kb.act(junk, ht, AF.Square, accum_out=ss)
        rstd_inplace(kb, ss, 1.0 / D, EPS)
        kb.v("dve", "scalar_tensor_tensor", tmp, [ht, ss, G], ALU.mult, ALU.mult)
        kb.v("pool", "tensor_tensor", xn, [tmp, S], ALU.add)

    def norm_tiles_to_xT(self, ph, src, gi, si, consume):
        kb, cfg = self.kb, self.cfg
        xTs = [ph.sb([128, 8, 512], BF16, "xT") for _ in range(2)]
        hts = [ph.sb([128, D], F32, "ht") for _ in range(2)]
        junk = ph.sb([128, D], F32, "junk")
        sss = [ph.sb([128, 1], F32, "ss") for _ in range(2)]
        tmps = [ph.sb([128, D], F32, "tmp") for _ in range(2)]
        xns = [ph.sb([128, D], BF16, "xn") for _ in range(2)]
        ptr = [ph.ps([128, 1024], BF16, "ptr") for _ in range(2)]
        k = 0
        for ti, (t0, TT) in enumerate(cfg.tiles):
            xT = xTs[ti % 2]
            mod = self.modC if ti == 0 else self.modL
            for s in range(TT // 128):
                ht, ss, tmp, xn, pt = hts[k % 2], sss[k % 2], tmps[k % 2], xns[k % 2], ptr[k % 2]
                k += 1
                r0 = t0 + s * 128
                kb.dma(ht[:], src[r0:r0 + 128, :])
                self.norm_sub(ht[:], junk[:], ss[:], tmp[:], xn[:], mod[gi][:], mod[si][:])
                for c in range(8):
                    kb.tr(pt[:, c * 128:(c + 1) * 128], xn[:, c * 128:(c + 1) * 128], self.ident[:])
                self.evac(xT[:, :, s * 128:(s + 1) * 128], pt[:].rearrange("p (c t) -> p c t", c=8))
            consume(ti, t0, TT, xT)

    def even_layer(self, l):
        j = l // 2
        self.phase_mod(l)
        sm = {k: self.inp(f"e{j}_{k}", s) for k, s in EVEN_SMALL_SHAPES.items()}
        self.phase_e1(j)
        self.phase_e2(j, sm)
        self.phase_e3(j)
        self.phase_e4(j, sm)
        self.phase_e5(j, sm)
        self.phase_e6(l, j, sm)
        self.moe(l)

    def phase_e1(self, j):
        kb, cfg = self.kb, self.cfg
        W = self.inp(f"hy_w_in__{j}", [D, 2480])
        self.UF = self.scratch(f"UF{j}", [UF_ROWS, cfg.T])
        self.UT = self.scratch(f"UT{j}", [cfg.T, UT_COLS])
        with kb.phase() as ph:
            Wfm = ph.sb([128, 8, UF_ROWS], BF16, "Wfm")
            Wtm = ph.sb([128, 8, UT_COLS], BF16, "Wtm")
            Wv = W.rearrange("(c p) n -> p c n", p=128)
            kb.dma(Wfm[:, :, 0:416], Wv[:, :, 0:416], eng="pool")
            for (d0, s0) in ((0, 8), (8, 0), (16, 24), (24, 16)):
                kb.dma(Wfm[:, :, 416 + d0:416 + d0 + 8], Wv[:, :, 384 + s0:384 + s0 + 8], eng="pool")
            kb.dma(Wfm[:, :, 448:1984], Wv[:, :, 416:1952], eng="pool")
            kb.dma(Wtm[:, :, :], Wv[:, :, 1952:2480], eng="pool")
            pmm = [ph.ps() for _ in range(4)]
            ost = [ph.sb([128, 512], F32, "ost") for _ in range(4)]
            cnt = [0]

            def consume(ti, t0, TT, xT):
                for m in range(16):
                    rows = min(128, UF_ROWS - m * 128)
                    pm, ob = pmm[cnt[0] % 4], ost[cnt[0] % 4]
                    cnt[0] += 1
                    for c in range(8):
                        kb.mm(pm[0:rows, 0:TT], Wfm[:, c, m * 128:m * 128 + rows], xT[:, c, 0:TT], start=(c == 0), stop=(c == 7))
                    self.evac(ob[0:rows, 0:TT], pm[0:rows, 0:TT])
                    kb.dma(self.UF[m * 128:m * 128 + rows, t0:t0 + TT], ob[0:rows, 0:TT])
                for s in range(TT // 128):
                    for (n0, n1) in ((0, 512), (512, 528)):
                        pm, ob = pmm[cnt[0] % 4], ost[cnt[0] % 4]
                        cnt[0] += 1
                        for c in range(8):
                            kb.mm(pm[:, 0:n1 - n0], xT[:, c, s * 128:(s + 1) * 128], Wtm[:, c, n0:n1], start=(c == 0), stop=(c == 7))
                        self.evac(ob[:, 0:n1 - n0], pm[:, 0:n1 - n0])
                        kb.dma(self.UT[t0 + s * 128:t0 + (s + 1) * 128, n0:n1], ob[:, 0:n1 - n0])

            self.norm_tiles_to_xT(ph, self.H, 1, 0, consume)

    def phase_e2(self, j, sm):
        kb, cfg = self.kb, self.cfg
        Wqb = self.inp(f"mla_w_qb__{j}", [256, 768])
        Wkvb = self.inp(f"mla_w_kvb__{j}", [128, 1024])
        self.QF = self.scratch(f"QF{j}", [NH, 96, cfg.T], BF16)
        self.KF = self.scratch(f"KF{j}", [NH, 96, cfg.T], BF16)
        self.VT = self.scratch(f"VT{j}", [cfg.T, 512], BF16)
        with kb.phase() as ph:
            Wq = ph.sb([128, 2, 768], BF16, "Wq")
            Wqs = ph.sb([128, 2, NH, 32], BF16, "Wqs")
            Wk = ph.sb([128, NH, 64], BF16, "Wk")
            Wvv = ph.sb([128, NH, 64], BF16, "Wv")
            kb.dma(Wq[:], Wqb.rearrange("(c p) n -> p c n", p=128), eng="pool")
            Wq4 = Wqb.rearrange("(c p) (h d) -> p c h d", p=128, d=96)
            for (d0, s0) in ((0, 8), (8, 0), (16, 24), (24, 16)):
                for c in range(2):
                    kb.dma(Wqs[:, c, :, d0:d0 + 8], Wq4[:, c, :, 64 + s0:64 + s0 + 8], eng="pool")
            Wkv3 = Wkvb.rearrange("p (h d) -> p h d", d=128)
            kb.dma(Wk[:], Wkv3[:, :, 0:64], eng="pool")
            kb.dma(Wvv[:], Wkv3[:, :, 64:128], eng="pool")
            qa_g = ph.sb([128, 2], F32, "qa_g")
            kva_g = ph.sb([128, 1], F32, "kva_g")
            qn = ph.sb([96, 2], F32, "qn")
            kn = ph.sb([96, 2], F32, "kn")
            kb.dma(qa_g[:], sm["qa_g"])
            kb.dma(kva_g[:], sm["kva_g"])
            kb.dma(qn[:], sm["qn_col"])
            kb.dma(kn[:], sm["kn_col"])
            cq = ph.sb([128, 2, 512], F32, "cq")
            ckv = ph.sb([128, 512], F32, "ckv")
            kpe = ph.sb([96, 512], F32, "kpe")
            kpes = ph.sb([96, 512], F32, "kpes")
            cs = ph.sb([96, 512], F32, "cs")
            sn = ph.sb([96, 512], F32, "sn")
            sq = ph.sb([128, 2, 512], BF16, "sq")
            rs = ph.sb([128, 512], F32, "rs")
            cqn = ph.sb([128, 2, 512], BF16, "cqn")
            ckvn = ph.sb([128, 512], BF16, "ckvn")
            rk = ph.sb([96, 512], F32, "rk")
            t1s = [ph.sb([96, 512], F32, "t1") for _ in range(2)]
            t2s = [ph.sb([96, 512], F32, "t2") for _ in range(2)]
            sqhs = [ph.sb([96, 512], BF16, "sqh") for _ in range(2)]
            rshs = [ph.sb([96, 512], F32, "rsh") for _ in range(2)]
            ohs = [ph.sb([96, 512], BF16, "oh") for _ in range(2)]
            vsb = [ph.sb([128, 512], BF16, "vsb") for _ in range(2)]
            pA = [ph.ps() for _ in range(2)]
            pB = [ph.ps() for _ in range(2)]
            pS = [ph.ps() for _ in range(2)]
            pR = ph.ps()
            pV = ph.ps()
            u = 0
            for ti, (t0, TT) in enumerate(cfg.tiles):
                kb.dma(cq[:, :, 0:TT], self.UF[0:256, t0:t0 + TT].rearrange("(c p) t -> p c t", p=128))
                kb.dma(ckv[:, 0:TT], self.UF[256:384, t0:t0 + TT])
                kb.dma(kpe[64:96, 0:TT], self.UF[384:416, t0:t0 + TT])
                kb.dma(kpes[64:96, 0:TT], self.UF[416:448, t0:t0 + TT])
                kb.dma(cs[64:96, 0:TT], self.cin["cosT"][64:96, t0:t0 + TT])
                kb.dma(sn[64:96, 0:TT], self.cin["sinT"][64:96, t0:t0 + TT])
                kb.act(sq[:, :, 0:TT], cq[:, :, 0:TT], AF.Square)
                for c in range(2):
                    kb.mm(pR[:, 0:TT], self.ones[:], sq[:, c, 0:TT], start=(c == 0), stop=(c == 1))
                rstd_inplace(kb, rs[:, 0:TT], 1.0 / 256, EPS, src=pR[:, 0:TT])
                for c in range(2):
                    kb.v("dve", "scalar_tensor_tensor", cqn[:, c, 0:TT], [cq[:, c, 0:TT], qa_g[:, c:c + 1], rs[:, 0:TT]], ALU.mult, ALU.mult)
                kb.act(sq[:, 0, 0:TT], ckv[:, 0:TT], AF.Square)
                kb.mm(pR[:, 0:TT], self.ones[:], sq[:, 0, 0:TT])
                rstd_inplace(kb, rs[:, 0:TT], 1.0 / 128, EPS, src=pR[:, 0:TT])
                kb.v("dve", "scalar_tensor_tensor", ckvn[:, 0:TT], [ckv[:, 0:TT], kva_g[:, 0:1], rs[:, 0:TT]], ALU.mult, ALU.mult)
                kb.v("dve", "scalar_tensor_tensor", rk[64:96, 0:TT], [kpe[64:96, 0:TT], kn[64:96, 0:1], cs[64:96, 0:TT]], ALU.mult, ALU.mult)
                kb.v("dve", "scalar_tensor_tensor", t2s[0][64:96, 0:TT], [kpes[64:96, 0:TT], kn[64:96, 1:2], sn[64:96, 0:TT]], ALU.mult, ALU.mult)
                kb.v("pool", "tensor_tensor", rk[64:96, 0:TT], [rk[64:96, 0:TT], t2s[0][64:96, 0:TT]], ALU.add)
                for h in range(NH):
                    a, b2, s_, t1, t2, sqh, rsh, oh = pA[u % 2], pB[u % 2], pS[u % 2], t1s[u % 2], t2s[u % 2], sqhs[u % 2], rshs[u % 2], ohs[u % 2]
                    u += 1
                    for c in range(2):
                        kb.mm(a[0:96, 0:TT], Wq[:, c, h * 96:(h + 1) * 96], cqn[:, c, 0:TT], start=(c == 0), stop=(c == 1))
                    for c in range(2):
                        kb.mm(b2[64:96, 0:TT], Wqs[:, c, h, :], cqn[:, c, 0:TT], start=(c == 0), stop=(c == 1))
                    kb.act(sqh[0:96, 0:TT], a[0:96, 0:TT], AF.Square)
                    kb.mm(s_[0:96, 0:TT], self.ones[0:96, 0:96], sqh[0:96, 0:TT])
                    rstd_inplace(kb, rsh[0:96, 0:TT], 1.0 / 96, EPS, src=s_[0:96, 0:TT])
                    kb.v("dve", "scalar_tensor_tensor", oh[0:64, 0:TT], [a[0:64, 0:TT], qn[0:64, 0:1], rsh[0:64, 0:TT]], ALU.mult, ALU.mult)
                    kb.v("dve", "scalar_tensor_tensor", t1[64:96, 0:TT], [a[64:96, 0:TT], qn[64:96, 0:1], cs[64:96, 0:TT]], ALU.mult, ALU.mult)
                    kb.v("dve", "scalar_tensor_tensor", t2[64:96, 0:TT], [b2[64:96, 0:TT], qn[64:96, 1:2], sn[64:96, 0:TT]], ALU.mult, ALU.mult)
                    kb.v("pool", "tensor_tensor", t1[64:96, 0:TT], [t1[64:96, 0:TT], t2[64:96, 0:TT]], ALU.add)
                    kb.v("dve", "tensor_tensor", oh[64:96, 0:TT], [t1[64:96, 0:TT], rsh[64:96, 0:TT]], ALU.mult)
                    kb.dma(self.QF[h, :, t0:t0 + TT], oh[0:96, 0:TT])
                    a, s_, sqh, rsh, oh = pA[u % 2], pS[u % 2], sqhs[u % 2], rshs[u % 2], ohs[u % 2]
                    u += 1
                    kb.mm(a[0:64, 0:TT], Wk[:, h, :], ckvn[:, 0:TT])
                    kb.act(sqh[0:64, 0:TT], a[0:64, 0:TT], AF.Square)
                    kb.act(sqh[64:96, 0:TT], kpe[64:96, 0:TT], AF.Square)
                    kb.mm(s_[0:96, 0:TT], self.ones[0:96, 0:96], sqh[0:96, 0:TT])
                    rstd_inplace(kb, rsh[0:96, 0:TT], 1.0 / 96, EPS, src=s_[0:96, 0:TT])
                    kb.v("dve", "scalar_tensor_tensor", oh[0:64, 0:TT], [a[0:64, 0:TT], kn[0:64, 0:1], rsh[0:64, 0:TT]], ALU.mult, ALU.mult)
                    kb.v("dve", "tensor_tensor", oh[64:96, 0:TT], [rk[64:96, 0:TT], rsh[64:96, 0:TT]], ALU.mult)
                    kb.dma(self.KF[h, :, t0:t0 + TT], oh[0:96, 0:TT])
                for s in range(TT // 128):
                    vb = vsb[s % 2]
                    kb.mm(pV[:], ckvn[:, s * 128:(s + 1) * 128], Wvv[:].rearrange("p h d -> p (h d)"))
                    self.evac(vb[:], pV[:])
                    kb.dma(self.VT[t0 + s * 128:t0 + (s + 1) * 128, :], vb[:])

    def phase_e3(self, j):
        kb, cfg = self.kb, self.cfg
        self.AF_ = self.scratch(f"AF{j}", [512, cfg.T], BF16)
        nsub = cfg.nsub
        with kb.phase() as ph:
            Vall = ph.sb([128, nsub, NH, 65], BF16, "Vall")
            kb.v("pool", "memset", Vall[:], [], 1.0)
            for h_ in range(NH):
                kb.dma(Vall[:, :, h_, 0:64], self.VT[:, h_ * 64:(h_ + 1) * 64].rearrange("(b p) n -> p b n", p=128))
            sel = ph.sb([65, 64], F32, "sel")
            kb.v("pool", "memset", sel[:], [], 0.0)
            kb.v("pool", "memset", sel[64:65, :], [], 1.0)
            oext = ph.sb([65, 512], F32, "oext")
            Khs = [ph.sb([96, cfg.T], BF16, "Kh") for _ in range(2)]
            Qts = [ph.sb([96, 512], BF16, "Qt") for _ in range(2)]
            PTs = [ph.sb([128, 512], BF16, "PT") for _ in range(4)]
            rden = ph.sb([64, 512], F32, "rden")
            osb = [ph.sb([64, 512], BF16, "osb") for _ in range(2)]
            pS = [ph.ps() for _ in range(4)]
            pO = [ph.ps() for _ in range(2)]
            pD = [ph.ps() for _ in range(2)]
            DEPTH = 3
            units = []
            gi = 0
            for h in range(NH):
                for ti, (t0, TT) in enumerate(cfg.tiles):
                    nkb = 2 if ti == 0 else nsub
                    for kbk in range(nkb):
                        units.append((h, ti, t0, TT, kbk, nkb, gi))
                    gi += 1
            cur_h = [-1]
            cur_g = [-1]
            for idx in range(len(units) + DEPTH):
                if idx < len(units):
                    h, ti, t0, TT, kbk, nkb, g = units[idx]
                    if h != cur_h[0]:
                        cur_h[0] = h
                        kb.dma(Khs[h % 2][:], self.KF[h])
                    if g != cur_g[0]:
                        cur_g[0] = g
                        kb.dma(Qts[g % 2][:, 0:TT], self.QF[h, :, t0:t0 + TT])
                    ps, pt = pS[idx % 4], PTs[idx % 4]
                    kb.mm(ps[:, 0:TT], Khs[h % 2][:, kbk * 128:(kbk + 1) * 128], Qts[g % 2][:, 0:TT])
                    kb.act(pt[:, 0:TT], ps[:, 0:TT], AF.Exp, scale=MLA_SCALE)
                j2 = idx - DEPTH
                if j2 >= 0:
                    h, ti, t0, TT, kbk, nkb, g = units[j2]
                    pt = PTs[j2 % 4]
                    po, pd, ob = pO[g % 2], pD[g % 2], osb[g % 2]
                    kb.mm(po[0:65, 0:TT], Vall[:, kbk, h, :], pt[:, 0:TT], start=(kbk == 0), stop=(kbk == nkb - 1))
                    if kbk == nkb - 1:
                        kb.act(oext[:, 0:TT], po[0:65, 0:TT], AF.Copy)
                        kb.mm(pd[0:64, 0:TT], sel[:], oext[:, 0:TT])
                        kb.v("dve", "reciprocal", rden[:, 0:TT], [pd[0:64, 0:TT]])
                        kb.v("dve", "tensor_tensor", ob[:, 0:TT], [po[0:64, 0:TT], rden[:, 0:TT]], ALU.mult)
                        kb.dma(self.AF_[h * 64:(h + 1) * 64, t0:t0 + TT], ob[:, 0:TT])
    def load_const_tile(self, ph, name, dtype=F32):
        shp = CONST_SHAPES(self.cfg)[name]
        t = ph.sb(list(shp), F32, name)
        self.kb.dma(t[:], self.cin[name])
        if dtype == F32:
            return t
        t2 = ph.sb(list(shp), dtype, name + "b")
        self.kb.v("dve", "tensor_copy", t2[:], [t[:]])
        return t2

    def phase_e4(self, j, sm):
        kb, cfg = self.kb, self.cfg
        T = cfg.T
        self.GQ = self.scratch(f"GQ{j}", [GH, 128, T], BF16)
        self.GK = self.scratch(f"GK{j}", [GH, 128, T], BF16)
        self.GKT = self.scratch(f"GKT{j}", [T, 512], BF16)
        self.GVT = self.scratch(f"GVT{j}", [T, 512], BF16)
        last = len(cfg.tiles) - 1
        with kb.phase() as ph:
            cw = ph.sb([128, 12, 5], F32, "cw")
            kb.dma(cw[:], sm["convw"])
            xs = [ph.sb([128, 516], F32, "x") for _ in range(2)]
            accs = [ph.sb([128, 512], F32, "acc") for _ in range(2)]
            ys = [ph.sb([128, 512], F32, "y") for _ in range(2)]
            sqs = [ph.sb([128, 512], BF16, "sq") for _ in range(2)]
            rs = ph.sb([128, 512], F32, "rs")
            yns = [ph.sb([128, 512], BF16, "yn") for _ in range(2)]
            tsb = [ph.sb([128, 4, 128], BF16, "tsb") for _ in range(2)]
            pR = [ph.ps() for _ in range(2)]
            ptr = [ph.ps([128, 1024], BF16, "ptr") for _ in range(2)]
            u = 0
            for ti, (t0, TT) in enumerate(cfg.tiles):
                for cc in range(12):
                    x, acc, y, sq, yn, pr, pt, tb = xs[u % 2], accs[u % 2], ys[u % 2], sqs[u % 2], yns[u % 2], pR[u % 2], ptr[u % 2], tsb[u % 2]
                    u += 1
                    r0 = 448 + cc * 128
                    kb.dma(x[:, 2:TT + 2], self.UF[r0:r0 + 128, t0:t0 + TT])
                    if ti in (0, 1):
                        kb.v("pool", "memset", x[:, 0:2], [], 0.0)
                    else:
                        kb.dma(x[:, 0:2], self.UF[r0:r0 + 128, t0 - 2:t0])
                    if ti in (0, last):
                        kb.v("pool", "memset", x[:, TT + 2:TT + 4], [], 0.0)
                    else:
                        kb.dma(x[:, TT + 2:TT + 4], self.UF[r0:r0 + 128, t0 + TT:t0 + TT + 2])
                    kb.v("dve", "tensor_scalar", acc[:, 0:TT], [x[:, 0:TT]], cw[:, cc, 0:1], None, ALU.mult)
                    for jj in range(1, 5):
                        kb.v("dve", "scalar_tensor_tensor", acc[:, 0:TT], [x[:, jj:jj + TT], cw[:, cc, jj:jj + 1], acc[:, 0:TT]], ALU.mult, ALU.add)
                    kb.act(y[:, 0:TT], acc[:, 0:TT], AF.Silu)
                    if cc < 8:
                        kb.act(sq[:, 0:TT], y[:, 0:TT], AF.Square)
                        kb.mm(pr[:, 0:TT], self.ones[:], sq[:, 0:TT])
                        rstd_inplace(kb, rs[:, 0:TT], 1.0, EPS, src=pr[:, 0:TT])
                        kb.v("dve", "scalar_tensor_tensor", yn[:, 0:TT], [y[:, 0:TT], (128 ** -0.5) if cc < 4 else 1.0, rs[:, 0:TT]], ALU.mult, ALU.mult)
                        dst = self.GQ[cc] if cc < 4 else self.GK[cc - 4]
                        kb.dma(dst[:, t0:t0 + TT], yn[:, 0:TT])
                    else:
                        kb.v("pool", "tensor_copy", yn[:, 0:TT], [y[:, 0:TT]])
                    if cc >= 4:
                        ns = TT // 128
                        for s in range(ns):
                            kb.tr(pt[:, s * 128:(s + 1) * 128], yn[:, s * 128:(s + 1) * 128], self.ident[:])
                        self.evac(tb[:, 0:ns, :], pt[:, 0:TT].rearrange("p (s c) -> p s c", c=128))
                        dstT = self.GKT if cc < 8 else self.GVT
                        hh = (cc - 4) % 4
                        kb.dma(dstT[t0:t0 + TT, hh * 128:(hh + 1) * 128].rearrange("(s p) c -> p s c", p=128), tb[:, 0:ns, :])

    def phase_e5(self, j, sm):
        kb, cfg = self.kb, self.cfg
        T, nsub = cfg.T, cfg.nsub
        self.OD = [self.scratch(f"OD{j}_{d}", [T, 512]) for d in range(2)]
        fwd = list(range(nsub))
        bwd = [1, 0] + list(range(nsub - 1, 1, -1))
        with kb.phase() as ph:
            mle = self.load_const_tile(ph, "m_le")
            mge = self.load_const_tile(ph, "m_ge")
            mlt = self.load_const_tile(ph, "m_lt")
            mgt = self.load_const_tile(ph, "m_gt")
            msame = self.load_const_tile(ph, "m_same")
            mh0 = self.load_const_tile(ph, "m_h0")
            mh1 = self.load_const_tile(ph, "m_h1")
            onesf = self.load_const_tile(ph, "ones_f")
            gv = ph.sb([128, 16], F32, "gv")
            kb.dma(gv[:], sm["gdn_vec"].broadcast_to([128, 16]))
            negA = ph.sb([128, 8], F32, "negA")
            kb.act(negA[:], gv[:, 0:8], AF.Exp)
            kb.v("dve", "tensor_scalar", negA[:], [negA[:]], -1.0, None, ALU.mult)
            S = [[ph.sb([128, 128], F32, f"S{d}{h}") for h in range(GH)] for d in range(2)]
            Sb = [[ph.sb([128, 128], BF16, f"Sb{d}{h}") for h in range(GH)] for d in range(2)]
            for d in range(2):
                for h in range(GH):
                    kb.v("pool", "memset", S[d][h][:], [], 0.0)
                    kb.v("pool", "memset", Sb[d][h][:], [], 0.0)
            banks = [ph.ps() for _ in range(7)]
            tbank = ph.ps([128, 1024], BF16, "tbank")
            slot = [0]
            tslot = [0]

            def ps128t():
                i = tslot[0] % 8
                tslot[0] += 1
                return V(tbank[:, i * 128:(i + 1) * 128], tbank.name)

            def ps128(bf=False):
                i = slot[0] % 7
                slot[0] += 1
                b = banks[i]
                return V(b[:, 0:128], b.name)

            def sub(v, r, cs=None):
                a = v.ap[r, :] if cs is None else v.ap[r, cs]
                return V(a, v.key)

            UB = []
            for d in range(4):
                b = {}
                b["ab"] = ph.sb([128, 16], F32, "ab")
                b["KT"] = ph.sb([128, GH, 128], BF16, "KT")
                b["QT"] = ph.sb([128, GH, 128], BF16, "QT")
                b["Ktm"] = ph.sb([128, 512], BF16, "Ktm")
                b["Vtm"] = ph.sb([128, 512], BF16, "Vtm")
                for nm in ("xa", "g", "beta", "nbeta", "gc", "gt", "eg", "ek", "beg", "egl0", "egl1"):
                    b[nm] = ph.sb([128, 4], F32, nm)
                b["osb"] = ph.sb([128, 512], F32, "osb")
                UB.append(b)
            HB = []
            for i in range(8):
                b = {}
                for nm in ("diag", "e1", "e2", "Dst", "DT", "EG", "usb"):
                    b[nm] = ph.sb([128, 128], F32, nm)
                for nm in ("X0", "X1", "XT0", "XT1", "AT0", "AT1", "Vb", "Kbg", "Khat", "QgT", "AqkT", "wT", "vnew"):
                    b[nm] = ph.sb([128, 128], BF16, nm)
                HB.append(b)
            def unit(d, h, B, Hb):
                hs = slice(h * 128, (h + 1) * 128)
                gcol = B["gc"][:, h:h + 1]
                kb.v("dve", "tensor_scalar", Hb["diag"][:], [self.ident_f[:]], gcol, None, ALU.mult)
                pg = ps128()
                kb.mm(pg, onesf[:], Hb["diag"][:])
                kb.v("dve", "tensor_scalar", Hb["e1"][:], [pg], gcol, 0.0, ALU.subtract, ALU.max)
                kb.v("dve", "tensor_scalar", Hb["e2"][:], [pg], gcol, 0.0, ALU.subtract, ALU.min)
                kb.act(Hb["EG"][:], pg, AF.Exp)
                kb.act(Hb["e1"][:], Hb["e1"][:], AF.Exp, scale=-1.0)
                kb.act(Hb["e2"][:], Hb["e2"][:], AF.Exp)
                kb.v("pool", "tensor_tensor", Hb["Dst"][:], [Hb["e1"][:], (mgt if d == 0 else mlt)[:]], ALU.mult)
                kb.v("pool", "tensor_tensor", Hb["DT"][:], [Hb["e2"][:], (mle if d == 0 else mge)[:]], ALU.mult)
                yield
                pkk = ps128()
                kb.mm(pkk, B["KT"][:, h, :], B["KT"][:, h, :])
                kb.v("dve", "scalar_tensor_tensor", Hb["X0"][:], [pkk, B["nbeta"][:, h:h + 1], Hb["Dst"][:]], ALU.mult, ALU.mult)
                pkq = ps128()
                kb.mm(pkq, B["KT"][:, h, :], B["QT"][:, h, :])
                kb.v("dve", "tensor_tensor", Hb["AqkT"][:], [pkq, Hb["DT"][:]], ALU.mult)
                yield
                pxt_b = ps128t()
                kb.tr(pxt_b, Hb["X0"][:], self.ident[:])
                kb.v("dve", "tensor_copy", Hb["XT0"][:], [pxt_b])
                kb.v("pool", "tensor_tensor", Hb["AT0"][:], [Hb["XT0"][:], self.ident[:]], ALU.add)
                yield
                X, XT, AT = Hb["X0"], Hb["XT0"], Hb["AT0"]
                for js in range(1, 6):
                    Xn = Hb["X1"] if X is Hb["X0"] else Hb["X0"]
                    XTn = Hb["XT1"] if XT is Hb["XT0"] else Hb["XT0"]
                    ATn = Hb["AT1"] if AT is Hb["AT0"] else Hb["AT0"]
                    px = ps128()
                    kb.mm(px, XT[:], X[:])
                    if js < 5:
                        pxT = ps128()
                        kb.mm(pxT, X[:], XT[:])
                    kb.act(Xn[:], px, AF.Copy)
                    if js < 5:
                        kb.v("dve", "tensor_copy", XTn[:], [pxT])
                    yield
                    pa = ps128()
                    kb.mm(pa, Xn[:], AT[:])
                    kb.v("dve", "tensor_tensor", ATn[:], [pa, AT[:]], ALU.add)
                    X, XT, AT = Xn, XTn, ATn
                    yield
                kb.v("pool", "tensor_scalar", Hb["Vb"][:], [B["Vtm"][:, hs]], B["beta"][:, h:h + 1], 1.0, ALU.mult, ALU.mult)
                kb.v("pool", "tensor_scalar", Hb["Kbg"][:], [B["Ktm"][:, hs]], B["beg"][:, h:h + 1], 1.0, ALU.mult, ALU.mult)
                kb.act(Hb["Khat"][:], B["Ktm"][:, hs], AF.Copy, scale=B["ek"][:, h:h + 1])
                kb.v("pool", "tensor_tensor", Hb["QgT"][:], [B["QT"][:, h, :], Hb["EG"][:]], ALU.mult)
                pu = ps128()
                kb.mm(pu, AT[:], Hb["Vb"][:])
                kb.act(Hb["usb"][:], pu, AF.Copy)
                pw = ps128()
                kb.mm(pw, Hb["Kbg"][:], AT[:])
                kb.act(Hb["wT"][:], pw, AF.Copy)
                yield
                for half in ((0, 1) if d == 0 else (1, 0)):
                    r = slice(half * 64, half * 64 + 64)
                    egl = B["egl0"] if half == 0 else B["egl1"]
                    oq = V(B["osb"][r, hs], (B["osb"].name, h))
                    p1 = ps128()
                    kb.mm(sub(p1, r), Hb["wT"][:, r], Sb[d][h][:])
                    kb.v("dve", "tensor_tensor", Hb["vnew"][r, :], [Hb["usb"][r, :], sub(p1, r)], ALU.subtract)
                    yield
                    p2 = ps128()
                    kb.mm(sub(p2, r), Hb["QgT"][:, r], Sb[d][h][:], start=True, stop=False)
                    kb.mm(sub(p2, r), Hb["AqkT"][r, r], Hb["vnew"][r, :], start=False, stop=True)
                    kb.act(oq, sub(p2, r), AF.Copy)
                    p3 = ps128()
                    kb.mm(p3, Hb["Khat"][r, :], Hb["vnew"][r, :])
                    kb.v("dve", "scalar_tensor_tensor", S[d][h][:], [S[d][h][:], egl[:, h:h + 1], p3], ALU.mult, ALU.add)
                    kb.act(Sb[d][h][:], S[d][h][:], AF.Copy)
                    yield

            for step in range(nsub):
                makers = []
                Bs = []
                for d in range(2):
                    sb_ = fwd[step] if d == 0 else bwd[step]
                    r0 = sb_ * 128
                    B = UB[(step % 2) * 2 + d]
                    Bs.append((B, r0, d))
                    kb.dma(B["ab"][:], self.UT[r0:r0 + 128, 512:528])
                    kb.dma(B["KT"][:], self.GK[:, :, r0:r0 + 128].rearrange("h c t -> c h t"))
                    kb.dma(B["QT"][:], self.GQ[:, :, r0:r0 + 128].rearrange("h c t -> c h t"))
                    kb.dma(B["Ktm"][:], self.GKT[r0:r0 + 128, :])
                    kb.dma(B["Vtm"][:], self.GVT[r0:r0 + 128, :])
                    ab = B["ab"]
                    kb.v("dve", "tensor_tensor", B["xa"][:], [ab[:, d * 8:d * 8 + 4], gv[:, 8 + d * 4:12 + d * 4]], ALU.add)
                    kb.act(B["xa"][:], B["xa"][:], AF.Exp)
                    kb.act(B["xa"][:], B["xa"][:], AF.Ln, bias=1.0)
                    kb.v("dve", "tensor_tensor", B["g"][:], [B["xa"][:], negA[:, d * 4:d * 4 + 4]], ALU.mult)
                    kb.act(B["beta"][:], ab[:, d * 8 + 4:d * 8 + 8], AF.Sigmoid)
                    kb.v("dve", "tensor_scalar", B["nbeta"][:], [B["beta"][:]], -1.0, None, ALU.mult)
                    Mc = mle if d == 0 else mge
                    p_gc, p_gt, p_0, p_1 = ps128(), ps128(), ps128(), ps128()
                    kb.mm(sub(p_gc, slice(0, 128), slice(0, 4)), Mc[:], B["g"][:])
                    kb.mm(sub(p_gt, slice(0, 128), slice(0, 4)), msame[:], B["g"][:])
                    kb.mm(sub(p_0, slice(0, 128), slice(0, 4)), mh0[:], B["g"][:])
                    kb.mm(sub(p_1, slice(0, 128), slice(0, 4)), mh1[:], B["g"][:])
                    kb.v("dve", "tensor_copy", B["gc"][:], [sub(p_gc, slice(0, 128), slice(0, 4))])
                    kb.v("dve", "tensor_tensor", B["gt"][:], [sub(p_gt, slice(0, 128), slice(0, 4)), B["gc"][:]], ALU.subtract)
                    kb.act(B["ek"][:], B["gt"][:], AF.Exp)
                    kb.act(B["eg"][:], B["gc"][:], AF.Exp)
                    kb.act(B["egl0"][:], sub(p_0, slice(0, 128), slice(0, 4)), AF.Exp)
                    kb.act(B["egl1"][:], sub(p_1, slice(0, 128), slice(0, 4)), AF.Exp)
                    kb.v("dve", "tensor_tensor", B["beg"][:], [B["beta"][:], B["eg"][:]], ALU.mult)
                for h in range(GH):
                    for (B, r0, d) in Bs:
                        makers.append(lambda slot_, d=d, h=h, B=B: unit(d, h, B, HB[slot_]))
                interleave(makers, 8)
                for (B, r0, d) in Bs:
                    kb.dma(self.OD[d][r0:r0 + 128, :], B["osb"][:], extra_reads=[(B["osb"].name, h) for h in range(GH)])

    def phase_e6(self, l, j, sm):
        kb, cfg = self.kb, self.cfg
        Wout = self.inp(f"hy_w_out__{j}", [D, D])
        self.Fm = self.scratch(f"F{l}", [cfg.T, D], BF16)
        with kb.phase() as ph:
            Wo = ph.sb([128, 8, D], BF16, "Wo")
            kb.dma(Wo[:], Wout.rearrange("(c p) n -> p c n", p=128), eng="pool")
            og = ph.sb([128, 128], F32, "og")
            kb.dma(og[:], sm["outg"].broadcast_to([128, 128]))
            o0s = [ph.sb([128, 512], F32, "o0") for _ in range(2)]
            o1s = [ph.sb([128, 512], F32, "o1") for _ in range(2)]
            zs = [ph.sb([128, 512], F32, "z") for _ in range(2)]
            sqo = ph.sb([128, 512], F32, "sqo")
            ssq = [ph.sb([128, 4], F32, "ssq") for _ in range(2)]
            on = ph.sb([128, 512], F32, "on")
            dtm = [ph.sb([128, 512], BF16, "dtm") for _ in range(2)]
            XT = [ph.sb([128, 8, 128], BF16, "XT") for _ in range(2)]
            ptr = [ph.ps([128, 1024], BF16, "ptr") for _ in range(2)]
            py = [ph.ps() for _ in range(4)]
            hts = [ph.sb([128, D], F32, "ht") for _ in range(2)]
            tmp = [ph.sb([128, D], F32, "tmp") for _ in range(2)]
            junk = ph.sb([128, D], F32, "junk")
            sss = [ph.sb([128, 1], F32, "ss") for _ in range(2)]
            tmp2 = [ph.sb([128, D], F32, "tmp2") for _ in range(2)]
            fb = [ph.sb([128, D], BF16, "fb") for _ in range(2)]
            for s in range(cfg.nsub):
                mod = self.modC if s < 2 else self.modL
                r0 = s * 128
                i = s % 2
                kb.dma(o0s[i][:], self.OD[0][r0:r0 + 128, :])
                kb.dma(o1s[i][:], self.OD[1][r0:r0 + 128, :])
                kb.dma(zs[i][:], self.UT[r0:r0 + 128, 0:512])
                kb.dma(XT[i][:, 0:4, :], self.AF_[:, r0:r0 + 128].rearrange("(c p) t -> p c t", p=128))
                kb.dma(hts[i][:], self.H[r0:r0 + 128, :])
                kb.v("pool", "tensor_tensor", o0s[i][:], [o0s[i][:], o1s[i][:]], ALU.add)
                kb.v("dve", "tensor_tensor", sqo[:], [o0s[i][:], o0s[i][:]], ALU.mult)
                kb.v("dve", "tensor_reduce", ssq[i][:], [sqo[:].rearrange("p (h v) -> p h v", h=4)], AX.X, ALU.add)
                rstd_inplace(kb, ssq[i][:], 1.0 / 128, EPS)
                for h in range(4):
                    hs = slice(h * 128, (h + 1) * 128)
                    kb.v("dve", "scalar_tensor_tensor", on[:, hs], [o0s[i][:, hs], ssq[i][:, h:h + 1], og[:]], ALU.mult, ALU.mult)
                kb.act(zs[i][:], zs[i][:], AF.Silu)
                kb.v("pool", "tensor_tensor", dtm[i][:], [on[:], zs[i][:]], ALU.mult)
                for c in range(4):
                    kb.tr(ptr[i][:, c * 128:(c + 1) * 128], dtm[i][:, c * 128:(c + 1) * 128], self.ident[:])
                self.evac(XT[i][:, 4:8, :], ptr[i][:, 0:512].rearrange("p (c t) -> p c t", c=4))
                for n in range(2):
                    pp = py[(2 * s + n) % 4]
                    for c in range(8):
                        kb.mm(pp[:], XT[i][:, c, :], Wo[:, c, n * 512:(n + 1) * 512], start=(c == 0), stop=(c == 7))
                    kb.v("dve", "tensor_tensor", tmp[i][:, n * 512:(n + 1) * 512], [pp[:], mod[2][:, n * 512:(n + 1) * 512]], ALU.mult)
                kb.v("pool", "tensor_tensor", hts[i][:], [tmp[i][:], hts[i][:]], ALU.add)
                kb.dma(self.H[r0:r0 + 128, :], hts[i][:])
                self.norm_sub(hts[i][:], junk[:], sss[i][:], tmp2[i][:], fb[i][:], mod[4][:], mod[3][:])
                kb.dma(self.Fm[r0:r0 + 128, :], fb[i][:])
    def moe_setup(self):
        kb, cfg = self.kb, self.cfg
        if hasattr(self, "OH"):
            return
        ns, nb = cfg.nsub, cfg.nblk
        self.OH = kb.sb([128, ns, 64], BF16, "OH")
        self.WTS = kb.sb([128, ns, 2], F32, "WTS")
        self.DEST = kb.sb([128, ns, 2], I32, "DEST")
        self.I1 = kb.sb([128, nb, 8], I32, "I1")
        self.I2 = kb.sb([128, nb, 4], I32, "I2")
        self.offs = kb.sb([128, 32], F32, "offs")
        self.XS = self.scratch("XS", [cfg.nslot, D], BF16)
        self.YS = self.scratch("YS", [cfg.nslot, D], F32)

    def moe(self, l):
        self.moe_setup()
        kb, cfg = self.kb, self.cfg
        ns, nb = cfg.nsub, cfg.nblk
        wg = self.inp(f"moe_w_group__{l}", [D, 4])
        bg = self.inp(f"moe_b_group__{l}", [1, 4])
        we = self.inp(f"moe_w_expert__{l}", [D, 32])
        be = self.inp(f"moe_b_expert__{l}", [1, 32])
        w1 = self.inp(f"moe_w1__{l}", [32 * D, 512])
        w3 = self.inp(f"moe_w3__{l}", [32 * D, 512])
        w2 = self.inp(f"moe_w2__{l}", [32 * 512, D])
        OH, WTS, DEST, I1, I2, offs = self.OH, self.WTS, self.DEST, self.I1, self.I2, self.offs
        with kb.phase() as ph:
            Wr = ph.sb([128, 8, 36], BF16, "Wr")
            kb.dma(Wr[:, :, 0:4], wg.rearrange("(c p) n -> p c n", p=128), eng="pool")
            kb.dma(Wr[:, :, 4:36], we.rearrange("(c p) n -> p c n", p=128), eng="pool")
            br = ph.sb([128, 36], F32, "br")
            kb.dma(br[:, 0:4], bg.broadcast_to([128, 4]))
            kb.dma(br[:, 4:36], be.broadcast_to([128, 32]))
            zt = ph.sb([128, D], BF16, "zt")
            kb.v("pool", "memset", zt[:], [], 0.0)
            for i in range(cfg.nslot // 128):
                kb.dma(self.XS[i * 128:(i + 1) * 128, :], zt[:], wkey=("XS", "z", i))
            fts = [ph.sb([128, D], BF16, "ft") for _ in range(2)]
            fT = [ph.sb([128, 8, 128], BF16, "fT") for _ in range(2)]
            ptr = [ph.ps([128, 1024], BF16, "ptr") for _ in range(2)]
            plg = [ph.ps() for _ in range(2)]
            pcnt = ph.ps()
            sm_ = [{nm: ph.sb([128, w], F32, nm) for nm, w in (("lg", 36), ("mx", 1), ("nmx", 1), ("eg", 4), ("sg", 1), ("pg", 1), ("G1", 4),
                                                               ("pen", 4), ("lem", 32), ("top", 8), ("dif", 1), ("r", 1), ("den", 1))} for _ in range(2)]
            osum = [ph.sb([128, 32], BF16, "osum") for _ in range(2)]
            for s in range(ns):
                i = s % 2
                ft, t = fts[i], sm_[i]
                kb.dma(ft[:], self.Fm[s * 128:(s + 1) * 128, :])
                for c in range(8):
                    kb.tr(ptr[i][:, c * 128:(c + 1) * 128], ft[:, c * 128:(c + 1) * 128], self.ident[:])
                self.evac(fT[i][:], ptr[i][:].rearrange("p (c t) -> p c t", c=8))
                for c in range(8):
                    kb.mm(plg[i][:, 0:36], fT[i][:, c, :], Wr[:, c, :], start=(c == 0), stop=(c == 7))
                kb.v("dve", "tensor_tensor", t["lg"][:], [plg[i][:, 0:36], br[:]], ALU.add)
                kb.v("dve", "reduce_max", t["mx"][:], [t["lg"][:, 0:4]], AX.X)
                kb.v("dve", "tensor_scalar", t["nmx"][:], [t["mx"][:]], -1.0, None, ALU.mult)
                kb.act(t["eg"][:], t["lg"][:, 0:4], AF.Exp, bias=t["nmx"][:], accum_out=t["sg"][:])
                kb.v("dve", "reciprocal", t["pg"][:], [t["sg"][:]])
                kb.v("dve", "tensor_scalar", t["G1"][:], [t["lg"][:, 0:4]], t["mx"][:], None, ALU.is_equal)
                kb.v("dve", "tensor_scalar", t["pen"][:], [t["G1"][:]], 1e30, -1e30, ALU.mult, ALU.add)
                for g in range(4):
                    kb.v("dve", "tensor_scalar", t["lem"][:, g * 8:(g + 1) * 8], [t["lg"][:, 4 + g * 8:12 + g * 8]], t["pen"][:, g:g + 1], None, ALU.add)
                kb.v("dve", "max", t["top"][:], [t["lem"][:]])
                kb.v("dve", "tensor_scalar", OH[:, s, 0:32], [t["lem"][:]], t["top"][:, 0:1], None, ALU.is_equal)
                kb.v("dve", "tensor_scalar", OH[:, s, 32:64], [t["lem"][:]], t["top"][:, 1:2], None, ALU.is_equal)
                kb.v("dve", "tensor_tensor", t["dif"][:], [t["top"][:, 1:2], t["top"][:, 0:1]], ALU.subtract)
                kb.act(t["r"][:], t["dif"][:], AF.Exp)
                kb.v("dve", "tensor_scalar", t["den"][:], [t["r"][:]], 1.0, None, ALU.add)
                kb.v("dve", "reciprocal", t["den"][:], [t["den"][:]])
                kb.v("dve", "tensor_tensor", WTS[:, s, 0:1], [t["pg"][:], t["den"][:]], ALU.mult)
                kb.v("dve", "tensor_tensor", WTS[:, s, 1:2], [WTS[:, s, 0:1], t["r"][:]], ALU.mult)
                kb.v("pool", "tensor_tensor", osum[i][:], [OH[:, s, 0:32], OH[:, s, 32:64]], ALU.add)
                kb.mm(pcnt[:, 0:32], self.ones[:], osum[i][:], start=(s == 0), stop=(s == ns - 1))
            cnt = ph.sb([128, 32], F32, "cnt")
            kb.v("dve", "tensor_copy", cnt[:], [pcnt[:, 0:32]])
            thr = ph.sb([128, 34], F32, "thr")
            kb.dma(thr[:], self.cin["thr"])
            cmp = ph.sb([128, 32, 34], F32, "cmp")
            kb.v("dve", "tensor_tensor", cmp[:], [cnt[:].unsqueeze(2).to_broadcast([128, 32, 34]), thr[:].unsqueeze(1).to_broadcast([128, 32, 34])], ALU.is_gt)
            padded = ph.sb([128, 32], F32, "padded")
            kb.v("dve", "tensor_reduce", padded[:], [cmp[:]], AX.X, ALU.add)
            kb.v("dve", "tensor_scalar", padded[:], [padded[:]], float(MOE_BS), None, ALU.mult)
            onesr = ph.sb([128, 32], F32, "onesr")
            kb.v("pool", "memset", onesr[:], [], 1.0)
            pend = ph.sb([128, 32], F32, "pend")
            kb.v("dve", "tensor_tensor_scan", pend[:], [onesr[:], padded[:]], 0.0, ALU.mult, ALU.add)
            kb.v("dve", "tensor_tensor", offs[:], [pend[:], padded[:]], ALU.subtract)
            bst = ph.sb([128, nb], F32, "bst")
            kb.dma(bst[:], self.cin["blk_start"])
            cmp2 = ph.sb([128, nb, 32], F32, "cmp2")
            kb.v("dve", "tensor_tensor", cmp2[:], [pend[:].unsqueeze(1).to_broadcast([128, nb, 32]), bst[:].unsqueeze(2).to_broadcast([128, nb, 32])], ALU.is_le)
            blke = ph.sb([128, nb], F32, "blke")
            kb.v("dve", "tensor_reduce", blke[:], [cmp2[:]], AX.X, ALU.add)
            kb.v("dve", "tensor_scalar", blke[:], [blke[:]], 31.0, None, ALU.min)
            iop = ph.sb([128, 1], F32, "iop")
            kb.dma(iop[:], self.cin["iota_p"])
            b1 = ph.sb([128, nb], F32, "b1")
            b2 = ph.sb([128, nb], F32, "b2")
            kb.v("dve", "tensor_scalar", b1[:], [blke[:]], 1024.0, iop[:, 0:1], ALU.mult, ALU.add)
            kb.v("dve", "tensor_scalar", b2[:], [blke[:]], 512.0, iop[:, 0:1], ALU.mult, ALU.add)
            for c in range(8):
                kb.v("dve", "tensor_scalar", I1[:, :, c], [b1[:]], float(c * 128), None, ALU.add)
            for c in range(4):
                kb.v("dve", "tensor_scalar", I2[:, :, c], [b2[:]], float(c * 128), None, ALU.add)
            tri = ph.sb([128, 128], F32, "trif")
            kb.dma(tri[:], self.cin["tri_lt_full"])
            trib = ph.sb([128, 128], BF16, "trib")
            kb.v("dve", "tensor_copy", trib[:], [tri[:]])
            kb.barrier()
            basep = ph.sb([128, 32], F32, "basep")
            kb.v("dve", "tensor_copy", basep[:], [offs[:]])
            pcx = [ph.ps() for _ in range(2)]
            pos = [ph.sb([128, 32], F32, "pos") for _ in range(2)]
            tm = [ph.sb([128, 32], F32, "tm") for _ in range(2)]
            dd = [ph.sb([128, 2], F32, "dd") for _ in range(2)]
            for s in range(ns):
                i = s % 2
                kb.v("pool", "tensor_tensor", osum[i][:], [OH[:, s, 0:32], OH[:, s, 32:64]], ALU.add)
                kb.mm(pcx[i][:, 0:32], trib[:], osum[i][:])
                kb.mm(pcx[i][:, 32:64], self.ones[:], osum[i][:])
                kb.v("dve", "tensor_tensor", pos[i][:], [pcx[i][:, 0:32], basep[:]], ALU.add)
                kb.v("dve", "tensor_tensor", basep[:], [pcx[i][:, 32:64], basep[:]], ALU.add)
                for k in range(2):
                    kb.v("dve", "tensor_tensor", tm[i][:], [OH[:, s, k * 32:(k + 1) * 32], pos[i][:]], ALU.mult)
                    kb.v("dve", "tensor_reduce", dd[i][:, k:k + 1], [tm[i][:]], AX.X, ALU.add)
                kb.v("dve", "tensor_copy", DEST[:, s, :], [dd[i][:]])
                ft = fts[i]
                kb.dma(ft[:], self.Fm[s * 128:(s + 1) * 128, :])
                for k in range(2):
                    kb.scatter(self.XS, ft[:], DEST[:, s, k:k + 1], wkey=("XS", "sc"))
        with kb.phase() as ph:
            W1 = [ph.sb([128, 8, 512], BF16, "W1") for _ in range(2)]
            W3 = [ph.sb([128, 8, 512], BF16, "W3") for _ in range(2)]
            W2 = [ph.sb([128, 4, D], BF16, "W2") for _ in range(2)]
            xs = [ph.sb([128, D], BF16, "xs") for _ in range(2)]
            xT = [ph.sb([128, 8, 512], BF16, "xT") for _ in range(2)]
            sil = [ph.sb([128, 512], F32, "sil") for _ in range(2)]
            hid = [ph.sb([128, 4, 512], BF16, "hid") for _ in range(2)]
            ysb = [ph.sb([128, D], F32, "ysb") for _ in range(2)]
            ptr = [ph.ps([128, 1024], BF16, "ptr") for _ in range(2)]
            p1 = [ph.ps() for _ in range(2)]
            p3 = [ph.ps() for _ in range(2)]
            py = [ph.ps() for _ in range(2)]
            u = 0
            for b in range(nb):
                i = b % 2
                for c in range(8):
                    kb.gather(W1[i][:, c, :], w1, I1[:, b, c:c + 1])
                    kb.gather(W3[i][:, c, :], w3, I1[:, b, c:c + 1])
                for c in range(4):
                    kb.gather(W2[i][:, c, :], w2, I2[:, b, c:c + 1])
                for s in range(4):
                    x = xs[u % 2]
                    pt = ptr[u % 2]
                    u += 1
                    r0 = b * MOE_BS + s * 128
                    kb.dma(x[:], self.XS[r0:r0 + 128, :], rkey=("XS", "sc"))
                    for c in range(8):
                        kb.tr(pt[:, c * 128:(c + 1) * 128], x[:, c * 128:(c + 1) * 128], self.ident[:])
                    self.evac(xT[i][:, :, s * 128:(s + 1) * 128], pt[:].rearrange("p (c t) -> p c t", c=8))
                for hc in range(4):
                    a, b3 = p1[hc % 2], p3[hc % 2]
                    for c in range(8):
                        kb.mm(a[:], W1[i][:, c, hc * 128:(hc + 1) * 128], xT[i][:, c, :], start=(c == 0), stop=(c == 7))
                    for c in range(8):
                        kb.mm(b3[:], W3[i][:, c, hc * 128:(hc + 1) * 128], xT[i][:, c, :], start=(c == 0), stop=(c == 7))
                    kb.act(sil[hc % 2][:], a[:], AF.Silu)
                    kb.v("dve", "tensor_tensor", hid[i][:, hc, :], [b3[:], sil[hc % 2][:]], ALU.mult)
                for s in range(4):
                    yb = ysb[s % 2]
                    for n in range(2):
                        pp = py[n]
                        for hc in range(4):
                            kb.mm(pp[:], hid[i][:, hc, s * 128:(s + 1) * 128], W2[i][:, hc, n * 512:(n + 1) * 512], start=(hc == 0), stop=(hc == 3))
                        self.evac(yb[:, n * 512:(n + 1) * 512], pp[:])
                    r0 = b * MOE_BS + s * 128
                    kb.dma(self.YS[r0:r0 + 128, :], yb[:], wkey=("YS", "w"))
        with kb.phase() as ph:
            y1 = [ph.sb([128, D], F32, "y1") for _ in range(2)]
            y2 = [ph.sb([128, D], F32, "y2") for _ in range(2)]
            hts = [ph.sb([128, D], F32, "ht") for _ in range(2)]
            for s in range(ns):
                i = s % 2
                mod = self.modC if s < 2 else self.modL
                kb.gather(y1[i][:], self.YS, DEST[:, s, 0:1], rkey=("YS", "w"))
                kb.gather(y2[i][:], self.YS, DEST[:, s, 1:2], rkey=("YS", "w"))
                kb.dma(hts[i][:], self.H[s * 128:(s + 1) * 128, :])
                kb.v("dve", "tensor_scalar", y1[i][:], [y1[i][:]], WTS[:, s, 0:1], None, ALU.mult)
                kb.v("dve", "scalar_tensor_tensor", y1[i][:], [y2[i][:], WTS[:, s, 1:2], y1[i][:]], ALU.mult, ALU.add)
                kb.v("pool", "tensor_tensor", y1[i][:], [y1[i][:], mod[5][:]], ALU.mult)
                kb.v("dve", "tensor_tensor", hts[i][:], [hts[i][:], y1[i][:]], ALU.add)
                kb.dma(self.H[s * 128:(s + 1) * 128, :], hts[i][:])
    def odd_layer(self, l):
        j = l // 2
        self.phase_mod(l)
        sm = {k: self.inp(f"o{j}_{k}", s) for k, s in ODD_SMALL_SHAPES.items()}
        self.phase_r1()
        self.phase_r2a(l, j, sm)
        self.phase_r2b(j, sm)
        self.phase_r3(j)
        self.phase_r4(l, j, sm)
        self.moe(l)

    def phase_r1(self):
        kb, cfg = self.kb, self.cfg
        if not hasattr(self, "XN"):
            self.XN = self.scratch("XN", [D, cfg.T], BF16)
        with kb.phase() as ph:
            def consume(ti, t0, TT, xT):
                kb.dma(self.XN[:, t0:t0 + TT].rearrange("(c p) t -> p c t", p=128), xT[:, :, 0:TT])
            self.norm_tiles_to_xT(ph, self.H, 1, 0, consume)

    def wload(self, ph, src, shape, name, view=None):
        t = ph.sb(shape, BF16, name)
        self.kb.dma(t[:], view if view is not None else src, eng="pool")
        return t

    def phase_r2a(self, l, j, sm):
        kb, cfg = self.kb, self.cfg
        T = cfg.T
        has_vres = j > 0
        wr = self.inp(f"rk_wr__{j}", [D, D])
        wk = self.inp(f"rk_wk__{j}", [D, D])
        wv = self.inp(f"rk_wv__{j}", [D, D])
        w1 = self.inp(f"rk_w1__{j}", [2, D, 64])
        a1 = self.inp(f"rk_a1__{j}", [2, D, 64])
        g1 = self.inp(f"rk_g1__{j}", [D, 160])
        g2 = self.inp(f"rk_g2__{j}", [160, D])
        if has_vres:
            v1 = self.inp(f"rk_v1__{j - 1}", [D, 32])
            v2 = self.inp(f"rk_v2__{j - 1}", [32, D])
        if not hasattr(self, "RF"):
            self.RF = self.scratch("RF", [D, T], BF16)
            self.KFm = self.scratch("KFm", [D, T], BF16)
            self.KKF = self.scratch("KKF", [D, T], BF16)
            self.TW = self.scratch("TW", [2, 64, T], BF16)
            self.TA = self.scratch("TA", [2, 64, T], BF16)
            self.VTM = self.scratch("VTM", [T, D], BF16)
            self.VF = self.scratch("VF", [T, D], F32)
            self.GATE = self.scratch("GATE", [T, D], BF16)
        last = len(cfg.tiles) - 1
        with kb.phase() as ph:
            r3 = lambda w: w.rearrange("(c p) n -> p c n", p=128)
            Wr = self.wload(ph, None, [128, 8, D], "Wr", r3(wr))
            Wk = self.wload(ph, None, [128, 8, D], "Wk", r3(wk))
            Wv = self.wload(ph, None, [128, 8, D], "Wv", r3(wv))
            W1 = [self.wload(ph, None, [128, 8, 64], "W1", r3(w1[d])) for d in range(2)]
            A1 = [self.wload(ph, None, [128, 8, 64], "A1", r3(a1[d])) for d in range(2)]
            G1 = self.wload(ph, None, [128, 8, 160], "G1", r3(g1))
            G2a = self.wload(ph, None, [128, D], "G2a", g2[0:128, :])
            G2b = self.wload(ph, None, [32, D], "G2b", g2[128:160, :])
            if has_vres:
                V1 = self.wload(ph, None, [128, 8, 32], "V1", r3(v1))
                V2 = self.wload(ph, None, [32, D], "V2", v2)
                v0b = ph.sb([128, D], F32, "v0b")
                kb.dma(v0b[:], sm["v0"].broadcast_to([128, D]))
            mu = ph.sb([128, 6, 8], F32, "mu")
            kb.dma(mu[:], sm["mu"])
            kkc = ph.sb([128, 8], F32, "kkc")
            kb.dma(kkc[:], sm["kkcol"])
            bones = ph.sb([128, 128], F32, "bonesf")
            kb.dma(bones[:], self.cin["m_same"])
            bonesb = ph.sb([128, 128], BF16, "bonesb")
            kb.v("dve", "tensor_copy", bonesb[:], [bones[:]])
            x = ph.sb([128, 8, 514], BF16, "x")
            tmpf = ph.sb([128, 8, 512], F32, "tmpf")
            xx = ph.sb([128, 8, 512], BF16, "xx")
            xms = [ph.sb([128, 8, 512], BF16, "xm") for _ in range(2)]
            pm = [ph.ps() for _ in range(4)]
            pR = ph.ps()
            ob = [ph.sb([128, 512], BF16, "ob") for _ in range(3)]
            of = [ph.sb([128, 512], F32, "of") for _ in range(2)]
            sq = ph.sb([128, 512], BF16, "sq")
            rs = ph.sb([128, 512], F32, "rs")
            sg = ph.sb([128, 2, 512], BF16, "sg")
            lvT = ph.sb([32, 512], BF16, "lvT")
            vf = [ph.sb([128, 512], F32, "vf") for _ in range(2)]
            cnt = [0]

            def mix(i, TT):
                xm = xms[cnt[0] % 2]
                cnt[0] += 1
                for c in range(8):
                    kb.v("dve", "scalar_tensor_tensor", xm[:, c, 0:TT], [xx[:, c, 0:TT], mu[:, i, c:c + 1], x[:, c, 1:TT + 1]], ALU.mult, ALU.add)
                return xm

            def proj_fm(xm, Wt, ncol0, rows, TT, pmi):
                p = pm[pmi % 4]
                for c in range(8):
                    kb.mm(p[0:rows, 0:TT], Wt[:, c, ncol0:ncol0 + rows], xm[:, c, 0:TT], start=(c == 0), stop=(c == 7))
                return p

            u = 0
            for ti, (t0, TT) in enumerate(cfg.tiles):
                lo = 0 if ti in (0, 1) else 1
                hi = 0 if ti in (0, last) else 1
                kb.dma(x[:, :, 1 - lo:TT + 1 + hi], self.XN[:, t0 - lo:t0 + TT + hi].rearrange("(c p) t -> p c t", p=128))
                if not lo:
                    kb.v("pool", "memset", x[:, :, 0:1], [], 0.0)
                if not hi:
                    kb.v("pool", "memset", x[:, :, TT + 1:TT + 2], [], 0.0)
                kb.v("dve", "tensor_tensor", tmpf[:, :, 0:TT], [x[:, :, 0:TT], x[:, :, 2:TT + 2]], ALU.add)
                kb.v("dve", "scalar_tensor_tensor", xx[:, :, 0:TT], [tmpf[:, :, 0:TT], 0.5, x[:, :, 1:TT + 1]], ALU.mult, ALU.subtract)
                xm = mix(0, TT)
                for m in range(8):
                    p = proj_fm(xm, Wr, m * 128, 128, TT, u)
                    o = ob[u % 3]
                    u += 1
                    self.evac(o[:, 0:TT], p[:, 0:TT])
                    kb.dma(self.RF[m * 128:(m + 1) * 128, t0:t0 + TT], o[:, 0:TT])
                xm = mix(2, TT)
                for m in range(8):
                    p = proj_fm(xm, Wk, m * 128, 128, TT, u)
                    o = ob[u % 3]
                    f = of[u % 2]
                    u += 1
                    kb.act(o[:, 0:TT], p[:, 0:TT], AF.Copy)
                    kb.dma(self.KFm[m * 128:(m + 1) * 128, t0:t0 + TT], o[:, 0:TT])
                    kb.v("dve", "tensor_scalar", f[:, 0:TT], [p[:, 0:TT]], kkc[:, m:m + 1], None, ALU.mult)
                    kb.act(sq[:, 0:TT], f[:, 0:TT], AF.Square)
                    kb.mm(pR[:, 0:TT], bonesb[:], sq[:, 0:TT])
                    rstd_inplace(kb, rs[:, 0:TT], 1.0, EPS, src=pR[:, 0:TT])
                    o2 = ob[u % 3]
                    u += 1
                    kb.v("dve", "tensor_tensor", o2[:, 0:TT], [f[:, 0:TT], rs[:, 0:TT]], ALU.mult)
                    kb.dma(self.KKF[m * 128:(m + 1) * 128, t0:t0 + TT], o2[:, 0:TT])
                xm = mix(3, TT)
                if has_vres:
                    p = proj_fm(xm, V1, 0, 32, TT, u)
                    u += 1
                    kb.act(lvT[:, 0:TT], p[0:32, 0:TT], AF.Copy)
                for s in range(TT // 128):
                    r0 = t0 + s * 128
                    for n in range(2):
                        p = pm[u % 4]
                        o = ob[u % 3]
                        f = of[u % 2]
                        u += 1
                        for c in range(8):
                            kb.mm(p[:], xm[:, c, s * 128:(s + 1) * 128], Wv[:, c, n * 512:(n + 1) * 512], start=(c == 0), stop=(c == 7))
                        if not has_vres:
                            kb.act(f[:], p[:], AF.Copy)
                            kb.dma(self.VF[r0:r0 + 128, n * 512:(n + 1) * 512], f[:])
                            kb.v("dve", "tensor_copy", o[:], [p[:]])
                        else:
                            p2 = pm[u % 4]
                            u += 1
                            vft = vf[n]
                            kb.dma(vft[:], self.VF[r0:r0 + 128, n * 512:(n + 1) * 512])
                            kb.mm(p2[:], lvT[:, s * 128:(s + 1) * 128], V2[:, n * 512:(n + 1) * 512])
                            kb.v("dve", "tensor_tensor", f[:], [p2[:], v0b[:, n * 512:(n + 1) * 512]], ALU.add)
                            kb.act(f[:], f[:], AF.Sigmoid)
                            kb.v("dve", "tensor_tensor", vft[:], [vft[:], p[:]], ALU.subtract)
                            kb.v("pool", "tensor_tensor", vft[:], [vft[:], f[:]], ALU.mult)
                            kb.v("dve", "tensor_tensor", o[:], [vft[:], p[:]], ALU.add)
                        kb.dma(self.VTM[r0:r0 + 128, n * 512:(n + 1) * 512], o[:])
                xm = mix(5, TT)
                p = proj_fm(xm, G1, 0, 128, TT, u)
                u += 1
                kb.act(sg[:, 0, 0:TT], p[:, 0:TT], AF.Sigmoid)
                p = proj_fm(xm, G1, 128, 32, TT, u)
                u += 1
                kb.act(sg[0:32, 1, 0:TT], p[0:32, 0:TT], AF.Sigmoid)
                for s in range(TT // 128):
                    r0 = t0 + s * 128
                    for n in range(2):
                        p = pm[u % 4]
                        o = ob[u % 3]
                        u += 1
                        kb.mm(p[:], sg[:, 0, s * 128:(s + 1) * 128], G2a[:, n * 512:(n + 1) * 512], start=True, stop=False)
                        kb.mm(p[:], sg[0:32, 1, s * 128:(s + 1) * 128], G2b[:, n * 512:(n + 1) * 512], start=False, stop=True)
                        self.evac(o[:], p[:])
                        kb.dma(self.GATE[r0:r0 + 128, n * 512:(n + 1) * 512], o[:])
                xm = mix(1, TT)
                for d in range(2):
                    p = proj_fm(xm, W1[d], 0, 64, TT, u)
                    o = ob[u % 3]
                    u += 1
                    kb.act(o[0:64, 0:TT], p[0:64, 0:TT], AF.Tanh)
                    kb.dma(self.TW[d, :, t0:t0 + TT], o[0:64, 0:TT])
                xm = mix(4, TT)
                for d in range(2):
                    p = proj_fm(xm, A1[d], 0, 64, TT, u)
                    o = ob[u % 3]
                    u += 1
                    kb.act(o[0:64, 0:TT], p[0:64, 0:TT], AF.Copy)
                    kb.dma(self.TA[d, :, t0:t0 + TT], o[0:64, 0:TT])

    def phase_r2b(self, j, sm):
        kb, cfg = self.kb, self.cfg
        T = cfg.T
        nch = T // 64
        w2 = self.inp(f"rk_w2__{j}", [2, 64, D])
        a2 = self.inp(f"rk_a2__{j}", [2, 64, D])
        if not hasattr(self, "RT"):
            for nm in ("RT", "AT", "BT", "KT"):
                setattr(self, nm, [self.scratch(f"{nm}{d}", [D, T], BF16) for d in range(2)])
            for nm in ("ATM", "KHAT", "BHAT"):
                setattr(self, nm, [self.scratch(f"{nm}{d}", [T, D], BF16) for d in range(2)])
            self.GCd = [self.scratch(f"GC{d}", [D, nch], F32) for d in range(2)]
            self.SBN = self.scratch("SBN", [T, 16], F32)
        with kb.phase() as ph:
            W2 = [self.wload(ph, None, [64, D], "W2", w2[d]) for d in range(2)]
            A2 = [self.wload(ph, None, [64, D], "A2", a2[d]) for d in range(2)]
            w0 = ph.sb([128, 2, 8], F32, "w0")
            a0 = ph.sb([128, 2, 8], F32, "a0")
            kac = ph.sb([128, 8], F32, "kac")
            omka = ph.sb([128, 8], F32, "omka")
            rkc = ph.sb([128, 8], F32, "rkc")
            kb.dma(w0[:], sm["w0"])
            kb.dma(a0[:], sm["a0"])
            kb.dma(kac[:], sm["kacol"])
            kb.dma(rkc[:], sm["rkcol"])
            kb.v("dve", "tensor_scalar", omka[:], [kac[:]], -1.0, 1.0, ALU.mult, ALU.add)
            rmask = ph.sb([128, 512], F32, "rmask")
            kb.dma(rmask[:], self.cin["rmask"])
            hsel = ph.sb([128, 2], F32, "hself")
            kb.dma(hsel[:], self.cin["hsel"])
            hselb = ph.sb([128, 2], BF16, "hselb")
            kb.v("dve", "tensor_copy", hselb[:], [hsel[:]])
            tw = [ph.sb([64, 512], BF16, "tw") for _ in range(2)]
            ta = [ph.sb([64, 512], BF16, "ta") for _ in range(2)]
            rt = [ph.sb([128, 512], BF16, "rt") for _ in range(2)]
            kt = [ph.sb([128, 512], BF16, "kt") for _ in range(2)]
            kkt = [ph.sb([128, 512], BF16, "kkt") for _ in range(2)]
            F = lambda nm: ph.sb([128, 512], F32, nm)
            lw, av, keys, bv, cs, tot, e1, e2, ex, tmp, prod = F("lw"), F("av"), F("keys"), F("bv"), F("cs"), F("tot"), F("e1"), F("e2"), F("ex"), F("tmp"), F("prod")
            obs = [ph.sb([128, 512], BF16, "ob") for _ in range(4)]
            tms = [ph.sb([128, 3, 512], BF16, "tms") for _ in range(2)]
            tsb = [ph.sb([128, 4, 128], BF16, "tsb") for _ in range(3)]
            gcs = ph.sb([128, 8], F32, "gcs")
            prodb = ph.sb([128, 512], BF16, "prodb")
            sbn = ph.sb([128, 4, 16], F32, "sbn")
            pz = [ph.ps() for _ in range(2)]
            pa = [ph.ps() for _ in range(2)]
            ptr = [ph.ps([128, 1024], BF16, "ptr") for _ in range(3)]
            psb = ph.ps()
            u = 0
            tq = 0
            for ti, (t0, TT) in enumerate(cfg.tiles):
                ns = TT // 128
                nc_ = TT // 64
                for d in range(2):
                    kb.dma(tw[d][:, 0:TT], self.TW[d, :, t0:t0 + TT])
                    kb.dma(ta[d][:, 0:TT], self.TA[d, :, t0:t0 + TT])
                for c in range(8):
                    i = c % 2
                    kb.dma(rt[i][:, 0:TT], self.RF[c * 128:(c + 1) * 128, t0:t0 + TT])
                    kb.dma(kt[i][:, 0:TT], self.KFm[c * 128:(c + 1) * 128, t0:t0 + TT])
                    kb.dma(kkt[i][:, 0:TT], self.KKF[c * 128:(c + 1) * 128, t0:t0 + TT])
                    for d in range(2):
                        z, a_ = pz[u % 2], pa[u % 2]
                        tm3 = tms[u % 2]
                        u += 1
                        kb.mm(z[:, 0:TT], W2[d][:, c * 128:(c + 1) * 128], tw[d][:, 0:TT])
                        kb.mm(a_[:, 0:TT], A2[d][:, c * 128:(c + 1) * 128], ta[d][:, 0:TT])
                        kb.act(lw[:, 0:TT], z[:, 0:TT], AF.Sigmoid, bias=w0[:, d, c:c + 1])
                        kb.v("dve", "tensor_scalar", lw[:, 0:TT], [lw[:, 0:TT]], -0.6065306597126334, None, ALU.mult)
                        kb.act(av[:, 0:TT], a_[:, 0:TT], AF.Sigmoid, bias=a0[:, d, c:c + 1])
                        kb.v("dve", "tensor_scalar", tmp[:, 0:TT], [av[:, 0:TT]], kac[:, c:c + 1], omka[:, c:c + 1], ALU.mult, ALU.add)
                        kb.v("dve", "tensor_tensor", keys[:, 0:TT], [tmp[:, 0:TT], kt[i][:, 0:TT]], ALU.mult)
                        kb.v("pool", "tensor_tensor", bv[:, 0:TT], [av[:, 0:TT], kkt[i][:, 0:TT]], ALU.mult)
                        kb.v("dve", "tensor_tensor_scan", cs[:, 0:TT], [rmask[:, 0:TT], lw[:, 0:TT]], 0.0, ALU.mult, ALU.add)
                        cs3 = cs[:, 0:TT].rearrange("p (n k) -> p n k", k=64)
                        kb.v("pool", "tensor_copy", tot[:, 0:TT].rearrange("p (n k) -> p n k", k=64), [cs3[:, :, 63:64].to_broadcast([128, nc_, 64])])
                        if d == 0:
                            E1 = cs
                        else:
                            kb.v("dve", "tensor_tensor", e1[:, 0:TT], [tot[:, 0:TT], cs[:, 0:TT]], ALU.subtract)
                            kb.v("dve", "tensor_tensor", e1[:, 0:TT], [e1[:, 0:TT], lw[:, 0:TT]], ALU.add)
                            E1 = e1
                        kb.v("pool", "tensor_tensor", e2[:, 0:TT], [E1[:, 0:TT], lw[:, 0:TT]], ALU.subtract)
                        o = obs[tq % 4]; tq += 1
                        kb.act(ex[:, 0:TT], E1[:, 0:TT], AF.Exp)
                        kb.v("dve", "tensor_tensor", o[:, 0:TT], [ex[:, 0:TT], rt[i][:, 0:TT]], ALU.mult)
                        kb.dma(self.RT[d][c * 128:(c + 1) * 128, t0:t0 + TT], o[:, 0:TT])
                        o = obs[tq % 4]; tq += 1
                        kb.act(ex[:, 0:TT], e2[:, 0:TT], AF.Exp)
                        kb.v("dve", "scalar_tensor_tensor", o[:, 0:TT], [ex[:, 0:TT], -1.0, kkt[i][:, 0:TT]], ALU.mult, ALU.mult)
                        kb.dma(self.AT[d][c * 128:(c + 1) * 128, t0:t0 + TT], o[:, 0:TT])
                        kb.v("pool", "tensor_copy", tm3[:, 0, 0:TT], [o[:, 0:TT]])
                        kb.act(ex[:, 0:TT], E1[:, 0:TT], AF.Exp, scale=-1.0)
                        o = obs[tq % 4]; tq += 1
                        kb.v("dve", "tensor_tensor", o[:, 0:TT], [ex[:, 0:TT], bv[:, 0:TT]], ALU.mult)
                        kb.dma(self.BT[d][c * 128:(c + 1) * 128, t0:t0 + TT], o[:, 0:TT])
                        o = obs[tq % 4]; tq += 1
                        kb.v("dve", "tensor_tensor", o[:, 0:TT], [ex[:, 0:TT], keys[:, 0:TT]], ALU.mult)
                        kb.dma(self.KT[d][c * 128:(c + 1) * 128, t0:t0 + TT], o[:, 0:TT])
                        kb.v("pool", "tensor_tensor", tmp[:, 0:TT], [tot[:, 0:TT], E1[:, 0:TT]], ALU.subtract)
                        kb.act(ex[:, 0:TT], tmp[:, 0:TT], AF.Exp)
                        kb.v("dve", "tensor_tensor", tm3[:, 1, 0:TT], [ex[:, 0:TT], keys[:, 0:TT]], ALU.mult)
                        kb.v("dve", "tensor_tensor", tm3[:, 2, 0:TT], [ex[:, 0:TT], bv[:, 0:TT]], ALU.mult)
                        kb.act(gcs[:, 0:nc_], cs3[:, :, 63], AF.Exp)
                        kb.dma(self.GCd[d][c * 128:(c + 1) * 128, t0 // 64:t0 // 64 + nc_], gcs[:, 0:nc_])
                        for k3, dst in enumerate((self.ATM[d], self.KHAT[d], self.BHAT[d])):
                            pt, tb = ptr[k3], tsb[k3]
                            for s in range(ns):
                                kb.tr(pt[:, s * 128:(s + 1) * 128], tm3[:, k3, s * 128:(s + 1) * 128], self.ident[:])
                            self.evac(tb[:, 0:ns, :], pt[:, 0:TT].rearrange("p (s c) -> p s c", c=128))
                            kb.dma(dst[t0:t0 + TT, c * 128:(c + 1) * 128].rearrange("(s p) c -> p s c", p=128), tb[:, 0:ns, :])
                        if d == 0:
                            kb.v("dve", "scalar_tensor_tensor", prod[:, 0:TT], [keys[:, 0:TT], rkc[:, c:c + 1], rt[i][:, 0:TT]], ALU.mult, ALU.mult)
                        else:
                            kb.v("dve", "scalar_tensor_tensor", tmp[:, 0:TT], [keys[:, 0:TT], rkc[:, c:c + 1], rt[i][:, 0:TT]], ALU.mult, ALU.mult)
                            kb.v("dve", "scalar_tensor_tensor", prodb[:, 0:TT], [tmp[:, 0:TT], 1.0, prod[:, 0:TT]], ALU.mult, ALU.add)
                    for s in range(ns):
                        kb.mm(psb[:, (s * 8 + c) * 2:(s * 8 + c) * 2 + 2], prodb[:, s * 128:(s + 1) * 128], hselb[:])
                kb.v("dve", "tensor_scalar", sbn[:, 0:ns, :], [psb[:, 0:ns * 16].rearrange("p (s h) -> p s h", h=16)], 0.5, None, ALU.mult)
                kb.dma(self.SBN[t0:t0 + TT, :].rearrange("(s p) h -> p s h", p=128), sbn[:, 0:ns, :])
    def phase_r3(self, j):
        kb, cfg = self.kb, self.cfg
        T, nsub = cfg.T, cfg.nsub
        W = 16
        if not hasattr(self, "YD"):
            self.YD = [self.scratch(f"YD{d}", [T, D]) for d in range(2)]
        fwd = list(range(nsub))
        bwd = [1, 0] + list(range(nsub - 1, 1, -1))
        with kb.phase() as ph:
            mle = self.load_const_tile(ph, "m_le")
            mge = self.load_const_tile(ph, "m_ge")
            mlt = self.load_const_tile(ph, "m_lt")
            mgt = self.load_const_tile(ph, "m_gt")
            Hs = [[ph.sb([128, 64], F32, f"Hs{d}_{h}") for h in range(16)] for d in range(2)]
            Hb = [[ph.sb([128, 64], BF16, f"Hb{d}_{h}") for h in range(16)] for d in range(2)]
            for d in range(2):
                for h in range(16):
                    kb.v("pool", "memset", Hs[d][h][:], [], 0.0)
                    kb.v("pool", "memset", Hb[d][h][:], [], 0.0)
            banks = [ph.ps() for _ in range(8)]
            slot = [0]

            def ps128():
                i = slot[0] % 8
                slot[0] += 1
                return banks[i]

            UB = []
            for i in range(4):
                b = {}
                for nm in ("RT", "AT", "BT", "KT"):
                    b[nm] = ph.sb([128, 8, 128], BF16, nm)
                for nm in ("ATM", "KHAT", "BHAT", "V"):
                    b[nm] = ph.sb([128, D], BF16, nm)
                b["GC"] = ph.sb([128, 8, 2], F32, "GC")
                b["ysb"] = ph.sb([128, D], F32, "ysb")
                UB.append(b)
            HB = []
            for i in range(W):
                b = {}
                for nm in ("X0", "X1", "XT0", "XT1", "AT0", "AT1", "AakT", "ArkT", "ArbT", "PT"):
                    b[nm] = ph.sb([128, 128], BF16, nm)
                b["Z"] = ph.sb([128, 64], BF16, "Z")
                b["U"] = ph.sb([128, 64], BF16, "U")
                b["U0"] = ph.sb([128, 64], F32, "U0")
                HB.append(b)

            def unit(d, h, B, Hh):
                MsT = mlt if d == 0 else mgt
                Ms = mgt if d == 0 else mlt
                MiT = mle if d == 0 else mge
                c = h // 2
                rs = slice(64 * (h % 2), 64 * (h % 2) + 64)
                hs = slice(h * 64, (h + 1) * 64)
                RT, AT_, BT, KT = B["RT"][rs, c, :], B["AT"][rs, c, :], B["BT"][rs, c, :], B["KT"][rs, c, :]
                for (l_, r_, dst, msk) in ((BT, AT_, "XT0", MsT), (AT_, BT, "X0", Ms), (KT, AT_, "AakT", MsT), (KT, RT, "ArkT", MiT), (BT, RT, "ArbT", MiT)):
                    p = ps128()
                    kb.mm(p[:, 0:128], l_, r_)
                    kb.v("dve", "tensor_tensor", Hh[dst][:], [p[:, 0:128], msk[:]], ALU.mult)
                    yield
                kb.v("pool", "tensor_tensor", Hh["AT0"][:], [Hh["XT0"][:], self.ident[:]], ALU.add)
                X, XT, AT = Hh["X0"], Hh["XT0"], Hh["AT0"]
                for js in range(1, 6):
                    Xn = Hh["X1"] if X is Hh["X0"] else Hh["X0"]
                    XTn = Hh["XT1"] if XT is Hh["XT0"] else Hh["XT0"]
                    ATn = Hh["AT1"] if AT is Hh["AT0"] else Hh["AT0"]
                    px = ps128()
                    kb.mm(px[:, 0:128], XT[:], X[:])
                    if js < 5:
                        pxT = ps128()
                        kb.mm(pxT[:, 0:128], X[:], XT[:])
                    kb.act(Xn[:], px[:, 0:128], AF.Copy)
                    if js < 5:
                        kb.v("dve", "tensor_copy", XTn[:], [pxT[:, 0:128]])
                    yield
                    pa = ps128()
                    kb.mm(pa[:, 0:128], Xn[:], AT[:])
                    kb.v("dve", "tensor_tensor", ATn[:], [pa[:, 0:128], AT[:]], ALU.add)
                    X, XT, AT = Xn, XTn, ATn
                    yield
                p = ps128()
                kb.mm(p[:, 0:64], Hh["AakT"][:], B["V"][:, hs])
                kb.act(Hh["Z"][:], p[:, 0:64], AF.Copy)
                p = ps128()
                kb.mm(p[rs, 0:128], B["ATM"][:, hs], AT[:])
                kb.act(Hh["PT"][rs, :], p[rs, 0:128], AF.Copy)
                yield
                p = ps128()
                kb.mm(p[:, 0:64], AT[:], Hh["Z"][:])
                kb.act(Hh["U0"][:], p[:, 0:64], AF.Copy)
                yield
                for half in ((0, 1) if d == 0 else (1, 0)):
                    q = slice(half * 64, half * 64 + 64)
                    yq = V(B["ysb"][q, hs], (B["ysb"].name, h))
                    p1 = ps128()
                    kb.mm(p1[q, 0:64], Hh["PT"][rs, q], Hb[d][h][rs, :])
                    kb.v("dve", "tensor_tensor", Hh["U"][q, :], [p1[q, 0:64], Hh["U0"][q, :]], ALU.add)
                    p2a = ps128()
                    kb.mm(p2a[q, 0:64], B["RT"][rs, c, q], Hb[d][h][rs, :])
                    kb.act(yq, p2a[q, 0:64], AF.Copy)
                    yield
                    p2 = ps128()
                    kb.mm(p2[q, 0:64], Hh["ArkT"][q, q], B["V"][q, hs], start=True, stop=False)
                    kb.mm(p2[q, 0:64], Hh["ArbT"][q, q], Hh["U"][q, :], start=False, stop=True)
                    kb.v("dve", "tensor_tensor", yq, [p2[q, 0:64], yq], ALU.add)
                    p3 = ps128()
                    kb.mm(p3[rs, 0:64], B["KHAT"][q, hs], B["V"][q, hs], start=True, stop=False)
                    kb.mm(p3[rs, 0:64], B["BHAT"][q, hs], Hh["U"][q, :], start=False, stop=True)
                    kb.v("dve", "scalar_tensor_tensor", Hs[d][h][rs, :], [Hs[d][h][rs, :], B["GC"][rs, c, half:half + 1], p3[rs, 0:64]], ALU.mult, ALU.add)
                    kb.act(Hb[d][h][rs, :], Hs[d][h][rs, :], AF.Copy)
                    yield

            for step in range(nsub):
                makers = []
                Bs = []
                for d in range(2):
                    sb_ = fwd[step] if d == 0 else bwd[step]
                    r0 = sb_ * 128
                    B = UB[(step % 2) * 2 + d]
                    Bs.append((B, r0, d))
                    for nm, src in (("RT", self.RT), ("AT", self.AT), ("BT", self.BT), ("KT", self.KT)):
                        kb.dma(B[nm][:], src[d][:, r0:r0 + 128].rearrange("(c p) t -> p c t", p=128))
                    for nm, src in (("ATM", self.ATM[d]), ("KHAT", self.KHAT[d]), ("BHAT", self.BHAT[d]), ("V", self.VTM)):
                        kb.dma(B[nm][:], src[r0:r0 + 128, :])
                    kb.dma(B["GC"][:], self.GCd[d][:, sb_ * 2:sb_ * 2 + 2].rearrange("(c p) n -> p c n", p=128))
                for h in range(16):
                    for (B, r0, d) in Bs:
                        makers.append(lambda slot_, d=d, h=h, B=B: unit(d, h, B, HB[slot_]))
                interleave(makers, W)
                for (B, r0, d) in Bs:
                    kb.dma(self.YD[d][r0:r0 + 128, :], B["ysb"][:], extra_reads=[(B["ysb"].name, h) for h in range(16)])

    def phase_r4(self, l, j, sm):
        kb, cfg = self.kb, self.cfg
        Wout = self.inp(f"rk_wo__{j}", [D, D])
        self.Fm = self.scratch(f"F{l}", [cfg.T, D], BF16)
        with kb.phase() as ph:
            Wo = ph.sb([128, 8, D], BF16, "Wo")
            kb.dma(Wo[:], Wout.rearrange("(c p) n -> p c n", p=128), eng="pool")
            lnw = ph.sb([128, D], F32, "lnw")
            lnb = ph.sb([128, D], F32, "lnb")
            kb.dma(lnw[:], sm["lnw"].broadcast_to([128, D]))
            kb.dma(lnb[:], sm["lnb"].broadcast_to([128, D]))
            y0 = [ph.sb([128, D], F32, "y0") for _ in range(2)]
            y1 = [ph.sb([128, D], F32, "y1") for _ in range(2)]
            vt = [ph.sb([128, D], BF16, "vt") for _ in range(2)]
            gt = [ph.sb([128, D], BF16, "gt") for _ in range(2)]
            sbn = [ph.sb([128, 16], F32, "sbn") for _ in range(2)]
            st = [ph.sb([128, 16], F32, "st") for _ in range(2)]
            sqt = ph.sb([128, D], F32, "sqt")
            bon = ph.sb([128, D], F32, "bon")
            zt = [ph.sb([128, D], BF16, "zt") for _ in range(2)]
            XT = [ph.sb([128, 8, 128], BF16, "XT") for _ in range(2)]
            ptr = [ph.ps([128, 1024], BF16, "ptr") for _ in range(2)]
            py = [ph.ps() for _ in range(4)]
            hts = [ph.sb([128, D], F32, "ht") for _ in range(2)]
            tmp = [ph.sb([128, D], F32, "tmp") for _ in range(2)]
            junk = ph.sb([128, D], F32, "junk")
            sss = [ph.sb([128, 1], F32, "ss") for _ in range(2)]
            tmp2 = [ph.sb([128, D], F32, "tmp2") for _ in range(2)]
            fb = [ph.sb([128, D], BF16, "fb") for _ in range(2)]
            v3 = lambda t: t[:].rearrange("p (h n) -> p h n", n=64)
            b3 = lambda t: t[:].unsqueeze(2).to_broadcast([128, 16, 64])
            for s in range(cfg.nsub):
                mod = self.modC if s < 2 else self.modL
                r0 = s * 128
                i = s % 2
                kb.dma(y0[i][:], self.YD[0][r0:r0 + 128, :])
                kb.dma(y1[i][:], self.YD[1][r0:r0 + 128, :])
                kb.dma(vt[i][:], self.VTM[r0:r0 + 128, :])
                kb.dma(gt[i][:], self.GATE[r0:r0 + 128, :])
                kb.dma(sbn[i][:], self.SBN[r0:r0 + 128, :])
                kb.dma(hts[i][:], self.H[r0:r0 + 128, :])
                kb.v("pool", "tensor_tensor", y0[i][:], [y0[i][:], y1[i][:]], ALU.add)
                kb.v("dve", "tensor_reduce", st[i][:], [v3(y0[i])], AX.X, ALU.add)
                kb.v("dve", "tensor_scalar", st[i][:], [st[i][:]], 1.0 / 64, None, ALU.mult)
                kb.v("dve", "tensor_tensor", v3(y0[i]), [v3(y0[i]), b3(st[i])], ALU.subtract)
                kb.v("pool", "tensor_tensor", sqt[:], [y0[i][:], y0[i][:]], ALU.mult)
                kb.v("dve", "tensor_reduce", st[i][:], [v3(sqt)], AX.X, ALU.add)
                rstd_inplace(kb, st[i][:], 1.0 / 64, 64e-5)
                kb.v("dve", "tensor_tensor", v3(y0[i]), [v3(y0[i]), b3(st[i])], ALU.mult)
                kb.v("pool", "tensor_tensor", y0[i][:], [y0[i][:], lnw[:]], ALU.mult)
                kb.v("pool", "tensor_tensor", y0[i][:], [y0[i][:], lnb[:]], ALU.add)
                kb.v("dve", "tensor_tensor", v3(bon), [v3(vt[i]), b3(sbn[i])], ALU.mult)
                kb.v("dve", "tensor_tensor", y0[i][:], [y0[i][:], bon[:]], ALU.add)
                kb.v("pool", "tensor_tensor", zt[i][:], [y0[i][:], gt[i][:]], ALU.mult)
                for c in range(8):
                    kb.tr(ptr[i][:, c * 128:(c + 1) * 128], zt[i][:, c * 128:(c + 1) * 128], self.ident[:])
                self.evac(XT[i][:], ptr[i][:].rearrange("p (c t) -> p c t", c=8))
                for n in range(2):
                    pp = py[(2 * s + n) % 4]
                    for c in range(8):
                        kb.mm(pp[:], XT[i][:, c, :], Wo[:, c, n * 512:(n + 1) * 512], start=(c == 0), stop=(c == 7))
                    kb.v("dve", "tensor_tensor", tmp[i][:, n * 512:(n + 1) * 512], [pp[:], mod[2][:, n * 512:(n + 1) * 512]], ALU.mult)
                kb.v("pool", "tensor_tensor", hts[i][:], [tmp[i][:], hts[i][:]], ALU.add)
                kb.dma(self.H[r0:r0 + 128, :], hts[i][:])
                self.norm_sub(hts[i][:], junk[:], sss[i][:], tmp2[i][:], fb[i][:], mod[4][:], mod[3][:])
                kb.dma(self.Fm[r0:r0 + 128, :], fb[i][:])
import re


def resolve_input(name, cfg, inp, b, consts, cache):
    if name == "h0":
        return np.ascontiguousarray(np.concatenate([inp["ctx"][b], inp["x"][b][:cfg.T - CTX]], 0))
    if name == "cvec":
        return np.ascontiguousarray(np.concatenate([inp["c"][b].reshape(8, 128).T, inp["c_ctx"].reshape(8, 128).T], 1))
    if name.startswith("c_"):
        return consts[name[2:]]
    key = ("shared", name)
    if key in cache:
        return cache[key]
    m = re.match(r"^e(\d+)_(\w+)$", name)
    if m:
        j = int(m.group(1))
        if ("es", j) not in cache:
            cache[("es", j)] = host_even_smalls(inp, j)
        arr = cache[("es", j)][m.group(2)]
    else:
        m = re.match(r"^o(\d+)_(\w+)$", name)
        if m:
            j = int(m.group(1))
            if ("os", j) not in cache:
                cache[("os", j)] = host_odd_smalls(inp, j)
            arr = cache[("os", j)][m.group(2)]
        else:
            m = re.match(r"^(.+)__(\d+)$", name)
            base, idx = m.group(1), int(m.group(2))
            assert base in ALL_INPUTS, base
            a = inp[base][idx]
            if a.ndim == 1:
                a = a.reshape(1, -1)
            elif base in ("moe_w1", "moe_w3", "moe_w2"):
                a = a.reshape(-1, a.shape[-1])
            arr = np.ascontiguousarray(a)
    cache[key] = arr
    return arr


ALL_INPUTS = ('x', 'c', 'ctx', 'c_ctx', 'ada_w', 'ada_b', 'norm_mix', 'norm_ffn', 'hy_w_in', 'hy_w_out', 'mla_qa_norm', 'mla_w_qb',
              'mla_kva_norm', 'mla_w_kvb', 'mla_q_norm', 'mla_k_norm', 'gdn_conv', 'gdn_a_log', 'gdn_dt_bias', 'gdn_out_norm',
              'rk_mu', 'rk_wr', 'rk_wk', 'rk_wv', 'rk_wo', 'rk_w0', 'rk_w1', 'rk_w2', 'rk_a0', 'rk_a1', 'rk_a2', 'rk_g1', 'rk_g2',
              'rk_kk', 'rk_ka', 'rk_rk', 'rk_ln_w', 'rk_ln_b', 'rk_v0', 'rk_v1', 'rk_v2', 'moe_w_group', 'moe_b_group',
              'moe_w_expert', 'moe_b_expert', 'moe_w1', 'moe_w3', 'moe_w2')

_PROG_CACHE = {}


def get_prog(cfg_key):
    if cfg_key not in _PROG_CACHE:
        nlt, layers, debug = cfg_key
        cfg = Cfg(nlt=nlt, layers=layers, debug=debug)
        p = Prog(cfg)
        p.setup()
        for l in cfg.layers:
            if l % 2 == 0:
                p.even_layer(l)
            else:
                p.odd_layer(l)
        p.finish()
        _PROG_CACHE[cfg_key] = p
    return _PROG_CACHE[cfg_key]


def run_prog(p, inp, batches):
    cfg = p.cfg
    consts = host_consts(cfg)
    cache = {}
    in_maps = []
    for b in batches:
        m = {}
        for name, (shape, dt) in p.in_shapes.items():
            a = resolve_input(name, cfg, inp, b, consts, cache)
            assert tuple(a.shape) == tuple(shape), (name, a.shape, shape)
            m[name] = a
        in_maps.append(m)
    res = run_bass_kernel_spmd(p.nc, in_maps, core_ids=list(range(len(batches))))
    return res


def kernel(**inputs):
    inp = {k: np.asarray(v) for k, v in inputs.items()}
    p = get_prog((16, (0, 1, 2, 3), False))
    res = run_prog(p, inp, list(range(8)))
    return np.stack([np.asarray(r["out"], dtype=np.float32) for r in res.results], 0)
```

```python
import contextlib
import numpy as np
import concourse.bass as bass
import concourse.mybir as mybir
from concourse.bass_utils import run_bass_kernel_spmd

F32 = mybir.dt.float32
BF16 = mybir.dt.bfloat16
I32 = mybir.dt.int32
AF = mybir.ActivationFunctionType
ALU = mybir.AluOpType
AX = mybir.AxisListType

ENGS = ("pe", "act", "dve", "pool", "sp")
SAME_ENGINE_SYNC = True
DMA_WINDOW = 8
SEM_MAXV = 30000


class Op:
    __slots__ = ("eng", "fn", "reads", "writes", "is_dma", "deps", "signals", "event", "pre_wait", "barrier")

    def __init__(self, eng, fn, reads, writes, is_dma, barrier=False):
        self.eng = eng
        self.fn = fn
        self.reads = reads
        self.writes = writes
        self.is_dma = is_dma
        self.deps = set()
        self.signals = False
        self.event = None
        self.pre_wait = None
        self.barrier = barrier


class V:
    __slots__ = ("ap", "key")

    def __init__(self, ap, key):
        self.ap = ap
        self.key = key


def _a(x):
    return x.ap if isinstance(x, V) else x


def _key(x):
    if isinstance(x, V):
        return x.key
    if isinstance(x, (str, tuple)):
        return x
    if hasattr(x, "tensor"):
        return x.tensor.name
    return x.name


class Phase:
    def __init__(self, kb):
        self.kb = kb
        self.es = contextlib.ExitStack()

    def sb(self, shape, dtype=F32, name="t"):
        self.kb._n += 1
        return self.es.enter_context(self.kb.nc.sbuf_tensor(f"{name}_{self.kb._n}", list(shape), dtype))

    def ps(self, shape=(128, 512), dtype=F32, name="p"):
        self.kb._n += 1
        nm = f"{name}_{self.kb._n}"
        self.kb.psum_names.add(nm)
        return self.es.enter_context(self.kb.nc.psum_tensor(nm, list(shape), dtype))

    def __enter__(self):
        return self

    def __exit__(self, *a):
        self.kb.barrier()
        self.es.close()
        return False


def interleave(makers, width):
    it = iter(makers)
    active = []
    for slot in range(width):
        m = next(it, None)
        if m is not None:
            active.append((slot, m(slot)))
    while active:
        for entry in list(active):
            slot, g = entry
            try:
                next(g)
            except StopIteration:
                i = active.index(entry)
                m = next(it, None)
                if m is not None:
                    active[i] = (slot, m(slot))
                else:
                    active.pop(i)


class KB:
    def __init__(self, nc):
        self.nc = nc
        self.ops = []
        self._n = 0
        self.psum_names = set()

    def phase(self):
        return Phase(self)

    def sb(self, shape, dtype=F32, name="g"):
        self._n += 1
        return self.nc.alloc_sbuf_tensor(f"{name}_{self._n}", list(shape), dtype)

    def dram(self, shape, dtype=F32, name="dr"):
        self._n += 1
        return self.nc.dram_tensor(f"{name}_{self._n}", list(shape), dtype, kind="Internal")

    def barrier(self):
        self.ops.append(Op(None, None, [], [], False, barrier=True))

    def op(self, eng, fn, reads, writes, is_dma=False):
        o = Op(eng, fn, [_key(r) for r in reads], [_key(w) for w in writes], is_dma)
        self.ops.append(o)
        return o

    def mm(self, out, lhsT, rhs, start=True, stop=True, **kw):
        return self.op("pe", lambda e: e.matmul(_a(out), _a(lhsT), _a(rhs), start=start, stop=stop, **kw), [lhsT, rhs], [out])

    def tr(self, out, in_, ident):
        return self.op("pe", lambda e: e.transpose(_a(out), _a(in_), _a(ident)), [in_, ident], [out])

    def act(self, out, in_, func, bias=None, scale=None, accum_out=None):
        kw = {}
        reads = [in_]
        if bias is not None:
            kw["bias"] = bias
            if not isinstance(bias, (int, float)):
                reads.append(bias)
        if scale is not None:
            kw["scale"] = scale
            if not isinstance(scale, (int, float)):
                reads.append(scale)
        writes = [out]
        if accum_out is not None:
            kw["accum_out"] = accum_out
            writes.append(accum_out)
        kw = {k: _a(x) for k, x in kw.items()}
        return self.op("act", lambda e: e.activation(_a(out), _a(in_), func, **kw), reads, writes)

    def v(self, eng, method, out, ins, *args, **kw):
        isap = lambda a: hasattr(a, "tensor") or isinstance(a, V)
        reads = [a for a in ins if isap(a)]
        reads += [a for a in args if isap(a)]
        reads += [a for a in kw.values() if isap(a)]
        ins2 = [_a(a) for a in ins]
        args2 = [_a(a) for a in args]
        kw2 = {k: _a(x) for k, x in kw.items()}
        return self.op(eng, lambda e: getattr(e, method)(_a(out), *ins2, *args2, **kw2), reads, [out])

    def dma(self, out, in_, eng="sp", rkey=None, wkey=None, extra_reads=(), **kw):
        return self.op(eng, lambda e: e.dma_start(out=_a(out), in_=_a(in_), **kw), [rkey or in_] + list(extra_reads), [wkey or out], is_dma=True)

    def gather(self, out, src, idx, rkey=None):
        return self.op("pool", lambda e: e.indirect_dma_start(out=out, out_offset=None, in_=src,
                       in_offset=bass.IndirectOffsetOnAxis(ap=idx, axis=0)), [rkey or src, idx], [out], is_dma=True)

    def scatter(self, dst, src, idx, wkey=None):
        return self.op("pool", lambda e: e.indirect_dma_start(out=dst, out_offset=bass.IndirectOffsetOnAxis(ap=idx, axis=0),
                       in_=src, in_offset=None), [src, idx], [wkey or dst], is_dma=True)

    def finalize(self):
        nc = self.nc
        allops = self.ops
        ops = [o for o in allops if not o.barrier]
        idx_of = {id(o): i for i, o in enumerate(ops)}
        last_w = {}
        readers = {}
        last_on = {e: None for e in ENGS}
        recent_dma = {e: [] for e in ENGS}
        pending = {e: set() for e in ENGS}
        for o in allops:
            if o.barrier:
                src = set()
                for e in ENGS:
                    if last_on[e] is not None:
                        src.add(last_on[e])
                    src.update(recent_dma[e])
                for e in ENGS:
                    pending[e] |= src
                continue
            i = idx_of[id(o)]
            deps = set()
            for k in o.reads:
                if k in last_w:
                    deps.add(last_w[k])
                if k in self.psum_names:
                    for r in readers.get(k, ()):
                        if ops[r].eng != o.eng:
                            deps.add(r)
            for k in o.writes:
                if k in last_w:
                    deps.add(last_w[k])
                deps.update(readers.get(k, ()))
            deps |= pending[o.eng]
            pending[o.eng] = set()
            deps.discard(i)
            for k in o.reads:
                readers.setdefault(k, []).append(i)
            for k in o.writes:
                last_w[k] = i
                readers[k] = []
            fd = set()
            for d in deps:
                p = ops[d]
                if p.eng == o.eng and not p.is_dma and (o.eng == "pe" or not SAME_ENGINE_SYNC):
                    continue
                fd.add(d)
            best = {}
            keep = set()
            for d in fd:
                pe_ = ops[d]
                if pe_.is_dma or pe_.eng not in ("pe", "act", "dve"):
                    keep.add(d)
                elif pe_.eng not in best or d > best[pe_.eng]:
                    best[pe_.eng] = d
            fd = keep | set(best.values())
            o.deps = fd
            for d in fd:
                ops[d].signals = True
            last_on[o.eng] = i
            if o.is_dma:
                recent_dma[o.eng] = (recent_dma[o.eng] + [i])[-DMA_WINDOW:]
        n_sig = {e: 0 for e in ENGS}
        n_dma = {e: 0 for e in ENGS}
        for o in ops:
            if o.is_dma:
                o.signals = True
                n_dma[o.eng] += 1
            elif o.signals:
                n_sig[o.eng] += 1
        sems = {e: [nc.alloc_semaphore(f"s_{e}_{j}") for j in range(max(1, -(-n_sig[e] // SEM_MAXV)))] for e in ENGS}
        dsems = {e: [nc.alloc_semaphore(f"d_{e}_{j}") for j in range(DMA_WINDOW)] for e in ENGS if n_dma[e]}
        cnt = {e: 0 for e in ENGS}
        dcnt = {e: 0 for e in ENGS}
        for o in ops:
            if o.is_dma:
                n = dcnt[o.eng]
                dcnt[o.eng] += 1
                sem = dsems[o.eng][n % DMA_WINDOW]
                o.event = (sem, 16 * (n // DMA_WINDOW + 1))
                if n >= DMA_WINDOW:
                    o.pre_wait = (sem, 16 * (n // DMA_WINDOW))
            elif o.signals:
                n = cnt[o.eng]
                cnt[o.eng] += 1
                o.event = (sems[o.eng][n // SEM_MAXV], n % SEM_MAXV + 1)
        self.stats = dict(n_ops=len(ops), per_eng={e: sum(1 for o in ops if o.eng == e) for e in ENGS}, n_sig=n_sig, n_dma=n_dma)
        per_eng = {e: [o for o in ops if o.eng == e] for e in ENGS}
        final_waits = []
        for e in ENGS:
            for j in range(min(DMA_WINDOW, dcnt[e])):
                final_waits.append((dsems[e][j], 16 * ((dcnt[e] - 1 - j) // DMA_WINDOW + 1)))

        def emit(engname, eh):
            waited = {}

            def wait(sem, v):
                if waited.get(sem.name, 0) >= v:
                    return
                waited[sem.name] = v
                eh.wait_ge(sem, v)

            for o in per_eng[engname]:
                if o.pre_wait is not None:
                    wait(*o.pre_wait)
                for d in sorted(o.deps):
                    wait(*ops[d].event)
                ins = o.fn(eh)
                if o.event is not None:
                    ins.then_inc(o.event[0], 16 if o.is_dma else 1)
            if engname == "sp":
                for s, v in final_waits:
                    wait(s, v)

        with nc.Block() as block:
            @block.tensor
            def _(e):
                emit("pe", e)

            @block.scalar
            def _(e):
                emit("act", e)

            @block.vector
            def _(e):
                emit("dve", e)

            @block.gpsimd
            def _(e):
                emit("pool", e)

            @block.sync
            def _(e):
                emit("sp", e)
D = 1024
CTX = 256
GRID_W = 64
EPS = 1e-6
NH = 8
QK = 96
GH = 4
UF_ROWS = 1984
UT_COLS = 528
MOE_BS = 512


class Cfg:
    def __init__(self, nlt=16, layers=(0, 1, 2, 3), debug=False):
        self.nlt = nlt
        self.T = CTX + 512 * nlt
        self.tiles = [(0, CTX)] + [(CTX + 512 * i, 512) for i in range(nlt)]
        self.nsub = self.T // 128
        self.layers = tuple(layers)
        self.debug = debug
        self.nblk = -(-(2 * self.T) // MOE_BS) + 32
        self.nslot = self.nblk * MOE_BS


def rope_perm():
    p = np.zeros(32, np.int64)
    for ax in range(2):
        for half in range(2):
            for f in range(8):
                p[ax * 16 + half * 8 + f] = ax * 16 + (1 - half) * 8 + f
    return p


def rope_tables(cfg):
    S = cfg.T - CTX
    pos = np.arange(S)
    row = (pos // GRID_W).astype(np.float32)
    col = (pos % GRID_W).astype(np.float32)
    inv = (10000.0 ** (-np.arange(8, dtype=np.float32) / 8)).astype(np.float32)
    cosT = np.zeros((96, cfg.T), np.float32)
    sinT = np.zeros((96, cfg.T), np.float32)
    cosT[64:96, :CTX] = 1.0
    for ax in range(2):
        p = row if ax == 0 else col
        ang = p[None, :] * inv[:, None]
        for half in range(2):
            r0 = 64 + ax * 16 + half * 8
            cosT[r0:r0 + 8, CTX:] = np.cos(ang)
            sinT[r0:r0 + 8, CTX:] = np.sin(ang) * (-1.0 if half == 0 else 1.0)
    return cosT, sinT


def host_consts(cfg):
    c = {}
    c["ident_f"] = np.eye(128, dtype=np.float32)
    i = np.arange(128)
    same = (i[:, None] // 64) == (i[None, :] // 64)
    c["m_le"] = (same & (i[:, None] <= i[None, :])).astype(np.float32)
    c["m_ge"] = (same & (i[:, None] >= i[None, :])).astype(np.float32)
    c["m_lt"] = (same & (i[:, None] < i[None, :])).astype(np.float32)
    c["m_gt"] = (same & (i[:, None] > i[None, :])).astype(np.float32)
    c["m_same"] = same.astype(np.float32)
    c["ones_f"] = np.ones((128, 128), np.float32)
    c["m_h0"] = np.repeat((i[:, None] < 64), 128, 1).astype(np.float32)
    c["m_h1"] = np.repeat((i[:, None] >= 64), 128, 1).astype(np.float32)
    c["tri_lt_full"] = (i[:, None] < i[None, :]).astype(np.float32)
    cosT, sinT = rope_tables(cfg)
    c["cosT"] = cosT
    c["sinT"] = sinT
    c["thr"] = np.tile((np.arange(34, dtype=np.float32) * MOE_BS)[None, :], (128, 1))
    rm = np.ones((128, 512), np.float32)
    rm[:, ::64] = 0.0
    c["rmask"] = rm
    hs = np.zeros((128, 2), np.float32)
    hs[:64, 0] = 1.0
    hs[64:, 1] = 1.0
    c["hsel"] = hs
    c["iota_p"] = i.astype(np.float32).reshape(128, 1)
    c["blk_start"] = np.tile((np.arange(cfg.nblk, dtype=np.float32) * MOE_BS)[None, :], (128, 1))
    return c


CONST_SHAPES = lambda cfg: {
    "ident_f": (128, 128), "m_le": (128, 128), "m_ge": (128, 128), "m_lt": (128, 128), "m_gt": (128, 128),
    "m_same": (128, 128), "ones_f": (128, 128), "m_h0": (128, 128), "m_h1": (128, 128), "tri_lt_full": (128, 128),
    "cosT": (96, cfg.T), "sinT": (96, cfg.T), "iota_p": (128, 1), "rmask": (128, 512), "hsel": (128, 2), "thr": (128, 34), "blk_start": (128, cfg.nblk),
}


def host_even_smalls(inp, j):
    perm = rope_perm()
    s = {}
    s["qa_g"] = np.ascontiguousarray(inp["mla_qa_norm"][j].reshape(2, 128).T)
    s["kva_g"] = np.ascontiguousarray(inp["mla_kva_norm"][j].reshape(128, 1))
    for nm, key in (("qn_col", "mla_q_norm"), ("kn_col", "mla_k_norm")):
        g = inp[key][j]
        colv = np.zeros((96, 2), np.float32)
        colv[:, 0] = g
        colv[64:96, 1] = g[64 + perm]
        s[nm] = colv
    s["convw"] = np.ascontiguousarray(inp["gdn_conv"][j].reshape(5, 12, 128).transpose(2, 1, 0))
    s["gdn_vec"] = np.concatenate([inp["gdn_a_log"][j].reshape(-1), inp["gdn_dt_bias"][j].reshape(-1)]).reshape(1, 16).astype(np.float32)
    s["outg"] = np.ascontiguousarray(inp["gdn_out_norm"][j].reshape(1, 128))
    return s


EVEN_SMALL_SHAPES = {"qa_g": (128, 2), "kva_g": (128, 1), "qn_col": (96, 2), "kn_col": (96, 2),
                     "convw": (128, 12, 5), "gdn_vec": (1, 16), "outg": (1, 128)}


def host_odd_smalls(inp, j):
    s = {}
    col = lambda v: np.ascontiguousarray(v.reshape(8, 128).T)
    s["mu"] = np.ascontiguousarray(inp["rk_mu"][j].reshape(6, 8, 128).transpose(2, 0, 1))
    s["w0"] = np.ascontiguousarray(inp["rk_w0"][j].reshape(2, 8, 128).transpose(2, 0, 1))
    s["a0"] = np.ascontiguousarray(inp["rk_a0"][j].reshape(2, 8, 128).transpose(2, 0, 1))
    s["kkcol"] = col(inp["rk_kk"][j])
    s["kacol"] = col(inp["rk_ka"][j])
    s["rkcol"] = col(inp["rk_rk"][j].reshape(-1))
    s["lnw"] = np.ascontiguousarray(inp["rk_ln_w"][j].reshape(1, -1))
    s["lnb"] = np.ascontiguousarray(inp["rk_ln_b"][j].reshape(1, -1))
    s["v0"] = np.ascontiguousarray(inp["rk_v0"][j - 1].reshape(1, -1)) if j > 0 else np.zeros((1, D), np.float32)
    return s


ODD_SMALL_SHAPES = {"mu": (128, 6, 8), "w0": (128, 2, 8), "a0": (128, 2, 8), "kkcol": (128, 8), "kacol": (128, 8),
                    "rkcol": (128, 8), "lnw": (1, D), "lnb": (1, D), "v0": (1, D)}
MLA_SCALE = 96 ** -0.5


def rstd_inplace(kb, t, inv_n, eps, src=None):
    kb.v("dve", "tensor_scalar", t, [src if src is not None else t], inv_n, eps, ALU.mult, ALU.add)
    kb.act(t, t, AF.Ln)
    kb.act(t, t, AF.Exp, scale=-0.5)


class Prog:
    def __init__(self, cfg):
        self.cfg = cfg
        self.nc = bass.Bass("TRN2", target_bir_lowering=False)
        self.kb = KB(self.nc)
        self.in_shapes = {}
        self.dbg = {}
        self._rr = 0

    def inp(self, name, shape, dtype=F32):
        self.in_shapes[name] = (tuple(shape), dtype)
        return self.nc.dram_tensor(name, list(shape), dtype, kind="ExternalInput").ap()

    def scratch(self, name, shape, dtype=F32):
        if self.cfg.debug:
            t = self.nc.dram_tensor("dbg_" + name, list(shape), dtype, kind="ExternalOutput")
            self.dbg[name] = "dbg_" + name
        else:
            t = self.nc.dram_tensor("scr_" + name, list(shape), dtype, kind="Internal")
        return t.ap()

    def evac(self, out, in_):
        self._rr += 1
        if self._rr % 2:
            self.kb.act(out, in_, AF.Copy)
        else:
            self.kb.v("dve", "tensor_copy", out, [in_])

    def setup(self):
        kb, cfg = self.kb, self.cfg
        self.h0 = self.inp("h0", [cfg.T, D])
        self.cvec = self.inp("cvec", [128, 16])
        self.out = self.nc.dram_tensor("out", [cfg.T - CTX, D], F32, kind="ExternalOutput").ap()
        self.H = self.scratch("H", [cfg.T, D])
        self.cin = {k: self.inp("c_" + k, s) for k, s in CONST_SHAPES(cfg).items()}
        self.ident_f = kb.sb([128, 128], F32, "identf")
        self.ident = kb.sb([128, 128], BF16, "ident")
        self.ones = kb.sb([128, 128], BF16, "ones")
        kb.dma(self.ident_f[:], self.cin["ident_f"])
        kb.v("dve", "tensor_copy", self.ident[:], [self.ident_f[:]])
        kb.v("pool", "memset", self.ones[:], [], 1.0)
        self.modL = [kb.sb([128, D], F32, f"modL{i}") for i in range(6)]
        self.modC = [kb.sb([128, D], F32, f"modC{i}") for i in range(6)]
        with kb.phase() as ph:
            bufs = [ph.sb([128, D], F32, "cp") for _ in range(4)]
            for s in range(cfg.nsub):
                b = bufs[s % 4]
                kb.dma(b[:], self.h0[s * 128:(s + 1) * 128, :])
                kb.dma(self.H[s * 128:(s + 1) * 128, :], b[:])

    def finish(self):
        kb, cfg = self.kb, self.cfg
        with kb.phase() as ph:
            bufs = [ph.sb([128, D], F32, "cp") for _ in range(4)]
            for s in range(2, cfg.nsub):
                b = bufs[s % 4]
                kb.dma(b[:], self.H[s * 128:(s + 1) * 128, :])
                kb.dma(self.out[(s - 2) * 128:(s - 1) * 128, :], b[:])
        kb.finalize()

    def phase_mod(self, l):
        kb = self.kb
        ada_w = self.inp(f"ada_w__{l}", [D, 6 * D])
        ada_b = self.inp(f"ada_b__{l}", [1, 6 * D])
        nmix = self.inp(f"norm_mix__{l}", [1, D])
        nffn = self.inp(f"norm_ffn__{l}", [1, D])
        with kb.phase() as ph:
            sc = ph.sb([128, 16], F32, "sc")
            kb.dma(sc[:], self.cvec)
            kb.act(sc[:], sc[:], AF.Silu)
            lhs = ph.sb([128, 16, 128], F32, "lhs")
            for c in range(16):
                kb.v("dve", "tensor_copy", lhs[:, c, :], [sc[:, c:c + 1].to_broadcast([128, 128])])
            bb = ph.sb([128, 6 * D], F32, "bb")
            kb.dma(bb[:], ada_b.broadcast_to([128, 6 * D]))
            wst = [ph.sb([128, 8, 512], F32, "wst") for _ in range(2)]
            pl = [ph.ps() for _ in range(2)]
            pc = [ph.ps() for _ in range(2)]
            awv = ada_w.rearrange("(c p) n -> p c n", p=128)
            for n in range(12):
                w = wst[n % 2]
                kb.dma(w[:], awv[:, :, n * 512:(n + 1) * 512])
                for c in range(8):
                    kb.mm(pl[n % 2][:], lhs[:, c, :], w[:, c, :], start=(c == 0), stop=(c == 7))
                for c in range(8):
                    kb.mm(pc[n % 2][:], lhs[:, 8 + c, :], w[:, c, :], start=(c == 0), stop=(c == 7))
                jj, hf = n // 2, n % 2
                kb.v("dve", "tensor_tensor", self.modL[jj][:, hf * 512:(hf + 1) * 512], [pl[n % 2][:], bb[:, n * 512:(n + 1) * 512]], ALU.add)
                kb.v("dve", "tensor_tensor", self.modC[jj][:, hf * 512:(hf + 1) * 512], [pc[n % 2][:], bb[:, n * 512:(n + 1) * 512]], ALU.add)
            nm = ph.sb([128, D], F32, "nm")
            nf = ph.sb([128, D], F32, "nf")
            kb.dma(nm[:], nmix.broadcast_to([128, D]))
            kb.dma(nf[:], nffn.broadcast_to([128, D]))
            for mod in (self.modL, self.modC):
                kb.v("dve", "scalar_tensor_tensor", mod[1][:], [mod[1][:], 1.0, nm[:]], ALU.add, ALU.mult)
                kb.v("dve", "scalar_tensor_tensor", mod[4][:], [mod[4][:], 1.0, nf[:]], ALU.add, ALU.mult)

    def norm_sub(self, ht, junk, ss, tmp, xn, G, S):
        kb = self.kb
        kb.act(junk, ht, AF.Square, accum_out=ss)
        rstd_inplace(kb, ss, 1.0 / D, EPS)
        kb.v("dve", "scalar_tensor_tensor", tmp, [ht, ss, G], ALU.mult, ALU.mult)
        kb.v("pool", "tensor_tensor", xn, [tmp, S], ALU.add)

    def norm_tiles_to_xT(self, ph, src, gi, si, consume):
        kb, cfg = self.kb, self.cfg
        xTs = [ph.sb([128, 8, 512], BF16, "xT") for _ in range(2)]
        hts = [ph.sb([128, D], F32, "ht") for _ in range(2)]
        junk = ph.sb([128, D], F32, "junk")
        sss = [ph.sb([128, 1], F32, "ss") for _ in range(2)]
        tmps = [ph.sb([128, D], F32, "tmp") for _ in range(2)]
        xns = [ph.sb([128, D], BF16, "xn") for _ in range(2)]
        ptr = [ph.ps([128, 1024], BF16, "ptr") for _ in range(2)]
        k = 0
        for ti, (t0, TT) in enumerate(cfg.tiles):
            xT = xTs[ti % 2]
            mod = self.modC if ti == 0 else self.modL
            for s in range(TT // 128):
                ht, ss, tmp, xn, pt = hts[k % 2], sss[k % 2], tmps[k % 2], xns[k % 2], ptr[k % 2]
                k += 1
                r0 = t0 + s * 128
                kb.dma(ht[:], src[r0:r0 + 128, :])
                self.norm_sub(ht[:], junk[:], ss[:], tmp[:], xn[:], mod[gi][:], mod[si][:])
                for c in range(8):
                    kb.tr(pt[:, c * 128:(c + 1) * 128], xn[:, c * 128:(c + 1) * 128], self.ident[:])
                self.evac(xT[:, :, s * 128:(s + 1) * 128], pt[:].rearrange("p (c t) -> p c t", c=8))
            consume(ti, t0, TT, xT)

    def even_layer(self, l):
        j = l // 2
        self.phase_mod(l)
        sm = {k: self.inp(f"e{j}_{k}", s) for k, s in EVEN_SMALL_SHAPES.items()}
        self.phase_e1(j)
        self.phase_e2(j, sm)
        self.phase_e3(j)
        self.phase_e4(j, sm)
        self.phase_e5(j, sm)
        self.phase_e6(l, j, sm)
        self.moe(l)

    def phase_e1(self, j):
        kb, cfg = self.kb, self.cfg
        W = self.inp(f"hy_w_in__{j}", [D, 2480])
        self.UF = self.scratch(f"UF{j}", [UF_ROWS, cfg.T])
        self.UT = self.scratch(f"UT{j}", [cfg.T, UT_COLS])
        with kb.phase() as ph:
            Wfm = ph.sb([128, 8, UF_ROWS], BF16, "Wfm")
            Wtm = ph.sb([128, 8, UT_COLS], BF16, "Wtm")
            Wv = W.rearrange("(c p) n -> p c n", p=128)
            kb.dma(Wfm[:, :, 0:416], Wv[:, :, 0:416], eng="pool")
            for (d0, s0) in ((0, 8), (8, 0), (16, 24), (24, 16)):
                kb.dma(Wfm[:, :, 416 + d0:416 + d0 + 8], Wv[:, :, 384 + s0:384 + s0 + 8], eng="pool")
            kb.dma(Wfm[:, :, 448:1984], Wv[:, :, 416:1952], eng="pool")
            kb.dma(Wtm[:, :, :], Wv[:, :, 1952:2480], eng="pool")
            pmm = [ph.ps() for _ in range(4)]
            ost = [ph.sb([128, 512], F32, "ost") for _ in range(4)]
            cnt = [0]

            def consume(ti, t0, TT, xT):
                for m in range(16):
                    rows = min(128, UF_ROWS - m * 128)
                    pm, ob = pmm[cnt[0] % 4], ost[cnt[0] % 4]
                    cnt[0] += 1
                    for c in range(8):
                        kb.mm(pm[0:rows, 0:TT], Wfm[:, c, m * 128:m * 128 + rows], xT[:, c, 0:TT], start=(c == 0), stop=(c == 7))
                    self.evac(ob[0:rows, 0:TT], pm[0:rows, 0:TT])
                    kb.dma(self.UF[m * 128:m * 128 + rows, t0:t0 + TT], ob[0:rows, 0:TT])
                for s in range(TT // 128):
                    for (n0, n1) in ((0, 512), (512, 528)):
                        pm, ob = pmm[cnt[0] % 4], ost[cnt[0] % 4]
                        cnt[0] += 1
                        for c in range(8):
                            kb.mm(pm[:, 0:n1 - n0], xT[:, c, s * 128:(s + 1) * 128], Wtm[:, c, n0:n1], start=(c == 0), stop=(c == 7))
                        self.evac(ob[:, 0:n1 - n0], pm[:, 0:n1 - n0])
                        kb.dma(self.UT[t0 + s * 128:t0 + (s + 1) * 128, n0:n1], ob[:, 0:n1 - n0])

            self.norm_tiles_to_xT(ph, self.H, 1, 0, consume)

    def phase_e2(self, j, sm):
        kb, cfg = self.kb, self.cfg
        Wqb = self.inp(f"mla_w_qb__{j}", [256, 768])
        Wkvb = self.inp(f"mla_w_kvb__{j}", [128, 1024])
        self.QF = self.scratch(f"QF{j}", [NH, 96, cfg.T], BF16)
        self.KF = self.scratch(f"KF{j}", [NH, 96, cfg.T], BF16)
        self.VT = self.scratch(f"VT{j}", [cfg.T, 512], BF16)
        with kb.phase() as ph:
            Wq = ph.sb([128, 2, 768], BF16, "Wq")
            Wqs = ph.sb([128, 2, NH, 32], BF16, "Wqs")
            Wk = ph.sb([128, NH, 64], BF16, "Wk")
            Wvv = ph.sb([128, NH, 64], BF16, "Wv")
            kb.dma(Wq[:], Wqb.rearrange("(c p) n -> p c n", p=128), eng="pool")
            Wq4 = Wqb.rearrange("(c p) (h d) -> p c h d", p=128, d=96)
            for (d0, s0) in ((0, 8), (8, 0), (16, 24), (24, 16)):
                for c in range(2):
                    kb.dma(Wqs[:, c, :, d0:d0 + 8], Wq4[:, c, :, 64 + s0:64 + s0 + 8], eng="pool")
            Wkv3 = Wkvb.rearrange("p (h d) -> p h d", d=128)
            kb.dma(Wk[:], Wkv3[:, :, 0:64], eng="pool")
            kb.dma(Wvv[:], Wkv3[:, :, 64:128], eng="pool")
            qa_g = ph.sb([128, 2], F32, "qa_g")
            kva_g = ph.sb([128, 1], F32, "kva_g")
            qn = ph.sb([96, 2], F32, "qn")
            kn = ph.sb([96, 2], F32, "kn")
            kb.dma(qa_g[:], sm["qa_g"])
            kb.dma(kva_g[:], sm["kva_g"])
            kb.dma(qn[:], sm["qn_col"])
            kb.dma(kn[:], sm["kn_col"])
            cq = ph.sb([128, 2, 512], F32, "cq")
            ckv = ph.sb([128, 512], F32, "ckv")
            kpe = ph.sb([96, 512], F32, "kpe")
            kpes = ph.sb([96, 512], F32, "kpes")
            cs = ph.sb([96, 512], F32, "cs")
            sn = ph.sb([96, 512], F32, "sn")
            sq = ph.sb([128, 2, 512], BF16, "sq")
            rs = ph.sb([128, 512], F32, "rs")
            cqn = ph.sb([128, 2, 512], BF16, "cqn")
            ckvn = ph.sb([128, 512], BF16, "ckvn")
            rk = ph.sb([96, 512], F32, "rk")
            t1s = [ph.sb([96, 512], F32, "t1") for _ in range(2)]
            t2s = [ph.sb([96, 512], F32, "t2") for _ in range(2)]
            sqhs = [ph.sb([96, 512], BF16, "sqh") for _ in range(2)]
            rshs = [ph.sb([96, 512], F32, "rsh") for _ in range(2)]
            ohs = [ph.sb([96, 512], BF16, "oh") for _ in range(2)]
            vsb = [ph.sb([128, 512], BF16, "vsb") for _ in range(2)]
            pA = [ph.ps() for _ in range(2)]
            pB = [ph.ps() for _ in range(2)]
            pS = [ph.ps() for _ in range(2)]
            pR = ph.ps()
            pV = ph.ps()
            u = 0
            for ti, (t0, TT) in enumerate(cfg.tiles):
                kb.dma(cq[:, :, 0:TT], self.UF[0:256, t0:t0 + TT].rearrange("(c p) t -> p c t", p=128))
                kb.dma(ckv[:, 0:TT], self.UF[256:384, t0:t0 + TT])
                kb.dma(kpe[64:96, 0:TT], self.UF[384:416, t0:t0 + TT])
                kb.dma(kpes[64:96, 0:TT], self.UF[416:448, t0:t0 + TT])
                kb.dma(cs[64:96, 0:TT], self.cin["cosT"][64:96, t0:t0 + TT])
                kb.dma(sn[64:96, 0:TT], self.cin["sinT"][64:96, t0:t0 + TT])
                kb.act(sq[:, :, 0:TT], cq[:, :, 0:TT], AF.Square)
                for c in range(2):
                    kb.mm(pR[:, 0:TT], self.ones[:], sq[:, c, 0:TT], start=(c == 0), stop=(c == 1))
                rstd_inplace(kb, rs[:, 0:TT], 1.0 / 256, EPS, src=pR[:, 0:TT])
                for c in range(2):
                    kb.v("dve", "scalar_tensor_tensor", cqn[:, c, 0:TT], [cq[:, c, 0:TT], qa_g[:, c:c + 1], rs[:, 0:TT]], ALU.mult, ALU.mult)
                kb.act(sq[:, 0, 0:TT], ckv[:, 0:TT], AF.Square)
                kb.mm(pR[:, 0:TT], self.ones[:], sq[:, 0, 0:TT])
                rstd_inplace(kb, rs[:, 0:TT], 1.0 / 128, EPS, src=pR[:, 0:TT])
                kb.v("dve", "scalar_tensor_tensor", ckvn[:, 0:TT], [ckv[:, 0:TT], kva_g[:, 0:1], rs[:, 0:TT]], ALU.mult, ALU.mult)
                kb.v("dve", "scalar_tensor_tensor", rk[64:96, 0:TT], [kpe[64:96, 0:TT], kn[64:96, 0:1], cs[64:96, 0:TT]], ALU.mult, ALU.mult)
                kb.v("dve", "scalar_tensor_tensor", t2s[0][64:96, 0:TT], [kpes[64:96, 0:TT], kn[64:96, 1:2], sn[64:96, 0:TT]], ALU.mult, ALU.mult)
                kb.v("pool", "tensor_tensor", rk[64:96, 0:TT], [rk[64:96, 0:TT], t2s[0][64:96, 0:TT]], ALU.add)
                for h in range(NH):
                    a, b2, s_, t1, t2, sqh, rsh, oh = pA[u % 2], pB[u % 2], pS[u % 2], t1s[u % 2], t2s[u % 2], sqhs[u % 2], rshs[u % 2], ohs[u % 2]
                    u += 1
                    for c in range(2):
                        kb.mm(a[0:96, 0:TT], Wq[:, c, h * 96:(h + 1) * 96], cqn[:, c, 0:TT], start=(c == 0), stop=(c == 1))
                    for c in range(2):
                        kb.mm(b2[64:96, 0:TT], Wqs[:, c, h, :], cqn[:, c, 0:TT], start=(c == 0), stop=(c == 1))
                    kb.act(sqh[0:96, 0:TT], a[0:96, 0:TT], AF.Square)
                    kb.mm(s_[0:96, 0:TT], self.ones[0:96, 0:96], sqh[0:96, 0:TT])
                    rstd_inplace(kb, rsh[0:96, 0:TT], 1.0 / 96, EPS, src=s_[0:96, 0:TT])
                    kb.v("dve", "scalar_tensor_tensor", oh[0:64, 0:TT], [a[0:64, 0:TT], qn[0:64, 0:1], rsh[0:64, 0:TT]], ALU.mult, ALU.mult)
                    kb.v("dve", "scalar_tensor_tensor", t1[64:96, 0:TT], [a[64:96, 0:TT], qn[64:96, 0:1], cs[64:96, 0:TT]], ALU.mult, ALU.mult)
                    kb.v("dve", "scalar_tensor_tensor", t2[64:96, 0:TT], [b2[64:96, 0:TT], qn[64:96, 1:2], sn[64:96, 0:TT]], ALU.mult, ALU.mult)
                    kb.v("pool", "tensor_tensor", t1[64:96, 0:TT], [t1[64:96, 0:TT], t2[64:96, 0:TT]], ALU.add)
                    kb.v("dve", "tensor_tensor", oh[64:96, 0:TT], [t1[64:96, 0:TT], rsh[64:96, 0:TT]], ALU.mult)
                    kb.dma(self.QF[h, :, t0:t0 + TT], oh[0:96, 0:TT])
                    a, s_, sqh, rsh, oh = pA[u % 2], pS[u % 2], sqhs[u % 2], rshs[u % 2], ohs[u % 2]
                    u += 1
                    kb.mm(a[0:64, 0:TT], Wk[:, h, :], ckvn[:, 0:TT])
                    kb.act(sqh[0:64, 0:TT], a[0:64, 0:TT], AF.Square)
                    kb.act(sqh[64:96, 0:TT], kpe[64:96, 0:TT], AF.Square)
                    kb.mm(s_[0:96, 0:TT], self.ones[0:96, 0:96], sqh[0:96, 0:TT])
                    rstd_inplace(kb, rsh[0:96, 0:TT], 1.0 / 96, EPS, src=s_[0:96, 0:TT])
                    kb.v("dve", "scalar_tensor_tensor", oh[0:64, 0:TT], [a[0:64, 0:TT], kn[0:64, 0:1], rsh[0:64, 0:TT]], ALU.mult, ALU.mult)
                    kb.v("dve", "tensor_tensor", oh[64:96, 0:TT], [rk[64:96, 0:TT], rsh[64:96, 0:TT]], ALU.mult)
                    kb.dma(self.KF[h, :, t0:t0 + TT], oh[0:96, 0:TT])
                for s in range(TT // 128):
                    vb = vsb[s % 2]
                    kb.mm(pV[:], ckvn[:, s * 128:(s + 1) * 128], Wvv[:].rearrange("p h d -> p (h d)"))
                    self.evac(vb[:], pV[:])
                    kb.dma(self.VT[t0 + s * 128:t0 + (s + 1) * 128, :], vb[:])

    def phase_e3(self, j):
        kb, cfg = self.kb, self.cfg
        self.AF_ = self.scratch(f"AF{j}", [512, cfg.T], BF16)
        nsub = cfg.nsub
        with kb.phase() as ph:
            Vall = ph.sb([128, nsub, NH, 65], BF16, "Vall")
            kb.v("pool", "memset", Vall[:], [], 1.0)
            for h_ in range(NH):
                kb.dma(Vall[:, :, h_, 0:64], self.VT[:, h_ * 64:(h_ + 1) * 64].rearrange("(b p) n -> p b n", p=128))
            sel = ph.sb([65, 64], F32, "sel")
            kb.v("pool", "memset", sel[:], [], 0.0)
            kb.v("pool", "memset", sel[64:65, :], [], 1.0)
            oext = ph.sb([65, 512], F32, "oext")
            Khs = [ph.sb([96, cfg.T], BF16, "Kh") for _ in range(2)]
            Qts = [ph.sb([96, 512], BF16, "Qt") for _ in range(2)]
            PTs = [ph.sb([128, 512], BF16, "PT") for _ in range(4)]
            rden = ph.sb([64, 512], F32, "rden")
            osb = [ph.sb([64, 512], BF16, "osb") for _ in range(2)]
            pS = [ph.ps() for _ in range(4)]
            pO = [ph.ps() for _ in range(2)]
            pD = [ph.ps() for _ in range(2)]
            DEPTH = 3
            units = []
            gi = 0
            for h in range(NH):
                for ti, (t0, TT) in enumerate(cfg.tiles):
                    nkb = 2 if ti == 0 else nsub
                    for kbk in range(nkb):
                        units.append((h, ti, t0, TT, kbk, nkb, gi))
                    gi += 1
            cur_h = [-1]
            cur_g = [-1]
            for idx in range(len(units) + DEPTH):
                if idx < len(units):
                    h, ti, t0, TT, kbk, nkb, g = units[idx]
                    if h != cur_h[0]:
                        cur_h[0] = h
                        kb.dma(Khs[h % 2][:], self.KF[h])
                    if g != cur_g[0]:
                        cur_g[0] = g
                        kb.dma(Qts[g % 2][:, 0:TT], self.QF[h, :, t0:t0 + TT])
                    ps, pt = pS[idx % 4], PTs[idx % 4]
                    kb.mm(ps[:, 0:TT], Khs[h % 2][:, kbk * 128:(kbk + 1) * 128], Qts[g % 2][:, 0:TT])
                    kb.act(pt[:, 0:TT], ps[:, 0:TT], AF.Exp, scale=MLA_SCALE)
                j2 = idx - DEPTH
                if j2 >= 0:
                    h, ti, t0, TT, kbk, nkb, g = units[j2]
                    pt = PTs[j2 % 4]
                    po, pd, ob = pO[g % 2], pD[g % 2], osb[g % 2]
                    kb.mm(po[0:65, 0:TT], Vall[:, kbk, h, :], pt[:, 0:TT], start=(kbk == 0), stop=(kbk == nkb - 1))
                    if kbk == nkb - 1:
                        kb.act(oext[:, 0:TT], po[0:65, 0:TT], AF.Copy)
                        kb.mm(pd[0:64, 0:TT], sel[:], oext[:, 0:TT])
                        kb.v("dve", "reciprocal", rden[:, 0:TT], [pd[0:64, 0:TT]])
                        kb.v("dve", "tensor_tensor", ob[:, 0:TT], [po[0:64, 0:TT], rden[:, 0:TT]], ALU.mult)
                        kb.dma(self.AF_[h * 64:(h + 1) * 64, t0:t0 + TT], ob[:, 0:TT])
    def load_const_tile(self, ph, name, dtype=F32):
        shp = CONST_SHAPES(self.cfg)[name]
        t = ph.sb(list(shp), F32, name)
        self.kb.dma(t[:], self.cin[name])
        if dtype == F32:
            return t
        t2 = ph.sb(list(shp), dtype, name + "b")
        self.kb.v("dve", "tensor_copy", t2[:], [t[:]])
        return t2

    def phase_e4(self, j, sm):
        kb, cfg = self.kb, self.cfg
        T = cfg.T
        self.GQ = self.scratch(f"GQ{j}", [GH, 128, T], BF16)
        self.GK = self.scratch(f"GK{j}", [GH, 128, T], BF16)
        self.GKT = self.scratch(f"GKT{j}", [T, 512], BF16)
        self.GVT = self.scratch(f"GVT{j}", [T, 512], BF16)
        last = len(cfg.tiles) - 1
        with kb.phase() as ph:
            cw = ph.sb([128, 12, 5], F32, "cw")
            kb.dma(cw[:], sm["convw"])
            xs = [ph.sb([128, 516], F32, "x") for _ in range(2)]
            accs = [ph.sb([128, 512], F32, "acc") for _ in range(2)]
            ys = [ph.sb([128, 512], F32, "y") for _ in range(2)]
            sqs = [ph.sb([128, 512], BF16, "sq") for _ in range(2)]
            rs = ph.sb([128, 512], F32, "rs")
            yns = [ph.sb([128, 512], BF16, "yn") for _ in range(2)]
            tsb = [ph.sb([128, 4, 128], BF16, "tsb") for _ in range(2)]
            pR = [ph.ps() for _ in range(2)]
            ptr = [ph.ps([128, 1024], BF16, "ptr") for _ in range(2)]
            u = 0
            for ti, (t0, TT) in enumerate(cfg.tiles):
                for cc in range(12):
                    x, acc, y, sq, yn, pr, pt, tb = xs[u % 2], accs[u % 2], ys[u % 2], sqs[u % 2], yns[u % 2], pR[u % 2], ptr[u % 2], tsb[u % 2]
                    u += 1
                    r0 = 448 + cc * 128
                    kb.dma(x[:, 2:TT + 2], self.UF[r0:r0 + 128, t0:t0 + TT])
                    if ti in (0, 1):
                        kb.v("pool", "memset", x[:, 0:2], [], 0.0)
                    else:
                        kb.dma(x[:, 0:2], self.UF[r0:r0 + 128, t0 - 2:t0])
                    if ti in (0, last):
                        kb.v("pool", "memset", x[:, TT + 2:TT + 4], [], 0.0)
                    else:
                        kb.dma(x[:, TT + 2:TT + 4], self.UF[r0:r0 + 128, t0 + TT:t0 + TT + 2])
                    kb.v("dve", "tensor_scalar", acc[:, 0:TT], [x[:, 0:TT]], cw[:, cc, 0:1], None, ALU.mult)
                    for jj in range(1, 5):
                        kb.v("dve", "scalar_tensor_tensor", acc[:, 0:TT], [x[:, jj:jj + TT], cw[:, cc, jj:jj + 1], acc[:, 0:TT]], ALU.mult, ALU.add)
                    kb.act(y[:, 0:TT], acc[:, 0:TT], AF.Silu)
                    if cc < 8:
                        kb.act(sq[:, 0:TT], y[:, 0:TT], AF.Square)
                        kb.mm(pr[:, 0:TT], self.ones[:], sq[:, 0:TT])
                        rstd_inplace(kb, rs[:, 0:TT], 1.0, EPS, src=pr[:, 0:TT])
                        kb.v("dve", "scalar_tensor_tensor", yn[:, 0:TT], [y[:, 0:TT], (128 ** -0.5) if cc < 4 else 1.0, rs[:, 0:TT]], ALU.mult, ALU.mult)
                        dst = self.GQ[cc] if cc < 4 else self.GK[cc - 4]
                        kb.dma(dst[:, t0:t0 + TT], yn[:, 0:TT])
                    else:
                        kb.v("pool", "tensor_copy", yn[:, 0:TT], [y[:, 0:TT]])
                    if cc >= 4:
                        ns = TT // 128
                        for s in range(ns):
                            kb.tr(pt[:, s * 128:(s + 1) * 128], yn[:, s * 128:(s + 1) * 128], self.ident[:])
                        self.evac(tb[:, 0:ns, :], pt[:, 0:TT].rearrange("p (s c) -> p s c", c=128))
                        dstT = self.GKT if cc < 8 else self.GVT
                        hh = (cc - 4) % 4
                        kb.dma(dstT[t0:t0 + TT, hh * 128:(hh + 1) * 128].rearrange("(s p) c -> p s c", p=128), tb[:, 0:ns, :])

    def phase_e5(self, j, sm):
        kb, cfg = self.kb, self.cfg
        T, nsub = cfg.T, cfg.nsub
        self.OD = [self.scratch(f"OD{j}_{d}", [T, 512]) for d in range(2)]
        fwd = list(range(nsub))
        bwd = [1, 0] + list(range(nsub - 1, 1, -1))
        with kb.phase() as ph:
            mle = self.load_const_tile(ph, "m_le")
            mge = self.load_const_tile(ph, "m_ge")
            mlt = self.load_const_tile(ph, "m_lt")
            mgt = self.load_const_tile(ph, "m_gt")
            msame = self.load_const_tile(ph, "m_same")
            mh0 = self.load_const_tile(ph, "m_h0")
            mh1 = self.load_const_tile(ph, "m_h1")
            onesf = self.load_const_tile(ph, "ones_f")
            gv = ph.sb([128, 16], F32, "gv")
            kb.dma(gv[:], sm["gdn_vec"].broadcast_to([128, 16]))
            negA = ph.sb([128, 8], F32, "negA")
            kb.act(negA[:], gv[:, 0:8], AF.Exp)
            kb.v("dve", "tensor_scalar", negA[:], [negA[:]], -1.0, None, ALU.mult)
            S = [[ph.sb([128, 128], F32, f"S{d}{h}") for h in range(GH)] for d in range(2)]
            Sb = [[ph.sb([128, 128], BF16, f"Sb{d}{h}") for h in range(GH)] for d in range(2)]
            for d in range(2):
                for h in range(GH):
                    kb.v("pool", "memset", S[d][h][:], [], 0.0)
                    kb.v("pool", "memset", Sb[d][h][:], [], 0.0)
            banks = [ph.ps() for _ in range(7)]
            tbank = ph.ps([128, 1024], BF16, "tbank")
            slot = [0]
            tslot = [0]

            def ps128t():
                i = tslot[0] % 8
                tslot[0] += 1
                return V(tbank[:, i * 128:(i + 1) * 128], tbank.name)

            def ps128(bf=False):
                i = slot[0] % 7
                slot[0] += 1
                b = banks[i]
                return V(b[:, 0:128], b.name)

            def sub(v, r, cs=None):
                a = v.ap[r, :] if cs is None else v.ap[r, cs]
                return V(a, v.key)

            UB = []
            for d in range(4):
                b = {}
                b["ab"] = ph.sb([128, 16], F32, "ab")
                b["KT"] = ph.sb([128, GH, 128], BF16, "KT")
                b["QT"] = ph.sb([128, GH, 128], BF16, "QT")
                b["Ktm"] = ph.sb([128, 512], BF16, "Ktm")
                b["Vtm"] = ph.sb([128, 512], BF16, "Vtm")
                for nm in ("xa", "g", "beta", "nbeta", "gc", "gt", "eg", "ek", "beg", "egl0", "egl1"):
                    b[nm] = ph.sb([128, 4], F32, nm)
                b["osb"] = ph.sb([128, 512], F32, "osb")
                UB.append(b)
            HB = []
            for i in range(8):
                b = {}
                for nm in ("diag", "e1", "e2", "Dst", "DT", "EG", "usb"):
                    b[nm] = ph.sb([128, 128], F32, nm)
                for nm in ("X0", "X1", "XT0", "XT1", "AT0", "AT1", "Vb", "Kbg", "Khat", "QgT", "AqkT", "wT", "vnew"):
                    b[nm] = ph.sb([128, 128], BF16, nm)
                HB.append(b)
            def unit(d, h, B, Hb):
                hs = slice(h * 128, (h + 1) * 128)
                gcol = B["gc"][:, h:h + 1]
                kb.v("dve", "tensor_scalar", Hb["diag"][:], [self.ident_f[:]], gcol, None, ALU.mult)
                pg = ps128()
                kb.mm(pg, onesf[:], Hb["diag"][:])
                kb.v("dve", "tensor_scalar", Hb["e1"][:], [pg], gcol, 0.0, ALU.subtract, ALU.max)
                kb.v("dve", "tensor_scalar", Hb["e2"][:], [pg], gcol, 0.0, ALU.subtract, ALU.min)
                kb.act(Hb["EG"][:], pg, AF.Exp)
                kb.act(Hb["e1"][:], Hb["e1"][:], AF.Exp, scale=-1.0)
                kb.act(Hb["e2"][:], Hb["e2"][:], AF.Exp)
                kb.v("pool", "tensor_tensor", Hb["Dst"][:], [Hb["e1"][:], (mgt if d == 0 else mlt)[:]], ALU.mult)
                kb.v("pool", "tensor_tensor", Hb["DT"][:], [Hb["e2"][:], (mle if d == 0 else mge)[:]], ALU.mult)
                yield
                pkk = ps128()
                kb.mm(pkk, B["KT"][:, h, :], B["KT"][:, h, :])
                kb.v("dve", "scalar_tensor_tensor", Hb["X0"][:], [pkk, B["nbeta"][:, h:h + 1], Hb["Dst"][:]], ALU.mult, ALU.mult)
                pkq = ps128()
                kb.mm(pkq, B["KT"][:, h, :], B["QT"][:, h, :])
                kb.v("dve", "tensor_tensor", Hb["AqkT"][:], [pkq, Hb["DT"][:]], ALU.mult)
                yield
                pxt_b = ps128t()
                kb.tr(pxt_b, Hb["X0"][:], self.ident[:])
                kb.v("dve", "tensor_copy", Hb["XT0"][:], [pxt_b])
                kb.v("pool", "tensor_tensor", Hb["AT0"][:], [Hb["XT0"][:], self.ident[:]], ALU.add)
                yield
                X, XT, AT = Hb["X0"], Hb["XT0"], Hb["AT0"]
                for js in range(1, 6):
                    Xn = Hb["X1"] if X is Hb["X0"] else Hb["X0"]
                    XTn = Hb["XT1"] if XT is Hb["XT0"] else Hb["XT0"]
                    ATn = Hb["AT1"] if AT is Hb["AT0"] else Hb["AT0"]
                    px = ps128()
                    kb.mm(px, XT[:], X[:])
                    if js < 5:
                        pxT = ps128()
                        kb.mm(pxT, X[:], XT[:])
                    kb.act(Xn[:], px, AF.Copy)
                    if js < 5:
                        kb.v("dve", "tensor_copy", XTn[:], [pxT])
                    yield
                    pa = ps128()
                    kb.mm(pa, Xn[:], AT[:])
                    kb.v("dve", "tensor_tensor", ATn[:], [pa, AT[:]], ALU.add)
                    X, XT, AT = Xn, XTn, ATn
                    yield
                kb.v("pool", "tensor_scalar", Hb["Vb"][:], [B["Vtm"][:, hs]], B["beta"][:, h:h + 1], 1.0, ALU.mult, ALU.mult)
                kb.v("pool", "tensor_scalar", Hb["Kbg"][:], [B["Ktm"][:, hs]], B["beg"][:, h:h + 1], 1.0, ALU.mult, ALU.mult)
                kb.act(Hb["Khat"][:], B["Ktm"][:, hs], AF.Copy, scale=B["ek"][:, h:h + 1])
                kb.v("pool", "tensor_tensor", Hb["QgT"][:], [B["QT"][:, h, :], Hb["EG"][:]], ALU.mult)
                pu = ps128()
                kb.mm(pu, AT[:], Hb["Vb"][:])
                kb.act(Hb["usb"][:], pu, AF.Copy)
                pw = ps128()
                kb.mm(pw, Hb["Kbg"][:], AT[:])
                kb.act(Hb["wT"][:], pw, AF.Copy)
                yield
                for half in ((0, 1) if d == 0 else (1, 0)):
                    r = slice(half * 64, half * 64 + 64)
                    egl = B["egl0"] if half == 0 else B["egl1"]
                    oq = V(B["osb"][r, hs], (B["osb"].name, h))
                    p1 = ps128()
                    kb.mm(sub(p1, r), Hb["wT"][:, r], Sb[d][h][:])
                    kb.v("dve", "tensor_tensor", Hb["vnew"][r, :], [Hb["usb"][r, :], sub(p1, r)], ALU.subtract)
                    yield
                    p2 = ps128()
                    kb.mm(sub(p2, r), Hb["QgT"][:, r], Sb[d][h][:], start=True, stop=False)
                    kb.mm(sub(p2, r), Hb["AqkT"][r, r], Hb["vnew"][r, :], start=False, stop=True)
                    kb.act(oq, sub(p2, r), AF.Copy)
                    p3 = ps128()
                    kb.mm(p3, Hb["Khat"][r, :], Hb["vnew"][r, :])
                    kb.v("dve", "scalar_tensor_tensor", S[d][h][:], [S[d][h][:], egl[:, h:h + 1], p3], ALU.mult, ALU.add)
                    kb.act(Sb[d][h][:], S[d][h][:], AF.Copy)
                    yield

            for step in range(nsub):
                makers = []
                Bs = []
                for d in range(2):
                    sb_ = fwd[step] if d == 0 else bwd[step]
                    r0 = sb_ * 128
                    B = UB[(step % 2) * 2 + d]
                    Bs.append((B, r0, d))
                    kb.dma(B["ab"][:], self.UT[r0:r0 + 128, 512:528])
                    kb.dma(B["KT"][:], self.GK[:, :, r0:r0 + 128].rearrange("h c t -> c h t"))
                    kb.dma(B["QT"][:], self.GQ[:, :, r0:r0 + 128].rearrange("h c t -> c h t"))
                    kb.dma(B["Ktm"][:], self.GKT[r0:r0 + 128, :])
                    kb.dma(B["Vtm"][:], self.GVT[r0:r0 + 128, :])
                    ab = B["ab"]
                    kb.v("dve", "tensor_tensor", B["xa"][:], [ab[:, d * 8:d * 8 + 4], gv[:, 8 + d * 4:12 + d * 4]], ALU.add)
                    kb.act(B["xa"][:], B["xa"][:], AF.Exp)
                    kb.act(B["xa"][:], B["xa"][:], AF.Ln, bias=1.0)
                    kb.v("dve", "tensor_tensor", B["g"][:], [B["xa"][:], negA[:, d * 4:d * 4 + 4]], ALU.mult)
                    kb.act(B["beta"][:], ab[:, d * 8 + 4:d * 8 + 8], AF.Sigmoid)
                    kb.v("dve", "tensor_scalar", B["nbeta"][:], [B["beta"][:]], -1.0, None, ALU.mult)
                    Mc = mle if d == 0 else mge
                    p_gc, p_gt, p_0, p_1 = ps128(), ps128(), ps128(), ps128()
                    kb.mm(sub(p_gc, slice(0, 128), slice(0, 4)), Mc[:], B["g"][:])
                    kb.mm(sub(p_gt, slice(0, 128), slice(0, 4)), msame[:], B["g"][:])
                    kb.mm(sub(p_0, slice(0, 128), slice(0, 4)), mh0[:], B["g"][:])
                    kb.mm(sub(p_1, slice(0, 128), slice(0, 4)), mh1[:], B["g"][:])
                    kb.v("dve", "tensor_copy", B["gc"][:], [sub(p_gc, slice(0, 128), slice(0, 4))])
                    kb.v("dve", "tensor_tensor", B["gt"][:], [sub(p_gt, slice(0, 128), slice(0, 4)), B["gc"][:]], ALU.subtract)
                    kb.act(B["ek"][:], B["gt"][:], AF.Exp)
                    kb.act(B["eg"][:], B["gc"][:], AF.Exp)
                    kb.act(B["egl0"][:], sub(p_0, slice(0, 128), slice(0, 4)), AF.Exp)
                    kb.act(B["egl1"][:], sub(p_1, slice(0, 128), slice(0, 4)), AF.Exp)
                    kb.v("dve", "tensor_tensor", B["beg"][:], [B["beta"][:], B["eg"][:]], ALU.mult)
                for h in range(GH):
                    for (B, r0, d) in Bs:
                        makers.append(lambda slot_, d=d, h=h, B=B: unit(d, h, B, HB[slot_]))
                interleave(makers, 8)
                for (B, r0, d) in Bs:
                    kb.dma(self.OD[d][r0:r0 + 128, :], B["osb"][:], extra_reads=[(B["osb"].name, h) for h in range(GH)])

    def phase_e6(self, l, j, sm):
        kb, cfg = self.kb, self.cfg
        Wout = self.inp(f"hy_w_out__{j}", [D, D])
        self.Fm = self.scratch(f"F{l}", [cfg.T, D], BF16)
        with kb.phase() as ph:
            Wo = ph.sb([128, 8, D], BF16, "Wo")
            kb.dma(Wo[:], Wout.rearrange("(c p) n -> p c n", p=128), eng="pool")
            og = ph.sb([128, 128], F32, "og")
            kb.dma(og[:], sm["outg"].broadcast_to([128, 128]))
            o0s = [ph.sb([128, 512], F32, "o0") for _ in range(2)]
            o1s = [ph.sb([128, 512], F32, "o1") for _ in range(2)]
            zs = [ph.sb([128, 512], F32, "z") for _ in range(2)]
            sqo = ph.sb([128, 512], F32, "sqo")
            ssq = [ph.sb([128, 4], F32, "ssq") for _ in range(2)]
            on = ph.sb([128, 512], F32, "on")
            dtm = [ph.sb([128, 512], BF16, "dtm") for _ in range(2)]
            XT = [ph.sb([128, 8, 128], BF16, "XT") for _ in range(2)]
            ptr = [ph.ps([128, 1024], BF16, "ptr") for _ in range(2)]
            py = [ph.ps() for _ in range(4)]
            hts = [ph.sb([128, D], F32, "ht") for _ in range(2)]
            tmp = [ph.sb([128, D], F32, "tmp") for _ in range(2)]
            junk = ph.sb([128, D], F32, "junk")
            sss = [ph.sb([128, 1], F32, "ss") for _ in range(2)]
            tmp2 = [ph.sb([128, D], F32, "tmp2") for _ in range(2)]
            fb = [ph.sb([128, D], BF16, "fb") for _ in range(2)]
            for s in range(cfg.nsub):
                mod = self.modC if s < 2 else self.modL
                r0 = s * 128
                i = s % 2
                kb.dma(o0s[i][:], self.OD[0][r0:r0 + 128, :])
                kb.dma(o1s[i][:], self.OD[1][r0:r0 + 128, :])
                kb.dma(zs[i][:], self.UT[r0:r0 + 128, 0:512])
                kb.dma(XT[i][:, 0:4, :], self.AF_[:, r0:r0 + 128].rearrange("(c p) t -> p c t", p=128))
                kb.dma(hts[i][:], self.H[r0:r0 + 128, :])
                kb.v("pool", "tensor_tensor", o0s[i][:], [o0s[i][:], o1s[i][:]], ALU.add)
                kb.v("dve", "tensor_tensor", sqo[:], [o0s[i][:], o0s[i][:]], ALU.mult)
                kb.v("dve", "tensor_reduce", ssq[i][:], [sqo[:].rearrange("p (h v) -> p h v", h=4)], AX.X, ALU.add)
                rstd_inplace(kb, ssq[i][:], 1.0 / 128, EPS)
                for h in range(4):
                    hs = slice(h * 128, (h + 1) * 128)
                    kb.v("dve", "scalar_tensor_tensor", on[:, hs], [o0s[i][:, hs], ssq[i][:, h:h + 1], og[:]], ALU.mult, ALU.mult)
                kb.act(zs[i][:], zs[i][:], AF.Silu)
                kb.v("pool", "tensor_tensor", dtm[i][:], [on[:], zs[i][:]], ALU.mult)
                for c in range(4):
                    kb.tr(ptr[i][:, c * 128:(c + 1) * 128], dtm[i][:, c * 128:(c + 1) * 128], self.ident[:])
                self.evac(XT[i][:, 4:8, :], ptr[i][:, 0:512].rearrange("p (c t) -> p c t", c=4))
                for n in range(2):
                    pp = py[(2 * s + n) % 4]
                    for c in range(8):
                        kb.mm(pp[:], XT[i][:, c, :], Wo[:, c, n * 512:(n + 1) * 512], start=(c == 0), stop=(c == 7))
                    kb.v("dve", "tensor_tensor", tmp[i][:, n * 512:(n + 1) * 512], [pp[:], mod[2][:, n * 512:(n + 1) * 512]], ALU.mult)
                kb.v("pool", "tensor_tensor", hts[i][:], [tmp[i][:], hts[i][:]], ALU.add)
                kb.dma(self.H[r0:r0 + 128, :], hts[i][:])
                self.norm_sub(hts[i][:], junk[:], sss[i][:], tmp2[i][:], fb[i][:], mod[4][:], mod[3][:])
                kb.dma(self.Fm[r0:r0 + 128, :], fb[i][:])
    def moe_setup(self):
        kb, cfg = self.kb, self.cfg
        if hasattr(self, "OH"):
            return
        ns, nb = cfg.nsub, cfg.nblk
        self.OH = kb.sb([128, ns, 64], BF16, "OH")
        self.WTS = kb.sb([128, ns, 2], F32, "WTS")
        self.DEST = kb.sb([128, ns, 2], I32, "DEST")
        self.I1 = kb.sb([128, nb, 8], I32, "I1")
        self.I2 = kb.sb([128, nb, 4], I32, "I2")
        self.offs = kb.sb([128, 32], F32, "offs")
        self.XS = self.scratch("XS", [cfg.nslot, D], BF16)
        self.YS = self.scratch("YS", [cfg.nslot, D], F32)

    def moe(self, l):
        self.moe_setup()
        kb, cfg = self.kb, self.cfg
        ns, nb = cfg.nsub, cfg.nblk
        wg = self.inp(f"moe_w_group__{l}", [D, 4])
        bg = self.inp(f"moe_b_group__{l}", [1, 4])
        we = self.inp(f"moe_w_expert__{l}", [D, 32])
        be = self.inp(f"moe_b_expert__{l}", [1, 32])
        w1 = self.inp(f"moe_w1__{l}", [32 * D, 512])
        w3 = self.inp(f"moe_w3__{l}", [32 * D, 512])
        w2 = self.inp(f"moe_w2__{l}", [32 * 512, D])
        OH, WTS, DEST, I1, I2, offs = self.OH, self.WTS, self.DEST, self.I1, self.I2, self.offs
        with kb.phase() as ph:
            Wr = ph.sb([128, 8, 36], BF16, "Wr")
            kb.dma(Wr[:, :, 0:4], wg.rearrange("(c p) n -> p c n", p=128), eng="pool")
            kb.dma(Wr[:, :, 4:36], we.rearrange("(c p) n -> p c n", p=128), eng="pool")
            br = ph.sb([128, 36], F32, "br")
            kb.dma(br[:, 0:4], bg.broadcast_to([128, 4]))
            kb.dma(br[:, 4:36], be.broadcast_to([128, 32]))
            zt = ph.sb([128, D], BF16, "zt")
            kb.v("pool", "memset", zt[:], [], 0.0)
            for i in range(cfg.nslot // 128):
                kb.dma(self.XS[i * 128:(i + 1) * 128, :], zt[:], wkey=("XS", "z", i))
            fts = [ph.sb([128, D], BF16, "ft") for _ in range(2)]
            fT = [ph.sb([128, 8, 128], BF16, "fT") for _ in range(2)]
            ptr = [ph.ps([128, 1024], BF16, "ptr") for _ in range(2)]
            plg = [ph.ps() for _ in range(2)]
            pcnt = ph.ps()
            sm_ = [{nm: ph.sb([128, w], F32, nm) for nm, w in (("lg", 36), ("mx", 1), ("nmx", 1), ("eg", 4), ("sg", 1), ("pg", 1), ("G1", 4),
                                                               ("pen", 4), ("lem", 32), ("top", 8), ("dif", 1), ("r", 1), ("den", 1))} for _ in range(2)]
            osum = [ph.sb([128, 32], BF16, "osum") for _ in range(2)]
            for s in range(ns):
                i = s % 2
                ft, t = fts[i], sm_[i]
                kb.dma(ft[:], self.Fm[s * 128:(s + 1) * 128, :])
                for c in range(8):
                    kb.tr(ptr[i][:, c * 128:(c + 1) * 128], ft[:, c * 128:(c + 1) * 128], self.ident[:])
                self.evac(fT[i][:], ptr[i][:].rearrange("p (c t) -> p c t", c=8))
                for c in range(8):
                    kb.mm(plg[i][:, 0:36], fT[i][:, c, :], Wr[:, c, :], start=(c == 0), stop=(c == 7))
                kb.v("dve", "tensor_tensor", t["lg"][:], [plg[i][:, 0:36], br[:]], ALU.add)
                kb.v("dve", "reduce_max", t["mx"][:], [t["lg"][:, 0:4]], AX.X)
                kb.v("dve", "tensor_scalar", t["nmx"][:], [t["mx"][:]], -1.0, None, ALU.mult)
                kb.act(t["eg"][:], t["lg"][:, 0:4], AF.Exp, bias=t["nmx"][:], accum_out=t["sg"][:])
                kb.v("dve", "reciprocal", t["pg"][:], [t["sg"][:]])
                kb.v("dve", "tensor_scalar", t["G1"][:], [t["lg"][:, 0:4]], t["mx"][:], None, ALU.is_equal)
                kb.v("dve", "tensor_scalar", t["pen"][:], [t["G1"][:]], 1e30, -1e30, ALU.mult, ALU.add)
                for g in range(4):
                    kb.v("dve", "tensor_scalar", t["lem"][:, g * 8:(g + 1) * 8], [t["lg"][:, 4 + g * 8:12 + g * 8]], t["pen"][:, g:g + 1], None, ALU.add)
                kb.v("dve", "max", t["top"][:], [t["lem"][:]])
                kb.v("dve", "tensor_scalar", OH[:, s, 0:32], [t["lem"][:]], t["top"][:, 0:1], None, ALU.is_equal)
                kb.v("dve", "tensor_scalar", OH[:, s, 32:64], [t["lem"][:]], t["top"][:, 1:2], None, ALU.is_equal)
                kb.v("dve", "tensor_tensor", t["dif"][:], [t["top"][:, 1:2], t["top"][:, 0:1]], ALU.subtract)
                kb.act(t["r"][:], t["dif"][:], AF.Exp)
                kb.v("dve", "tensor_scalar", t["den"][:], [t["r"][:]], 1.0, None, ALU.add)
                kb.v("dve", "reciprocal", t["den"][:], [t["den"][:]])
                kb.v("dve", "tensor_tensor", WTS[:, s, 0:1], [t["pg"][:], t["den"][:]], ALU.mult)
                kb.v("dve", "tensor_tensor", WTS[:, s, 1:2], [WTS[:, s, 0:1], t["r"][:]], ALU.mult)
                kb.v("pool", "tensor_tensor", osum[i][:], [OH[:, s, 0:32], OH[:, s, 32:64]], ALU.add)
                kb.mm(pcnt[:, 0:32], self.ones[:], osum[i][:], start=(s == 0), stop=(s == ns - 1))
            cnt = ph.sb([128, 32], F32, "cnt")
            kb.v("dve", "tensor_copy", cnt[:], [pcnt[:, 0:32]])
            thr = ph.sb([128, 34], F32, "thr")
            kb.dma(thr[:], self.cin["thr"])
            cmp = ph.sb([128, 32, 34], F32, "cmp")
            kb.v("dve", "tensor_tensor", cmp[:], [cnt[:].unsqueeze(2).to_broadcast([128, 32, 34]), thr[:].unsqueeze(1).to_broadcast([128, 32, 34])], ALU.is_gt)
            padded = ph.sb([128, 32], F32, "padded")
            kb.v("dve", "tensor_reduce", padded[:], [cmp[:]], AX.X, ALU.add)
            kb.v("dve", "tensor_scalar", padded[:], [padded[:]], float(MOE_BS), None, ALU.mult)
            onesr = ph.sb([128, 32], F32, "onesr")
            kb.v("pool", "memset", onesr[:], [], 1.0)
            pend = ph.sb([128, 32], F32, "pend")
            kb.v("dve", "tensor_tensor_scan", pend[:], [onesr[:], padded[:]], 0.0, ALU.mult, ALU.add)
            kb.v("dve", "tensor_tensor", offs[:], [pend[:], padded[:]], ALU.subtract)
            bst = ph.sb([128, nb], F32, "bst")
            kb.dma(bst[:], self.cin["blk_start"])
            cmp2 = ph.sb([128, nb, 32], F32, "cmp2")
            kb.v("dve", "tensor_tensor", cmp2[:], [pend[:].unsqueeze(1).to_broadcast([128, nb, 32]), bst[:].unsqueeze(2).to_broadcast([128, nb, 32])], ALU.is_le)
            blke = ph.sb([128, nb], F32, "blke")
            kb.v("dve", "tensor_reduce", blke[:], [cmp2[:]], AX.X, ALU.add)
            kb.v("dve", "tensor_scalar", blke[:], [blke[:]], 31.0, None, ALU.min)
            iop = ph.sb([128, 1], F32, "iop")
            kb.dma(iop[:], self.cin["iota_p"])
            b1 = ph.sb([128, nb], F32, "b1")
            b2 = ph.sb([128, nb], F32, "b2")
            kb.v("dve", "tensor_scalar", b1[:], [blke[:]], 1024.0, iop[:, 0:1], ALU.mult, ALU.add)
            kb.v("dve", "tensor_scalar", b2[:], [blke[:]], 512.0, iop[:, 0:1], ALU.mult, ALU.add)
            for c in range(8):
                kb.v("dve", "tensor_scalar", I1[:, :, c], [b1[:]], float(c * 128), None, ALU.add)
            for c in range(4):
                kb.v("dve", "tensor_scalar", I2[:, :, c], [b2[:]], float(c * 128), None, ALU.add)
            tri = ph.sb([128, 128], F32, "trif")
            kb.dma(tri[:], self.cin["tri_lt_full"])
            trib = ph.sb([128, 128], BF16, "trib")
            kb.v("dve", "tensor_copy", trib[:], [tri[:]])
            kb.barrier()
            basep = ph.sb([128, 32], F32, "basep")
            kb.v("dve", "tensor_copy", basep[:], [offs[:]])
            pcx = [ph.ps() for _ in range(2)]
            pos = [ph.sb([128, 32], F32, "pos") for _ in range(2)]
            tm = [ph.sb([128, 32], F32, "tm") for _ in range(2)]
            dd = [ph.sb([128, 2], F32, "dd") for _ in range(2)]
            for s in range(ns):
                i = s % 2
                kb.v("pool", "tensor_tensor", osum[i][:], [OH[:, s, 0:32], OH[:, s, 32:64]], ALU.add)
                kb.mm(pcx[i][:, 0:32], trib[:], osum[i][:])
                kb.mm(pcx[i][:, 32:64], self.ones[:], osum[i][:])
                kb.v("dve", "tensor_tensor", pos[i][:], [pcx[i][:, 0:32], basep[:]], ALU.add)
                kb.v("dve", "tensor_tensor", basep[:], [pcx[i][:, 32:64], basep[:]], ALU.add)
                for k in range(2):
                    kb.v("dve", "tensor_tensor", tm[i][:], [OH[:, s, k * 32:(k + 1) * 32], pos[i][:]], ALU.mult)
                    kb.v("dve", "tensor_reduce", dd[i][:, k:k + 1], [tm[i][:]], AX.X, ALU.add)
                kb.v("dve", "tensor_copy", DEST[:, s, :], [dd[i][:]])
                ft = fts[i]
                kb.dma(ft[:], self.Fm[s * 128:(s + 1) * 128, :])
                for k in range(2):
                    kb.scatter(self.XS, ft[:], DEST[:, s, k:k + 1], wkey=("XS", "sc"))
        with kb.phase() as ph:
            W1 = [ph.sb([128, 8, 512], BF16, "W1") for _ in range(2)]
            W3 = [ph.sb([128, 8, 512], BF16, "W3") for _ in range(2)]
            W2 = [ph.sb([128, 4, D], BF16, "W2") for _ in range(2)]
            xs = [ph.sb([128, D], BF16, "xs") for _ in range(2)]
            xT = [ph.sb([128, 8, 512], BF16, "xT") for _ in range(2)]
            sil = [ph.sb([128, 512], F32, "sil") for _ in range(2)]
            hid = [ph.sb([128, 4, 512], BF16, "hid") for _ in range(2)]
            ysb = [ph.sb([128, D], F32, "ysb") for _ in range(2)]
            ptr = [ph.ps([128, 1024], BF16, "ptr") for _ in range(2)]
            p1 = [ph.ps() for _ in range(2)]
            p3 = [ph.ps() for _ in range(2)]
            py = [ph.ps() for _ in range(2)]
            u = 0
            for b in range(nb):
                i = b % 2
                for c in range(8):
                    kb.gather(W1[i][:, c, :], w1, I1[:, b, c:c + 1])
                    kb.gather(W3[i][:, c, :], w3, I1[:, b, c:c + 1])
                for c in range(4):
                    kb.gather(W2[i][:, c, :], w2, I2[:, b, c:c + 1])
                for s in range(4):
                    x = xs[u % 2]
                    pt = ptr[u % 2]
                    u += 1
                    r0 = b * MOE_BS + s * 128
                    kb.dma(x[:], self.XS[r0:r0 + 128, :], rkey=("XS", "sc"))
                    for c in range(8):
                        kb.tr(pt[:, c * 128:(c + 1) * 128], x[:, c * 128:(c + 1) * 128], self.ident[:])
                    self.evac(xT[i][:, :, s * 128:(s + 1) * 128], pt[:].rearrange("p (c t) -> p c t", c=8))
                for hc in range(4):
                    a, b3 = p1[hc % 2], p3[hc % 2]
                    for c in range(8):
                        kb.mm(a[:], W1[i][:, c, hc * 128:(hc + 1) * 128], xT[i][:, c, :], start=(c == 0), stop=(c == 7))
                    for c in range(8):
                        kb.mm(b3[:], W3[i][:, c, hc * 128:(hc + 1) * 128], xT[i][:, c, :], start=(c == 0), stop=(c == 7))
                    kb.act(sil[hc % 2][:], a[:], AF.Silu)
                    kb.v("dve", "tensor_tensor", hid[i][:, hc, :], [b3[:], sil[hc % 2][:]], ALU.mult)
                for s in range(4):
                    yb = ysb[s % 2]
                    for n in range(2):
                        pp = py[n]
                        for hc in range(4):
                            kb.mm(pp[:], hid[i][:, hc, s * 128:(s + 1) * 128], W2[i][:, hc, n * 512:(n + 1) * 512], start=(hc == 0), stop=(hc == 3))
                        self.evac(yb[:, n * 512:(n + 1) * 512], pp[:])
                    r0 = b * MOE_BS + s * 128
                    kb.dma(self.YS[r0:r0 + 128, :], yb[:], wkey=("YS", "w"))
        with kb.phase() as ph:
            y1 = [ph.sb([128, D], F32, "y1") for _ in range(2)]
            y2 = [ph.sb([128, D], F32, "y2") for _ in range(2)]
            hts = [ph.sb([128, D], F32, "ht") for _ in range(2)]
            for s in range(ns):
                i = s % 2
                mod = self.modC if s < 2 else self.modL
                kb.gather(y1[i][:], self.YS, DEST[:, s, 0:1], rkey=("YS", "w"))
                kb.gather(y2[i][:], self.YS, DEST[:, s, 1:2], rkey=("YS", "w"))
                kb.dma(hts[i][:], self.H[s * 128:(s + 1) * 128, :])
                kb.v("dve", "tensor_scalar", y1[i][:], [y1[i][:]], WTS[:, s, 0:1], None, ALU.mult)
                kb.v("dve", "scalar_tensor_tensor", y1[i][:], [y2[i][:], WTS[:, s, 1:2], y1[i][:]], ALU.mult, ALU.add)
                kb.v("pool", "tensor_tensor", y1[i][:], [y1[i][:], mod[5][:]], ALU.mult)
                kb.v("dve", "tensor_tensor", hts[i][:], [hts[i][:], y1[i][:]], ALU.add)
                kb.dma(self.H[s * 128:(s + 1) * 128, :], hts[i][:])
    def odd_layer(self, l):
        j = l // 2
        self.phase_mod(l)
        sm = {k: self.inp(f"o{j}_{k}", s) for k, s in ODD_SMALL_SHAPES.items()}
        self.phase_r1()
        self.phase_r2a(l, j, sm)
        self.phase_r2b(j, sm)
        self.phase_r3(j)
        self.phase_r4(l, j, sm)
        self.moe(l)

    def phase_r1(self):
        kb, cfg = self.kb, self.cfg
        if not hasattr(self, "XN"):
            self.XN = self.scratch("XN", [D, cfg.T], BF16)
        with kb.phase() as ph:
            def consume(ti, t0, TT, xT):
                kb.dma(self.XN[:, t0:t0 + TT].rearrange("(c p) t -> p c t", p=128), xT[:, :, 0:TT])
            self.norm_tiles_to_xT(ph, self.H, 1, 0, consume)

    def wload(self, ph, src, shape, name, view=None):
        t = ph.sb(shape, BF16, name)
        self.kb.dma(t[:], view if view is not None else src, eng="pool")
        return t

    def phase_r2a(self, l, j, sm):
        kb, cfg = self.kb, self.cfg
        T = cfg.T
        has_vres = j > 0
        wr = self.inp(f"rk_wr__{j}", [D, D])
        wk = self.inp(f"rk_wk__{j}", [D, D])
        wv = self.inp(f"rk_wv__{j}", [D, D])
        w1 = self.inp(f"rk_w1__{j}", [2, D, 64])
        a1 = self.inp(f"rk_a1__{j}", [2, D, 64])
        g1 = self.inp(f"rk_g1__{j}", [D, 160])
        g2 = self.inp(f"rk_g2__{j}", [160, D])
        if has_vres:
            v1 = self.inp(f"rk_v1__{j - 1}", [D, 32])
            v2 = self.inp(f"rk_v2__{j - 1}", [32, D])
        if not hasattr(self, "RF"):
            self.RF = self.scratch("RF", [D, T], BF16)
            self.KFm = self.scratch("KFm", [D, T], BF16)
            self.KKF = self.scratch("KKF", [D, T], BF16)
            self.TW = self.scratch("TW", [2, 64, T], BF16)
            self.TA = self.scratch("TA", [2, 64, T], BF16)
            self.VTM = self.scratch("VTM", [T, D], BF16)
            self.VF = self.scratch("VF", [T, D], F32)
            self.GATE = self.scratch("GATE", [T, D], BF16)
        last = len(cfg.tiles) - 1
        with kb.phase() as ph:
            r3 = lambda w: w.rearrange("(c p) n -> p c n", p=128)
            Wr = self.wload(ph, None, [128, 8, D], "Wr", r3(wr))
            Wk = self.wload(ph, None, [128, 8, D], "Wk", r3(wk))
            Wv = self.wload(ph, None, [128, 8, D], "Wv", r3(wv))
            W1 = [self.wload(ph, None, [128, 8, 64], "W1", r3(w1[d])) for d in range(2)]
            A1 = [self.wload(ph, None, [128, 8, 64], "A1", r3(a1[d])) for d in range(2)]
            G1 = self.wload(ph, None, [128, 8, 160], "G1", r3(g1))
            G2a = self.wload(ph, None, [128, D], "G2a", g2[0:128, :])
            G2b = self.wload(ph, None, [32, D], "G2b", g2[128:160, :])
            if has_vres:
                V1 = self.wload(ph, None, [128, 8, 32], "V1", r3(v1))
                V2 = self.wload(ph, None, [32, D], "V2", v2)
                v0b = ph.sb([128, D], F32, "v0b")
                kb.dma(v0b[:], sm["v0"].broadcast_to([128, D]))
            mu = ph.sb([128, 6, 8], F32, "mu")
            kb.dma(mu[:], sm["mu"])
            kkc = ph.sb([128, 8], F32, "kkc")
            kb.dma(kkc[:], sm["kkcol"])
            bones = ph.sb([128, 128], F32, "bonesf")
            kb.dma(bones[:], self.cin["m_same"])
            bonesb = ph.sb([128, 128], BF16, "bonesb")
            kb.v("dve", "tensor_copy", bonesb[:], [bones[:]])
            x = ph.sb([128, 8, 514], BF16, "x")
            tmpf = ph.sb([128, 8, 512], F32, "tmpf")
            xx = ph.sb([128, 8, 512], BF16, "xx")
            xms = [ph.sb([128, 8, 512], BF16, "xm") for _ in range(2)]
            pm = [ph.ps() for _ in range(4)]
            pR = ph.ps()
            ob = [ph.sb([128, 512], BF16, "ob") for _ in range(3)]
            of = [ph.sb([128, 512], F32, "of") for _ in range(2)]
            sq = ph.sb([128, 512], BF16, "sq")
            rs = ph.sb([128, 512], F32, "rs")
            sg = ph.sb([128, 2, 512], BF16, "sg")
            lvT = ph.sb([32, 512], BF16, "lvT")
            vf = [ph.sb([128, 512], F32, "vf") for _ in range(2)]
            cnt = [0]

            def mix(i, TT):
                xm = xms[cnt[0] % 2]
                cnt[0] += 1
                for c in range(8):
                    kb.v("dve", "scalar_tensor_tensor", xm[:, c, 0:TT], [xx[:, c, 0:TT], mu[:, i, c:c + 1], x[:, c, 1:TT + 1]], ALU.mult, ALU.add)
                return xm

            def proj_fm(xm, Wt, ncol0, rows, TT, pmi):
                p = pm[pmi % 4]
                for c in range(8):
                    kb.mm(p[0:rows, 0:TT], Wt[:, c, ncol0:ncol0 + rows], xm[:, c, 0:TT], start=(c == 0), stop=(c == 7))
                return p

            u = 0
            for ti, (t0, TT) in enumerate(cfg.tiles):
                lo = 0 if ti in (0, 1) else 1
                hi = 0 if ti in (0, last) else 1
                kb.dma(x[:, :, 1 - lo:TT + 1 + hi], self.XN[:, t0 - lo:t0 + TT + hi].rearrange("(c p) t -> p c t", p=128))
                if not lo:
                    kb.v("pool", "memset", x[:, :, 0:1], [], 0.0)
                if not hi:
                    kb.v("pool", "memset", x[:, :, TT + 1:TT + 2], [], 0.0)
                kb.v("dve", "tensor_tensor", tmpf[:, :, 0:TT], [x[:, :, 0:TT], x[:, :, 2:TT + 2]], ALU.add)
                kb.v("dve", "scalar_tensor_tensor", xx[:, :, 0:TT], [tmpf[:, :, 0:TT], 0.5, x[:, :, 1:TT + 1]], ALU.mult, ALU.subtract)
                xm = mix(0, TT)
                for m in range(8):
                    p = proj_fm(xm, Wr, m * 128, 128, TT, u)
                    o = ob[u % 3]
                    u += 1
                    self.evac(o[:, 0:TT], p[:, 0:TT])
                    kb.dma(self.RF[m * 128:(m + 1) * 128, t0:t0 + TT], o[:, 0:TT])
                xm = mix(2, TT)
                for m in range(8):
                    p = proj_fm(xm, Wk, m * 128, 128, TT, u)
                    o = ob[u % 3]
                    f = of[u % 2]
                    u += 1
                    kb.act(o[:, 0:TT], p[:, 0:TT], AF.Copy)
                    kb.dma(self.KFm[m * 128:(m + 1) * 128, t0:t0 + TT], o[:, 0:TT])
                    kb.v("dve", "tensor_scalar", f[:, 0:TT], [p[:, 0:TT]], kkc[:, m:m + 1], None, ALU.mult)
                    kb.act(sq[:, 0:TT], f[:, 0:TT], AF.Square)
                    kb.mm(pR[:, 0:TT], bonesb[:], sq[:, 0:TT])
                    rstd_inplace(kb, rs[:, 0:TT], 1.0, EPS, src=pR[:, 0:TT])
                    o2 = ob[u % 3]
                    u += 1
                    kb.v("dve", "tensor_tensor", o2[:, 0:TT], [f[:, 0:TT], rs[:, 0:TT]], ALU.mult)
                    kb.dma(self.KKF[m * 128:(m + 1) * 128, t0:t0 + TT], o2[:, 0:TT])
                xm = mix(3, TT)
                if has_vres:
                    p = proj_fm(xm, V1, 0, 32, TT, u)
                    u += 1
                    kb.act(lvT[:, 0:TT], p[0:32, 0:TT], AF.Copy)
                for s in range(TT // 128):
                    r0 = t0 + s * 128
                    for n in range(2):
                        p = pm[u % 4]
                        o = ob[u % 3]
                        f = of[u % 2]
                        u += 1
                        for c in range(8):
                            kb.mm(p[:], xm[:, c, s * 128:(s + 1) * 128], Wv[:, c, n * 512:(n + 1) * 512], start=(c == 0), stop=(c == 7))
                        if not has_vres:
                            kb.act(f[:], p[:], AF.Copy)
                            kb.dma(self.VF[r0:r0 + 128, n * 512:(n + 1) * 512], f[:])
                            kb.v("dve", "tensor_copy", o[:], [p[:]])
                        else:
                            p2 = pm[u % 4]
                            u += 1
                            vft = vf[n]
                            kb.dma(vft[:], self.VF[r0:r0 + 128, n * 512:(n + 1) * 512])
                            kb.mm(p2[:], lvT[:, s * 128:(s + 1) * 128], V2[:, n * 512:(n + 1) * 512])
                            kb.v("dve", "tensor_tensor", f[:], [p2[:], v0b[:, n * 512:(n + 1) * 512]], ALU.add)
                            kb.act(f[:], f[:], AF.Sigmoid)
                            kb.v("dve", "tensor_tensor", vft[:], [vft[:], p[:]], ALU.subtract)
                            kb.v("pool", "tensor_tensor", vft[:], [vft[:], f[:]], ALU.mult)
                            kb.v("dve", "tensor_tensor", o[:], [vft[:], p[:]], ALU.add)
                        kb.dma(self.VTM[r0:r0 + 128, n * 512:(n + 1) * 512], o[:])
                xm = mix(5, TT)
                p = proj_fm(xm, G1, 0, 128, TT, u)
                u += 1
                kb.act(sg[:, 0, 0:TT], p[:, 0:TT], AF.Sigmoid)
                p = proj_fm(xm, G1, 128, 32, TT, u)
                u += 1
                kb.act(sg[0:32, 1, 0:TT], p[0:32, 0:TT], AF.Sigmoid)
                for s in range(TT // 128):
                    r0 = t0 + s * 128
                    for n in range(2):
                        p = pm[u % 4]
                        o = ob[u % 3]
                        u += 1
                        kb.mm(p[:], sg[:, 0, s * 128:(s + 1) * 128], G2a[:, n * 512:(n + 1) * 512], start=True, stop=False)
                        kb.mm(p[:], sg[0:32, 1, s * 128:(s + 1) * 128], G2b[:, n * 512:(n + 1) * 512], start=False, stop=True)
                        self.evac(o[:], p[:])
                        kb.dma(self.GATE[r0:r0 + 128, n * 512:(n + 1) * 512], o[:])
                xm = mix(1, TT)
                for d in range(2):
                    p = proj_fm(xm, W1[d], 0, 64, TT, u)
                    o = ob[u % 3]
                    u += 1
                    kb.act(o[0:64, 0:TT], p[0:64, 0:TT], AF.Tanh)
                    kb.dma(self.TW[d, :, t0:t0 + TT], o[0:64, 0:TT])
                xm = mix(4, TT)
                for d in range(2):
                    p = proj_fm(xm, A1[d], 0, 64, TT, u)
                    o = ob[u % 3]
                    u += 1
                    kb.act(o[0:64, 0:TT], p[0:64, 0:TT], AF.Copy)
                    kb.dma(self.TA[d, :, t0:t0 + TT], o[0:64, 0:TT])

    def phase_r2b(self, j, sm):
        kb, cfg = self.kb, self.cfg
        T = cfg.T
        nch = T // 64
        w2 = self.inp(f"rk_w2__{j}", [2, 64, D])
        a2 = self.inp(f"rk_a2__{j}", [2, 64, D])
        if not hasattr(self, "RT"):
            for nm in ("RT", "AT", "BT", "KT"):
                setattr(self, nm, [self.scratch(f"{nm}{d}", [D, T], BF16) for d in range(2)])
            for nm in ("ATM", "KHAT", "BHAT"):
                setattr(self, nm, [self.scratch(f"{nm}{d}", [T, D], BF16) for d in range(2)])
            self.GCd = [self.scratch(f"GC{d}", [D, nch], F32) for d in range(2)]
            self.SBN = self.scratch("SBN", [T, 16], F32)
        with kb.phase() as ph:
            W2 = [self.wload(ph, None, [64, D], "W2", w2[d]) for d in range(2)]
            A2 = [self.wload(ph, None, [64, D], "A2", a2[d]) for d in range(2)]
            w0 = ph.sb([128, 2, 8], F32, "w0")
            a0 = ph.sb([128, 2, 8], F32, "a0")
            kac = ph.sb([128, 8], F32, "kac")
            omka = ph.sb([128, 8], F32, "omka")
            rkc = ph.sb([128, 8], F32, "rkc")
            kb.dma(w0[:], sm["w0"])
            kb.dma(a0[:], sm["a0"])
            kb.dma(kac[:], sm["kacol"])
            kb.dma(rkc[:], sm["rkcol"])
            kb.v("dve", "tensor_scalar", omka[:], [kac[:]], -1.0, 1.0, ALU.mult, ALU.add)
            rmask = ph.sb([128, 512], F32, "rmask")
            kb.dma(rmask[:], self.cin["rmask"])
            hsel = ph.sb([128, 2], F32, "hself")
            kb.dma(hsel[:], self.cin["hsel"])
            hselb = ph.sb([128, 2], BF16, "hselb")
            kb.v("dve", "tensor_copy", hselb[:], [hsel[:]])
            tw = [ph.sb([64, 512], BF16, "tw") for _ in range(2)]
            ta = [ph.sb([64, 512], BF16, "ta") for _ in range(2)]
            rt = [ph.sb([128, 512], BF16, "rt") for _ in range(2)]
            kt = [ph.sb([128, 512], BF16, "kt") for _ in range(2)]
            kkt = [ph.sb([128, 512], BF16, "kkt") for _ in range(2)]
            F = lambda nm: ph.sb([128, 512], F32, nm)
            FS = [{nm: F(nm) for nm in ("lw", "av", "keys", "bv", "cs", "tot", "e1", "e2", "tmp", "tmp2")} for _ in range(2)]
            EX = [F("ex") for _ in range(4)]
            exi = [0]
            prod = F("prod")
            obs = [ph.sb([128, 512], BF16, "ob") for _ in range(4)]
            tms = [ph.sb([128, 3, 512], BF16, "tms") for _ in range(2)]
            tsb = [ph.sb([128, 4, 128], BF16, "tsb") for _ in range(3)]
            gcs = ph.sb([128, 8], F32, "gcs")
            prodb = ph.sb([128, 512], BF16, "prodb")
            sbn = ph.sb([128, 4, 16], F32, "sbn")
            pz = [ph.ps() for _ in range(2)]
            pa = [ph.ps() for _ in range(2)]
            ptr = [ph.ps([128, 1024], BF16, "ptr") for _ in range(3)]
            psb = ph.ps()
            u = 0
            tq = 0
            for ti, (t0, TT) in enumerate(cfg.tiles):
                ns = TT // 128
                nc_ = TT // 64
                for d in range(2):
                    kb.dma(tw[d][:, 0:TT], self.TW[d, :, t0:t0 + TT])
                    kb.dma(ta[d][:, 0:TT], self.TA[d, :, t0:t0 + TT])
                for c in range(8):
                    i = c % 2
                    kb.dma(rt[i][:, 0:TT], self.RF[c * 128:(c + 1) * 128, t0:t0 + TT])
                    kb.dma(kt[i][:, 0:TT], self.KFm[c * 128:(c + 1) * 128, t0:t0 + TT])
                    kb.dma(kkt[i][:, 0:TT], self.KKF[c * 128:(c + 1) * 128, t0:t0 + TT])
                    for d in range(2):
                        z, a_ = pz[u % 2], pa[u % 2]
                        tm3 = tms[u % 2]
                        fs = FS[u % 2]
                        lw, av, keys, bv, cs, tot, e1, e2, tmp, tmp2 = (fs[k_] for k_ in ("lw", "av", "keys", "bv", "cs", "tot", "e1", "e2", "tmp", "tmp2"))
                        u += 1
                        kb.mm(z[:, 0:TT], W2[d][:, c * 128:(c + 1) * 128], tw[d][:, 0:TT])
                        kb.mm(a_[:, 0:TT], A2[d][:, c * 128:(c + 1) * 128], ta[d][:, 0:TT])
                        kb.act(lw[:, 0:TT], z[:, 0:TT], AF.Sigmoid, bias=w0[:, d, c:c + 1])
                        kb.v("dve", "tensor_scalar", lw[:, 0:TT], [lw[:, 0:TT]], -0.6065306597126334, None, ALU.mult)
                        kb.act(av[:, 0:TT], a_[:, 0:TT], AF.Sigmoid, bias=a0[:, d, c:c + 1])
                        kb.v("dve", "tensor_scalar", tmp[:, 0:TT], [av[:, 0:TT]], kac[:, c:c + 1], omka[:, c:c + 1], ALU.mult, ALU.add)
                        kb.v("dve", "tensor_tensor", keys[:, 0:TT], [tmp[:, 0:TT], kt[i][:, 0:TT]], ALU.mult)
                        kb.v("pool", "tensor_tensor", bv[:, 0:TT], [av[:, 0:TT], kkt[i][:, 0:TT]], ALU.mult)
                        kb.v("dve", "tensor_tensor_scan", cs[:, 0:TT], [rmask[:, 0:TT], lw[:, 0:TT]], 0.0, ALU.mult, ALU.add)
                        cs3 = cs[:, 0:TT].rearrange("p (n k) -> p n k", k=64)
                        kb.v("pool", "tensor_copy", tot[:, 0:TT].rearrange("p (n k) -> p n k", k=64), [cs3[:, :, 63:64].to_broadcast([128, nc_, 64])])
                        if d == 0:
                            E1 = cs
                        else:
                            kb.v("dve", "tensor_tensor", e1[:, 0:TT], [tot[:, 0:TT], cs[:, 0:TT]], ALU.subtract)
                            kb.v("dve", "tensor_tensor", e1[:, 0:TT], [e1[:, 0:TT], lw[:, 0:TT]], ALU.add)
                            E1 = e1
                        kb.v("pool", "tensor_tensor", e2[:, 0:TT], [E1[:, 0:TT], lw[:, 0:TT]], ALU.subtract)
                        o = obs[tq % 4]; tq += 1
                        ex = EX[exi[0] % 4]; exi[0] += 1
                        kb.act(ex[:, 0:TT], E1[:, 0:TT], AF.Exp)
                        kb.v("dve", "tensor_tensor", o[:, 0:TT], [ex[:, 0:TT], rt[i][:, 0:TT]], ALU.mult)
                        kb.dma(self.RT[d][c * 128:(c + 1) * 128, t0:t0 + TT], o[:, 0:TT])
                        o = obs[tq % 4]; tq += 1
                        ex = EX[exi[0] % 4]; exi[0] += 1
                        kb.act(ex[:, 0:TT], e2[:, 0:TT], AF.Exp)
                        kb.v("dve", "scalar_tensor_tensor", o[:, 0:TT], [ex[:, 0:TT], -1.0, kkt[i][:, 0:TT]], ALU.mult, ALU.mult)
                        kb.dma(self.AT[d][c * 128:(c + 1) * 128, t0:t0 + TT], o[:, 0:TT])
                        kb.v("pool", "tensor_copy", tm3[:, 0, 0:TT], [o[:, 0:TT]])
                        ex = EX[exi[0] % 4]; exi[0] += 1
                        kb.act(ex[:, 0:TT], E1[:, 0:TT], AF.Exp, scale=-1.0)
                        o = obs[tq % 4]; tq += 1
                        kb.v("dve", "tensor_tensor", o[:, 0:TT], [ex[:, 0:TT], bv[:, 0:TT]], ALU.mult)
                        kb.dma(self.BT[d][c * 128:(c + 1) * 128, t0:t0 + TT], o[:, 0:TT])
                        o = obs[tq % 4]; tq += 1
                        kb.v("dve", "tensor_tensor", o[:, 0:TT], [ex[:, 0:TT], keys[:, 0:TT]], ALU.mult)
                        kb.dma(self.KT[d][c * 128:(c + 1) * 128, t0:t0 + TT], o[:, 0:TT])
                        kb.v("pool", "tensor_tensor", tmp2[:, 0:TT], [tot[:, 0:TT], E1[:, 0:TT]], ALU.subtract)
                        ex = EX[exi[0] % 4]; exi[0] += 1
                        kb.act(ex[:, 0:TT], tmp2[:, 0:TT], AF.Exp)
                        kb.v("dve", "tensor_tensor", tm3[:, 1, 0:TT], [ex[:, 0:TT], keys[:, 0:TT]], ALU.mult)
                        kb.v("dve", "tensor_tensor", tm3[:, 2, 0:TT], [ex[:, 0:TT], bv[:, 0:TT]], ALU.mult)
                        kb.act(gcs[:, 0:nc_], cs3[:, :, 63], AF.Exp)
                        kb.dma(self.GCd[d][c * 128:(c + 1) * 128, t0 // 64:t0 // 64 + nc_], gcs[:, 0:nc_])
                        for k3, dst in enumerate((self.ATM[d], self.KHAT[d], self.BHAT[d])):
                            pt, tb = ptr[k3], tsb[k3]
                            for s in range(ns):
                                kb.tr(pt[:, s * 128:(s + 1) * 128], tm3[:, k3, s * 128:(s + 1) * 128], self.ident[:])
                            self.evac(tb[:, 0:ns, :], pt[:, 0:TT].rearrange("p (s c) -> p s c", c=128))
                            kb.dma(dst[t0:t0 + TT, c * 128:(c + 1) * 128].rearrange("(s p) c -> p s c", p=128), tb[:, 0:ns, :])
                        if d == 0:
                            kb.v("dve", "scalar_tensor_tensor", prod[:, 0:TT], [keys[:, 0:TT], rkc[:, c:c + 1], rt[i][:, 0:TT]], ALU.mult, ALU.mult)
                        else:
                            kb.v("dve", "scalar_tensor_tensor", tmp[:, 0:TT], [keys[:, 0:TT], rkc[:, c:c + 1], rt[i][:, 0:TT]], ALU.mult, ALU.mult)
                            kb.v("dve", "scalar_tensor_tensor", prodb[:, 0:TT], [tmp[:, 0:TT], 1.0, prod[:, 0:TT]], ALU.mult, ALU.add)
                    for s in range(ns):
                        kb.mm(psb[:, (s * 8 + c) * 2:(s * 8 + c) * 2 + 2], prodb[:, s * 128:(s + 1) * 128], hselb[:])
                kb.v("dve", "tensor_scalar", sbn[:, 0:ns, :], [psb[:, 0:ns * 16].rearrange("p (s h) -> p s h", h=16)], 0.5, None, ALU.mult)
                kb.dma(self.SBN[t0:t0 + TT, :].rearrange("(s p) h -> p s h", p=128), sbn[:, 0:ns, :])
    def phase_r3(self, j):
        kb, cfg = self.kb, self.cfg
        T, nsub = cfg.T, cfg.nsub
        W = 16
        if not hasattr(self, "YD"):
            self.YD = [self.scratch(f"YD{d}", [T, D]) for d in range(2)]
        fwd = list(range(nsub))
        bwd = [1, 0] + list(range(nsub - 1, 1, -1))
        with kb.phase() as ph:
            mle = self.load_const_tile(ph, "m_le")
            mge = self.load_const_tile(ph, "m_ge")
            mlt = self.load_const_tile(ph, "m_lt")
            mgt = self.load_const_tile(ph, "m_gt")
            Hs = [[ph.sb([128, 64], F32, f"Hs{d}_{h}") for h in range(16)] for d in range(2)]
            Hb = [[ph.sb([128, 64], BF16, f"Hb{d}_{h}") for h in range(16)] for d in range(2)]
            for d in range(2):
                for h in range(16):
                    kb.v("pool", "memset", Hs[d][h][:], [], 0.0)
                    kb.v("pool", "memset", Hb[d][h][:], [], 0.0)
            banks = [ph.ps() for _ in range(8)]
            slot = [0]

            def ps128():
                i = slot[0] % 8
                slot[0] += 1
                return banks[i]

            UB = []
            for i in range(4):
                b = {}
                for nm in ("RT", "AT", "BT", "KT"):
                    b[nm] = ph.sb([128, 8, 128], BF16, nm)
                for nm in ("ATM", "KHAT", "BHAT", "V"):
                    b[nm] = ph.sb([128, D], BF16, nm)
                b["GC"] = ph.sb([128, 8, 2], F32, "GC")
                b["ysb"] = ph.sb([128, D], F32, "ysb")
                UB.append(b)
            HB = []
            for i in range(W):
                b = {}
                for nm in ("X0", "X1", "XT0", "XT1", "AT0", "AT1", "AakT", "ArkT", "ArbT", "PT"):
                    b[nm] = ph.sb([128, 128], BF16, nm)
                b["Z"] = ph.sb([128, 64], BF16, "Z")
                b["U"] = ph.sb([128, 64], BF16, "U")
                b["U0"] = ph.sb([128, 64], F32, "U0")
                HB.append(b)

            def unit(d, h, B, Hh):
                MsT = mlt if d == 0 else mgt
                Ms = mgt if d == 0 else mlt
                MiT = mle if d == 0 else mge
                c = h // 2
                rs = slice(64 * (h % 2), 64 * (h % 2) + 64)
                hs = slice(h * 64, (h + 1) * 64)
                RT, AT_, BT, KT = B["RT"][rs, c, :], B["AT"][rs, c, :], B["BT"][rs, c, :], B["KT"][rs, c, :]
                for (l_, r_, dst, msk) in ((BT, AT_, "XT0", MsT), (AT_, BT, "X0", Ms), (KT, AT_, "AakT", MsT), (KT, RT, "ArkT", MiT), (BT, RT, "ArbT", MiT)):
                    p = ps128()
                    kb.mm(p[:, 0:128], l_, r_)
                    kb.v("dve", "tensor_tensor", Hh[dst][:], [p[:, 0:128], msk[:]], ALU.mult)
                    yield
                kb.v("pool", "tensor_tensor", Hh["AT0"][:], [Hh["XT0"][:], self.ident[:]], ALU.add)
                X, XT, AT = Hh["X0"], Hh["XT0"], Hh["AT0"]
                for js in range(1, 6):
                    Xn = Hh["X1"] if X is Hh["X0"] else Hh["X0"]
                    XTn = Hh["XT1"] if XT is Hh["XT0"] else Hh["XT0"]
                    ATn = Hh["AT1"] if AT is Hh["AT0"] else Hh["AT0"]
                    px = ps128()
                    kb.mm(px[:, 0:128], XT[:], X[:])
                    if js < 5:
                        pxT = ps128()
                        kb.mm(pxT[:, 0:128], X[:], XT[:])
                    kb.act(Xn[:], px[:, 0:128], AF.Copy)
                    if js < 5:
                        kb.v("dve", "tensor_copy", XTn[:], [pxT[:, 0:128]])
                    yield
                    pa = ps128()
                    kb.mm(pa[:, 0:128], Xn[:], AT[:])
                    kb.v("dve", "tensor_tensor", ATn[:], [pa[:, 0:128], AT[:]], ALU.add)
                    X, XT, AT = Xn, XTn, ATn
                    yield
                p = ps128()
                kb.mm(p[:, 0:64], Hh["AakT"][:], B["V"][:, hs])
                kb.act(Hh["Z"][:], p[:, 0:64], AF.Copy)
                p = ps128()
                kb.mm(p[rs, 0:128], B["ATM"][:, hs], AT[:])
                kb.act(Hh["PT"][rs, :], p[rs, 0:128], AF.Copy)
                yield
                p = ps128()
                kb.mm(p[:, 0:64], AT[:], Hh["Z"][:])
                kb.act(Hh["U0"][:], p[:, 0:64], AF.Copy)
                yield
                for half in ((0, 1) if d == 0 else (1, 0)):
                    q = slice(half * 64, half * 64 + 64)
                    yq = V(B["ysb"][q, hs], (B["ysb"].name, h))
                    p1 = ps128()
                    kb.mm(p1[q, 0:64], Hh["PT"][rs, q], Hb[d][h][rs, :])
                    kb.v("dve", "tensor_tensor", Hh["U"][q, :], [p1[q, 0:64], Hh["U0"][q, :]], ALU.add)
                    p2a = ps128()
                    kb.mm(p2a[q, 0:64], B["RT"][rs, c, q], Hb[d][h][rs, :])
                    kb.act(yq, p2a[q, 0:64], AF.Copy)
                    yield
                    p2 = ps128()
                    kb.mm(p2[q, 0:64], Hh["ArkT"][q, q], B["V"][q, hs], start=True, stop=False)
                    kb.mm(p2[q, 0:64], Hh["ArbT"][q, q], Hh["U"][q, :], start=False, stop=True)
                    kb.v("dve", "tensor_tensor", yq, [p2[q, 0:64], yq], ALU.add)
                    p3 = ps128()
                    kb.mm(p3[rs, 0:64], B["KHAT"][q, hs], B["V"][q, hs], start=True, stop=False)
                    kb.mm(p3[rs, 0:64], B["BHAT"][q, hs], Hh["U"][q, :], start=False, stop=True)
                    kb.v("dve", "scalar_tensor_tensor", Hs[d][h][rs, :], [Hs[d][h][rs, :], B["GC"][rs, c, half:half + 1], p3[rs, 0:64]], ALU.mult, ALU.add)
                    kb.act(Hb[d][h][rs, :], Hs[d][h][rs, :], AF.Copy)
                    yield

            for step in range(nsub):
                makers = []
                Bs = []
                for d in range(2):
                    sb_ = fwd[step] if d == 0 else bwd[step]
                    r0 = sb_ * 128
                    B = UB[(step % 2) * 2 + d]
                    Bs.append((B, r0, d))
                    for nm, src in (("RT", self.RT), ("AT", self.AT), ("BT", self.BT), ("KT", self.KT)):
                        kb.dma(B[nm][:], src[d][:, r0:r0 + 128].rearrange("(c p) t -> p c t", p=128))
                    for nm, src in (("ATM", self.ATM[d]), ("KHAT", self.KHAT[d]), ("BHAT", self.BHAT[d]), ("V", self.VTM)):
                        kb.dma(B[nm][:], src[r0:r0 + 128, :])
                    kb.dma(B["GC"][:], self.GCd[d][:, sb_ * 2:sb_ * 2 + 2].rearrange("(c p) n -> p c n", p=128))
                for h in range(16):
                    for (B, r0, d) in Bs:
                        makers.append(lambda slot_, d=d, h=h, B=B: unit(d, h, B, HB[slot_]))
                interleave(makers, W)
                for (B, r0, d) in Bs:
                    kb.dma(self.YD[d][r0:r0 + 128, :], B["ysb"][:], extra_reads=[(B["ysb"].name, h) for h in range(16)])

    def phase_r4(self, l, j, sm):
        kb, cfg = self.kb, self.cfg
        Wout = self.inp(f"rk_wo__{j}", [D, D])
        self.Fm = self.scratch(f"F{l}", [cfg.T, D], BF16)
        with kb.phase() as ph:
            Wo = ph.sb([128, 8, D], BF16, "Wo")
            kb.dma(Wo[:], Wout.rearrange("(c p) n -> p c n", p=128), eng="pool")
            lnw = ph.sb([128, D], F32, "lnw")
            lnb = ph.sb([128, D], F32, "lnb")
            kb.dma(lnw[:], sm["lnw"].broadcast_to([128, D]))
            kb.dma(lnb[:], sm["lnb"].broadcast_to([128, D]))
            y0 = [ph.sb([128, D], F32, "y0") for _ in range(2)]
            y1 = [ph.sb([128, D], F32, "y1") for _ in range(2)]
            vt = [ph.sb([128, D], BF16, "vt") for _ in range(2)]
            gt = [ph.sb([128, D], BF16, "gt") for _ in range(2)]
            sbn = [ph.sb([128, 16], F32, "sbn") for _ in range(2)]
            st = [ph.sb([128, 16], F32, "st") for _ in range(2)]
            sqt = ph.sb([128, D], F32, "sqt")
            bon = ph.sb([128, D], F32, "bon")
            zt = [ph.sb([128, D], BF16, "zt") for _ in range(2)]
            XT = [ph.sb([128, 8, 128], BF16, "XT") for _ in range(2)]
            ptr = [ph.ps([128, 1024], BF16, "ptr") for _ in range(2)]
            py = [ph.ps() for _ in range(4)]
            hts = [ph.sb([128, D], F32, "ht") for _ in range(2)]
            tmp = [ph.sb([128, D], F32, "tmp") for _ in range(2)]
            junk = ph.sb([128, D], F32, "junk")
            sss = [ph.sb([128, 1], F32, "ss") for _ in range(2)]
            tmp2 = [ph.sb([128, D], F32, "tmp2") for _ in range(2)]
            fb = [ph.sb([128, D], BF16, "fb") for _ in range(2)]
            v3 = lambda t: t[:].rearrange("p (h n) -> p h n", n=64)
            b3 = lambda t: t[:].unsqueeze(2).to_broadcast([128, 16, 64])
            for s in range(cfg.nsub):
                mod = self.modC if s < 2 else self.modL
                r0 = s * 128
                i = s % 2
                kb.dma(y0[i][:], self.YD[0][r0:r0 + 128, :])
                kb.dma(y1[i][:], self.YD[1][r0:r0 + 128, :])
                kb.dma(vt[i][:], self.VTM[r0:r0 + 128, :])
                kb.dma(gt[i][:], self.GATE[r0:r0 + 128, :])
                kb.dma(sbn[i][:], self.SBN[r0:r0 + 128, :])
                kb.dma(hts[i][:], self.H[r0:r0 + 128, :])
                kb.v("pool", "tensor_tensor", y0[i][:], [y0[i][:], y1[i][:]], ALU.add)
                kb.v("dve", "tensor_reduce", st[i][:], [v3(y0[i])], AX.X, ALU.add)
                kb.v("dve", "tensor_scalar", st[i][:], [st[i][:]], 1.0 / 64, None, ALU.mult)
                kb.v("dve", "tensor_tensor", v3(y0[i]), [v3(y0[i]), b3(st[i])], ALU.subtract)
                kb.v("pool", "tensor_tensor", sqt[:], [y0[i][:], y0[i][:]], ALU.mult)
                kb.v("dve", "tensor_reduce", st[i][:], [v3(sqt)], AX.X, ALU.add)
                rstd_inplace(kb, st[i][:], 1.0 / 64, 64e-5)
                kb.v("dve", "tensor_tensor", v3(y0[i]), [v3(y0[i]), b3(st[i])], ALU.mult)
                kb.v("pool", "tensor_tensor", y0[i][:], [y0[i][:], lnw[:]], ALU.mult)
                kb.v("pool", "tensor_tensor", y0[i][:], [y0[i][:], lnb[:]], ALU.add)
                kb.v("dve", "tensor_tensor", v3(bon), [v3(vt[i]), b3(sbn[i])], ALU.mult)
                kb.v("dve", "tensor_tensor", y0[i][:], [y0[i][:], bon[:]], ALU.add)
                kb.v("pool", "tensor_tensor", zt[i][:], [y0[i][:], gt[i][:]], ALU.mult)
                for c in range(8):
                    kb.tr(ptr[i][:, c * 128:(c + 1) * 128], zt[i][:, c * 128:(c + 1) * 128], self.ident[:])
                self.evac(XT[i][:], ptr[i][:].rearrange("p (c t) -> p c t", c=8))
                for n in range(2):
                    pp = py[(2 * s + n) % 4]
                    for c in range(8):
                        kb.mm(pp[:], XT[i][:, c, :], Wo[:, c, n * 512:(n + 1) * 512], start=(c == 0), stop=(c == 7))
                    kb.v("dve", "tensor_tensor", tmp[i][:, n * 512:(n + 1) * 512], [pp[:], mod[2][:, n * 512:(n + 1) * 512]], ALU.mult)
                kb.v("pool", "tensor_tensor", hts[i][:], [tmp[i][:], hts[i][:]], ALU.add)
                kb.dma(self.H[r0:r0 + 128, :], hts[i][:])
                self.norm_sub(hts[i][:], junk[:], sss[i][:], tmp2[i][:], fb[i][:], mod[4][:], mod[3][:])
                kb.dma(self.Fm[r0:r0 + 128, :], fb[i][:])
import re


def resolve_input(name, cfg, inp, b, consts, cache):
    if name == "h0":
        return np.ascontiguousarray(np.concatenate([inp["ctx"][b], inp["x"][b][:cfg.T - CTX]], 0))
    if name == "cvec":
        return np.ascontiguousarray(np.concatenate([inp["c"][b].reshape(8, 128).T, inp["c_ctx"].reshape(8, 128).T], 1))
    if name.startswith("c_"):
        return consts[name[2:]]
    key = ("shared", name)
    if key in cache:
        return cache[key]
    m = re.match(r"^e(\d+)_(\w+)$", name)
    if m:
        j = int(m.group(1))
        if ("es", j) not in cache:
            cache[("es", j)] = host_even_smalls(inp, j)
        arr = cache[("es", j)][m.group(2)]
    else:
        m = re.match(r"^o(\d+)_(\w+)$", name)
        if m:
            j = int(m.group(1))
            if ("os", j) not in cache:
                cache[("os", j)] = host_odd_smalls(inp, j)
            arr = cache[("os", j)][m.group(2)]
        else:
            m = re.match(r"^(.+)__(\d+)$", name)
            base, idx = m.group(1), int(m.group(2))
            assert base in ALL_INPUTS, base
            a = inp[base][idx]
            if a.ndim == 1:
                a = a.reshape(1, -1)
            elif base in ("moe_w1", "moe_w3", "moe_w2"):
                a = a.reshape(-1, a.shape[-1])
            arr = np.ascontiguousarray(a)
    cache[key] = arr
    return arr


ALL_INPUTS = ('x', 'c', 'ctx', 'c_ctx', 'ada_w', 'ada_b', 'norm_mix', 'norm_ffn', 'hy_w_in', 'hy_w_out', 'mla_qa_norm', 'mla_w_qb',
              'mla_kva_norm', 'mla_w_kvb', 'mla_q_norm', 'mla_k_norm', 'gdn_conv', 'gdn_a_log', 'gdn_dt_bias', 'gdn_out_norm',
              'rk_mu', 'rk_wr', 'rk_wk', 'rk_wv', 'rk_wo', 'rk_w0', 'rk_w1', 'rk_w2', 'rk_a0', 'rk_a1', 'rk_a2', 'rk_g1', 'rk_g2',
              'rk_kk', 'rk_ka', 'rk_rk', 'rk_ln_w', 'rk_ln_b', 'rk_v0', 'rk_v1', 'rk_v2', 'moe_w_group', 'moe_b_group',
              'moe_w_expert', 'moe_b_expert', 'moe_w1', 'moe_w3', 'moe_w2')

_PROG_CACHE = {}


def get_prog(cfg_key):
    if cfg_key not in _PROG_CACHE:
        nlt, layers, debug = cfg_key
        cfg = Cfg(nlt=nlt, layers=layers, debug=debug)
        p = Prog(cfg)
        p.setup()
        for l in cfg.layers:
            if l % 2 == 0:
                p.even_layer(l)
            else:
                p.odd_layer(l)
        p.finish()
        _PROG_CACHE[cfg_key] = p
    return _PROG_CACHE[cfg_key]


def run_prog(p, inp, batches):
    cfg = p.cfg
    consts = host_consts(cfg)
    cache = {}
    in_maps = []
    for b in batches:
        m = {}
        for name, (shape, dt) in p.in_shapes.items():
            a = resolve_input(name, cfg, inp, b, consts, cache)
            assert tuple(a.shape) == tuple(shape), (name, a.shape, shape)
            m[name] = a
        in_maps.append(m)
    res = run_bass_kernel_spmd(p.nc, in_maps, core_ids=list(range(len(batches))))
    return res


def kernel(**inputs):
    inp = {k: np.asarray(v) for k, v in inputs.items()}
    p = get_prog((16, (0, 1, 2, 3), False))
    res = run_prog(p, inp, list(range(8)))
    return np.stack([np.asarray(r["out"], dtype=np.float32) for r in res.results], 0)
```

```python
import contextlib
import numpy as np
import concourse.bass as bass
import concourse.mybir as mybir
from concourse.bass_utils import run_bass_kernel_spmd

F32 = mybir.dt.float32
BF16 = mybir.dt.bfloat16
I32 = mybir.dt.int32
AF = mybir.ActivationFunctionType
ALU = mybir.AluOpType
AX = mybir.AxisListType

ENGS = ("pe", "act", "dve", "pool", "sp")
SAME_ENGINE_SYNC = True
DMA_WINDOW = 8
SEM_MAXV = 30000


class Op:
    __slots__ = ("eng", "fn", "reads", "writes", "is_dma", "deps", "signals", "event", "pre_wait", "barrier")

    def __init__(self, eng, fn, reads, writes, is_dma, barrier=False):
        self.eng = eng
        self.fn = fn
        self.reads = reads
        self.writes = writes
        self.is_dma = is_dma
        self.deps = set()
        self.signals = False
        self.event = None
        self.pre_wait = None
        self.barrier = barrier


class V:
    __slots__ = ("ap", "key")

    def __init__(self, ap, key):
        self.ap = ap
        self.key = key


def _a(x):
    return x.ap if isinstance(x, V) else x


def _key(x):
    if isinstance(x, V):
        return x.key
    if isinstance(x, (str, tuple)):
        return x
    if hasattr(x, "tensor"):
        return x.tensor.name
    return x.name


class Phase:
    def __init__(self, kb):
        self.kb = kb
        self.es = contextlib.ExitStack()

    def sb(self, shape, dtype=F32, name="t"):
        self.kb._n += 1
        return self.es.enter_context(self.kb.nc.sbuf_tensor(f"{name}_{self.kb._n}", list(shape), dtype))

    def ps(self, shape=(128, 512), dtype=F32, name="p"):
        self.kb._n += 1
        nm = f"{name}_{self.kb._n}"
        self.kb.psum_names.add(nm)
        return self.es.enter_context(self.kb.nc.psum_tensor(nm, list(shape), dtype))

    def __enter__(self):
        return self

    def __exit__(self, *a):
        self.kb.barrier()
        self.es.close()
        return False


def interleave(makers, width):
    it = iter(makers)
    active = []
    for slot in range(width):
        m = next(it, None)
        if m is not None:
            active.append((slot, m(slot)))
    while active:
        for entry in list(active):
            slot, g = entry
            try:
                next(g)
            except StopIteration:
                i = active.index(entry)
                m = next(it, None)
                if m is not None:
                    active[i] = (slot, m(slot))
                else:
                    active.pop(i)


class KB:
    def __init__(self, nc):
        self.nc = nc
        self.ops = []
        self._n = 0
        self.psum_names = set()

    def phase(self):
        return Phase(self)

    def sb(self, shape, dtype=F32, name="g"):
        self._n += 1
        return self.nc.alloc_sbuf_tensor(f"{name}_{self._n}", list(shape), dtype)

    def dram(self, shape, dtype=F32, name="dr"):
        self._n += 1
        return self.nc.dram_tensor(f"{name}_{self._n}", list(shape), dtype, kind="Internal")

    def barrier(self):
        self.ops.append(Op(None, None, [], [], False, barrier=True))

    def op(self, eng, fn, reads, writes, is_dma=False):
        o = Op(eng, fn, [_key(r) for r in reads], [_key(w) for w in writes], is_dma)
        self.ops.append(o)
        return o

    def mm(self, out, lhsT, rhs, start=True, stop=True, **kw):
        return self.op("pe", lambda e: e.matmul(_a(out), _a(lhsT), _a(rhs), start=start, stop=stop, **kw), [lhsT, rhs], [out])

    def tr(self, out, in_, ident):
        return self.op("pe", lambda e: e.transpose(_a(out), _a(in_), _a(ident)), [in_, ident], [out])

    def act(self, out, in_, func, bias=None, scale=None, accum_out=None):
        kw = {}
        reads = [in_]
        if bias is not None:
            kw["bias"] = bias
            if not isinstance(bias, (int, float)):
                reads.append(bias)
        if scale is not None:
            kw["scale"] = scale
            if not isinstance(scale, (int, float)):
                reads.append(scale)
        writes = [out]
        if accum_out is not None:
            kw["accum_out"] = accum_out
            writes.append(accum_out)
        kw = {k: _a(x) for k, x in kw.items()}
        return self.op("act", lambda e: e.activation(_a(out), _a(in_), func, **kw), reads, writes)

    def v(self, eng, method, out, ins, *args, **kw):
        isap = lambda a: hasattr(a, "tensor") or isinstance(a, V)
        reads = [a for a in ins if isap(a)]
        reads += [a for a in args if isap(a)]
        reads += [a for a in kw.values() if isap(a)]
        ins2 = [_a(a) for a in ins]
        args2 = [_a(a) for a in args]
        kw2 = {k: _a(x) for k, x in kw.items()}
        return self.op(eng, lambda e: getattr(e, method)(_a(out), *ins2, *args2, **kw2), reads, [out])

    def dma(self, out, in_, eng="sp", rkey=None, wkey=None, extra_reads=(), **kw):
        return self.op(eng, lambda e: e.dma_start(out=_a(out), in_=_a(in_), **kw), [rkey or in_] + list(extra_reads), [wkey or out], is_dma=True)

    def gather(self, out, src, idx, rkey=None):
        return self.op("pool", lambda e: e.indirect_dma_start(out=out, out_offset=None, in_=src,
                       in_offset=bass.IndirectOffsetOnAxis(ap=idx, axis=0)), [rkey or src, idx], [out], is_dma=True)

    def scatter(self, dst, src, idx, wkey=None):
        return self.op("pool", lambda e: e.indirect_dma_start(out=dst, out_offset=bass.IndirectOffsetOnAxis(ap=idx, axis=0),
                       in_=src, in_offset=None), [src, idx], [wkey or dst], is_dma=True)

    def finalize(self):
        nc = self.nc
        allops = self.ops
        ops = [o for o in allops if not o.barrier]
        idx_of = {id(o): i for i, o in enumerate(ops)}
        last_w = {}
        readers = {}
        last_on = {e: None for e in ENGS}
        recent_dma = {e: [] for e in ENGS}
        pending = {e: set() for e in ENGS}
        for o in allops:
            if o.barrier:
                src = set()
                for e in ENGS:
                    if last_on[e] is not None:
                        src.add(last_on[e])
                    src.update(recent_dma[e])
                for e in ENGS:
                    pending[e] |= src
                continue
            i = idx_of[id(o)]
            deps = set()
            for k in o.reads:
                if k in last_w:
                    deps.add(last_w[k])
                if k in self.psum_names:
                    for r in readers.get(k, ()):
                        if ops[r].eng != o.eng:
                            deps.add(r)
            for k in o.writes:
                if k in last_w:
                    deps.add(last_w[k])
                deps.update(readers.get(k, ()))
            deps |= pending[o.eng]
            pending[o.eng] = set()
            deps.discard(i)
            for k in o.reads:
                readers.setdefault(k, []).append(i)
            for k in o.writes:
                last_w[k] = i
                readers[k] = []
            fd = set()
            for d in deps:
                p = ops[d]
                if p.eng == o.eng and not p.is_dma and (o.eng == "pe" or not SAME_ENGINE_SYNC):
                    continue
                fd.add(d)
            best = {}
            keep = set()
            for d in fd:
                pe_ = ops[d]
                if pe_.is_dma or pe_.eng not in ("pe", "act", "dve"):
                    keep.add(d)
                elif pe_.eng not in best or d > best[pe_.eng]:
                    best[pe_.eng] = d
            fd = keep | set(best.values())
            o.deps = fd
            for d in fd:
                ops[d].signals = True
            last_on[o.eng] = i
            if o.is_dma:
                recent_dma[o.eng] = (recent_dma[o.eng] + [i])[-DMA_WINDOW:]
        n_sig = {e: 0 for e in ENGS}
        n_dma = {e: 0 for e in ENGS}
        for o in ops:
            if o.is_dma:
                o.signals = True
                n_dma[o.eng] += 1
            elif o.signals:
                n_sig[o.eng] += 1
        sems = {e: [nc.alloc_semaphore(f"s_{e}_{j}") for j in range(max(1, -(-n_sig[e] // SEM_MAXV)))] for e in ENGS}
        dsems = {e: [nc.alloc_semaphore(f"d_{e}_{j}") for j in range(DMA_WINDOW)] for e in ENGS if n_dma[e]}
        cnt = {e: 0 for e in ENGS}
        dcnt = {e: 0 for e in ENGS}
        for o in ops:
            if o.is_dma:
                n = dcnt[o.eng]
                dcnt[o.eng] += 1
                sem = dsems[o.eng][n % DMA_WINDOW]
                o.event = (sem, 16 * (n // DMA_WINDOW + 1))
                if n >= DMA_WINDOW:
                    o.pre_wait = (sem, 16 * (n // DMA_WINDOW))
            elif o.signals:
                n = cnt[o.eng]
                cnt[o.eng] += 1
                o.event = (sems[o.eng][n // SEM_MAXV], n % SEM_MAXV + 1)
        self.stats = dict(n_ops=len(ops), per_eng={e: sum(1 for o in ops if o.eng == e) for e in ENGS}, n_sig=n_sig, n_dma=n_dma)
        per_eng = {e: [o for o in ops if o.eng == e] for e in ENGS}
        final_waits = []
        for e in ENGS:
            for j in range(min(DMA_WINDOW, dcnt[e])):
                final_waits.append((dsems[e][j], 16 * ((dcnt[e] - 1 - j) // DMA_WINDOW + 1)))

        def emit(engname, eh):
            waited = {}

            def wait(sem, v):
                if waited.get(sem.name, 0) >= v:
                    return
                waited[sem.name] = v
                eh.wait_ge(sem, v)

            for o in per_eng[engname]:
                if o.pre_wait is not None:
                    wait(*o.pre_wait)
                for d in sorted(o.deps):
                    wait(*ops[d].event)
                ins = o.fn(eh)
                if o.event is not None:
                    ins.then_inc(o.event[0], 16 if o.is_dma else 1)
            if engname == "sp":
                for s, v in final_waits:
                    wait(s, v)

        with nc.Block() as block:
            @block.tensor
            def _(e):
                emit("pe", e)

            @block.scalar
            def _(e):
                emit("act", e)

            @block.vector
            def _(e):
                emit("dve", e)

            @block.gpsimd
            def _(e):
                emit("pool", e)

            @block.sync
            def _(e):
                emit("sp", e)
D = 1024
CTX = 256
GRID_W = 64
EPS = 1e-6
NH = 8
QK = 96
GH = 4
UF_ROWS = 1984
UT_COLS = 528
MOE_BS = 512


class Cfg:
    def __init__(self, nlt=16, layers=(0, 1, 2, 3), debug=False):
        self.nlt = nlt
        self.T = CTX + 512 * nlt
        self.tiles = [(0, CTX)] + [(CTX + 512 * i, 512) for i in range(nlt)]
        self.nsub = self.T // 128
        self.layers = tuple(layers)
        self.debug = debug
        self.nblk = -(-(2 * self.T) // MOE_BS) + 32
        self.nslot = self.nblk * MOE_BS


def rope_perm():
    p = np.zeros(32, np.int64)
    for ax in range(2):
        for half in range(2):
            for f in range(8):
                p[ax * 16 + half * 8 + f] = ax * 16 + (1 - half) * 8 + f
    return p


def rope_tables(cfg):
    S = cfg.T - CTX
    pos = np.arange(S)
    row = (pos // GRID_W).astype(np.float32)
    col = (pos % GRID_W).astype(np.float32)
    inv = (10000.0 ** (-np.arange(8, dtype=np.float32) / 8)).astype(np.float32)
    cosT = np.zeros((96, cfg.T), np.float32)
    sinT = np.zeros((96, cfg.T), np.float32)
    cosT[64:96, :CTX] = 1.0
    for ax in range(2):
        p = row if ax == 0 else col
        ang = p[None, :] * inv[:, None]
        for half in range(2):
            r0 = 64 + ax * 16 + half * 8
            cosT[r0:r0 + 8, CTX:] = np.cos(ang)
            sinT[r0:r0 + 8, CTX:] = np.sin(ang) * (-1.0 if half == 0 else 1.0)
    return cosT, sinT


def host_consts(cfg):
    c = {}
    c["ident_f"] = np.eye(128, dtype=np.float32)
    i = np.arange(128)
    same = (i[:, None] // 64) == (i[None, :] // 64)
    c["m_le"] = (same & (i[:, None] <= i[None, :])).astype(np.float32)
    c["m_ge"] = (same & (i[:, None] >= i[None, :])).astype(np.float32)
    c["m_lt"] = (same & (i[:, None] < i[None, :])).astype(np.float32)
    c["m_gt"] = (same & (i[:, None] > i[None, :])).astype(np.float32)
    c["m_same"] = same.astype(np.float32)
    c["ones_f"] = np.ones((128, 128), np.float32)
    c["m_h0"] = np.repeat((i[:, None] < 64), 128, 1).astype(np.float32)
    c["m_h1"] = np.repeat((i[:, None] >= 64), 128, 1).astype(np.float32)
    c["tri_lt_full"] = (i[:, None] < i[None, :]).astype(np.float32)
    cosT, sinT = rope_tables(cfg)
    c["cosT"] = cosT
    c["sinT"] = sinT
    c["thr"] = np.tile((np.arange(34, dtype=np.float32) * MOE_BS)[None, :], (128, 1))
    rm = np.ones((128, 512), np.float32)
    rm[:, ::64] = 0.0
    c["rmask"] = rm
    hs = np.zeros((128, 2), np.float32)
    hs[:64, 0] = 1.0
    hs[64:, 1] = 1.0
    c["hsel"] = hs
    c["iota_p"] = i.astype(np.float32).reshape(128, 1)
    c["blk_start"] = np.tile((np.arange(cfg.nblk, dtype=np.float32) * MOE_BS)[None, :], (128, 1))
    return c


CONST_SHAPES = lambda cfg: {
    "ident_f": (128, 128), "m_le": (128, 128), "m_ge": (128, 128), "m_lt": (128, 128), "m_gt": (128, 128),
    "m_same": (128, 128), "ones_f": (128, 128), "m_h0": (128, 128), "m_h1": (128, 128), "tri_lt_full": (128, 128),
    "cosT": (96, cfg.T), "sinT": (96, cfg.T), "iota_p": (128, 1), "rmask": (128, 512), "hsel": (128, 2), "thr": (128, 34), "blk_start": (128, cfg.nblk),
}


def host_even_smalls(inp, j):
    perm = rope_perm()
    s = {}
    s["qa_g"] = np.ascontiguousarray(inp["mla_qa_norm"][j].reshape(2, 128).T)
    s["kva_g"] = np.ascontiguousarray(inp["mla_kva_norm"][j].reshape(128, 1))
    for nm, key in (("qn_col", "mla_q_norm"), ("kn_col", "mla_k_norm")):
        g = inp[key][j]
        colv = np.zeros((96, 2), np.float32)
        colv[:, 0] = g
        colv[64:96, 1] = g[64 + perm]
        s[nm] = colv
    s["convw"] = np.ascontiguousarray(inp["gdn_conv"][j].reshape(5, 12, 128).transpose(2, 1, 0))
    s["gdn_vec"] = np.concatenate([inp["gdn_a_log"][j].reshape(-1), inp["gdn_dt_bias"][j].reshape(-1)]).reshape(1, 16).astype(np.float32)
    s["outg"] = np.ascontiguousarray(inp["gdn_out_norm"][j].reshape(1, 128))
    return s


EVEN_SMALL_SHAPES = {"qa_g": (128, 2), "kva_g": (128, 1), "qn_col": (96, 2), "kn_col": (96, 2),
                     "convw": (128, 12, 5), "gdn_vec": (1, 16), "outg": (1, 128)}


def host_odd_smalls(inp, j):
    s = {}
    col = lambda v: np.ascontiguousarray(v.reshape(8, 128).T)
    s["mu"] = np.ascontiguousarray(inp["rk_mu"][j].reshape(6, 8, 128).transpose(2, 0, 1))
    s["w0"] = np.ascontiguousarray(inp["rk_w0"][j].reshape(2, 8, 128).transpose(2, 0, 1))
    s["a0"] = np.ascontiguousarray(inp["rk_a0"][j].reshape(2, 8, 128).transpose(2, 0, 1))
    s["kkcol"] = col(inp["rk_kk"][j])
    s["kacol"] = col(inp["rk_ka"][j])
    s["rkcol"] = col(inp["rk_rk"][j].reshape(-1))
    s["lnw"] = np.ascontiguousarray(inp["rk_ln_w"][j].reshape(1, -1))
    s["lnb"] = np.ascontiguousarray(inp["rk_ln_b"][j].reshape(1, -1))
    s["v0"] = np.ascontiguousarray(inp["rk_v0"][j - 1].reshape(1, -1)) if j > 0 else np.zeros((1, D), np.float32)
    return s


ODD_SMALL_SHAPES = {"mu": (128, 6, 8), "w0": (128, 2, 8), "a0": (128, 2, 8), "kkcol": (128, 8), "kacol": (128, 8),
                    "rkcol": (128, 8), "lnw": (1, D), "lnb": (1, D), "v0": (1, D)}
MLA_SCALE = 96 ** -0.5


def rstd_inplace(kb, t, inv_n, eps, src=None):
    kb.v("dve", "tensor_scalar", t, [src if src is not None else t], inv_n, eps, ALU.mult, ALU.add)
    kb.act(t, t, AF.Ln)
    kb.act(t, t, AF.Exp, scale=-0.5)


class Prog:
    def __init__(self, cfg):
        self.cfg = cfg
        self.nc = bass.Bass("TRN2", target_bir_lowering=False)
        self.kb = KB(self.nc)
        self.in_shapes = {}
        self.dbg = {}
        self._rr = 0

    def inp(self, name, shape, dtype=F32):
        self.in_shapes[name] = (tuple(shape), dtype)
        return self.nc.dram_tensor(name, list(shape), dtype, kind="ExternalInput").ap()

    def scratch(self, name, shape, dtype=F32):
        if self.cfg.debug:
            t = self.nc.dram_tensor("dbg_" + name, list(shape), dtype, kind="ExternalOutput")
            self.dbg[name] = "dbg_" + name
        else:
            t = self.nc.dram_tensor("scr_" + name, list(shape), dtype, kind="Internal")
        return t.ap()

    def evac(self, out, in_):
        self._rr += 1
        if self._rr % 2:
            self.kb.act(out, in_, AF.Copy)
        else:
            self.kb.v("dve", "tensor_copy", out, [in_])

    def setup(self):
        kb, cfg = self.kb, self.cfg
        self.h0 = self.inp("h0", [cfg.T, D])
        self.cvec = self.inp("cvec", [128, 16])
        self.out = self.nc.dram_tensor("out", [cfg.T - CTX, D], F32, kind="ExternalOutput").ap()
        self.H = self.scratch("H", [cfg.T, D])
        self.cin = {k: self.inp("c_" + k, s) for k, s in CONST_SHAPES(cfg).items()}
        self.ident_f = kb.sb([128, 128], F32, "identf")
        self.ident = kb.sb([128, 128], BF16, "ident")
        self.ones = kb.sb([128, 128], BF16, "ones")
        kb.dma(self.ident_f[:], self.cin["ident_f"])
        kb.v("dve", "tensor_copy", self.ident[:], [self.ident_f[:]])
        kb.v("pool", "memset", self.ones[:], [], 1.0)
        self.modL = [kb.sb([128, D], F32, f"modL{i}") for i in range(6)]
        self.modC = [kb.sb([128, D], F32, f"modC{i}") for i in range(6)]
        with kb.phase() as ph:
            bufs = [ph.sb([128, D], F32, "cp") for _ in range(4)]
            for s in range(cfg.nsub):
                b = bufs[s % 4]
                kb.dma(b[:], self.h0[s * 128:(s + 1) * 128, :])
                kb.dma(self.H[s * 128:(s + 1) * 128, :], b[:])

    def finish(self):
        kb, cfg = self.kb, self.cfg
        with kb.phase() as ph:
            bufs = [ph.sb([128, D], F32, "cp") for _ in range(4)]
            for s in range(2, cfg.nsub):
                b = bufs[s % 4]
                kb.dma(b[:], self.H[s * 128:(s + 1) * 128, :])
                kb.dma(self.out[(s - 2) * 128:(s - 1) * 128, :], b[:])
        kb.finalize()

    def phase_mod(self, l):
        kb = self.kb
        ada_w = self.inp(f"ada_w__{l}", [D, 6 * D])
        ada_b = self.inp(f"ada_b__{l}", [1, 6 * D])
        nmix = self.inp(f"norm_mix__{l}", [1, D])
        nffn = self.inp(f"norm_ffn__{l}", [1, D])
        with kb.phase() as ph:
            sc = ph.sb([128, 16], F32, "sc")
            kb.dma(sc[:], self.cvec)
            kb.act(sc[:], sc[:], AF.Silu)
            lhs = ph.sb([128, 16, 128], F32, "lhs")
            for c in range(16):
                kb.v("dve", "tensor_copy", lhs[:, c, :], [sc[:, c:c + 1].to_broadcast([128, 128])])
            bb = ph.sb([128, 6 * D], F32, "bb")
            kb.dma(bb[:], ada_b.broadcast_to([128, 6 * D]))
            wst = [ph.sb([128, 8, 512], F32, "wst") for _ in range(2)]
            pl = [ph.ps() for _ in range(2)]
            pc = [ph.ps() for _ in range(2)]
            awv = ada_w.rearrange("(c p) n -> p c n", p=128)
            for n in range(12):
                w = wst[n % 2]
                kb.dma(w[:], awv[:, :, n * 512:(n + 1) * 512])
                for c in range(8):
                    kb.mm(pl[n % 2][:], lhs[:, c, :], w[:, c, :], start=(c == 0), stop=(c == 7))
                for c in range(8):
                    kb.mm(pc[n % 2][:], lhs[:, 8 + c, :], w[:, c, :], start=(c == 0), stop=(c == 7))
                jj, hf = n // 2, n % 2
                kb.v("dve", "tensor_tensor", self.modL[jj][:, hf * 512:(hf + 1) * 512], [pl[n % 2][:], bb[:, n * 512:(n + 1) * 512]], ALU.add)
                kb.v("dve", "tensor_tensor", self.modC[jj][:, hf * 512:(hf + 1) * 512], [pc[n % 2][:], bb[:, n * 512:(n + 1) * 512]], ALU.add)
            nm = ph.sb([128, D], F32, "nm")
            nf = ph.sb([128, D], F32, "nf")
            kb.dma(nm[:], nmix.broadcast_to([128, D]))
            kb.dma(nf[:], nffn.broadcast_to([128, D]))
            for mod in (self.modL, self.modC):
                kb.v("dve", "scalar_tensor_tensor", mod[1][:], [mod[1][:], 1.0, nm[:]], ALU.add, ALU.mult)
                kb.v("dve", "scalar_tensor_tensor", mod[4][:], [mod[4][:], 1.0, nf[:]], ALU.add, ALU.mult)

    def norm_sub(self, ht, junk, ss, tmp, xn, G, S):
        kb = self.kb
        kb.act(junk, ht, AF.Square, accum_out=ss)
        rstd_inplace(kb, ss, 1.0 / D, EPS)
        kb.v("dve", "scalar_tensor_tensor", tmp, [ht, ss, G], ALU.mult, ALU.mult)
        kb.v("pool", "tensor_tensor", xn, [tmp, S], ALU.add)

    def norm_tiles_to_xT(self, ph, src, gi, si, consume):
        kb, cfg = self.kb, self.cfg
        xTs = [ph.sb([128, 8, 512], BF16, "xT") for _ in range(2)]
        hts = [ph.sb([128, D], F32, "ht") for _ in range(2)]
        junk = ph.sb([128, D], F32, "junk")
        sss = [ph.sb([128, 1], F32, "ss") for _ in range(2)]
        tmps = [ph.sb([128, D], F32, "tmp") for _ in range(2)]
        xns = [ph.sb([128, D], BF16, "xn") for _ in range(2)]
        ptr = [ph.ps([128, 1024], BF16, "ptr") for _ in range(2)]
        k = 0
        for ti, (t0, TT) in enumerate(cfg.tiles):
            xT = xTs[ti % 2]
            mod = self.modC if ti == 0 else self.modL
            for s in range(TT // 128):
                ht, ss, tmp, xn, pt = hts[k % 2], sss[k % 2], tmps[k % 2], xns[k % 2], ptr[k % 2]
                k += 1
                r0 = t0 + s * 128
                kb.dma(ht[:], src[r0:r0 + 128, :])
                self.norm_sub(ht[:], junk[:], ss[:], tmp[:], xn[:], mod[gi][:], mod[si][:])
                for c in range(8):
                    kb.tr(pt[:, c * 128:(c + 1) * 128], xn[:, c * 128:(c + 1) * 128], self.ident[:])
                self.evac(xT[:, :, s * 128:(s + 1) * 128], pt[:].rearrange("p (c t) -> p c t", c=8))
            consume(ti, t0, TT, xT)

    def even_layer(self, l):
        j = l // 2
        self.phase_mod(l)
        sm = {k: self.inp(f"e{j}_{k}", s) for k, s in EVEN_SMALL_SHAPES.items()}
        self.phase_e1(j)
        self.phase_e2(j, sm)
        self.phase_e3(j)
        self.phase_e4(j, sm)
        self.phase_e5(j, sm)
        self.phase_e6(l, j, sm)
        self.moe(l)

    def phase_e1(self, j):
        kb, cfg = self.kb, self.cfg
        W = self.inp(f"hy_w_in__{j}", [D, 2480])
        self.UF = self.scratch(f"UF{j}", [UF_ROWS, cfg.T])
        self.UT = self.scratch(f"UT{j}", [cfg.T, UT_COLS])
        with kb.phase() as ph:
            Wfm = ph.sb([128, 8, UF_ROWS], BF16, "Wfm")
            Wtm = ph.sb([128, 8, UT_COLS], BF16, "Wtm")
            Wv = W.rearrange("(c p) n -> p c n", p=128)
            kb.dma(Wfm[:, :, 0:416], Wv[:, :, 0:416], eng="pool")
            for (d0, s0) in ((0, 8), (8, 0), (16, 24), (24, 16)):
                kb.dma(Wfm[:, :, 416 + d0:416 + d0 + 8], Wv[:, :, 384 + s0:384 + s0 + 8], eng="pool")
            kb.dma(Wfm[:, :, 448:1984], Wv[:, :, 416:1952], eng="pool")
            kb.dma(Wtm[:, :, :], Wv[:, :, 1952:2480], eng="pool")
            pmm = [ph.ps() for _ in range(4)]
            ost = [ph.sb([128, 512], F32, "ost") for _ in range(4)]
            cnt = [0]

            def consume(ti, t0, TT, xT):
                for m in range(16):
                    rows = min(128, UF_ROWS - m * 128)
                    pm, ob = pmm[cnt[0] % 4], ost[cnt[0] % 4]
                    cnt[0] += 1
                    for c in range(8):
                        kb.mm(pm[0:rows, 0:TT], Wfm[:, c, m * 128:m * 128 + rows], xT[:, c, 0:TT], start=(c == 0), stop=(c == 7))
                    self.evac(ob[0:rows, 0:TT], pm[0:rows, 0:TT])
                    kb.dma(self.UF[m * 128:m * 128 + rows, t0:t0 + TT], ob[0:rows, 0:TT])
                for s in range(TT // 128):
                    for (n0, n1) in ((0, 512), (512, 528)):
                        pm, ob = pmm[cnt[0] % 4], ost[cnt[0] % 4]
                        cnt[0] += 1
                        for c in range(8):
                            kb.mm(pm[:, 0:n1 - n0], xT[:, c, s * 128:(s + 1) * 128], Wtm[:, c, n0:n1], start=(c == 0), stop=(c == 7))
                        self.evac(ob[:, 0:n1 - n0], pm[:, 0:n1 - n0])
                        kb.dma(self.UT[t0 + s * 128:t0 + (s + 1) * 128, n0:n1], ob[:, 0:n1 - n0])

            self.norm_tiles_to_xT(ph, self.H, 1, 0, consume)

    def phase_e2(self, j, sm):
        kb, cfg = self.kb, self.cfg
        Wqb = self.inp(f"mla_w_qb__{j}", [256, 768])
        Wkvb = self.inp(f"mla_w_kvb__{j}", [128, 1024])
        self.QF = self.scratch(f"QF{j}", [NH, 96, cfg.T], BF16)
        self.KF = self.scratch(f"KF{j}", [NH, 96, cfg.T], BF16)
        self.VT = self.scratch(f"VT{j}", [cfg.T, 512], BF16)
        with kb.phase() as ph:
            Wq = ph.sb([128, 2, 768], BF16, "Wq")
            Wqs = ph.sb([128, 2, NH, 32], BF16, "Wqs")
            Wk = ph.sb([128, NH, 64], BF16, "Wk")
            Wvv = ph.sb([128, NH, 64], BF16, "Wv")
            kb.dma(Wq[:], Wqb.rearrange("(c p) n -> p c n", p=128), eng="pool")
            Wq4 = Wqb.rearrange("(c p) (h d) -> p c h d", p=128, d=96)
            for (d0, s0) in ((0, 8), (8, 0), (16, 24), (24, 16)):
                for c in range(2):
                    kb.dma(Wqs[:, c, :, d0:d0 + 8], Wq4[:, c, :, 64 + s0:64 + s0 + 8], eng="pool")
            Wkv3 = Wkvb.rearrange("p (h d) -> p h d", d=128)
            kb.dma(Wk[:], Wkv3[:, :, 0:64], eng="pool")
            kb.dma(Wvv[:], Wkv3[:, :, 64:128], eng="pool")
            qa_g = ph.sb([128, 2], F32, "qa_g")
            kva_g = ph.sb([128, 1], F32, "kva_g")
            qn = ph.sb([96, 2], F32, "qn")
            kn = ph.sb([96, 2], F32, "kn")
            kb.dma(qa_g[:], sm["qa_g"])
            kb.dma(kva_g[:], sm["kva_g"])
            kb.dma(qn[:], sm["qn_col"])
            kb.dma(kn[:], sm["kn_col"])
            cq = ph.sb([128, 2, 512], F32, "cq")
            ckv = ph.sb([128, 512], F32, "ckv")
            kpe = ph.sb([96, 512], F32, "kpe")
            kpes = ph.sb([96, 512], F32, "kpes")
            cs = ph.sb([96, 512], F32, "cs")
            sn = ph.sb([96, 512], F32, "sn")
            sq = ph.sb([128, 2, 512], BF16, "sq")
            rs = ph.sb([128, 512], F32, "rs")
            cqn = ph.sb([128, 2, 512], BF16, "cqn")
            ckvn = ph.sb([128, 512], BF16, "ckvn")
            rk = ph.sb([96, 512], F32, "rk")
            t1s = [ph.sb([96, 512], F32, "t1") for _ in range(2)]
            t2s = [ph.sb([96, 512], F32, "t2") for _ in range(2)]
            sqhs = [ph.sb([96, 512], BF16, "sqh") for _ in range(2)]
            rshs = [ph.sb([96, 512], F32, "rsh") for _ in range(2)]
            ohs = [ph.sb([96, 512], BF16, "oh") for _ in range(2)]
            vsb = [ph.sb([128, 512], BF16, "vsb") for _ in range(2)]
            pA = [ph.ps() for _ in range(2)]
            pB = [ph.ps() for _ in range(2)]
            pS = [ph.ps() for _ in range(2)]
            pR = ph.ps()
            pV = ph.ps()
            u = 0
            for ti, (t0, TT) in enumerate(cfg.tiles):
                kb.dma(cq[:, :, 0:TT], self.UF[0:256, t0:t0 + TT].rearrange("(c p) t -> p c t", p=128))
                kb.dma(ckv[:, 0:TT], self.UF[256:384, t0:t0 + TT])
                kb.dma(kpe[64:96, 0:TT], self.UF[384:416, t0:t0 + TT])
                kb.dma(kpes[64:96, 0:TT], self.UF[416:448, t0:t0 + TT])
                kb.dma(cs[64:96, 0:TT], self.cin["cosT"][64:96, t0:t0 + TT])
                kb.dma(sn[64:96, 0:TT], self.cin["sinT"][64:96, t0:t0 + TT])
                kb.act(sq[:, :, 0:TT], cq[:, :, 0:TT], AF.Square)
                for c in range(2):
                    kb.mm(pR[:, 0:TT], self.ones[:], sq[:, c, 0:TT], start=(c == 0), stop=(c == 1))
                rstd_inplace(kb, rs[:, 0:TT], 1.0 / 256, EPS, src=pR[:, 0:TT])
                for c in range(2):
                    kb.v("dve", "scalar_tensor_tensor", cqn[:, c, 0:TT], [cq[:, c, 0:TT], qa_g[:, c:c + 1], rs[:, 0:TT]], ALU.mult, ALU.mult)
                kb.act(sq[:, 0, 0:TT], ckv[:, 0:TT], AF.Square)
                kb.mm(pR[:, 0:TT], self.ones[:], sq[:, 0, 0:TT])
                rstd_inplace(kb, rs[:, 0:TT], 1.0 / 128, EPS, src=pR[:, 0:TT])
                kb.v("dve", "scalar_tensor_tensor", ckvn[:, 0:TT], [ckv[:, 0:TT], kva_g[:, 0:1], rs[:, 0:TT]], ALU.mult, ALU.mult)
                kb.v("dve", "scalar_tensor_tensor", rk[64:96, 0:TT], [kpe[64:96, 0:TT], kn[64:96, 0:1], cs[64:96, 0:TT]], ALU.mult, ALU.mult)
                kb.v("dve", "scalar_tensor_tensor", t2s[0][64:96, 0:TT], [kpes[64:96, 0:TT], kn[64:96, 1:2], sn[64:96, 0:TT]], ALU.mult, ALU.mult)
                kb.v("pool", "tensor_tensor", rk[64:96, 0:TT], [rk[64:96, 0:TT], t2s[0][64:96, 0:TT]], ALU.add)
                for h in range(NH):
                    a, b2, s_, t1, t2, sqh, rsh, oh = pA[u % 2], pB[u % 2], pS[u % 2], t1s[u % 2], t2s[u % 2], sqhs[u % 2], rshs[u % 2], ohs[u % 2]
                    u += 1
                    for c in range(2):
                        kb.mm(a[0:96, 0:TT], Wq[:, c, h * 96:(h + 1) * 96], cqn[:, c, 0:TT], start=(c == 0), stop=(c == 1))
                    for c in range(2):
                        kb.mm(b2[64:96, 0:TT], Wqs[:, c, h, :], cqn[:, c, 0:TT], start=(c == 0), stop=(c == 1))
                    kb.act(sqh[0:96, 0:TT], a[0:96, 0:TT], AF.Square)
                    kb.mm(s_[0:96, 0:TT], self.ones[0:96, 0:96], sqh[0:96, 0:TT])
                    rstd_inplace(kb, rsh[0:96, 0:TT], 1.0 / 96, EPS, src=s_[0:96, 0:TT])
                    kb.v("dve", "scalar_tensor_tensor", oh[0:64, 0:TT], [a[0:64, 0:TT], qn[0:64, 0:1], rsh[0:64, 0:TT]], ALU.mult, ALU.mult)
                    kb.v("dve", "scalar_tensor_tensor", t1[64:96, 0:TT], [a[64:96, 0:TT], qn[64:96, 0:1], cs[64:96, 0:TT]], ALU.mult, ALU.mult)
                    kb.v("dve", "scalar_tensor_tensor", t2[64:96, 0:TT], [b2[64:96, 0:TT], qn[64:96, 1:2], sn[64:96, 0:TT]], ALU.mult, ALU.mult)
                    kb.v("pool", "tensor_tensor", t1[64:96, 0:TT], [t1[64:96, 0:TT], t2[64:96, 0:TT]], ALU.add)
                    kb.v("dve", "tensor_tensor", oh[64:96, 0:TT], [t1[64:96, 0:TT], rsh[64:96, 0:TT]], ALU.mult)
                    kb.dma(self.QF[h, :, t0:t0 + TT], oh[0:96, 0:TT])
                    a, s_, sqh, rsh, oh = pA[u % 2], pS[u % 2], sqhs[u % 2], rshs[u % 2], ohs[u % 2]
                    u += 1
                    kb.mm(a[0:64, 0:TT], Wk[:, h, :], ckvn[:, 0:TT])
                    kb.act(sqh[0:64, 0:TT], a[0:64, 0:TT], AF.Square)
                    kb.act(sqh[64:96, 0:TT], kpe[64:96, 0:TT], AF.Square)
                    kb.mm(s_[0:96, 0:TT], self.ones[0:96, 0:96], sqh[0:96, 0:TT])
                    rstd_inplace(kb, rsh[0:96, 0:TT], 1.0 / 96, EPS, src=s_[0:96, 0:TT])
                    kb.v("dve", "scalar_tensor_tensor", oh[0:64, 0:TT], [a[0:64, 0:TT], kn[0:64, 0:1], rsh[0:64, 0:TT]], ALU.mult, ALU.mult)
                    kb.v("dve", "tensor_tensor", oh[64:96, 0:TT], [rk[64:96, 0:TT], rsh[64:96, 0:TT]], ALU.mult)
                    kb.dma(self.KF[h, :, t0:t0 + TT], oh[0:96, 0:TT])
                for s in range(TT // 128):
                    vb = vsb[s % 2]
                    kb.mm(pV[:], ckvn[:, s * 128:(s + 1) * 128], Wvv[:].rearrange("p h d -> p (h d)"))
                    self.evac(vb[:], pV[:])
                    kb.dma(self.VT[t0 + s * 128:t0 + (s + 1) * 128, :], vb[:])

    def phase_e3(self, j):
        kb, cfg = self.kb, self.cfg
        self.AF_ = self.scratch(f"AF{j}", [512, cfg.T], BF16)
        nsub = cfg.nsub
        with kb.phase() as ph:
            Vall = ph.sb([128, nsub, NH, 65], BF16, "Vall")
            kb.v("pool", "memset", Vall[:], [], 1.0)
            for h_ in range(NH):
                kb.dma(Vall[:, :, h_, 0:64], self.VT[:, h_ * 64:(h_ + 1) * 64].rearrange("(b p) n -> p b n", p=128))
            sel = ph.sb([65, 64], F32, "sel")
            kb.v("pool", "memset", sel[:], [], 0.0)
            kb.v("pool", "memset", sel[64:65, :], [], 1.0)
            oext = ph.sb([65, 512], F32, "oext")
            Khs = [ph.sb([96, cfg.T], BF16, "Kh") for _ in range(2)]
            Qts = [ph.sb([96, 512], BF16, "Qt") for _ in range(2)]
            PTs = [ph.sb([128, 512], BF16, "PT") for _ in range(5)]
            rden = ph.sb([64, 512], F32, "rden")
            osb = [ph.sb([64, 512], BF16, "osb") for _ in range(2)]
            pS = [ph.ps() for _ in range(5)]
            pO = [ph.ps() for _ in range(2)]
            pD = [ph.ps() for _ in range(1)]
            DEPTH = 4
            units = []
            gi = 0
            for h in range(NH):
                for ti, (t0, TT) in enumerate(cfg.tiles):
                    nkb = 2 if ti == 0 else nsub
                    for kbk in range(nkb):
                        units.append((h, ti, t0, TT, kbk, nkb, gi))
                    gi += 1
            cur_h = [-1]
            cur_g = [-1]
            for idx in range(len(units) + DEPTH):
                if idx < len(units):
                    h, ti, t0, TT, kbk, nkb, g = units[idx]
                    if h != cur_h[0]:
                        cur_h[0] = h
                        kb.dma(Khs[h % 2][:], self.KF[h])
                    if g != cur_g[0]:
                        cur_g[0] = g
                        kb.dma(Qts[g % 2][:, 0:TT], self.QF[h, :, t0:t0 + TT])
                    ps, pt = pS[idx % 5], PTs[idx % 5]
                    kb.mm(ps[:, 0:TT], Khs[h % 2][:, kbk * 128:(kbk + 1) * 128], Qts[g % 2][:, 0:TT])
                    kb.act(pt[:, 0:TT], ps[:, 0:TT], AF.Exp, scale=MLA_SCALE)
                j2 = idx - DEPTH
                if j2 >= 0:
                    h, ti, t0, TT, kbk, nkb, g = units[j2]
                    pt = PTs[j2 % 5]
                    po, pd, ob = pO[g % 2], pD[0], osb[g % 2]
                    kb.mm(po[0:65, 0:TT], Vall[:, kbk, h, :], pt[:, 0:TT], start=(kbk == 0), stop=(kbk == nkb - 1))
                    if kbk == nkb - 1:
                        kb.act(oext[:, 0:TT], po[0:65, 0:TT], AF.Copy)
                        kb.mm(pd[0:64, 0:TT], sel[:], oext[:, 0:TT])
                        kb.v("dve", "reciprocal", rden[:, 0:TT], [pd[0:64, 0:TT]])
                        kb.v("dve", "tensor_tensor", ob[:, 0:TT], [po[0:64, 0:TT], rden[:, 0:TT]], ALU.mult)
                        kb.dma(self.AF_[h * 64:(h + 1) * 64, t0:t0 + TT], ob[:, 0:TT])
    def load_const_tile(self, ph, name, dtype=F32):
        shp = CONST_SHAPES(self.cfg)[name]
        t = ph.sb(list(shp), F32, name)
        self.kb.dma(t[:], self.cin[name])
        if dtype == F32:
            return t
        t2 = ph.sb(list(shp), dtype, name + "b")
        self.kb.v("dve", "tensor_copy", t2[:], [t[:]])
        return t2

    def phase_e4(self, j, sm):
        kb, cfg = self.kb, self.cfg
        T = cfg.T
        self.GQ = self.scratch(f"GQ{j}", [GH, 128, T], BF16)
        self.GK = self.scratch(f"GK{j}", [GH, 128, T], BF16)
        self.GKT = self.scratch(f"GKT{j}", [T, 512], BF16)
        self.GVT = self.scratch(f"GVT{j}", [T, 512], BF16)
        last = len(cfg.tiles) - 1
        with kb.phase() as ph:
            cw = ph.sb([128, 12, 5], F32, "cw")
            kb.dma(cw[:], sm["convw"])
            xs = [ph.sb([128, 516], F32, "x") for _ in range(2)]
            accs = [ph.sb([128, 512], F32, "acc") for _ in range(2)]
            ys = [ph.sb([128, 512], F32, "y") for _ in range(2)]
            sqs = [ph.sb([128, 512], BF16, "sq") for _ in range(2)]
            rs = ph.sb([128, 512], F32, "rs")
            yns = [ph.sb([128, 512], BF16, "yn") for _ in range(2)]
            tsb = [ph.sb([128, 4, 128], BF16, "tsb") for _ in range(2)]
            pR = [ph.ps() for _ in range(2)]
            ptr = [ph.ps([128, 1024], BF16, "ptr") for _ in range(2)]
            u = 0
            for ti, (t0, TT) in enumerate(cfg.tiles):
                for cc in range(12):
                    x, acc, y, sq, yn, pr, pt, tb = xs[u % 2], accs[u % 2], ys[u % 2], sqs[u % 2], yns[u % 2], pR[u % 2], ptr[u % 2], tsb[u % 2]
                    u += 1
                    r0 = 448 + cc * 128
                    kb.dma(x[:, 2:TT + 2], self.UF[r0:r0 + 128, t0:t0 + TT])
                    if ti in (0, 1):
                        kb.v("pool", "memset", x[:, 0:2], [], 0.0)
                    else:
                        kb.dma(x[:, 0:2], self.UF[r0:r0 + 128, t0 - 2:t0])
                    if ti in (0, last):
                        kb.v("pool", "memset", x[:, TT + 2:TT + 4], [], 0.0)
                    else:
                        kb.dma(x[:, TT + 2:TT + 4], self.UF[r0:r0 + 128, t0 + TT:t0 + TT + 2])
                    kb.v("dve", "tensor_scalar", acc[:, 0:TT], [x[:, 0:TT]], cw[:, cc, 0:1], None, ALU.mult)
                    for jj in range(1, 5):
                        kb.v("dve", "scalar_tensor_tensor", acc[:, 0:TT], [x[:, jj:jj + TT], cw[:, cc, jj:jj + 1], acc[:, 0:TT]], ALU.mult, ALU.add)
                    kb.act(y[:, 0:TT], acc[:, 0:TT], AF.Silu)
                    if cc < 8:
                        kb.act(sq[:, 0:TT], y[:, 0:TT], AF.Square)
                        kb.mm(pr[:, 0:TT], self.ones[:], sq[:, 0:TT])
                        rstd_inplace(kb, rs[:, 0:TT], 1.0, EPS, src=pr[:, 0:TT])
                        kb.v("dve", "scalar_tensor_tensor", yn[:, 0:TT], [y[:, 0:TT], (128 ** -0.5) if cc < 4 else 1.0, rs[:, 0:TT]], ALU.mult, ALU.mult)
                        dst = self.GQ[cc] if cc < 4 else self.GK[cc - 4]
                        kb.dma(dst[:, t0:t0 + TT], yn[:, 0:TT])
                    else:
                        kb.v("pool", "tensor_copy", yn[:, 0:TT], [y[:, 0:TT]])
                    if cc >= 4:
                        ns = TT // 128
                        for s in range(ns):
                            kb.tr(pt[:, s * 128:(s + 1) * 128], yn[:, s * 128:(s + 1) * 128], self.ident[:])
                        self.evac(tb[:, 0:ns, :], pt[:, 0:TT].rearrange("p (s c) -> p s c", c=128))
                        dstT = self.GKT if cc < 8 else self.GVT
                        hh = (cc - 4) % 4
                        kb.dma(dstT[t0:t0 + TT, hh * 128:(hh + 1) * 128].rearrange("(s p) c -> p s c", p=128), tb[:, 0:ns, :])

    def phase_e5(self, j, sm):
        kb, cfg = self.kb, self.cfg
        T, nsub = cfg.T, cfg.nsub
        self.OD = [self.scratch(f"OD{j}_{d}", [T, 512]) for d in range(2)]
        fwd = list(range(nsub))
        bwd = [1, 0] + list(range(nsub - 1, 1, -1))
        with kb.phase() as ph:
            mle = self.load_const_tile(ph, "m_le")
            mge = self.load_const_tile(ph, "m_ge")
            mlt = self.load_const_tile(ph, "m_lt")
            mgt = self.load_const_tile(ph, "m_gt")
            msame = self.load_const_tile(ph, "m_same")
            mh0 = self.load_const_tile(ph, "m_h0")
            mh1 = self.load_const_tile(ph, "m_h1")
            onesf = self.load_const_tile(ph, "ones_f")
            gv = ph.sb([128, 16], F32, "gv")
            kb.dma(gv[:], sm["gdn_vec"].broadcast_to([128, 16]))
            negA = ph.sb([128, 8], F32, "negA")
            kb.act(negA[:], gv[:, 0:8], AF.Exp)
            kb.v("dve", "tensor_scalar", negA[:], [negA[:]], -1.0, None, ALU.mult)
            S = [[ph.sb([128, 128], F32, f"S{d}{h}") for h in range(GH)] for d in range(2)]
            Sb = [[ph.sb([128, 128], BF16, f"Sb{d}{h}") for h in range(GH)] for d in range(2)]
            for d in range(2):
                for h in range(GH):
                    kb.v("pool", "memset", S[d][h][:], [], 0.0)
                    kb.v("pool", "memset", Sb[d][h][:], [], 0.0)
            banks = [ph.ps() for _ in range(7)]
            tbank = ph.ps([128, 1024], BF16, "tbank")
            slot = [0]
            tslot = [0]

            def ps128t():
                i = tslot[0] % 8
                tslot[0] += 1
                return V(tbank[:, i * 128:(i + 1) * 128], tbank.name)

            def ps128(bf=False):
                i = slot[0] % 7
                slot[0] += 1
                b = banks[i]
                return V(b[:, 0:128], b.name)

            def sub(v, r, cs=None):
                a = v.ap[r, :] if cs is None else v.ap[r, cs]
                return V(a, v.key)

            UB = []
            for d in range(4):
                b = {}
                b["ab"] = ph.sb([128, 16], F32, "ab")
                b["KT"] = ph.sb([128, GH, 128], BF16, "KT")
                b["QT"] = ph.sb([128, GH, 128], BF16, "QT")
                b["Ktm"] = ph.sb([128, 512], BF16, "Ktm")
                b["Vtm"] = ph.sb([128, 512], BF16, "Vtm")
                for nm in ("xa", "g", "beta", "nbeta", "gc", "gt", "eg", "ek", "beg", "egl0", "egl1"):
                    b[nm] = ph.sb([128, 4], F32, nm)
                b["osb"] = ph.sb([128, 512], F32, "osb")
                UB.append(b)
            HB = []
            for i in range(8):
                b = {}
                for nm in ("diag", "e1", "e2", "Dst", "DT", "EG", "usb"):
                    b[nm] = ph.sb([128, 128], F32, nm)
                for nm in ("X0", "X1", "XT0", "XT1", "AT0", "AT1", "Vb", "Kbg", "Khat", "QgT", "AqkT", "wT", "vnew"):
                    b[nm] = ph.sb([128, 128], BF16, nm)
                HB.append(b)
            def unit(d, h, B, Hb):
                hs = slice(h * 128, (h + 1) * 128)
                gcol = B["gc"][:, h:h + 1]
                kb.v("dve", "tensor_scalar", Hb["diag"][:], [self.ident_f[:]], gcol, None, ALU.mult)
                pg = ps128()
                kb.mm(pg, onesf[:], Hb["diag"][:])
                kb.v("dve", "tensor_scalar", Hb["e1"][:], [pg], gcol, 0.0, ALU.subtract, ALU.max)
                kb.v("dve", "tensor_scalar", Hb["e2"][:], [pg], gcol, 0.0, ALU.subtract, ALU.min)
                kb.act(Hb["EG"][:], pg, AF.Exp)
                kb.act(Hb["e1"][:], Hb["e1"][:], AF.Exp, scale=-1.0)
                kb.act(Hb["e2"][:], Hb["e2"][:], AF.Exp)
                kb.v("pool", "tensor_tensor", Hb["Dst"][:], [Hb["e1"][:], (mgt if d == 0 else mlt)[:]], ALU.mult)
                kb.v("pool", "tensor_tensor", Hb["DT"][:], [Hb["e2"][:], (mle if d == 0 else mge)[:]], ALU.mult)
                yield
                pkk = ps128()
                kb.mm(pkk, B["KT"][:, h, :], B["KT"][:, h, :])
                kb.v("dve", "scalar_tensor_tensor", Hb["X0"][:], [pkk, B["nbeta"][:, h:h + 1], Hb["Dst"][:]], ALU.mult, ALU.mult)
                pkq = ps128()
                kb.mm(pkq, B["KT"][:, h, :], B["QT"][:, h, :])
                kb.v("dve", "tensor_tensor", Hb["AqkT"][:], [pkq, Hb["DT"][:]], ALU.mult)
                yield
                pxt_b = ps128t()
                kb.tr(pxt_b, Hb["X0"][:], self.ident[:])
                kb.v("dve", "tensor_copy", Hb["XT0"][:], [pxt_b])
                kb.v("pool", "tensor_tensor", Hb["AT0"][:], [Hb["XT0"][:], self.ident[:]], ALU.add)
                yield
                X, XT, AT = Hb["X0"], Hb["XT0"], Hb["AT0"]
                for js in range(1, 6):
                    Xn = Hb["X1"] if X is Hb["X0"] else Hb["X0"]
                    XTn = Hb["XT1"] if XT is Hb["XT0"] else Hb["XT0"]
                    ATn = Hb["AT1"] if AT is Hb["AT0"] else Hb["AT0"]
                    px = ps128()
                    kb.mm(px, XT[:], X[:])
                    if js < 5:
                        pxT = ps128()
                        kb.mm(pxT, X[:], XT[:])
                    kb.act(Xn[:], px, AF.Copy)
                    if js < 5:
                        kb.v("dve", "tensor_copy", XTn[:], [pxT])
                    yield
                    pa = ps128()
                    kb.mm(pa, Xn[:], AT[:])
                    kb.v("dve", "tensor_tensor", ATn[:], [pa, AT[:]], ALU.add)
                    X, XT, AT = Xn, XTn, ATn
                    yield
                kb.v("pool", "tensor_scalar", Hb["Vb"][:], [B["Vtm"][:, hs]], B["beta"][:, h:h + 1], 1.0, ALU.mult, ALU.mult)
                kb.v("pool", "tensor_scalar", Hb["Kbg"][:], [B["Ktm"][:, hs]], B["beg"][:, h:h + 1], 1.0, ALU.mult, ALU.mult)
                kb.act(Hb["Khat"][:], B["Ktm"][:, hs], AF.Copy, scale=B["ek"][:, h:h + 1])
                kb.v("pool", "tensor_tensor", Hb["QgT"][:], [B["QT"][:, h, :], Hb["EG"][:]], ALU.mult)
                pu = ps128()
                kb.mm(pu, AT[:], Hb["Vb"][:])
                kb.act(Hb["usb"][:], pu, AF.Copy)
                pw = ps128()
                kb.mm(pw, Hb["Kbg"][:], AT[:])
                kb.act(Hb["wT"][:], pw, AF.Copy)
                yield
                for half in ((0, 1) if d == 0 else (1, 0)):
                    r = slice(half * 64, half * 64 + 64)
                    egl = B["egl0"] if half == 0 else B["egl1"]
                    oq = V(B["osb"][r, hs], (B["osb"].name, h))
                    p1 = ps128()
                    kb.mm(sub(p1, r), Hb["wT"][:, r], Sb[d][h][:])
                    kb.v("dve", "tensor_tensor", Hb["vnew"][r, :], [Hb["usb"][r, :], sub(p1, r)], ALU.subtract)
                    yield
                    p2 = ps128()
                    kb.mm(sub(p2, r), Hb["QgT"][:, r], Sb[d][h][:], start=True, stop=False)
                    kb.mm(sub(p2, r), Hb["AqkT"][r, r], Hb["vnew"][r, :], start=False, stop=True)
                    kb.act(oq, sub(p2, r), AF.Copy)
                    p3 = ps128()
                    kb.mm(p3, Hb["Khat"][r, :], Hb["vnew"][r, :])
                    kb.v("dve", "scalar_tensor_tensor", S[d][h][:], [S[d][h][:], egl[:, h:h + 1], p3], ALU.mult, ALU.add)
                    kb.act(Sb[d][h][:], S[d][h][:], AF.Copy)
                    yield

            for step in range(nsub):
                makers = []
                Bs = []
                for d in range(2):
                    sb_ = fwd[step] if d == 0 else bwd[step]
                    r0 = sb_ * 128
                    B = UB[(step % 2) * 2 + d]
                    Bs.append((B, r0, d))
                    kb.dma(B["ab"][:], self.UT[r0:r0 + 128, 512:528])
                    kb.dma(B["KT"][:], self.GK[:, :, r0:r0 + 128].rearrange("h c t -> c h t"))
                    kb.dma(B["QT"][:], self.GQ[:, :, r0:r0 + 128].rearrange("h c t -> c h t"))
                    kb.dma(B["Ktm"][:], self.GKT[r0:r0 + 128, :])
                    kb.dma(B["Vtm"][:], self.GVT[r0:r0 + 128, :])
                    ab = B["ab"]
                    kb.v("dve", "tensor_tensor", B["xa"][:], [ab[:, d * 8:d * 8 + 4], gv[:, 8 + d * 4:12 + d * 4]], ALU.add)
                    kb.act(B["xa"][:], B["xa"][:], AF.Exp)
                    kb.act(B["xa"][:], B["xa"][:], AF.Ln, bias=1.0)
                    kb.v("dve", "tensor_tensor", B["g"][:], [B["xa"][:], negA[:, d * 4:d * 4 + 4]], ALU.mult)
                    kb.act(B["beta"][:], ab[:, d * 8 + 4:d * 8 + 8], AF.Sigmoid)
                    kb.v("dve", "tensor_scalar", B["nbeta"][:], [B["beta"][:]], -1.0, None, ALU.mult)
                    Mc = mle if d == 0 else mge
                    p_gc, p_gt, p_0, p_1 = ps128(), ps128(), ps128(), ps128()
                    kb.mm(sub(p_gc, slice(0, 128), slice(0, 4)), Mc[:], B["g"][:])
                    kb.mm(sub(p_gt, slice(0, 128), slice(0, 4)), msame[:], B["g"][:])
                    kb.mm(sub(p_0, slice(0, 128), slice(0, 4)), mh0[:], B["g"][:])
                    kb.mm(sub(p_1, slice(0, 128), slice(0, 4)), mh1[:], B["g"][:])
                    kb.v("dve", "tensor_copy", B["gc"][:], [sub(p_gc, slice(0, 128), slice(0, 4))])
                    kb.v("dve", "tensor_tensor", B["gt"][:], [sub(p_gt, slice(0, 128), slice(0, 4)), B["gc"][:]], ALU.subtract)
                    kb.act(B["ek"][:], B["gt"][:], AF.Exp)
                    kb.act(B["eg"][:], B["gc"][:], AF.Exp)
                    kb.act(B["egl0"][:], sub(p_0, slice(0, 128), slice(0, 4)), AF.Exp)
                    kb.act(B["egl1"][:], sub(p_1, slice(0, 128), slice(0, 4)), AF.Exp)
                    kb.v("dve", "tensor_tensor", B["beg"][:], [B["beta"][:], B["eg"][:]], ALU.mult)
                for h in range(GH):
                    for (B, r0, d) in Bs:
                        makers.append(lambda slot_, d=d, h=h, B=B: unit(d, h, B, HB[slot_]))
                interleave(makers, 8)
                for (B, r0, d) in Bs:
                    kb.dma(self.OD[d][r0:r0 + 128, :], B["osb"][:], extra_reads=[(B["osb"].name, h) for h in range(GH)])

    def phase_e6(self, l, j, sm):
        kb, cfg = self.kb, self.cfg
        Wout = self.inp(f"hy_w_out__{j}", [D, D])
        self.Fm = self.scratch(f"F{l}", [cfg.T, D], BF16)
        with kb.phase() as ph:
            Wo = ph.sb([128, 8, D], BF16, "Wo")
            kb.dma(Wo[:], Wout.rearrange("(c p) n -> p c n", p=128), eng="pool")
            og = ph.sb([128, 128], F32, "og")
            kb.dma(og[:], sm["outg"].broadcast_to([128, 128]))
            o0s = [ph.sb([128, 512], F32, "o0") for _ in range(2)]
            o1s = [ph.sb([128, 512], F32, "o1") for _ in range(2)]
            zs = [ph.sb([128, 512], F32, "z") for _ in range(2)]
            sqo = ph.sb([128, 512], F32, "sqo")
            ssq = [ph.sb([128, 4], F32, "ssq") for _ in range(2)]
            on = ph.sb([128, 512], F32, "on")
            dtm = [ph.sb([128, 512], BF16, "dtm") for _ in range(2)]
            XT = [ph.sb([128, 8, 128], BF16, "XT") for _ in range(2)]
            ptr = [ph.ps([128, 1024], BF16, "ptr") for _ in range(2)]
            py = [ph.ps() for _ in range(4)]
            hts = [ph.sb([128, D], F32, "ht") for _ in range(2)]
            tmp = [ph.sb([128, D], F32, "tmp") for _ in range(2)]
            junk = ph.sb([128, D], F32, "junk")
            sss = [ph.sb([128, 1], F32, "ss") for _ in range(2)]
            tmp2 = [ph.sb([128, D], F32, "tmp2") for _ in range(2)]
            fb = [ph.sb([128, D], BF16, "fb") for _ in range(2)]
            for s in range(cfg.nsub):
                mod = self.modC if s < 2 else self.modL
                r0 = s * 128
                i = s % 2
                kb.dma(o0s[i][:], self.OD[0][r0:r0 + 128, :])
                kb.dma(o1s[i][:], self.OD[1][r0:r0 + 128, :])
                kb.dma(zs[i][:], self.UT[r0:r0 + 128, 0:512])
                kb.dma(XT[i][:, 0:4, :], self.AF_[:, r0:r0 + 128].rearrange("(c p) t -> p c t", p=128))
                kb.dma(hts[i][:], self.H[r0:r0 + 128, :])
                kb.v("pool", "tensor_tensor", o0s[i][:], [o0s[i][:], o1s[i][:]], ALU.add)
                kb.v("dve", "tensor_tensor", sqo[:], [o0s[i][:], o0s[i][:]], ALU.mult)
                kb.v("dve", "tensor_reduce", ssq[i][:], [sqo[:].rearrange("p (h v) -> p h v", h=4)], AX.X, ALU.add)
                rstd_inplace(kb, ssq[i][:], 1.0 / 128, EPS)
                for h in range(4):
                    hs = slice(h * 128, (h + 1) * 128)
                    kb.v("dve", "scalar_tensor_tensor", on[:, hs], [o0s[i][:, hs], ssq[i][:, h:h + 1], og[:]], ALU.mult, ALU.mult)
                kb.act(zs[i][:], zs[i][:], AF.Silu)
                kb.v("pool", "tensor_tensor", dtm[i][:], [on[:], zs[i][:]], ALU.mult)
                for c in range(4):
                    kb.tr(ptr[i][:, c * 128:(c + 1) * 128], dtm[i][:, c * 128:(c + 1) * 128], self.ident[:])
                self.evac(XT[i][:, 4:8, :], ptr[i][:, 0:512].rearrange("p (c t) -> p c t", c=4))
                for n in range(2):
                    pp = py[(2 * s + n) % 4]
                    for c in range(8):
                        kb.mm(pp[:], XT[i][:, c, :], Wo[:, c, n * 512:(n + 1) * 512], start=(c == 0), stop=(c == 7))
                    kb.v("dve", "tensor_tensor", tmp[i][:, n * 512:(n + 1) * 512], [pp[:], mod[2][:, n * 512:(n + 1) * 512]], ALU.mult)
                kb.v("pool", "tensor_tensor", hts[i][:], [tmp[i][:], hts[i][:]], ALU.add)
                kb.dma(self.H[r0:r0 + 128, :], hts[i][:])
                self.norm_sub(hts[i][:], junk[:], sss[i][:], tmp2[i][:], fb[i][:], mod[4][:], mod[3][:])
                kb.dma(self.Fm[r0:r0 + 128, :], fb[i][:])
    def moe_setup(self):
        kb, cfg = self.kb, self.cfg
        if hasattr(self, "OH"):
            return
        ns, nb = cfg.nsub, cfg.nblk
        self.OH = kb.sb([128, ns, 64], BF16, "OH")
        self.WTS = kb.sb([128, ns, 2], F32, "WTS")
        self.DEST = kb.sb([128, ns, 2], I32, "DEST")
        self.I1 = kb.sb([128, nb, 8], I32, "I1")
        self.I2 = kb.sb([128, nb, 4], I32, "I2")
        self.offs = kb.sb([128, 32], F32, "offs")
        self.XS = self.scratch("XS", [cfg.nslot, D], BF16)
        self.YS = self.scratch("YS", [cfg.nslot, D], F32)

    def moe(self, l):
        self.moe_setup()
        kb, cfg = self.kb, self.cfg
        ns, nb = cfg.nsub, cfg.nblk
        wg = self.inp(f"moe_w_group__{l}", [D, 4])
        bg = self.inp(f"moe_b_group__{l}", [1, 4])
        we = self.inp(f"moe_w_expert__{l}", [D, 32])
        be = self.inp(f"moe_b_expert__{l}", [1, 32])
        w1 = self.inp(f"moe_w1__{l}", [32 * D, 512])
        w3 = self.inp(f"moe_w3__{l}", [32 * D, 512])
        w2 = self.inp(f"moe_w2__{l}", [32 * 512, D])
        OH, WTS, DEST, I1, I2, offs = self.OH, self.WTS, self.DEST, self.I1, self.I2, self.offs
        with kb.phase() as ph:
            Wr = ph.sb([128, 8, 36], BF16, "Wr")
            kb.dma(Wr[:, :, 0:4], wg.rearrange("(c p) n -> p c n", p=128), eng="pool")
            kb.dma(Wr[:, :, 4:36], we.rearrange("(c p) n -> p c n", p=128), eng="pool")
            br = ph.sb([128, 36], F32, "br")
            kb.dma(br[:, 0:4], bg.broadcast_to([128, 4]))
            kb.dma(br[:, 4:36], be.broadcast_to([128, 32]))
            zt = ph.sb([128, D], BF16, "zt")
            kb.v("pool", "memset", zt[:], [], 0.0)
            for i in range(cfg.nslot // 128):
                kb.dma(self.XS[i * 128:(i + 1) * 128, :], zt[:], wkey=("XS", "z", i))
            fts = [ph.sb([128, D], BF16, "ft") for _ in range(2)]
            fT = [ph.sb([128, 8, 128], BF16, "fT") for _ in range(2)]
            ptr = [ph.ps([128, 1024], BF16, "ptr") for _ in range(2)]
            plg = [ph.ps() for _ in range(2)]
            pcnt = ph.ps()
            sm_ = [{nm: ph.sb([128, w], F32, nm) for nm, w in (("lg", 36), ("mx", 1), ("nmx", 1), ("eg", 4), ("sg", 1), ("pg", 1), ("G1", 4),
                                                               ("pen", 4), ("lem", 32), ("top", 8), ("dif", 1), ("r", 1), ("den", 1))} for _ in range(2)]
            osum = [ph.sb([128, 32], BF16, "osum") for _ in range(2)]
            for s in range(ns):
                i = s % 2
                ft, t = fts[i], sm_[i]
                kb.dma(ft[:], self.Fm[s * 128:(s + 1) * 128, :])
                for c in range(8):
                    kb.tr(ptr[i][:, c * 128:(c + 1) * 128], ft[:, c * 128:(c + 1) * 128], self.ident[:])
                self.evac(fT[i][:], ptr[i][:].rearrange("p (c t) -> p c t", c=8))
                for c in range(8):
                    kb.mm(plg[i][:, 0:36], fT[i][:, c, :], Wr[:, c, :], start=(c == 0), stop=(c == 7))
                kb.v("dve", "tensor_tensor", t["lg"][:], [plg[i][:, 0:36], br[:]], ALU.add)
                kb.v("dve", "reduce_max", t["mx"][:], [t["lg"][:, 0:4]], AX.X)
                kb.v("dve", "tensor_scalar", t["nmx"][:], [t["mx"][:]], -1.0, None, ALU.mult)
                kb.act(t["eg"][:], t["lg"][:, 0:4], AF.Exp, bias=t["nmx"][:], accum_out=t["sg"][:])
                kb.v("dve", "reciprocal", t["pg"][:], [t["sg"][:]])
                kb.v("dve", "tensor_scalar", t["G1"][:], [t["lg"][:, 0:4]], t["mx"][:], None, ALU.is_equal)
                kb.v("dve", "tensor_scalar", t["pen"][:], [t["G1"][:]], 1e30, -1e30, ALU.mult, ALU.add)
                for g in range(4):
                    kb.v("dve", "tensor_scalar", t["lem"][:, g * 8:(g + 1) * 8], [t["lg"][:, 4 + g * 8:12 + g * 8]], t["pen"][:, g:g + 1], None, ALU.add)
                kb.v("dve", "max", t["top"][:], [t["lem"][:]])
                kb.v("dve", "tensor_scalar", OH[:, s, 0:32], [t["lem"][:]], t["top"][:, 0:1], None, ALU.is_equal)
                kb.v("dve", "tensor_scalar", OH[:, s, 32:64], [t["lem"][:]], t["top"][:, 1:2], None, ALU.is_equal)
                kb.v("dve", "tensor_tensor", t["dif"][:], [t["top"][:, 1:2], t["top"][:, 0:1]], ALU.subtract)
                kb.act(t["r"][:], t["dif"][:], AF.Exp)
                kb.v("dve", "tensor_scalar", t["den"][:], [t["r"][:]], 1.0, None, ALU.add)
                kb.v("dve", "reciprocal", t["den"][:], [t["den"][:]])
                kb.v("dve", "tensor_tensor", WTS[:, s, 0:1], [t["pg"][:], t["den"][:]], ALU.mult)
                kb.v("dve", "tensor_tensor", WTS[:, s, 1:2], [WTS[:, s, 0:1], t["r"][:]], ALU.mult)
                kb.v("pool", "tensor_tensor", osum[i][:], [OH[:, s, 0:32], OH[:, s, 32:64]], ALU.add)
                kb.mm(pcnt[:, 0:32], self.ones[:], osum[i][:], start=(s == 0), stop=(s == ns - 1))
            cnt = ph.sb([128, 32], F32, "cnt")
            kb.v("dve", "tensor_copy", cnt[:], [pcnt[:, 0:32]])
            thr = ph.sb([128, 34], F32, "thr")
            kb.dma(thr[:], self.cin["thr"])
            cmp = ph.sb([128, 32, 34], F32, "cmp")
            kb.v("dve", "tensor_tensor", cmp[:], [cnt[:].unsqueeze(2).to_broadcast([128, 32, 34]), thr[:].unsqueeze(1).to_broadcast([128, 32, 34])], ALU.is_gt)
            padded = ph.sb([128, 32], F32, "padded")
            kb.v("dve", "tensor_reduce", padded[:], [cmp[:]], AX.X, ALU.add)
            kb.v("dve", "tensor_scalar", padded[:], [padded[:]], float(MOE_BS), None, ALU.mult)
            onesr = ph.sb([128, 32], F32, "onesr")
            kb.v("pool", "memset", onesr[:], [], 1.0)
            pend = ph.sb([128, 32], F32, "pend")
            kb.v("dve", "tensor_tensor_scan", pend[:], [onesr[:], padded[:]], 0.0, ALU.mult, ALU.add)
            kb.v("dve", "tensor_tensor", offs[:], [pend[:], padded[:]], ALU.subtract)
            bst = ph.sb([128, nb], F32, "bst")
            kb.dma(bst[:], self.cin["blk_start"])
            cmp2 = ph.sb([128, nb, 32], F32, "cmp2")
            kb.v("dve", "tensor_tensor", cmp2[:], [pend[:].unsqueeze(1).to_broadcast([128, nb, 32]), bst[:].unsqueeze(2).to_broadcast([128, nb, 32])], ALU.is_le)
            blke = ph.sb([128, nb], F32, "blke")
            kb.v("dve", "tensor_reduce", blke[:], [cmp2[:]], AX.X, ALU.add)
            kb.v("dve", "tensor_scalar", blke[:], [blke[:]], 31.0, None, ALU.min)
            iop = ph.sb([128, 1], F32, "iop")
            kb.dma(iop[:], self.cin["iota_p"])
            b1 = ph.sb([128, nb], F32, "b1")
            b2 = ph.sb([128, nb], F32, "b2")
            kb.v("dve", "tensor_scalar", b1[:], [blke[:]], 1024.0, iop[:, 0:1], ALU.mult, ALU.add)
            kb.v("dve", "tensor_scalar", b2[:], [blke[:]], 512.0, iop[:, 0:1], ALU.mult, ALU.add)
            for c in range(8):
                kb.v("dve", "tensor_scalar", I1[:, :, c], [b1[:]], float(c * 128), None, ALU.add)
            for c in range(4):
                kb.v("dve", "tensor_scalar", I2[:, :, c], [b2[:]], float(c * 128), None, ALU.add)
            tri = ph.sb([128, 128], F32, "trif")
            kb.dma(tri[:], self.cin["tri_lt_full"])
            trib = ph.sb([128, 128], BF16, "trib")
            kb.v("dve", "tensor_copy", trib[:], [tri[:]])
            kb.barrier()
            basep = ph.sb([128, 32], F32, "basep")
            kb.v("dve", "tensor_copy", basep[:], [offs[:]])
            pcx = [ph.ps() for _ in range(2)]
            pos = [ph.sb([128, 32], F32, "pos") for _ in range(2)]
            tm = [ph.sb([128, 32], F32, "tm") for _ in range(2)]
            dd = [ph.sb([128, 2], F32, "dd") for _ in range(2)]
            for s in range(ns):
                i = s % 2
                kb.v("pool", "tensor_tensor", osum[i][:], [OH[:, s, 0:32], OH[:, s, 32:64]], ALU.add)
                kb.mm(pcx[i][:, 0:32], trib[:], osum[i][:])
                kb.mm(pcx[i][:, 32:64], self.ones[:], osum[i][:])
                kb.v("dve", "tensor_tensor", pos[i][:], [pcx[i][:, 0:32], basep[:]], ALU.add)
                kb.v("dve", "tensor_tensor", basep[:], [pcx[i][:, 32:64], basep[:]], ALU.add)
                for k in range(2):
                    kb.v("dve", "tensor_tensor", tm[i][:], [OH[:, s, k * 32:(k + 1) * 32], pos[i][:]], ALU.mult)
                    kb.v("dve", "tensor_reduce", dd[i][:, k:k + 1], [tm[i][:]], AX.X, ALU.add)
                kb.v("dve", "tensor_copy", DEST[:, s, :], [dd[i][:]])
                ft = fts[i]
                kb.dma(ft[:], self.Fm[s * 128:(s + 1) * 128, :])
                for k in range(2):
                    kb.scatter(self.XS, ft[:], DEST[:, s, k:k + 1], wkey=("XS", "sc"))
        with kb.phase() as ph:
            W1 = [ph.sb([128, 8, 512], BF16, "W1") for _ in range(2)]
            W3 = [ph.sb([128, 8, 512], BF16, "W3") for _ in range(2)]
            W2 = [ph.sb([128, 4, D], BF16, "W2") for _ in range(2)]
            xs = [ph.sb([128, D], BF16, "xs") for _ in range(2)]
            xT = [ph.sb([128, 8, 512], BF16, "xT") for _ in range(2)]
            sil = [ph.sb([128, 512], F32, "sil") for _ in range(2)]
            hid = [ph.sb([128, 4, 512], BF16, "hid") for _ in range(2)]
            ysb = [ph.sb([128, D], F32, "ysb") for _ in range(2)]
            ptr = [ph.ps([128, 1024], BF16, "ptr") for _ in range(2)]
            p1 = [ph.ps() for _ in range(2)]
            p3 = [ph.ps() for _ in range(2)]
            py = [ph.ps() for _ in range(2)]
            u = 0
            for b in range(nb):
                i = b % 2
                for c in range(8):
                    kb.gather(W1[i][:, c, :], w1, I1[:, b, c:c + 1])
                    kb.gather(W3[i][:, c, :], w3, I1[:, b, c:c + 1])
                for c in range(4):
                    kb.gather(W2[i][:, c, :], w2, I2[:, b, c:c + 1])
                for s in range(4):
                    x = xs[u % 2]
                    pt = ptr[u % 2]
                    u += 1
                    r0 = b * MOE_BS + s * 128
                    kb.dma(x[:], self.XS[r0:r0 + 128, :], rkey=("XS", "sc"))
                    for c in range(8):
                        kb.tr(pt[:, c * 128:(c + 1) * 128], x[:, c * 128:(c + 1) * 128], self.ident[:])
                    self.evac(xT[i][:, :, s * 128:(s + 1) * 128], pt[:].rearrange("p (c t) -> p c t", c=8))
                for hc in range(4):
                    a, b3 = p1[hc % 2], p3[hc % 2]
                    for c in range(8):
                        kb.mm(a[:], W1[i][:, c, hc * 128:(hc + 1) * 128], xT[i][:, c, :], start=(c == 0), stop=(c == 7))
                    for c in range(8):
                        kb.mm(b3[:], W3[i][:, c, hc * 128:(hc + 1) * 128], xT[i][:, c, :], start=(c == 0), stop=(c == 7))
                    kb.act(sil[hc % 2][:], a[:], AF.Silu)
                    kb.v("dve", "tensor_tensor", hid[i][:, hc, :], [b3[:], sil[hc % 2][:]], ALU.mult)
                for s in range(4):
                    yb = ysb[s % 2]
                    for n in range(2):
                        pp = py[n]
                        for hc in range(4):
                            kb.mm(pp[:], hid[i][:, hc, s * 128:(s + 1) * 128], W2[i][:, hc, n * 512:(n + 1) * 512], start=(hc == 0), stop=(hc == 3))
                        self.evac(yb[:, n * 512:(n + 1) * 512], pp[:])
                    r0 = b * MOE_BS + s * 128
                    kb.dma(self.YS[r0:r0 + 128, :], yb[:], wkey=("YS", "w"))
        with kb.phase() as ph:
            y1 = [ph.sb([128, D], F32, "y1") for _ in range(2)]
            y2 = [ph.sb([128, D], F32, "y2") for _ in range(2)]
            hts = [ph.sb([128, D], F32, "ht") for _ in range(2)]
            for s in range(ns):
                i = s % 2
                mod = self.modC if s < 2 else self.modL
                kb.gather(y1[i][:], self.YS, DEST[:, s, 0:1], rkey=("YS", "w"))
                kb.gather(y2[i][:], self.YS, DEST[:, s, 1:2], rkey=("YS", "w"))
                kb.dma(hts[i][:], self.H[s * 128:(s + 1) * 128, :])
                kb.v("dve", "tensor_scalar", y1[i][:], [y1[i][:]], WTS[:, s, 0:1], None, ALU.mult)
                kb.v("dve", "scalar_tensor_tensor", y1[i][:], [y2[i][:], WTS[:, s, 1:2], y1[i][:]], ALU.mult, ALU.add)
                kb.v("pool", "tensor_tensor", y1[i][:], [y1[i][:], mod[5][:]], ALU.mult)
                kb.v("dve", "tensor_tensor", hts[i][:], [hts[i][:], y1[i][:]], ALU.add)
                kb.dma(self.H[s * 128:(s + 1) * 128, :], hts[i][:])
    def odd_layer(self, l):
        j = l // 2
        self.phase_mod(l)
        sm = {k: self.inp(f"o{j}_{k}", s) for k, s in ODD_SMALL_SHAPES.items()}
        self.phase_r1()
        self.phase_r2a(l, j, sm)
        self.phase_r2b(j, sm)
        self.phase_r3(j)
        self.phase_r4(l, j, sm)
        self.moe(l)

    def phase_r1(self):
        kb, cfg = self.kb, self.cfg
        if not hasattr(self, "XN"):
            self.XN = self.scratch("XN", [D, cfg.T], BF16)
        with kb.phase() as ph:
            def consume(ti, t0, TT, xT):
                kb.dma(self.XN[:, t0:t0 + TT].rearrange("(c p) t -> p c t", p=128), xT[:, :, 0:TT])
            self.norm_tiles_to_xT(ph, self.H, 1, 0, consume)

    def wload(self, ph, src, shape, name, view=None):
        t = ph.sb(shape, BF16, name)
        self.kb.dma(t[:], view if view is not None else src, eng="pool")
        return t

    def phase_r2a(self, l, j, sm):
        kb, cfg = self.kb, self.cfg
        T = cfg.T
        has_vres = j > 0
        wr = self.inp(f"rk_wr__{j}", [D, D])
        wk = self.inp(f"rk_wk__{j}", [D, D])
        wv = self.inp(f"rk_wv__{j}", [D, D])
        w1 = self.inp(f"rk_w1__{j}", [2, D, 64])
        a1 = self.inp(f"rk_a1__{j}", [2, D, 64])
        g1 = self.inp(f"rk_g1__{j}", [D, 160])
        g2 = self.inp(f"rk_g2__{j}", [160, D])
        if has_vres:
            v1 = self.inp(f"rk_v1__{j - 1}", [D, 32])
            v2 = self.inp(f"rk_v2__{j - 1}", [32, D])
        if not hasattr(self, "RF"):
            self.RF = self.scratch("RF", [D, T], BF16)
            self.KFm = self.scratch("KFm", [D, T], BF16)
            self.KKF = self.scratch("KKF", [D, T], BF16)
            self.TW = self.scratch("TW", [2, 64, T], BF16)
            self.TA = self.scratch("TA", [2, 64, T], BF16)
            self.VTM = self.scratch("VTM", [T, D], BF16)
            self.VF = self.scratch("VF", [T, D], F32)
            self.GATE = self.scratch("GATE", [T, D], BF16)
        last = len(cfg.tiles) - 1
        with kb.phase() as ph:
            r3 = lambda w: w.rearrange("(c p) n -> p c n", p=128)
            Wr = self.wload(ph, None, [128, 8, D], "Wr", r3(wr))
            Wk = self.wload(ph, None, [128, 8, D], "Wk", r3(wk))
            Wv = self.wload(ph, None, [128, 8, D], "Wv", r3(wv))
            W1 = [self.wload(ph, None, [128, 8, 64], "W1", r3(w1[d])) for d in range(2)]
            A1 = [self.wload(ph, None, [128, 8, 64], "A1", r3(a1[d])) for d in range(2)]
            G1 = self.wload(ph, None, [128, 8, 160], "G1", r3(g1))
            G2a = self.wload(ph, None, [128, D], "G2a", g2[0:128, :])
            G2b = self.wload(ph, None, [32, D], "G2b", g2[128:160, :])
            if has_vres:
                V1 = self.wload(ph, None, [128, 8, 32], "V1", r3(v1))
                V2 = self.wload(ph, None, [32, D], "V2", v2)
                v0b = ph.sb([128, D], F32, "v0b")
                kb.dma(v0b[:], sm["v0"].broadcast_to([128, D]))
            mu = ph.sb([128, 6, 8], F32, "mu")
            kb.dma(mu[:], sm["mu"])
            kkc = ph.sb([128, 8], F32, "kkc")
            kb.dma(kkc[:], sm["kkcol"])
            bones = ph.sb([128, 128], F32, "bonesf")
            kb.dma(bones[:], self.cin["m_same"])
            bonesb = ph.sb([128, 128], BF16, "bonesb")
            kb.v("dve", "tensor_copy", bonesb[:], [bones[:]])
            x = ph.sb([128, 8, 514], BF16, "x")
            tmpf = ph.sb([128, 8, 512], F32, "tmpf")
            xx = ph.sb([128, 8, 512], BF16, "xx")
            xms = [ph.sb([128, 8, 512], BF16, "xm") for _ in range(2)]
            pm = [ph.ps() for _ in range(4)]
            pR = ph.ps()
            ob = [ph.sb([128, 512], BF16, "ob") for _ in range(3)]
            of = [ph.sb([128, 512], F32, "of") for _ in range(2)]
            sq = ph.sb([128, 512], BF16, "sq")
            rs = ph.sb([128, 512], F32, "rs")
            sg = ph.sb([128, 2, 512], BF16, "sg")
            lvT = ph.sb([32, 512], BF16, "lvT")
            vf = [ph.sb([128, 512], F32, "vf") for _ in range(2)]
            cnt = [0]

            def mix(i, TT):
                xm = xms[cnt[0] % 2]
                cnt[0] += 1
                for c in range(8):
                    kb.v("dve", "scalar_tensor_tensor", xm[:, c, 0:TT], [xx[:, c, 0:TT], mu[:, i, c:c + 1], x[:, c, 1:TT + 1]], ALU.mult, ALU.add)
                return xm

            def proj_fm(xm, Wt, ncol0, rows, TT, pmi):
                p = pm[pmi % 4]
                for c in range(8):
                    kb.mm(p[0:rows, 0:TT], Wt[:, c, ncol0:ncol0 + rows], xm[:, c, 0:TT], start=(c == 0), stop=(c == 7))
                return p

            u = 0
            for ti, (t0, TT) in enumerate(cfg.tiles):
                lo = 0 if ti in (0, 1) else 1
                hi = 0 if ti in (0, last) else 1
                kb.dma(x[:, :, 1 - lo:TT + 1 + hi], self.XN[:, t0 - lo:t0 + TT + hi].rearrange("(c p) t -> p c t", p=128))
                if not lo:
                    kb.v("pool", "memset", x[:, :, 0:1], [], 0.0)
                if not hi:
                    kb.v("pool", "memset", x[:, :, TT + 1:TT + 2], [], 0.0)
                kb.v("dve", "tensor_tensor", tmpf[:, :, 0:TT], [x[:, :, 0:TT], x[:, :, 2:TT + 2]], ALU.add)
                kb.v("dve", "scalar_tensor_tensor", xx[:, :, 0:TT], [tmpf[:, :, 0:TT], 0.5, x[:, :, 1:TT + 1]], ALU.mult, ALU.subtract)
                xm = mix(0, TT)
                for m in range(8):
                    p = proj_fm(xm, Wr, m * 128, 128, TT, u)
                    o = ob[u % 3]
                    u += 1
                    self.evac(o[:, 0:TT], p[:, 0:TT])
                    kb.dma(self.RF[m * 128:(m + 1) * 128, t0:t0 + TT], o[:, 0:TT])
                xm = mix(2, TT)
                for m in range(8):
                    p = proj_fm(xm, Wk, m * 128, 128, TT, u)
                    o = ob[u % 3]
                    f = of[u % 2]
                    u += 1
                    kb.act(o[:, 0:TT], p[:, 0:TT], AF.Copy)
                    kb.dma(self.KFm[m * 128:(m + 1) * 128, t0:t0 + TT], o[:, 0:TT])
                    kb.v("dve", "tensor_scalar", f[:, 0:TT], [p[:, 0:TT]], kkc[:, m:m + 1], None, ALU.mult)
                    kb.act(sq[:, 0:TT], f[:, 0:TT], AF.Square)
                    kb.mm(pR[:, 0:TT], bonesb[:], sq[:, 0:TT])
                    rstd_inplace(kb, rs[:, 0:TT], 1.0, EPS, src=pR[:, 0:TT])
                    o2 = ob[u % 3]
                    u += 1
                    kb.v("dve", "tensor_tensor", o2[:, 0:TT], [f[:, 0:TT], rs[:, 0:TT]], ALU.mult)
                    kb.dma(self.KKF[m * 128:(m + 1) * 128, t0:t0 + TT], o2[:, 0:TT])
                xm = mix(3, TT)
                if has_vres:
                    p = proj_fm(xm, V1, 0, 32, TT, u)
                    u += 1
                    kb.act(lvT[:, 0:TT], p[0:32, 0:TT], AF.Copy)
                for s in range(TT // 128):
                    r0 = t0 + s * 128
                    for n in range(2):
                        p = pm[u % 4]
                        o = ob[u % 3]
                        f = of[u % 2]
                        u += 1
                        for c in range(8):
                            kb.mm(p[:], xm[:, c, s * 128:(s + 1) * 128], Wv[:, c, n * 512:(n + 1) * 512], start=(c == 0), stop=(c == 7))
                        if not has_vres:
                            kb.act(f[:], p[:], AF.Copy)
                            kb.dma(self.VF[r0:r0 + 128, n * 512:(n + 1) * 512], f[:])
                            kb.v("dve", "tensor_copy", o[:], [p[:]])
                        else:
                            p2 = pm[u % 4]
                            u += 1
                            vft = vf[n]
                            kb.dma(vft[:], self.VF[r0:r0 + 128, n * 512:(n + 1) * 512])
                            kb.mm(p2[:], lvT[:, s * 128:(s + 1) * 128], V2[:, n * 512:(n + 1) * 512])
                            kb.v("dve", "tensor_tensor", f[:], [p2[:], v0b[:, n * 512:(n + 1) * 512]], ALU.add)
                            kb.act(f[:], f[:], AF.Sigmoid)
                            kb.v("dve", "tensor_tensor", vft[:], [vft[:], p[:]], ALU.subtract)
                            kb.v("pool", "tensor_tensor", vft[:], [vft[:], f[:]], ALU.mult)
                            kb.v("dve", "tensor_tensor", o[:], [vft[:], p[:]], ALU.add)
                        kb.dma(self.VTM[r0:r0 + 128, n * 512:(n + 1) * 512], o[:])
                xm = mix(5, TT)
                p = proj_fm(xm, G1, 0, 128, TT, u)
                u += 1
                kb.act(sg[:, 0, 0:TT], p[:, 0:TT], AF.Sigmoid)
                p = proj_fm(xm, G1, 128, 32, TT, u)
                u += 1
                kb.act(sg[0:32, 1, 0:TT], p[0:32, 0:TT], AF.Sigmoid)
                for s in range(TT // 128):
                    r0 = t0 + s * 128
                    for n in range(2):
                        p = pm[u % 4]
                        o = ob[u % 3]
                        u += 1
                        kb.mm(p[:], sg[:, 0, s * 128:(s + 1) * 128], G2a[:, n * 512:(n + 1) * 512], start=True, stop=False)
                        kb.mm(p[:], sg[0:32, 1, s * 128:(s + 1) * 128], G2b[:, n * 512:(n + 1) * 512], start=False, stop=True)
                        self.evac(o[:], p[:])
                        kb.dma(self.GATE[r0:r0 + 128, n * 512:(n + 1) * 512], o[:])
                xm = mix(1, TT)
                for d in range(2):
                    p = proj_fm(xm, W1[d], 0, 64, TT, u)
                    o = ob[u % 3]
                    u += 1
                    kb.act(o[0:64, 0:TT], p[0:64, 0:TT], AF.Tanh)
                    kb.dma(self.TW[d, :, t0:t0 + TT], o[0:64, 0:TT])
                xm = mix(4, TT)
                for d in range(2):
                    p = proj_fm(xm, A1[d], 0, 64, TT, u)
                    o = ob[u % 3]
                    u += 1
                    kb.act(o[0:64, 0:TT], p[0:64, 0:TT], AF.Copy)
                    kb.dma(self.TA[d, :, t0:t0 + TT], o[0:64, 0:TT])

    def phase_r2b(self, j, sm):
        kb, cfg = self.kb, self.cfg
        T = cfg.T
        nch = T // 64
        w2 = self.inp(f"rk_w2__{j}", [2, 64, D])
        a2 = self.inp(f"rk_a2__{j}", [2, 64, D])
        if not hasattr(self, "RT"):
            for nm in ("RT", "AT", "BT", "KT"):
                setattr(self, nm, [self.scratch(f"{nm}{d}", [D, T], BF16) for d in range(2)])
            for nm in ("ATM", "KHAT", "BHAT"):
                setattr(self, nm, [self.scratch(f"{nm}{d}", [T, D], BF16) for d in range(2)])
            self.GCd = [self.scratch(f"GC{d}", [D, nch], F32) for d in range(2)]
            self.SBN = self.scratch("SBN", [T, 16], F32)
        with kb.phase() as ph:
            W2 = [self.wload(ph, None, [64, D], "W2", w2[d]) for d in range(2)]
            A2 = [self.wload(ph, None, [64, D], "A2", a2[d]) for d in range(2)]
            w0 = ph.sb([128, 2, 8], F32, "w0")
            a0 = ph.sb([128, 2, 8], F32, "a0")
            kac = ph.sb([128, 8], F32, "kac")
            omka = ph.sb([128, 8], F32, "omka")
            rkc = ph.sb([128, 8], F32, "rkc")
            kb.dma(w0[:], sm["w0"])
            kb.dma(a0[:], sm["a0"])
            kb.dma(kac[:], sm["kacol"])
            kb.dma(rkc[:], sm["rkcol"])
            kb.v("dve", "tensor_scalar", omka[:], [kac[:]], -1.0, 1.0, ALU.mult, ALU.add)
            rmask = ph.sb([128, 512], F32, "rmask")
            kb.dma(rmask[:], self.cin["rmask"])
            hsel = ph.sb([128, 2], F32, "hself")
            kb.dma(hsel[:], self.cin["hsel"])
            hselb = ph.sb([128, 2], BF16, "hselb")
            kb.v("dve", "tensor_copy", hselb[:], [hsel[:]])
            tw = [ph.sb([64, 512], BF16, "tw") for _ in range(2)]
            ta = [ph.sb([64, 512], BF16, "ta") for _ in range(2)]
            rt = [ph.sb([128, 512], BF16, "rt") for _ in range(2)]
            kt = [ph.sb([128, 512], BF16, "kt") for _ in range(2)]
            kkt = [ph.sb([128, 512], BF16, "kkt") for _ in range(2)]
            F = lambda nm: ph.sb([128, 512], F32, nm)
            FS = [{nm: F(nm) for nm in ("lw", "av", "keys", "bv", "cs", "tot", "e1", "e2", "tmp", "tmp2")} for _ in range(2)]
            EX = [F("ex") for _ in range(4)]
            exi = [0]
            prod = F("prod")
            obs = [ph.sb([128, 512], BF16, "ob") for _ in range(4)]
            tms = [ph.sb([128, 3, 512], BF16, "tms") for _ in range(2)]
            tsb = [ph.sb([128, 4, 128], BF16, "tsb") for _ in range(3)]
            gcs = ph.sb([128, 8], F32, "gcs")
            prodb = ph.sb([128, 512], BF16, "prodb")
            sbn = ph.sb([128, 4, 16], F32, "sbn")
            pz = [ph.ps() for _ in range(2)]
            pa = [ph.ps() for _ in range(2)]
            ptr = [ph.ps([128, 1024], BF16, "ptr") for _ in range(3)]
            psb = ph.ps()
            u = 0
            tq = 0
            for ti, (t0, TT) in enumerate(cfg.tiles):
                ns = TT // 128
                nc_ = TT // 64
                for d in range(2):
                    kb.dma(tw[d][:, 0:TT], self.TW[d, :, t0:t0 + TT])
                    kb.dma(ta[d][:, 0:TT], self.TA[d, :, t0:t0 + TT])
                for c in range(8):
                    i = c % 2
                    kb.dma(rt[i][:, 0:TT], self.RF[c * 128:(c + 1) * 128, t0:t0 + TT])
                    kb.dma(kt[i][:, 0:TT], self.KFm[c * 128:(c + 1) * 128, t0:t0 + TT])
                    kb.dma(kkt[i][:, 0:TT], self.KKF[c * 128:(c + 1) * 128, t0:t0 + TT])
                    for d in range(2):
                        z, a_ = pz[u % 2], pa[u % 2]
                        tm3 = tms[u % 2]
                        fs = FS[u % 2]
                        lw, av, keys, bv, cs, tot, e1, e2, tmp, tmp2 = (fs[k_] for k_ in ("lw", "av", "keys", "bv", "cs", "tot", "e1", "e2", "tmp", "tmp2"))
                        u += 1
                        kb.mm(z[:, 0:TT], W2[d][:, c * 128:(c + 1) * 128], tw[d][:, 0:TT])
                        kb.mm(a_[:, 0:TT], A2[d][:, c * 128:(c + 1) * 128], ta[d][:, 0:TT])
                        kb.act(lw[:, 0:TT], z[:, 0:TT], AF.Sigmoid, bias=w0[:, d, c:c + 1])
                        kb.v("dve", "tensor_scalar", lw[:, 0:TT], [lw[:, 0:TT]], -0.6065306597126334, None, ALU.mult)
                        kb.act(av[:, 0:TT], a_[:, 0:TT], AF.Sigmoid, bias=a0[:, d, c:c + 1])
                        kb.v("dve", "tensor_scalar", tmp[:, 0:TT], [av[:, 0:TT]], kac[:, c:c + 1], omka[:, c:c + 1], ALU.mult, ALU.add)
                        kb.v("dve", "tensor_tensor", keys[:, 0:TT], [tmp[:, 0:TT], kt[i][:, 0:TT]], ALU.mult)
                        kb.v("pool", "tensor_tensor", bv[:, 0:TT], [av[:, 0:TT], kkt[i][:, 0:TT]], ALU.mult)
                        kb.v("dve", "tensor_tensor_scan", cs[:, 0:TT], [rmask[:, 0:TT], lw[:, 0:TT]], 0.0, ALU.mult, ALU.add)
                        cs3 = cs[:, 0:TT].rearrange("p (n k) -> p n k", k=64)
                        kb.v("pool", "tensor_copy", tot[:, 0:TT].rearrange("p (n k) -> p n k", k=64), [cs3[:, :, 63:64].to_broadcast([128, nc_, 64])])
                        if d == 0:
                            E1 = cs
                        else:
                            kb.v("dve", "tensor_tensor", e1[:, 0:TT], [tot[:, 0:TT], cs[:, 0:TT]], ALU.subtract)
                            kb.v("dve", "tensor_tensor", e1[:, 0:TT], [e1[:, 0:TT], lw[:, 0:TT]], ALU.add)
                            E1 = e1
                        kb.v("pool", "tensor_tensor", e2[:, 0:TT], [E1[:, 0:TT], lw[:, 0:TT]], ALU.subtract)
                        o = obs[tq % 4]; tq += 1
                        ex = EX[exi[0] % 4]; exi[0] += 1
                        kb.act(ex[:, 0:TT], E1[:, 0:TT], AF.Exp)
                        kb.v("dve", "tensor_tensor", o[:, 0:TT], [ex[:, 0:TT], rt[i][:, 0:TT]], ALU.mult)
                        kb.dma(self.RT[d][c * 128:(c + 1) * 128, t0:t0 + TT], o[:, 0:TT])
                        o = obs[tq % 4]; tq += 1
                        ex = EX[exi[0] % 4]; exi[0] += 1
                        kb.act(ex[:, 0:TT], e2[:, 0:TT], AF.Exp)
                        kb.v("dve", "scalar_tensor_tensor", o[:, 0:TT], [ex[:, 0:TT], -1.0, kkt[i][:, 0:TT]], ALU.mult, ALU.mult)
                        kb.dma(self.AT[d][c * 128:(c + 1) * 128, t0:t0 + TT], o[:, 0:TT])
                        kb.v("pool", "tensor_copy", tm3[:, 0, 0:TT], [o[:, 0:TT]])
                        ex = EX[exi[0] % 4]; exi[0] += 1
                        kb.act(ex[:, 0:TT], E1[:, 0:TT], AF.Exp, scale=-1.0)
                        o = obs[tq % 4]; tq += 1
                        kb.v("dve", "tensor_tensor", o[:, 0:TT], [ex[:, 0:TT], bv[:, 0:TT]], ALU.mult)
                        kb.dma(self.BT[d][c * 128:(c + 1) * 128, t0:t0 + TT], o[:, 0:TT])
                        o = obs[tq % 4]; tq += 1
                        kb.v("dve", "tensor_tensor", o[:, 0:TT], [ex[:, 0:TT], keys[:, 0:TT]], ALU.mult)
                        kb.dma(self.KT[d][c * 128:(c + 1) * 128, t0:t0 + TT], o[:, 0:TT])
                        kb.v("pool", "tensor_tensor", tmp2[:, 0:TT], [tot[:, 0:TT], E1[:, 0:TT]], ALU.subtract)
                        ex = EX[exi[0] % 4]; exi[0] += 1
                        kb.act(ex[:, 0:TT], tmp2[:, 0:TT], AF.Exp)
                        kb.v("dve", "tensor_tensor", tm3[:, 1, 0:TT], [ex[:, 0:TT], keys[:, 0:TT]], ALU.mult)
                        kb.v("dve", "tensor_tensor", tm3[:, 2, 0:TT], [ex[:, 0:TT], bv[:, 0:TT]], ALU.mult)
                        kb.act(gcs[:, 0:nc_], cs3[:, :, 63], AF.Exp)
                        kb.dma(self.GCd[d][c * 128:(c + 1) * 128, t0 // 64:t0 // 64 + nc_], gcs[:, 0:nc_])
                        for k3, dst in enumerate((self.ATM[d], self.KHAT[d], self.BHAT[d])):
                            pt, tb = ptr[k3], tsb[k3]
                            for s in range(ns):
                                kb.tr(pt[:, s * 128:(s + 1) * 128], tm3[:, k3, s * 128:(s + 1) * 128], self.ident[:])
                            self.evac(tb[:, 0:ns, :], pt[:, 0:TT].rearrange("p (s c) -> p s c", c=128))
                            kb.dma(dst[t0:t0 + TT, c * 128:(c + 1) * 128].rearrange("(s p) c -> p s c", p=128), tb[:, 0:ns, :])
                        if d == 0:
                            kb.v("dve", "scalar_tensor_tensor", prod[:, 0:TT], [keys[:, 0:TT], rkc[:, c:c + 1], rt[i][:, 0:TT]], ALU.mult, ALU.mult)
                        else:
                            kb.v("dve", "scalar_tensor_tensor", tmp[:, 0:TT], [keys[:, 0:TT], rkc[:, c:c + 1], rt[i][:, 0:TT]], ALU.mult, ALU.mult)
                            kb.v("dve", "scalar_tensor_tensor", prodb[:, 0:TT], [tmp[:, 0:TT], 1.0, prod[:, 0:TT]], ALU.mult, ALU.add)
                    for s in range(ns):
                        kb.mm(psb[:, (s * 8 + c) * 2:(s * 8 + c) * 2 + 2], prodb[:, s * 128:(s + 1) * 128], hselb[:])
                kb.v("dve", "tensor_scalar", sbn[:, 0:ns, :], [psb[:, 0:ns * 16].rearrange("p (s h) -> p s h", h=16)], 0.5, None, ALU.mult)
                kb.dma(self.SBN[t0:t0 + TT, :].rearrange("(s p) h -> p s h", p=128), sbn[:, 0:ns, :])
    def phase_r3(self, j):
        kb, cfg = self.kb, self.cfg
        T, nsub = cfg.T, cfg.nsub
        W = 16
        if not hasattr(self, "YD"):
            self.YD = [self.scratch(f"YD{d}", [T, D]) for d in range(2)]
        fwd = list(range(nsub))
        bwd = [1, 0] + list(range(nsub - 1, 1, -1))
        with kb.phase() as ph:
            mle = self.load_const_tile(ph, "m_le")
            mge = self.load_const_tile(ph, "m_ge")
            mlt = self.load_const_tile(ph, "m_lt")
            mgt = self.load_const_tile(ph, "m_gt")
            Hs = [[ph.sb([128, 64], F32, f"Hs{d}_{h}") for h in range(16)] for d in range(2)]
            Hb = [[ph.sb([128, 64], BF16, f"Hb{d}_{h}") for h in range(16)] for d in range(2)]
            for d in range(2):
                for h in range(16):
                    kb.v("pool", "memset", Hs[d][h][:], [], 0.0)
                    kb.v("pool", "memset", Hb[d][h][:], [], 0.0)
            banks = [ph.ps() for _ in range(8)]
            slot = [0]

            def ps128():
                i = slot[0] % 8
                slot[0] += 1
                return banks[i]

            UB = []
            for i in range(4):
                b = {}
                for nm in ("RT", "AT", "BT", "KT"):
                    b[nm] = ph.sb([128, 8, 128], BF16, nm)
                for nm in ("ATM", "KHAT", "BHAT", "V"):
                    b[nm] = ph.sb([128, D], BF16, nm)
                b["GC"] = ph.sb([128, 8, 2], F32, "GC")
                b["ysb"] = ph.sb([128, D], F32, "ysb")
                UB.append(b)
            HB = []
            for i in range(W):
                b = {}
                for nm in ("X0", "X1", "XT0", "XT1", "AT0", "AT1", "AakT", "ArkT", "ArbT", "PT"):
                    b[nm] = ph.sb([128, 128], BF16, nm)
                b["Z"] = ph.sb([128, 64], BF16, "Z")
                b["U"] = ph.sb([128, 64], BF16, "U")
                b["U0"] = ph.sb([128, 64], F32, "U0")
                HB.append(b)

            def unit(d, h, B, Hh):
                MsT = mlt if d == 0 else mgt
                Ms = mgt if d == 0 else mlt
                MiT = mle if d == 0 else mge
                c = h // 2
                rs = slice(64 * (h % 2), 64 * (h % 2) + 64)
                hs = slice(h * 64, (h + 1) * 64)
                RT, AT_, BT, KT = B["RT"][rs, c, :], B["AT"][rs, c, :], B["BT"][rs, c, :], B["KT"][rs, c, :]
                for (l_, r_, dst, msk) in ((BT, AT_, "XT0", MsT), (AT_, BT, "X0", Ms), (KT, AT_, "AakT", MsT), (KT, RT, "ArkT", MiT), (BT, RT, "ArbT", MiT)):
                    p = ps128()
                    kb.mm(p[:, 0:128], l_, r_)
                    kb.v("dve", "tensor_tensor", Hh[dst][:], [p[:, 0:128], msk[:]], ALU.mult)
                    yield
                kb.v("pool", "tensor_tensor", Hh["AT0"][:], [Hh["XT0"][:], self.ident[:]], ALU.add)
                X, XT, AT = Hh["X0"], Hh["XT0"], Hh["AT0"]
                for js in range(1, 6):
                    Xn = Hh["X1"] if X is Hh["X0"] else Hh["X0"]
                    XTn = Hh["XT1"] if XT is Hh["XT0"] else Hh["XT0"]
                    ATn = Hh["AT1"] if AT is Hh["AT0"] else Hh["AT0"]
                    px = ps128()
                    kb.mm(px[:, 0:128], XT[:], X[:])
                    if js < 5:
                        pxT = ps128()
                        kb.mm(pxT[:, 0:128], X[:], XT[:])
                    kb.act(Xn[:], px[:, 0:128], AF.Copy)
                    if js < 5:
                        kb.v("dve", "tensor_copy", XTn[:], [pxT[:, 0:128]])
                    yield
                    pa = ps128()
                    kb.mm(pa[:, 0:128], Xn[:], AT[:])
                    kb.v("dve", "tensor_tensor", ATn[:], [pa[:, 0:128], AT[:]], ALU.add)
                    X, XT, AT = Xn, XTn, ATn
                    yield
                p = ps128()
                kb.mm(p[:, 0:64], Hh["AakT"][:], B["V"][:, hs])
                kb.act(Hh["Z"][:], p[:, 0:64], AF.Copy)
                p = ps128()
                kb.mm(p[rs, 0:128], B["ATM"][:, hs], AT[:])
                kb.act(Hh["PT"][rs, :], p[rs, 0:128], AF.Copy)
                yield
                p = ps128()
                kb.mm(p[:, 0:64], AT[:], Hh["Z"][:])
                kb.act(Hh["U0"][:], p[:, 0:64], AF.Copy)
                yield
                for half in ((0, 1) if d == 0 else (1, 0)):
                    q = slice(half * 64, half * 64 + 64)
                    yq = V(B["ysb"][q, hs], (B["ysb"].name, h))
                    p1 = ps128()
                    kb.mm(p1[q, 0:64], Hh["PT"][rs, q], Hb[d][h][rs, :])
                    kb.v("dve", "tensor_tensor", Hh["U"][q, :], [p1[q, 0:64], Hh["U0"][q, :]], ALU.add)
                    p2a = ps128()
                    kb.mm(p2a[q, 0:64], B["RT"][rs, c, q], Hb[d][h][rs, :])
                    kb.act(yq, p2a[q, 0:64], AF.Copy)
                    yield
                    p2 = ps128()
                    kb.mm(p2[q, 0:64], Hh["ArkT"][q, q], B["V"][q, hs], start=True, stop=False)
                    kb.mm(p2[q, 0:64], Hh["ArbT"][q, q], Hh["U"][q, :], start=False, stop=True)
                    kb.v("dve", "tensor_tensor", yq, [p2[q, 0:64], yq], ALU.add)
                    p3 = ps128()
                    kb.mm(p3[rs, 0:64], B["KHAT"][q, hs], B["V"][q, hs], start=True, stop=False)
                    kb.mm(p3[rs, 0:64], B["BHAT"][q, hs], Hh["U"][q, :], start=False, stop=True)
                    kb.v("dve", "scalar_tensor_tensor", Hs[d][h][rs, :], [Hs[d][h][rs, :], B["GC"][rs, c, half:half + 1], p3[rs, 0:64]], ALU.mult, ALU.add)
                    kb.act(Hb[d][h][rs, :], Hs[d][h][rs, :], AF.Copy)
                    yield

            for step in range(nsub):
                makers = []
                Bs = []
                for d in range(2):
                    sb_ = fwd[step] if d == 0 else bwd[step]
                    r0 = sb_ * 128
                    B = UB[(step % 2) * 2 + d]
                    Bs.append((B, r0, d))
                    for nm, src in (("RT", self.RT), ("AT", self.AT), ("BT", self.BT), ("KT", self.KT)):
                        kb.dma(B[nm][:], src[d][:, r0:r0 + 128].rearrange("(c p) t -> p c t", p=128))
                    for nm, src in (("ATM", self.ATM[d]), ("KHAT", self.KHAT[d]), ("BHAT", self.BHAT[d]), ("V", self.VTM)):
                        kb.dma(B[nm][:], src[r0:r0 + 128, :])
                    kb.dma(B["GC"][:], self.GCd[d][:, sb_ * 2:sb_ * 2 + 2].rearrange("(c p) n -> p c n", p=128))
                for h in range(16):
                    for (B, r0, d) in Bs:
                        makers.append(lambda slot_, d=d, h=h, B=B: unit(d, h, B, HB[slot_]))
                interleave(makers, W)
                for (B, r0, d) in Bs:
                    kb.dma(self.YD[d][r0:r0 + 128, :], B["ysb"][:], extra_reads=[(B["ysb"].name, h) for h in range(16)])

    def phase_r4(self, l, j, sm):
        kb, cfg = self.kb, self.cfg
        Wout = self.inp(f"rk_wo__{j}", [D, D])
        self.Fm = self.scratch(f"F{l}", [cfg.T, D], BF16)
        with kb.phase() as ph:
            Wo = ph.sb([128, 8, D], BF16, "Wo")
            kb.dma(Wo[:], Wout.rearrange("(c p) n -> p c n", p=128), eng="pool")
            lnw = ph.sb([128, D], F32, "lnw")
            lnb = ph.sb([128, D], F32, "lnb")
            kb.dma(lnw[:], sm["lnw"].broadcast_to([128, D]))
            kb.dma(lnb[:], sm["lnb"].broadcast_to([128, D]))
            y0 = [ph.sb([128, D], F32, "y0") for _ in range(2)]
            y1 = [ph.sb([128, D], F32, "y1") for _ in range(2)]
            vt = [ph.sb([128, D], BF16, "vt") for _ in range(2)]
            gt = [ph.sb([128, D], BF16, "gt") for _ in range(2)]
            sbn = [ph.sb([128, 16], F32, "sbn") for _ in range(2)]
            st = [ph.sb([128, 16], F32, "st") for _ in range(2)]
            sqt = ph.sb([128, D], F32, "sqt")
            bon = ph.sb([128, D], F32, "bon")
            zt = [ph.sb([128, D], BF16, "zt") for _ in range(2)]
            XT = [ph.sb([128, 8, 128], BF16, "XT") for _ in range(2)]
            ptr = [ph.ps([128, 1024], BF16, "ptr") for _ in range(2)]
            py = [ph.ps() for _ in range(4)]
            hts = [ph.sb([128, D], F32, "ht") for _ in range(2)]
            tmp = [ph.sb([128, D], F32, "tmp") for _ in range(2)]
            junk = ph.sb([128, D], F32, "junk")
            sss = [ph.sb([128, 1], F32, "ss") for _ in range(2)]
            tmp2 = [ph.sb([128, D], F32, "tmp2") for _ in range(2)]
            fb = [ph.sb([128, D], BF16, "fb") for _ in range(2)]
            v3 = lambda t: t[:].rearrange("p (h n) -> p h n", n=64)
            b3 = lambda t: t[:].unsqueeze(2).to_broadcast([128, 16, 64])
            for s in range(cfg.nsub):
                mod = self.modC if s < 2 else self.modL
                r0 = s * 128
                i = s % 2
                kb.dma(y0[i][:], self.YD[0][r0:r0 + 128, :])
                kb.dma(y1[i][:], self.YD[1][r0:r0 + 128, :])
                kb.dma(vt[i][:], self.VTM[r0:r0 + 128, :])
                kb.dma(gt[i][:], self.GATE[r0:r0 + 128, :])
                kb.dma(sbn[i][:], self.SBN[r0:r0 + 128, :])
                kb.dma(hts[i][:], self.H[r0:r0 + 128, :])
                kb.v("pool", "tensor_tensor", y0[i][:], [y0[i][:], y1[i][:]], ALU.add)
                kb.v("dve", "tensor_reduce", st[i][:], [v3(y0[i])], AX.X, ALU.add)
                kb.v("dve", "tensor_scalar", st[i][:], [st[i][:]], 1.0 / 64, None, ALU.mult)
                kb.v("dve", "tensor_tensor", v3(y0[i]), [v3(y0[i]), b3(st[i])], ALU.subtract)
                kb.v("pool", "tensor_tensor", sqt[:], [y0[i][:], y0[i][:]], ALU.mult)
                kb.v("dve", "tensor_reduce", st[i][:], [v3(sqt)], AX.X, ALU.add)
                rstd_inplace(kb, st[i][:], 1.0 / 64, 64e-5)
                kb.v("dve", "tensor_tensor", v3(y0[i]), [v3(y0[i]), b3(st[i])], ALU.mult)
                kb.v("pool", "tensor_tensor", y0[i][:], [y0[i][:], lnw[:]], ALU.mult)
                kb.v("pool", "tensor_tensor", y0[i][:], [y0[i][:], lnb[:]], ALU.add)
                kb.v("dve", "tensor_tensor", v3(bon), [v3(vt[i]), b3(sbn[i])], ALU.mult)
                kb.v("dve", "tensor_tensor", y0[i][:], [y0[i][:], bon[:]], ALU.add)
                kb.v("pool", "tensor_tensor", zt[i][:], [y0[i][:], gt[i][:]], ALU.mult)
                for c in range(8):
                    kb.tr(ptr[i][:, c * 128:(c + 1) * 128], zt[i][:, c * 128:(c + 1) * 128], self.ident[:])
                self.evac(XT[i][:], ptr[i][:].rearrange("p (c t) -> p c t", c=8))
                for n in range(2):
                    pp = py[(2 * s + n) % 4]
                    for c in range(8):
                        kb.mm(pp[:], XT[i][:, c, :], Wo[:, c, n * 512:(n + 1) * 512], start=(c == 0), stop=(c == 7))
                    kb.v("dve", "tensor_tensor", tmp[i][:, n * 512:(n + 1) * 512], [pp[:], mod[2][:, n * 512:(n + 1) * 512]], ALU.mult)
                kb.v("pool", "tensor_tensor", hts[i][:], [tmp[i][:], hts[i][:]], ALU.add)
                kb.dma(self.H[r0:r0 + 128, :], hts[i][:])
                self.norm_sub(hts[i][:], junk[:], sss[i][:], tmp2[i][:], fb[i][:], mod[4][:], mod[3][:])
                kb.dma(self.Fm[r0:r0 + 128, :], fb[i][:])
import re


def resolve_input(name, cfg, inp, b, consts, cache):
    if name == "h0":
        return np.ascontiguousarray(np.concatenate([inp["ctx"][b], inp["x"][b][:cfg.T - CTX]], 0))
    if name == "cvec":
        return np.ascontiguousarray(np.concatenate([inp["c"][b].reshape(8, 128).T, inp["c_ctx"].reshape(8, 128).T], 1))
    if name.startswith("c_"):
        return consts[name[2:]]
    key = ("shared", name)
    if key in cache:
        return cache[key]
    m = re.match(r"^e(\d+)_(\w+)$", name)
    if m:
        j = int(m.group(1))
        if ("es", j) not in cache:
            cache[("es", j)] = host_even_smalls(inp, j)
        arr = cache[("es", j)][m.group(2)]
    else:
        m = re.match(r"^o(\d+)_(\w+)$", name)
        if m:
            j = int(m.group(1))
            if ("os", j) not in cache:
                cache[("os", j)] = host_odd_smalls(inp, j)
            arr = cache[("os", j)][m.group(2)]
        else:
            m = re.match(r"^(.+)__(\d+)$", name)
            base, idx = m.group(1), int(m.group(2))
            assert base in ALL_INPUTS, base
            a = inp[base][idx]
            if a.ndim == 1:
                a = a.reshape(1, -1)
            elif base in ("moe_w1", "moe_w3", "moe_w2"):
                a = a.reshape(-1, a.shape[-1])
            arr = np.ascontiguousarray(a)
    cache[key] = arr
    return arr


ALL_INPUTS = ('x', 'c', 'ctx', 'c_ctx', 'ada_w', 'ada_b', 'norm_mix', 'norm_ffn', 'hy_w_in', 'hy_w_out', 'mla_qa_norm', 'mla_w_qb',
              'mla_kva_norm', 'mla_w_kvb', 'mla_q_norm', 'mla_k_norm', 'gdn_conv', 'gdn_a_log', 'gdn_dt_bias', 'gdn_out_norm',
              'rk_mu', 'rk_wr', 'rk_wk', 'rk_wv', 'rk_wo', 'rk_w0', 'rk_w1', 'rk_w2', 'rk_a0', 'rk_a1', 'rk_a2', 'rk_g1', 'rk_g2',
              'rk_kk', 'rk_ka', 'rk_rk', 'rk_ln_w', 'rk_ln_b', 'rk_v0', 'rk_v1', 'rk_v2', 'moe_w_group', 'moe_b_group',
              'moe_w_expert', 'moe_b_expert', 'moe_w1', 'moe_w3', 'moe_w2')

_PROG_CACHE = {}


def get_prog(cfg_key):
    if cfg_key not in _PROG_CACHE:
        nlt, layers, debug = cfg_key
        cfg = Cfg(nlt=nlt, layers=layers, debug=debug)
        p = Prog(cfg)
        p.setup()
        for l in cfg.layers:
            if l % 2 == 0:
                p.even_layer(l)
            else:
                p.odd_layer(l)
        p.finish()
        _PROG_CACHE[cfg_key] = p
    return _PROG_CACHE[cfg_key]


def run_prog(p, inp, batches):
    cfg = p.cfg
    consts = host_consts(cfg)
    cache = {}
    in_maps = []
    for b in batches:
        m = {}
        for name, (shape, dt) in p.in_shapes.items():
            a = resolve_input(name, cfg, inp, b, consts, cache)
            assert tuple(a.shape) == tuple(shape), (name, a.shape, shape)
            m[name] = a
        in_maps.append(m)
    res = run_bass_kernel_spmd(p.nc, in_maps, core_ids=list(range(len(batches))))
    return res


def kernel(**inputs):
    inp = {k: np.asarray(v) for k, v in inputs.items()}
    p = get_prog((16, (0, 1, 2, 3), False))
    res = run_prog(p, inp, list(range(8)))
    return np.stack([np.asarray(r["out"], dtype=np.float32) for r in res.results], 0)
```
